# Optimizing a Trainium2 kernel written in Bass

```python
import math
import numpy as np
import jax
import jax.numpy as jnp
from jax import lax


D_MODEL = 1024
BATCH = 8
SEQ = 4096
DEPTH = 1

GRID_W = 64
CTX_LEN = 256
D_MIX = D_MODEL

GDN_HEADS = 4
GDN_DK = 128
GDN_DV = 128
GDN_CONV = 5
GDN_CHUNK = 64
GDN_QK_W = GDN_HEADS * GDN_DK
GDN_V_W = GDN_HEADS * GDN_DV
GDN_COLS = 2 * GDN_QK_W + 2 * GDN_V_W + 4 * GDN_HEADS

RWKV_HEADS = 8
RWKV_HD = 64
RWKV_W = RWKV_HEADS * RWKV_HD
RWKV_DECAY_LORA = 64
RWKV_ICLR_LORA = 64
RWKV_GATE_LORA = 128
RWKV_COLS = 3 * RWKV_W + 2 * RWKV_DECAY_LORA + 2 * RWKV_ICLR_LORA + RWKV_GATE_LORA
IN_COLS = GDN_COLS + RWKV_COLS

PEER_HEADS = 8
PEER_NKEYS = 128
PEER_EXPERTS = PEER_NKEYS * PEER_NKEYS
PEER_DKEY = 256
PEER_TOPK = 16
PEER_BLOCK = 128

NORM_EPS = 1e-6
L2_EPS = 1e-6
RWKV_GN_EPS = 64e-5
N_MOD = 6

kernel_name = 'hybrid_gdn_rwkv7_peer_dit_block'


def split_cols(t, widths):
    idx = [int(i) for i in np.cumsum(widths)[:-1]]
    return jnp.split(t, idx, axis=-1)


def rms_norm(x, gain):
    xf = x.astype(jnp.float32)
    y = xf * lax.rsqrt(jnp.mean(xf * xf, axis=-1, keepdims=True) + NORM_EPS)
    return (y * gain.astype(jnp.float32)).astype(x.dtype)


def l2_normalize(t):
    return t * lax.rsqrt(jnp.sum(t * t, axis=-1, keepdims=True) + L2_EPS)


def ada_mod(cond, w, b):
    m = (jax.nn.silu(cond) @ w + b)[:, None, :]
    return jnp.split(m, N_MOD, axis=-1)


def modulate(h, shift, scale):
    return h * (1 + scale) + shift


def seg_flip(t, n_ctx):
    return jnp.concatenate([jnp.flip(t[:, :n_ctx], axis=1), jnp.flip(t[:, n_ctx:], axis=1)], axis=1)


def dw_conv_centred(x, w):
    pad = (w.shape[0] - 1) // 2
    return lax.conv_general_dilated(x, w[:, None, :].astype(x.dtype), window_strides=(1,),
                                    padding=[(pad, pad)], dimension_numbers=('NWC', 'WIO', 'NWC'),
                                    feature_group_count=x.shape[-1])


def q_shift_grid(p):
    B, L, C = p.shape
    rows = L // GRID_W
    g = p.reshape(B, rows, GRID_W, C)
    q = C // 4
    left = jnp.pad(g[:, :, :-1, :q], ((0, 0), (0, 0), (1, 0), (0, 0)))
    right = jnp.pad(g[:, :, 1:, q:2 * q], ((0, 0), (0, 0), (0, 1), (0, 0)))
    up = jnp.pad(g[:, :-1, :, 2 * q:3 * q], ((0, 0), (1, 0), (0, 0), (0, 0)))
    down = jnp.pad(g[:, 1:, :, 3 * q:], ((0, 0), (0, 1), (0, 0), (0, 0)))
    return jnp.concatenate([left, right, up, down], axis=-1).reshape(B, L, C)


def bi_shift_seq(p):
    h = p.shape[-1] // 2
    prev = jnp.pad(p[:, :-1, :h], ((0, 0), (1, 0), (0, 0)))
    nxt = jnp.pad(p[:, 1:, h:], ((0, 0), (0, 1), (0, 0)))
    return jnp.concatenate([prev, nxt], axis=-1)


def gated_delta_chunked(q, k, v, g, beta):
    B, T, H, Dk = q.shape
    Dv = v.shape[-1]
    C = GDN_CHUNK
    N = T // C

    def to_chunks(t):
        t = t.reshape((B, N, C, H) + t.shape[3:])
        return jnp.moveaxis(t, (1, 3), (0, 2))

    qc, kc, vc = to_chunks(q), to_chunks(k), to_chunks(v)
    gc, bc = to_chunks(g), to_chunks(beta)
    G = jnp.cumsum(gc, axis=-1)
    incl = jnp.tril(jnp.ones((C, C), dtype=bool))
    strict = jnp.tril(jnp.ones((C, C), dtype=bool), -1)
    diff = G[..., :, None] - G[..., None, :]
    gamma = jnp.where(incl, jnp.exp(jnp.where(incl, diff, 0.0)), 0.0)
    kk = jnp.einsum('nbhid,nbhjd->nbhij', kc, kc)
    a_mat = jnp.where(strict, bc[..., :, None] * kk * gamma, 0.0) + jnp.eye(C, dtype=q.dtype)
    u = lax.linalg.triangular_solve(a_mat, bc[..., None] * vc, left_side=True, lower=True)
    w = lax.linalg.triangular_solve(a_mat, (bc * jnp.exp(G))[..., None] * kc, left_side=True, lower=True)
    qk = jnp.einsum('nbhid,nbhjd->nbhij', qc, kc) * gamma

    def step(S, inp):
        q_i, k_i, u_i, w_i, G_i, qk_i = inp
        v_new = u_i - jnp.einsum('bhcd,bhde->bhce', w_i, S)
        o = (jnp.einsum('bhcd,bhde->bhce', q_i * jnp.exp(G_i)[..., None], S)
             + jnp.einsum('bhij,bhje->bhie', qk_i, v_new))
        g_last = G_i[..., -1:]
        S = (S * jnp.exp(g_last)[..., None]
             + jnp.einsum('bhcd,bhce->bhde', k_i * jnp.exp(g_last - G_i)[..., None], v_new))
        return S, o

    S0 = jnp.zeros((B, H, Dk, Dv), q.dtype)
    _, o = lax.scan(step, S0, (qc, kc, u, w, G, qk))
    return jnp.moveaxis(o, (0, 2), (1, 3)).reshape(B, T, H, Dv)


def gdn_mixer(p, n_ctx, conv_w, a_log, dt_bias, norm_w):
    B, T, _ = p.shape
    f32 = jnp.float32
    qkv, z, alpha, beta_logit = split_cols(p, [2 * GDN_QK_W + GDN_V_W, GDN_V_W, 2 * GDN_HEADS, 2 * GDN_HEADS])
    qkv = jnp.concatenate([dw_conv_centred(qkv[:, :n_ctx], conv_w), dw_conv_centred(qkv[:, n_ctx:], conv_w)], axis=1)
    qkv = jax.nn.silu(qkv).astype(f32)
    q, k, v = split_cols(qkv, [GDN_QK_W, GDN_QK_W, GDN_V_W])
    q = l2_normalize(q.reshape(B, T, GDN_HEADS, GDN_DK)) * (GDN_DK ** -0.5)
    k = l2_normalize(k.reshape(B, T, GDN_HEADS, GDN_DK))
    v = v.reshape(B, T, GDN_HEADS, GDN_DV)
    alpha = alpha.astype(f32).reshape(B, T, 2, GDN_HEADS)
    g = -jnp.exp(a_log.astype(f32)) * jax.nn.softplus(alpha + dt_bias.astype(f32))
    beta = jax.nn.sigmoid(beta_logit.astype(f32).reshape(B, T, 2, GDN_HEADS))

    def both(t_fwd, t_bwd):
        return jnp.concatenate([t_fwd, seg_flip(t_bwd, n_ctx)], axis=0)

    o2 = gated_delta_chunked(both(q, q), both(k, k), both(v, v),
                             both(g[:, :, 0], g[:, :, 1]), both(beta[:, :, 0], beta[:, :, 1]))
    o = o2[:B] + seg_flip(o2[B:], n_ctx)
    o = (o * lax.rsqrt(jnp.mean(o * o, axis=-1, keepdims=True) + NORM_EPS) * norm_w.astype(f32)
         * jax.nn.silu(z.astype(f32).reshape(B, T, GDN_HEADS, GDN_DV)))
    return o.reshape(B, T, GDN_V_W).astype(p.dtype)


def rwkv7_scan(r, decay, k, v, kk, a):
    def step(S, inp):
        r_t, w_t, k_t, v_t, kk_t, a_t = inp
        sa = jnp.einsum('bhvk,bhk->bhv', S, -kk_t)
        S = (S * w_t[:, :, None, :] + sa[..., None] * (kk_t * a_t)[:, :, None, :]
             + v_t[..., None] * k_t[:, :, None, :])
        return S, jnp.einsum('bhvk,bhk->bhv', S, r_t)

    B2, T, H, N = r.shape
    S0 = jnp.zeros((B2, H, N, N), jnp.float32)
    xs = tuple(jnp.moveaxis(t, 1, 0) for t in (r, decay, k, v, kk, a))
    _, y = lax.scan(step, S0, xs)
    return jnp.moveaxis(y, 0, 1)


def rwkv7_mixer(p, n_ctx, mu, w0, w2, a0, a2, g2, k_k, k_a, r_k, gn_w, gn_b):
    B, T, _ = p.shape
    f32 = jnp.float32
    shifted = jnp.concatenate([bi_shift_seq(p[:, :n_ctx]), q_shift_grid(p[:, n_ctx:])], axis=1)
    p = (p + mu * (shifted - p)).astype(f32)
    r, k, v, xw, xa, xg = split_cols(p, [RWKV_W, RWKV_W, RWKV_W, 2 * RWKV_DECAY_LORA,
                                         2 * RWKV_ICLR_LORA, RWKV_GATE_LORA])
    xw = xw.reshape(B, T, 2, RWKV_DECAY_LORA)
    xa = xa.reshape(B, T, 2, RWKV_ICLR_LORA)
    w_pre = w0.astype(f32) + jnp.einsum('btdr,drc->btdc', jnp.tanh(xw), w2.astype(f32))
    log_w = -jnp.exp(-jax.nn.softplus(-w_pre) - 0.5)
    a = jax.nn.sigmoid(a0.astype(f32) + jnp.einsum('btdr,drc->btdc', xa, a2.astype(f32)))
    gate = jax.nn.sigmoid(xg) @ g2.astype(f32)
    kk = l2_normalize((k * k_k.astype(f32)).reshape(B, T, RWKV_HEADS, RWKV_HD))
    k_dir = k[:, :, None, :] * (1 + (a - 1) * k_a.astype(f32))

    def heads(t):
        return t.reshape(t.shape[:-1] + (RWKV_HEADS, RWKV_HD))

    def both(t_fwd, t_bwd):
        return jnp.concatenate([t_fwd, seg_flip(t_bwd, n_ctx)], axis=0)

    decay = jnp.exp(log_w)
    y2 = rwkv7_scan(heads(both(r, r)), heads(both(decay[:, :, 0], decay[:, :, 1])),
                    heads(both(k_dir[:, :, 0], k_dir[:, :, 1])), heads(both(v, v)),
                    both(kk, kk), heads(both(a[:, :, 0], a[:, :, 1])))
    y = y2[:B] + seg_flip(y2[B:], n_ctx)
    mean = jnp.mean(y, axis=-1, keepdims=True)
    var = jnp.mean(jnp.square(y - mean), axis=-1, keepdims=True)
    yn = ((y - mean) * lax.rsqrt(var + RWKV_GN_EPS)).reshape(B, T, RWKV_W) * gn_w.astype(f32) + gn_b.astype(f32)
    rk = (r[:, :, None, :] * k_dir * r_k.astype(f32)).reshape(B, T, 2, RWKV_HEADS, RWKV_HD)
    bonus = jnp.sum(jnp.sum(rk, axis=-1, keepdims=True), axis=2) * heads(v)
    out = (yn + bonus.reshape(B, T, RWKV_W)) * gate
    return out.astype(mu.dtype)


def peer_ffn(h, w_query, sub_keys, down, up):
    B, T, D = h.shape
    q = (h @ w_query).reshape(B, T, PEER_HEADS, 2, PEER_DKEY // 2)
    s = jnp.einsum('bthpd,hpkd->bthpk', q, sub_keys).astype(jnp.float32)
    s_top, i_top = lax.top_k(s, PEER_TOPK)
    cand_s = (s_top[..., 0, :, None] + s_top[..., 1, None, :]).reshape(B, T, PEER_HEADS, PEER_TOPK * PEER_TOPK)
    cand_i = (i_top[..., 0, :, None] * PEER_NKEYS + i_top[..., 1, None, :]).reshape(B, T, PEER_HEADS, PEER_TOPK * PEER_TOPK)
    best_s, pos = lax.top_k(cand_s, PEER_TOPK)
    idx = jnp.take_along_axis(cand_i, pos, axis=-1)
    gate = jax.nn.softmax(best_s, axis=-1)
    nb = (B * T) // PEER_BLOCK
    hb = h.reshape(nb, PEER_BLOCK, D)
    ib = idx.reshape(nb, PEER_BLOCK, PEER_HEADS * PEER_TOPK)
    gb = gate.reshape(nb, PEER_BLOCK, PEER_HEADS * PEER_TOPK).astype(h.dtype)

    def expert_block(args):
        h_blk, i_blk, g_blk = args
        act = jax.nn.gelu(jnp.einsum('td,ted->te', h_blk, down[i_blk]), approximate=False)
        return jnp.einsum('te,ted->td', act * g_blk, up[i_blk])

    y = lax.map(expert_block, (hb, ib, gb))
    return y.reshape(B, T, D)


def setup_inputs(seed: int = 0) -> dict:
    key = jax.random.key(seed)
    ks = jax.random.split(key, 30)
    f32 = jnp.float32

    def nrm(k, shape, s):
        return jax.random.normal(k, shape, f32) * s

    L = DEPTH
    dt = jnp.exp(jax.random.uniform(ks[10], (L, 2, GDN_HEADS), f32, math.log(1e-3), math.log(1e-1)))
    return {
        'x': nrm(ks[0], (BATCH, SEQ, D_MODEL), 1.0),
        'c': nrm(ks[1], (BATCH, D_MODEL), 1.0),
        'ctx': nrm(ks[2], (BATCH, CTX_LEN, D_MODEL), 1.0),
        'c_ctx': nrm(ks[3], (D_MODEL,), 1.0),
        'ada_w': nrm(ks[4], (L, D_MODEL, N_MOD * D_MODEL), 0.2 * D_MODEL ** -0.5),
        'ada_b': nrm(ks[5], (L, N_MOD * D_MODEL), 0.02),
        'norm1_g': 1.0 + nrm(ks[6], (L, D_MODEL), 0.02),
        'w_in': nrm(ks[7], (L, D_MODEL, IN_COLS), D_MODEL ** -0.5),
        'gdn_conv_w': nrm(ks[8], (L, GDN_CONV, 2 * GDN_QK_W + GDN_V_W), GDN_CONV ** -0.5),
        'gdn_a_log': jnp.log(jax.random.uniform(ks[9], (L, 2, GDN_HEADS), f32, 1.0, 16.0)),
        'gdn_dt_bias': dt + jnp.log(-jnp.expm1(-dt)),
        'gdn_norm_w': 1.0 + nrm(ks[11], (L, GDN_DV), 0.02),
        'rwkv_mu': jax.random.uniform(ks[12], (L, RWKV_COLS), f32, 0.0, 1.0),
        'rwkv_w0': jax.random.uniform(ks[13], (L, 2, RWKV_W), f32, -6.0, 0.0),
        'rwkv_w2': nrm(ks[14], (L, 2, RWKV_DECAY_LORA, RWKV_W), 0.5 * RWKV_DECAY_LORA ** -0.5),
        'rwkv_a0': nrm(ks[15], (L, 2, RWKV_W), 0.1),
        'rwkv_a2': nrm(ks[16], (L, 2, RWKV_ICLR_LORA, RWKV_W), 0.5 * RWKV_ICLR_LORA ** -0.5),
        'rwkv_g2': nrm(ks[17], (L, RWKV_GATE_LORA, RWKV_W), RWKV_GATE_LORA ** -0.5),
        'rwkv_k_k': 0.85 + nrm(ks[18], (L, RWKV_W), 0.02),
        'rwkv_k_a': 1.0 + nrm(ks[19], (L, RWKV_W), 0.02),
        'rwkv_r_k': nrm(ks[20], (L, RWKV_W), 0.1),
        'rwkv_gn_w': 1.0 + nrm(ks[21], (L, RWKV_W), 0.02),
        'rwkv_gn_b': nrm(ks[22], (L, RWKV_W), 0.02),
        'w_out': nrm(ks[23], (L, D_MIX, D_MODEL), D_MIX ** -0.5),
        'norm2_g': 1.0 + nrm(ks[24], (L, D_MODEL), 0.02),
        'peer_w_query': nrm(ks[25], (L, D_MODEL, PEER_HEADS * PEER_DKEY), D_MODEL ** -0.5),
        'peer_sub_keys': nrm(ks[26], (L, PEER_HEADS, 2, PEER_NKEYS, PEER_DKEY // 2), (PEER_DKEY // 2) ** -0.5),
        'peer_down': nrm(ks[27], (L, PEER_EXPERTS, D_MODEL), D_MODEL ** -0.5),
        'peer_up': nrm(ks[28], (L, PEER_EXPERTS, D_MODEL), 1.0),
        'final_norm_g': 1.0 + nrm(ks[29], (D_MODEL,), 0.02),
    }


def reference(x, c, ctx, c_ctx, ada_w, ada_b, norm1_g, w_in, gdn_conv_w, gdn_a_log, gdn_dt_bias,
              gdn_norm_w, rwkv_mu, rwkv_w0, rwkv_w2, rwkv_a0, rwkv_a2, rwkv_g2, rwkv_k_k, rwkv_k_a,
              rwkv_r_k, rwkv_gn_w, rwkv_gn_b, w_out, norm2_g, peer_w_query, peer_sub_keys,
              peer_down, peer_up, final_norm_g):
    n_ctx = ctx.shape[1]
    for layer in range(DEPTH):
        sh1, sc1, gt1, sh2, sc2, gt2 = ada_mod(c, ada_w[layer], ada_b[layer])
        csh1, csc1, cgt1, csh2, csc2, cgt2 = ada_mod(c_ctx[None, :], ada_w[layer], ada_b[layer])
        h = jnp.concatenate([modulate(rms_norm(ctx, norm1_g[layer]), csh1, csc1),
                             modulate(rms_norm(x, norm1_g[layer]), sh1, sc1)], axis=1)
        p = h @ w_in[layer]
        p_gdn, p_rwkv = split_cols(p, [GDN_COLS, RWKV_COLS])
        o_gdn = gdn_mixer(p_gdn, n_ctx, gdn_conv_w[layer], gdn_a_log[layer], gdn_dt_bias[layer],
                          gdn_norm_w[layer])
        o_rwkv = rwkv7_mixer(p_rwkv, n_ctx, rwkv_mu[layer], rwkv_w0[layer], rwkv_w2[layer],
                             rwkv_a0[layer], rwkv_a2[layer], rwkv_g2[layer], rwkv_k_k[layer],
                             rwkv_k_a[layer], rwkv_r_k[layer], rwkv_gn_w[layer], rwkv_gn_b[layer])
        o = jnp.concatenate([o_gdn, o_rwkv], axis=-1) @ w_out[layer]
        x = x + gt1 * o[:, n_ctx:]
        x = x + gt2 * peer_ffn(modulate(rms_norm(x, norm2_g[layer]), sh2, sc2), peer_w_query[layer],
                               peer_sub_keys[layer], peer_down[layer], peer_up[layer])
        if layer + 1 < DEPTH:
            ctx = ctx + cgt1 * o[:, :n_ctx]
            ctx = ctx + cgt2 * peer_ffn(modulate(rms_norm(ctx, norm2_g[layer]), csh2, csc2),
                                        peer_w_query[layer], peer_sub_keys[layer],
                                        peer_down[layer], peer_up[layer])
    return rms_norm(x, final_norm_g)
```

```python
from contextlib import ExitStack
import numpy as np
import concourse.bass as bass
import concourse.mybir as mybir
from concourse.bass_utils import run_bass_kernel_spmd

F32 = mybir.dt.float32
BF16 = mybir.dt.bfloat16
I32 = mybir.dt.int32
U32 = mybir.dt.uint32
AF = mybir.ActivationFunctionType
ALU = mybir.AluOpType
AX = mybir.AxisListType

D = 1024
TC = 256
IN_COLS = 3984
GDN_COLS = 2064
NORM_EPS = 1e-6
L2_EPS = 1e-6
GN_EPS = 64e-5


class Ctx:
    def __init__(self, nc):
        self.nc = nc
        self.es = ExitStack()
        self.eng = dict(pe=nc.tensor, act=nc.scalar, dve=nc.vector, pool=nc.gpsimd, sp=nc.sync)
        self.csem = {}
        self.cnt = {}
        for e in ("pe", "act", "dve", "pool"):
            self.csem[e] = self.es.enter_context(nc.semaphore("cs_" + e))
            self.cnt[e] = 0
        self.dsem = {}
        self.seen = {e: {} for e in self.eng}
        self.st = {}
        self.ninst = 0
        self._rec = None

    def _sem(self, sk):
        if sk[0] == "c":
            return self.csem[sk[1]]
        return self.dsem[sk[1]][0]

    def _deps(self, reads, writes, e=None):
        need = {}

        def add(m):
            if m is None:
                return
            sk, v = m
            if need.get(sk, 0) < v:
                need[sk] = v

        for r in reads:
            s = self.st.get(r)
            if s is not None:
                add(s[0])
                if isinstance(r, str) and r.startswith("ps") and r[2:].isdigit():
                    for sk, v in s[1].items():
                        if sk != ("c", e):
                            add((sk, v))
        for w in writes:
            s = self.st.get(w)
            if s is not None:
                add(s[0])
                for sk, v in s[1].items():
                    add((sk, v))
        return need

    def _wait(self, e, need):
        eng = self.eng[e]
        seen = self.seen[e]
        for sk, v in need.items():
            if e == "pe" and sk == ("c", "pe"):
                continue
            if sk[0] == "d":
                v = max(v, self.dsem[sk[1]][1])
            if seen.get(sk, 0) >= v:
                continue
            eng.wait_ge(self._sem(sk), v)
            seen[sk] = v

    def _mark(self, mark, reads, writes):
        for w in writes:
            self.st[w] = [mark, {}]
        for r in reads:
            s = self.st.get(r)
            if s is None:
                s = self.st[r] = [None, {}]
            sk, v = mark
            if s[1].get(sk, 0) < v:
                s[1][sk] = v

    def streams(self, n):
        lists = []
        for d in range(n):
            self._rec = []
            yield d
            lists.append(self._rec)
            self._rec = None
        idx = [0] * n
        left = sum(len(l) for l in lists)
        while left:
            for d in range(n):
                if idx[d] < len(lists[d]):
                    kind, args, kw = lists[d][idx[d]]
                    idx[d] += 1
                    left -= 1
                    getattr(self, kind)(*args, **kw)

    def op(self, e, fn, reads=(), writes=()):
        if self._rec is not None:
            self._rec.append(("op", (e, fn, tuple(reads), tuple(writes)), {}))
            return
        need = self._deps(reads, writes, e)
        self._wait(e, need)
        ins = fn(self.eng[e])
        ins.then_inc(self.csem[e], 1)
        self.cnt[e] += 1
        self.ninst += 1
        self._mark((("c", e), self.cnt[e]), reads, writes)

    def dma(self, out, in_, reads=(), writes=(), key=None, q="sp", **kw):
        if self._rec is not None:
            self._rec.append(("dma", (out, in_, tuple(reads), tuple(writes), key, q), kw))
            return
        if key not in self.dsem:
            self.dsem[key] = [self.es.enter_context(self.nc.semaphore("ds_%d" % len(self.dsem))), 0]
        need = self._deps(reads, writes)
        self._wait(q, need)
        d = self.dsem[key]
        self.eng[q].dma_start(out=out, in_=in_, **kw).then_inc(d[0], 16)
        d[1] += 16
        self.ninst += 1
        self._mark((("d", key), d[1]), reads, writes)

    def gather(self, out, in_, idx_ap, reads=(), writes=(), key=None):
        if self._rec is not None:
            self._rec.append(("gather", (out, in_, idx_ap, tuple(reads), tuple(writes), key), {}))
            return
        if key not in self.dsem:
            self.dsem[key] = [self.es.enter_context(self.nc.semaphore("ds_%d" % len(self.dsem))), 0]
        need = self._deps(reads, writes)
        self._wait("pool", need)
        d = self.dsem[key]
        self.nc.gpsimd.indirect_dma_start(
            out=out, out_offset=None, in_=in_,
            in_offset=bass.IndirectOffsetOnAxis(ap=idx_ap, axis=0)).then_inc(d[0], 16)
        d[1] += 16
        self.ninst += 1
        self._mark((("d", key), d[1]), reads, writes)

    def barrier(self):
        need = {("c", e): v for e, v in self.cnt.items() if v > 0}
        for k, d in self.dsem.items():
            if d[1] > 0:
                need[("d", k)] = d[1]
        for e in self.eng:
            self._wait(e, need)

    def finish(self, keys):
        need = self._deps(keys, ())
        self._wait("sp", need)


NM_MODE = ["f32"]
RW_NM = ["f32"]


class _Stop(Exception):
    pass


def build(nc, n_rows=64, dbg=(), stop=None):
    K = Ctx(nc)
    try:
        return _build(nc, K, n_rows, dbg, stop)
    except _Stop:
        K.barrier()
        return None


def _build(nc, K, n_rows, dbg, stop):
    def stop_at(name):
        if stop == name:
            raise _Stop()

    TL = 64 * n_rows
    T = TC + TL
    NT = T // 128
    NTL = TL // 128
    es = K.es

    def din(name, shape, dt=F32):
        return nc.dram_tensor(name, list(shape), dt, kind="ExternalInput").ap()

    x_d = din("x", [TL, D])
    c_d = din("c", [1, D])
    ctx_d = din("ctx", [TC, D])
    cctx_d = din("c_ctx", [1, D])
    adaw_d = din("ada_w", [D, 6144])
    adab_d = din("ada_b", [48, 128])
    adabr_d = din("ada_b_row", [1, 6144])
    g1_d = din("norm1_g", [8, 128])
    win_d = din("w_in", [D, IN_COLS])
    convw_d = din("gdn_conv_w", [60, 128])
    alog_d = din("gdn_a_log", [1, 8])
    dtb_d = din("gdn_dt_bias", [1, 8])
    gnorm_d = din("gdn_norm_w", [1, 128])
    mu_d = din("rwkv_mu", [15, 128])
    w0_d = din("rwkv_w0", [8, 128])
    w2_d = din("rwkv_w2", [128, 512])
    a0_d = din("rwkv_a0", [8, 128])
    a2_d = din("rwkv_a2", [128, 512])
    g2w_d = din("rwkv_g2", [128, 512])
    kk_d = din("rwkv_k_k", [4, 128])
    ka_d = din("rwkv_k_a", [4, 128])
    rk_d = din("rwkv_r_k", [4, 128])
    gnw_d = din("rwkv_gn_w", [4, 128])
    gnb_d = din("rwkv_gn_b", [4, 128])
    wout_d = din("w_out", [D, D])
    g2_d = din("norm2_g", [8, 128])
    wq_d = din("peer_w_query", [D, 2048])
    sk_d = din("peer_sub_keys", [16, 128, 128])
    down_d = din("peer_down", [16384, D])
    up_d = din("peer_up", [16384, D])
    fng_d = din("final_norm_g", [1, D])
    g2row_d = din("norm2_g_row", [1, D])
    out_d = nc.dram_tensor("out", [TL, D], F32, kind="ExternalOutput").ap()

    dbg_out = {}

    def dbgt(name, shape, dt=F32):
        dbg_out[name] = nc.dram_tensor("dbg_" + name, list(shape), dt, kind="ExternalOutput").ap()
        return dbg_out[name]

    pT_d = nc.dram_tensor("pT_s", [32, 128, T], F32).ap()

    def sb(name, shape, dt=F32, stack=es):
        return stack.enter_context(nc.sbuf_tensor(name, list(shape), dt))

    ps = [es.enter_context(nc.psum_tensor("ps%d" % i, [128, 512], F32)) for i in range(8)]

    dI = sb("dI", [128, 128], I32)
    ident = sb("ident", [128, 128])
    identb = sb("identb", [128, 128], BF16)
    ones = sb("ones", [128, 128])
    onesb = sb("onesb", [128, 128], BF16)
    m_lt = sb("m_lt", [128, 128])
    m_le = sb("m_le", [128, 128])
    m_gt = sb("m_gt", [128, 128])
    m_ge = sb("m_ge", [128, 128])
    e0 = sb("e0", [2, 128])
    K.op("pool", lambda e: e.iota(dI[:], pattern=[[1, 128]], base=0, channel_multiplier=-1), writes=["dI"])
    for t, op_, nm in ((ident, ALU.is_equal, "ident"), (m_lt, ALU.is_gt, "m_lt"), (m_le, ALU.is_ge, "m_le"),
                       (m_gt, ALU.is_lt, "m_gt"), (m_ge, ALU.is_le, "m_ge")):
        K.op("dve", lambda e, t=t, op_=op_: e.tensor_scalar(t[:], dI[:], 0.0, None, op0=op_), reads=["dI"], writes=[nm])
    K.op("dve", lambda e: e.tensor_copy(identb[:], ident[:]), reads=["ident"], writes=["identb"])
    K.op("pool", lambda e: e.memset(ones[:], 1.0), writes=["ones"])
    K.op("pool", lambda e: e.memset(onesb[:], 1.0), writes=["onesb"])
    e0i = sb("e0i", [2, 128], I32)
    K.op("pool", lambda e: e.iota(e0i[:], pattern=[[0, 128]], base=1, channel_multiplier=-1), writes=["e0i"])
    K.op("dve", lambda e: e.tensor_copy(e0[:], e0i[:]), reads=["e0i"], writes=["e0"])

    PK1 = dict(ADAB=0, G1=48, G2=56, CW=64)
    PK2 = dict(MU=0, W0=15, A0=23, KK=31, KA=35, RK=39, GNW=43, GNB=47, GNORM=51)
    pk1s = sb("pk1s", [128, 128])
    pk2s = sb("pk2s", [128, 128])
    pk1 = sb("pk1", [128, 128])
    pk2 = sb("pk2", [128, 128])
    K.op("pool", lambda e: e.memset(pk1s[:], 0.0), writes=["pk1s"])
    K.op("pool", lambda e: e.memset(pk2s[:], 0.0), writes=["pk2s"])
    for src, off, n in ((adab_d, 0, 48), (g1_d, 48, 8), (g2_d, 56, 8), (convw_d, 64, 60)):
        K.dma(pk1s[off:off + n, :], src[:, :], writes=["pk1s"], key="pk1s")
    for src, off, n in ((mu_d, 0, 15), (w0_d, 15, 8), (a0_d, 23, 8), (kk_d, 31, 4), (ka_d, 35, 4),
                        (rk_d, 39, 4), (gnw_d, 43, 4), (gnb_d, 47, 4), (gnorm_d, 51, 1)):
        K.dma(pk2s[off:off + n, :], src[:, :], writes=["pk2s"], key="pk2s")
    K.op("pe", lambda e: e.transpose(ps[0][:, 0:128], pk1s[:], ident[:]), reads=["pk1s", "ident"], writes=["ps0"])
    K.op("pe", lambda e: e.transpose(ps[0][:, 128:256], pk2s[:], ident[:]), reads=["pk2s", "ident"], writes=["ps0"])
    K.op("dve", lambda e: e.tensor_copy(pk1[:], ps[0][:, 0:128]), reads=["ps0"], writes=["pk1"])
    K.op("dve", lambda e: e.tensor_copy(pk2[:], ps[0][:, 128:256]), reads=["ps0"], writes=["pk2"])

    modT = sb("modT", [128, 48, 2])
    gt_d = nc.dram_tensor("gt_s", [128, 4 * D], F32).ap()
    A1 = sb("A1", [128, 8]); B1 = sb("B1", [128, 8])
    A1c = sb("A1c", [128, 8]); B1c = sb("B1c", [128, 8])
    A2 = sb("A2", [128, 8]); B2 = sb("B2", [128, 8])
    with ExitStack() as pa:
        cc = sb("cc", [2, D], stack=pa)
        scT = sb("scT", [128, 8, 2], stack=pa)
        aw = [sb("aw%d" % i, [128, 8, 1024], stack=pa) for i in range(2)]
        gtrow = sb("gtrow", [2, 4, D], stack=pa)
        gtB = sb("gtB", [128, 4, D], stack=pa)
        K.dma(cc[0:1, :], c_d[:, :], writes=["cc"], key="cc")
        K.dma(cc[1:2, :], cctx_d[:, :], writes=["cc"], key="cc")
        K.op("act", lambda e: e.activation(cc[:], cc[:], AF.Silu), reads=["cc"], writes=["cc"])
        for k in range(8):
            K.op("pe", lambda e, k=k: e.transpose(ps[1][:, 2 * k:2 * k + 2], cc[0:2, k * 128:(k + 1) * 128], ident[0:2, 0:2]),
                 reads=["cc", "ident"], writes=["ps1"])
        K.op("dve", lambda e: e.tensor_copy(scT[:].rearrange("p k c -> p (k c)"), ps[1][:, 0:16]), reads=["ps1"], writes=["scT"])
        K.op("pool", lambda e: e.memset(gtrow[:], 0.0), writes=["gtrow"])
        for q_ in range(4):
            K.dma(gtrow[0:1, q_, :], adabr_d[:, 2048 + q_ * 1024:3072 + q_ * 1024], writes=["gtrow"], key="gtrow")
        for g in range(6):
            a = aw[g % 2]
            an = "aw%d" % (g % 2)
            K.dma(a[:], adaw_d[:, g * 1024:(g + 1) * 1024].rearrange("(k p) c -> p k c", p=128), writes=[an], key=an)
            for j in range(8):
                col = (g * 8 + j) * 2
                for k in range(8):
                    K.op("pe", lambda e, a=a, j=j, k=k, col=col: e.matmul(
                        ps[2][:, col:col + 2], lhsT=a[:, k, j * 128:(j + 1) * 128], rhs=scT[:, k, :],
                        start=(k == 0), stop=(k == 7)), reads=[an, "scT"], writes=["ps2"])
            if g >= 2:
                q = g - 2
                for half in range(2):
                    for k in range(8):
                        K.op("pe", lambda e, a=a, k=k, half=half: e.matmul(
                            ps[3][0:2, :], lhsT=scT[:, k, :], rhs=a[:, k, half * 512:(half + 1) * 512],
                            start=(k == 0), stop=(k == 7)), reads=[an, "scT"], writes=["ps3"])
                    K.op("dve", lambda e, q=q, half=half: e.tensor_tensor(
                        gtrow[:, q, half * 512:(half + 1) * 512], ps[3][0:2, :], gtrow[:, q, half * 512:(half + 1) * 512], op=ALU.add),
                        reads=["ps3", "gtrow"], writes=["gtrow"])
                    K.op("pe", lambda e, q=q, half=half: e.matmul(
                        ps[4][:, :], lhsT=e0[:, :], rhs=gtrow[:, q, half * 512:(half + 1) * 512], start=True, stop=True),
                        reads=["e0", "gtrow"], writes=["ps4"])
                    K.op("act", lambda e, q=q, half=half: e.copy(gtB[:, q, half * 512:(half + 1) * 512], ps[4][:, :]),
                         reads=["ps4"], writes=["gtB"])
        K.op("dve", lambda e: e.tensor_tensor(
            modT[:], ps[2][:, 0:96].rearrange("p (j c) -> p j c", c=2),
            pk1[:, 0:48].unsqueeze(2).to_broadcast([128, 48, 2]), op=ALU.add), reads=["ps2", "pk1"], writes=["modT"])
        for (A, B, gname, sc0, sh0, col, nm) in ((A1, B1, "G1", 8, 0, 0, "1"), (A1c, B1c, "G1", 8, 0, 1, "1c"),
                                                 (A2, B2, "G2", 32, 24, 0, "2")):
            g0 = PK1[gname]
            K.op("dve", lambda e, A=A, g0=g0, sc0=sc0, col=col: e.scalar_tensor_tensor(
                A[:], modT[:, sc0:sc0 + 8, col], 1.0, pk1[:, g0:g0 + 8], op0=ALU.add, op1=ALU.mult),
                reads=["modT", "pk1"], writes=["A" + nm])
            K.op("dve", lambda e, B=B, sh0=sh0, col=col: e.tensor_copy(B[:], modT[:, sh0:sh0 + 8, col]),
                 reads=["modT"], writes=["B" + nm])

        K.dma(gt_d[:, :], gtB[:].rearrange("p q d -> p (q d)"), reads=["gtB"], writes=["gt_d"], key="st_gt")
        K.barrier()
    if "mod" in dbg:
        d_ = dbgt("mod", [128, 96])
        K.dma(d_[:, :], modT[:].rearrange("p j c -> p (j c)"), reads=["modT"], writes=["dbgmod"], key="dbg")

    def norm_tile(xt, xkey, A, B, hT, hkey, col0, pp, stage):
        junk, ss, rs, xs = stage
        K.op("act", lambda e: e.activation(junk[:], xt[:], AF.Square), reads=[xkey], writes=["n_junk"])
        K.op("dve", lambda e: e.reduce_sum(ss[:, 0:1], junk[:], axis=AX.X), reads=["n_junk"], writes=["n_ss"])
        K.op("act", lambda e: e.activation(rs[:, 0:1], ss[:, 0:1], AF.Sqrt, bias=eps_t[:, 0:1], scale=1.0 / D),
             reads=["n_ss", "eps"], writes=["n_rs"])
        K.op("dve", lambda e: e.reciprocal(rs[:, 1:2], rs[:, 0:1]), reads=["n_rs"], writes=["n_rs2"])
        K.op("dve", lambda e: e.tensor_scalar(xs[:], xt[:], rs[:, 1:2], None, op0=ALU.mult),
             reads=[xkey, "n_rs2"], writes=["n_xs"])
        for half in range(2):
            pt = ps[pp + half]
            pk = "ps%d" % (pp + half)
            for kk in range(4):
                k = half * 4 + kk
                K.op("pe", lambda e, k=k, kk=kk, pt=pt: e.transpose(pt[:, kk * 128:(kk + 1) * 128], xs[:, k * 128:(k + 1) * 128], ident[:]),
                     reads=["n_xs", "ident"], writes=[pk])
            for kk in range(4):
                k = half * 4 + kk
                eng = "act" if kk % 2 == 0 else "dve"
                if eng == "act":
                    K.op("act", lambda e, k=k, kk=kk, pt=pt: e.activation(
                        hT[:, k, col0:col0 + 128], pt[:, kk * 128:(kk + 1) * 128], AF.Identity,
                        bias=B[:, k:k + 1], scale=A[:, k:k + 1]), reads=[pk, "mods"], writes=[hkey])
                else:
                    K.op("dve", lambda e, k=k, kk=kk, pt=pt: e.tensor_scalar(
                        hT[:, k, col0:col0 + 128], pt[:, kk * 128:(kk + 1) * 128], A[:, k:k + 1], B[:, k:k + 1],
                        op0=ALU.mult, op1=ALU.add), reads=[pk, "mods"], writes=[hkey])

    eps_t = sb("eps_t", [128, 4])
    K.op("pool", lambda e: e.memset(eps_t[:, 0:1], NORM_EPS), writes=["eps"])
    K.op("pool", lambda e: e.memset(eps_t[:, 1:2], L2_EPS), writes=["eps"])
    K.op("pool", lambda e: e.memset(eps_t[:, 2:3], GN_EPS), writes=["eps"])
    K.op("pool", lambda e: e.memset(eps_t[:, 3:4], 1.0), writes=["eps"])
    K.op("dve", lambda e: e.tensor_copy(A1[:, 0:1], A1[:, 0:1]), reads=["A1", "B1", "A1c", "B1c", "A2", "B2"], writes=["mods"])

    with ExitStack() as pb:
        hT = sb("hT", [128, 8, T], BF16, stack=pb)
        junk = sb("junk", [128, D], stack=pb)
        ss = sb("ss", [128, 1], stack=pb)
        rs = sb("rs", [128, 2], stack=pb)
        xs = sb("xs", [128, D], stack=pb)
        xts = [sb("xt%d" % i, [128, D], stack=pb) for i in range(3)]
        for i in range(NT):
            xt = xts[i % 3]
            xk = "xt%d" % (i % 3)
            src = ctx_d[i * 128:(i + 1) * 128, :] if i < 2 else x_d[(i - 2) * 128:(i - 1) * 128, :]
            K.dma(xt[:], src, writes=[xk], key=xk)
            A, B = (A1c, B1c) if i < 2 else (A1, B1)
            norm_tile(xt, xk, A, B, hT, "hT", i * 128, 0, (junk, ss, rs, xs))
        if "hT" in dbg:
            d_ = dbgt("ss", [128, 1])
            K.dma(d_[:, :], ss[:, :], reads=["n_ss"], writes=["dbgss"], key="dbg")
            d_ = dbgt("rs", [128, 2])
            K.dma(d_[:, :], rs[:, :], reads=["n_rs", "n_rs2"], writes=["dbgrs"], key="dbg")
            d_ = dbgt("hT", [128, 8 * T], BF16)
            K.dma(d_[:, :], hT[:].rearrange("p k t -> p (k t)"), reads=["hT"], writes=["dbghT"], key="dbg")
        wst = [sb("wst%d" % i, [128, 8, 128], stack=pb) for i in range(2)]
        wbf = [sb("wbf%d" % i, [128, 8, 128], BF16, stack=pb) for i in range(2)]
        pcs = [sb("pc%d" % i, [128, T], stack=pb) for i in range(2)]
        nblk = (T + 511) // 512
        chunks = [(j, j * 128, 128) for j in range(16)] + [(16, 2048, 16)] + \
                 [(17 + j, GDN_COLS + j * 128, 128) for j in range(15)]
        ev = 0
        for ci, (dst, c0, ncol) in enumerate(chunks):
            w_s = wst[ci % 2]; w_b = wbf[ci % 2]; pc = pcs[ci % 2]
            ws_k = "wst%d" % (ci % 2); wb_k = "wbf%d" % (ci % 2); pc_k = "pc%d" % (ci % 2)
            K.dma(w_s[:, :, 0:ncol], win_d[:, c0:c0 + ncol].rearrange("(k p) c -> p k c", p=128), writes=[ws_k], key=ws_k)
            K.op("pool", lambda e, w_s=w_s, w_b=w_b, ncol=ncol: e.tensor_copy(w_b[:, :, 0:ncol], w_s[:, :, 0:ncol]),
                 reads=[ws_k], writes=[wb_k])
            for n in range(nblk):
                t0 = n * 512
                tn = min(512, T - t0)
                pt = ps[2 + (n % 4)]
                pk = "ps%d" % (2 + (n % 4))
                for k in range(8):
                    K.op("pe", lambda e, k=k, pt=pt, w_b=w_b, ncol=ncol, t0=t0, tn=tn: e.matmul(
                        pt[0:ncol, 0:tn], lhsT=w_b[:, k, 0:ncol], rhs=hT[:, k, t0:t0 + tn], start=(k == 0), stop=(k == 7)),
                        reads=[wb_k, "hT"], writes=[pk])
                eng = "act" if ev % 2 == 0 else "dve"
                ev += 1
                if eng == "act":
                    K.op("act", lambda e, pt=pt, pc=pc, ncol=ncol, t0=t0, tn=tn: e.copy(pc[0:ncol, t0:t0 + tn], pt[0:ncol, 0:tn]),
                         reads=[pk], writes=[pc_k])
                else:
                    K.op("dve", lambda e, pt=pt, pc=pc, ncol=ncol, t0=t0, tn=tn: e.tensor_copy(pc[0:ncol, t0:t0 + tn], pt[0:ncol, 0:tn]),
                         reads=[pk], writes=[pc_k])
            K.dma(pT_d[dst, 0:ncol, :], pc[0:ncol, :], reads=[pc_k], writes=[("pT", dst)], key="st_" + pc_k)

    K.barrier()
    if "pT" in dbg:
        d_ = dbgt("pT", [32, 128, T])
        K.dma(d_[:, :, :], pT_d[:, :, :], reads=[("pT", i) for i in range(32)], writes=["dbgpT"], key="dbg")


    mT_d = nc.dram_tensor("mT_s", [8, 128, TL], BF16).ap()
    psb = [p.bitcast(BF16) for p in ps]
    negm_le = sb("negm_le", [128, 128])
    negm_ge = sb("negm_ge", [128, 128])
    K.op("dve", lambda e: e.tensor_scalar(negm_le[:], m_le[:], 30000.0, -30000.0, op0=ALU.mult, op1=ALU.add), reads=["m_le"], writes=["negm_le"])
    K.op("dve", lambda e: e.tensor_scalar(negm_ge[:], m_ge[:], 30000.0, -30000.0, op0=ALU.mult, op1=ALU.add), reads=["m_ge"], writes=["negm_ge"])
    fwd_order = list(range(NT))
    bwd_order = [1, 0] + list(range(NT - 1, 1, -1))

    NMODE = NM_MODE[0]
    NDT = BF16 if NMODE == "bf16" else F32

    def mmv(ap):
        return ap

    identn = identb if NMODE == "bf16" else ident

    def neumann(Y, Yt, tag, pbank):
        PR = nm_tiles[tag]["PR"]; Pt = nm_tiles[tag]["Pt"]
        kPR = [tag + "PR0", tag + "PR1"]; kPt = [tag + "Pt0", tag + "Pt1"]
        pa_, pb_ = ps[pbank], ps[pbank + 1]
        ka, kb = "ps%d" % pbank, "ps%d" % (pbank + 1)
        K.op("pe", lambda e: e.matmul(pa_[:, 0:128], lhsT=mmv(Yt[:]), rhs=mmv(Y[:]), start=True, stop=True), reads=[tag + "Y", tag + "Yt"], writes=[ka])
        K.op("pe", lambda e: e.matmul(pb_[:, 0:128], lhsT=mmv(Y[:]), rhs=mmv(Yt[:]), start=True, stop=True), reads=[tag + "Y", tag + "Yt"], writes=[kb])
        K.op("act", lambda e: e.copy(PR[0][:, 0:128], pa_[:, 0:128]), reads=[ka], writes=[kPR[0]])
        K.op("dve", lambda e: e.tensor_tensor(PR[0][:, 128:256], Y[:], identn[:], op=ALU.add), reads=[tag + "Y", "identb", "ident"], writes=[kPR[0]])
        K.op("dve", lambda e: e.tensor_copy(Pt[0][:], pb_[:, 0:128]), reads=[kb], writes=[kPt[0]])
        cur = 0
        for l in range(1, 7):
            nxt = 1 - cur
            last = (l == 6)
            n0 = 128 if last else 0
            K.op("pe", lambda e, cur=cur, n0=n0: e.matmul(pa_[:, n0:256], lhsT=mmv(Pt[cur][:]), rhs=mmv(PR[cur][:, n0:256]), start=True, stop=False),
                 reads=[kPt[cur], kPR[cur]], writes=[ka])
            K.op("pe", lambda e, cur=cur: e.matmul(pa_[:, 128:256], lhsT=mmv(identn[:]), rhs=mmv(PR[cur][:, 128:256]), start=False, stop=True),
                 reads=["identb", "ident", kPR[cur]], writes=[ka])
            if not last:
                K.op("pe", lambda e, cur=cur: e.matmul(pb_[:, 0:128], lhsT=mmv(PR[cur][:, 0:128]), rhs=mmv(Pt[cur][:]), start=True, stop=True),
                     reads=[kPt[cur], kPR[cur]], writes=[kb])
            K.op("act", lambda e, nxt=nxt, n0=n0: e.copy(PR[nxt][:, n0:256], pa_[:, n0:256]), reads=[ka], writes=[kPR[nxt]])
            if not last:
                K.op("dve", lambda e, nxt=nxt: e.tensor_copy(Pt[nxt][:], pb_[:, 0:128]), reads=[kb], writes=[kPt[nxt]])
            cur = nxt
        if NMODE == "bf16":
            return PR[cur][:, 128:256], kPR[cur]
        fin = nm_tiles[tag]["fin"]
        K.op("act", lambda e, cur=cur: e.copy(fin[:], PR[cur][:, 128:256]), reads=[kPR[cur]], writes=[tag + "fin"])
        return fin[:], tag + "fin"

    nm_tiles = {}
    with ExitStack() as pg:
        for tag in ("n0", "n1"):
            nm_tiles[tag] = dict(PR=[sb(tag + "PR%d" % i, [128, 256], NDT, stack=pg) for i in range(2)],
                                 Pt=[sb(tag + "Pt%d" % i, [128, 128], NDT, stack=pg) for i in range(2)],
                                 fin=sb(tag + "fin", [128, 128], BF16, stack=pg))
        abT = sb("abT", [16, T], stack=pg)
        ab = sb("ab", [128, NT, 16], stack=pg)
        dtb_b = sb("dtb_b", [128, 8], stack=pg)
        nA_b = sb("nA_b", [128, 8], stack=pg)
        gg = sb("gg", [128, NT, 8], stack=pg)
        Gc = sb("Gc", [128, NT, 8], stack=pg)
        nbeta = sb("nbeta", [128, NT, 8], stack=pg)
        beta = sb("beta", [128, NT, 8], stack=pg)
        negeG = sb("negeG", [128, NT, 8], stack=pg)
        eG = sb("eG", [128, NT, 8], stack=pg)
        eTG = sb("eTG", [128, NT, 8], stack=pg)
        eTot = sb("eTot", [128, NT, 8], stack=pg)
        K.dma(abT[:, :], pT_d[16, 0:16, :], reads=[("pT", 16)], writes=["abT"], key="abT")
        K.dma(dtb_b[:, :], dtb_d.partition_broadcast(128), writes=["dtb_b"], key="dtb_b")
        K.dma(nA_b[:, :], alog_d.partition_broadcast(128), writes=["nA_b"], key="nA_b")
        for i in range(NT):
            K.op("pe", lambda e, i=i: e.transpose(ps[i // 32][:, (i % 32) * 16:(i % 32) * 16 + 16], abT[0:16, i * 128:(i + 1) * 128], ident[0:16, 0:16]),
                 reads=["abT", "ident"], writes=["ps%d" % (i // 32)])
        for b0 in range(0, NT, 32):
            nb = min(32, NT - b0)
            K.op("dve", lambda e, b0=b0, nb=nb: e.tensor_copy(ab[:, b0:b0 + nb, :].rearrange("p n c -> p (n c)"), ps[b0 // 32][:, 0:nb * 16]),
                 reads=["ps%d" % (b0 // 32)], writes=["ab"])
        K.op("act", lambda e: e.activation(nA_b[:], nA_b[:], AF.Exp), reads=["nA_b"], writes=["nA_b"])
        K.op("dve", lambda e: e.tensor_scalar(nA_b[:], nA_b[:], -1.0, None, op0=ALU.mult), reads=["nA_b"], writes=["nA_b"])
        K.op("dve", lambda e: e.tensor_tensor(gg[:], ab[:, :, 0:8], dtb_b[:].unsqueeze(1).to_broadcast([128, NT, 8]), op=ALU.add),
             reads=["ab", "dtb_b"], writes=["gg"])
        K.op("act", lambda e: e.activation(gg[:], gg[:], AF.Exp), reads=["gg"], writes=["gg"])
        K.op("act", lambda e: e.activation(gg[:], gg[:], AF.Ln, bias=eps_t[:, 3:4]), reads=["gg", "eps"], writes=["gg"])
        K.op("dve", lambda e: e.tensor_tensor(gg[:], gg[:], nA_b[:].unsqueeze(1).to_broadcast([128, NT, 8]), op=ALU.mult),
             reads=["gg", "nA_b"], writes=["gg"])
        K.op("act", lambda e: e.activation(beta[:], ab[:, :, 8:16], AF.Sigmoid), reads=["ab"], writes=["beta"])
        K.op("dve", lambda e: e.tensor_scalar(nbeta[:], beta[:], -1.0, None, op0=ALU.mult), reads=["beta"], writes=["nbeta"])
        ggf = gg[:].rearrange("p n c -> p (n c)")
        K.op("pe", lambda e: e.matmul(ps[2][:, 0:NT * 8], lhsT=m_le[:], rhs=ggf, start=True, stop=True), reads=["m_le", "gg"], writes=["ps2"])
        K.op("pe", lambda e: e.matmul(ps[3][:, 0:NT * 8], lhsT=m_ge[:], rhs=ggf, start=True, stop=True), reads=["m_ge", "gg"], writes=["ps3"])
        K.op("pe", lambda e: e.matmul(ps[4][:, 0:NT * 8], lhsT=ones[:], rhs=ggf, start=True, stop=True), reads=["ones", "gg"], writes=["ps4"])
        K.op("dve", lambda e: e.tensor_copy(Gc[:, :, 0:4], ps[2][:, 0:NT * 8].rearrange("p (n c) -> p n c", c=8)[:, :, 0:4]), reads=["ps2"], writes=["Gc"])
        K.op("dve", lambda e: e.tensor_copy(Gc[:, :, 4:8], ps[3][:, 0:NT * 8].rearrange("p (n c) -> p n c", c=8)[:, :, 4:8]), reads=["ps3"], writes=["Gc"])
        K.op("act", lambda e: e.activation(eG[:], Gc[:], AF.Exp), reads=["Gc"], writes=["eG"])
        K.op("dve", lambda e: e.tensor_scalar(negeG[:], eG[:], -1.0, None, op0=ALU.mult), reads=["eG"], writes=["negeG"])
        K.op("act", lambda e: e.activation(eTot[:].rearrange("p n c -> p (n c)"), ps[4][:, 0:NT * 8], AF.Exp), reads=["ps4"], writes=["eTot"])
        K.op("dve", lambda e: e.tensor_tensor(eTG[:].rearrange("p n c -> p (n c)"), ps[4][:, 0:NT * 8], Gc[:].rearrange("p n c -> p (n c)"), op=ALU.subtract),
             reads=["ps4", "Gc"], writes=["eTG"])
        K.op("act", lambda e: e.activation(eTG[:], eTG[:], AF.Exp), reads=["eTG"], writes=["eTG"])

        stop_at("gdn_scal")
        qT = sb("qT", [128, T], BF16, stack=pg)
        kT = sb("kT", [128, T], BF16, stack=pg)
        vT = sb("vT", [128, T], BF16, stack=pg)
        zs = sb("zs", [128, T], BF16, stack=pg)
        vtok = sb("vtok", [128, NT, 128], BF16, stack=pg)
        ktok = sb("ktok", [128, NT, 128], BF16, stack=pg)
        obuf = [sb("obuf%d" % d_, [128, NT, 128], stack=pg) for d_ in range(2)]
        pin = sb("pin", [128, T], stack=pg)
        cv = sb("cv", [128, T], stack=pg)
        sq = sb("sq", [128, T], stack=pg)
        rn = sb("rn", [128, 512], stack=pg)
        S = [sb("S%d" % d_, [128, 128], stack=pg) for d_ in range(2)]
        Sb = [sb("Sb%d" % d_, [128, 128], BF16, stack=pg) for d_ in range(2)]
        mst = sb("mst", [128, TL], BF16, stack=pg)
        dgl = [sb("dgl%d" % d_, [128, 128], stack=pg) for d_ in range(2)]
        arg = [sb("arg%d" % d_, [128, 128], stack=pg) for d_ in range(2)]
        DTi = [sb("DTi%d" % d_, [128, 128], stack=pg) for d_ in range(2)]
        DTs = [sb("DTs%d" % d_, [128, 128], stack=pg) for d_ in range(2)]
        Yb = [sb("Yb%d" % d_, [128, 128], NDT, stack=pg) for d_ in range(2)]
        Ytb = [sb("Ytb%d" % d_, [128, 128], NDT, stack=pg) for d_ in range(2)]
        Mqk = [sb("Mqk%d" % d_, [128, 128], BF16, stack=pg) for d_ in range(2)]
        Rb = [sb("Rb%d" % d_, [128, 128], BF16, stack=pg) for d_ in range(2)]
        Xb = [sb("Xb%d" % d_, [128, 128], BF16, stack=pg) for d_ in range(2)]
        Xs = [sb("Xs%d" % d_, [128, 128], BF16, stack=pg) for d_ in range(2)]
        QSe = [sb("QSe%d" % d_, [128, 128], stack=pg) for d_ in range(2)]
        on_ = sb("on_", [128, 128], stack=pg)
        oj = sb("oj", [128, 128], stack=pg)
        onn = sb("onn", [128, 128], stack=pg)
        oss = sb("oss", [128, 4], stack=pg)

        def conv_silu(cidx, dst, dkey, final_silu_to):
            cw0 = PK1["CW"]
            K.op("dve", lambda e: e.tensor_scalar(cv[:], pin[:], pk1[:, cw0 + 2 * 12 + cidx:cw0 + 2 * 12 + cidx + 1], None, op0=ALU.mult),
                 reads=["pin", "pk1"], writes=["cv"])
            for j in (0, 1, 3, 4):
                sh = j - 2
                wcol = pk1[:, cw0 + j * 12 + cidx:cw0 + j * 12 + cidx + 1]
                for (s0, s1) in ((0, TC), (TC, T)):
                    lo = max(s0, s0 - sh); hi = min(s1, s1 - sh)
                    K.op("dve", lambda e, lo=lo, hi=hi, sh=sh, wcol=wcol: e.scalar_tensor_tensor(
                        cv[:, lo:hi], pin[:, lo + sh:hi + sh], wcol, cv[:, lo:hi], op0=ALU.mult, op1=ALU.add),
                        reads=["pin", "pk1", "cv"], writes=["cv"])
            K.op("act", lambda e: e.activation(final_silu_to[:], cv[:], AF.Silu), reads=["cv"], writes=[dkey])

        def l2n(src, skey, dst, dkey, scale):
            K.op("pool", lambda e: e.tensor_tensor(sq[:], src[:], src[:], op=ALU.mult), reads=[skey], writes=["sq"])
            for n in range((T + 511) // 512):
                t0 = n * 512; tn = min(512, T - t0)
                pt = ps[5 + (n % 2)]; pk = "ps%d" % (5 + (n % 2))
                K.op("pe", lambda e, pt=pt, t0=t0, tn=tn: e.matmul(pt[:, 0:tn], lhsT=ones[:], rhs=sq[:, t0:t0 + tn], start=True, stop=True),
                     reads=["ones", "sq"], writes=[pk])
                K.op("act", lambda e, pt=pt, tn=tn: e.activation(rn[:, 0:tn], pt[:, 0:tn], AF.Sqrt, bias=eps_t[:, 1:2]), reads=[pk, "eps"], writes=["rn"])
                K.op("dve", lambda e, tn=tn: e.reciprocal(rn[:, 0:tn], rn[:, 0:tn]), reads=["rn"], writes=["rn"])
                K.op("dve", lambda e, t0=t0, tn=tn: e.scalar_tensor_tensor(dst[:, t0:t0 + tn], src[:, t0:t0 + tn], scale, rn[:, 0:tn], op0=ALU.mult, op1=ALU.mult),
                     reads=[skey, "rn"], writes=[dkey])

        def to_tok(src, skey, dst, dkey):
            for i in range(NT):
                pt = psb[5 + (i % 2)]; pk = "ps%d" % (5 + (i % 2))
                K.op("pe", lambda e, i=i, pt=pt: e.transpose(pt[:, 0:128], src[:, i * 128:(i + 1) * 128], identb[:]), reads=[skey, "identb"], writes=[pk])
                eng = "act" if i % 2 == 0 else "dve"
                if eng == "act":
                    K.op("act", lambda e, i=i, pt=pt: e.copy(dst[:, i, :], pt[:, 0:128]), reads=[pk], writes=[dkey])
                else:
                    K.op("dve", lambda e, i=i, pt=pt: e.tensor_copy(dst[:, i, :], pt[:, 0:128]), reads=[pk], writes=[dkey])

        for h in range(4):
            K.dma(pin[:, :], pT_d[h, :, :], reads=[("pT", h)], writes=["pin"], key="pin")
            conv_silu(h, cv, "cv", cv)
            l2n(cv, "cv", qT, "qT", float(128 ** -0.5))
            K.dma(pin[:, :], pT_d[4 + h, :, :], reads=[("pT", 4 + h)], writes=["pin"], key="pin")
            conv_silu(4 + h, cv, "cv", cv)
            l2n(cv, "cv", kT, "kT", 1.0)
            to_tok(kT, "kT", ktok, "ktok")
            K.dma(pin[:, :], pT_d[8 + h, :, :], reads=[("pT", 8 + h)], writes=["pin"], key="pin")
            conv_silu(8 + h, vT, "vT", vT)
            to_tok(vT, "vT", vtok, "vtok")
            K.dma(pin[:, :], pT_d[12 + h, :, :], reads=[("pT", 12 + h)], writes=["pin"], key="pin")
            K.op("act", lambda e: e.activation(zs[:], pin[:], AF.Silu), reads=["pin"], writes=["zs"])
            stop_at("gdn_prep")
            for d_ in range(2):
                K.op("pool", lambda e, d_=d_: e.memset(S[d_][:], 0.0), writes=["S%d" % d_])
                K.op("pool", lambda e, d_=d_: e.memset(Sb[d_][:], 0.0), writes=["Sb%d" % d_])
            for step in range(NT):
                for d_ in K.streams(2):
                    i = fwd_order[step] if d_ == 0 else bwd_order[step]
                    r = d_ * 4 + h
                    tag = "n%d" % d_
                    sl = slice(i * 128, (i + 1) * 128)
                    want_o = i >= 2
                    pA = ps[d_ * 4 + 0]; kA = "ps%d" % (d_ * 4 + 0)
                    pC = ps[d_ * 4 + 3]; kC = "ps%d" % (d_ * 4 + 3)
                    dk_ = "%d" % d_
                    K.op("dve", lambda e, i=i, r=r, d_=d_: e.tensor_scalar(dgl[d_][:], ident[:], Gc[:, i, r:r + 1], None, op0=ALU.mult),
                         reads=["ident", "Gc"], writes=["dgl" + dk_])
                    K.op("pe", lambda e, d_=d_, pA=pA: e.matmul(pA[:, 256:384], lhsT=ones[:], rhs=dgl[d_][:], start=True, stop=True),
                         reads=["ones", "dgl" + dk_], writes=[kA])
                    negm = negm_le if d_ == 0 else negm_ge
                    mstrict = m_lt if d_ == 0 else m_gt
                    K.op("dve", lambda e, i=i, r=r, d_=d_, pA=pA, negm=negm: e.scalar_tensor_tensor(
                        arg[d_][:], pA[:, 256:384], Gc[:, i, r:r + 1], negm[:], op0=ALU.subtract, op1=ALU.add),
                        reads=[kA, "Gc", "negm_le", "negm_ge"], writes=["arg" + dk_])
                    K.op("act", lambda e, d_=d_: e.activation(DTi[d_][:], arg[d_][:], AF.Exp), reads=["arg" + dk_], writes=["DTi" + dk_])
                    K.op("pool", lambda e, d_=d_, mstrict=mstrict: e.tensor_tensor(DTs[d_][:], DTi[d_][:], mstrict[:], op=ALU.mult),
                         reads=["DTi" + dk_, "m_lt", "m_gt"], writes=["DTs" + dk_])
                    stop_at("s_dt")
                    stop_at("dt@%d.%d" % (step, d_))
                    K.op("pe", lambda e, pA=pA, sl=sl: e.matmul(pA[:, 0:128], lhsT=kT[:, sl], rhs=kT[:, sl], start=True, stop=True),
                         reads=["kT"], writes=[kA])
                    K.op("dve", lambda e, i=i, r=r, d_=d_, pA=pA: e.scalar_tensor_tensor(
                        Yb[d_][:], pA[:, 0:128], nbeta[:, i, r:r + 1], DTs[d_][:], op0=ALU.mult, op1=ALU.mult),
                        reads=[kA, "nbeta", "DTs" + dk_], writes=[tag + "Y"])
                    if want_o:
                        K.op("pe", lambda e, pA=pA, sl=sl: e.matmul(pA[:, 128:256], lhsT=kT[:, sl], rhs=qT[:, sl], start=True, stop=True),
                             reads=["kT", "qT"], writes=[kA])
                        K.op("dve", lambda e, d_=d_, pA=pA: e.tensor_tensor(Mqk[d_][:], pA[:, 128:256], DTi[d_][:], op=ALU.mult),
                             reads=[kA, "DTi" + dk_], writes=["Mqk" + dk_])
                    pB = (psb if NMODE == "bf16" else ps)[d_ * 4 + 1]; kB = "ps%d" % (d_ * 4 + 1)
                    K.op("pe", lambda e, d_=d_, pB=pB: e.transpose(pB[:, 0:128], Yb[d_][:], identn[:]), reads=[tag + "Y", "identb", "ident"], writes=[kB])
                    K.op("act", lambda e, d_=d_, pB=pB: e.copy(Ytb[d_][:], pB[:, 0:128]), reads=[kB], writes=[tag + "Yt"])
                    stop_at("y@%d.%d" % (step, d_))
                    nm_in_Y = Yb[d_]; nm_in_Yt = Ytb[d_]
                    AinvT, kAinv = neumann(nm_in_Y, nm_in_Yt, tag, d_ * 4 + 1)
                    stop_at("nm@%d.%d" % (step, d_))
                    K.op("pe", lambda e, d_=d_, pC=pC, sl=sl: e.matmul(pC[:, 0:128], lhsT=kT[:, sl], rhs=Sb[d_][:], start=True, stop=True),
                         reads=["kT", "Sb" + dk_], writes=[kC])
                    if want_o:
                        K.op("pe", lambda e, d_=d_, pC=pC, sl=sl: e.matmul(pC[:, 128:256], lhsT=qT[:, sl], rhs=Sb[d_][:], start=True, stop=True),
                             reads=["qT", "Sb" + dk_], writes=[kC])
                    K.op("dve", lambda e, i=i, r=r, d_=d_, pC=pC: e.scalar_tensor_tensor(
                        Rb[d_][:], pC[:, 0:128], negeG[:, i, r:r + 1], vtok[:, i, :], op0=ALU.mult, op1=ALU.add),
                        reads=[kC, "negeG", "vtok"], writes=["Rb" + dk_])
                    if want_o:
                        K.op("act", lambda e, i=i, r=r, d_=d_, pC=pC: e.activation(QSe[d_][:], pC[:, 128:256], AF.Identity, scale=eG[:, i, r:r + 1]),
                             reads=[kC, "eG"], writes=["QSe" + dk_])
                    if want_o:
                        stop_at("s_o0")
                    K.op("pe", lambda e, d_=d_, pC=pC, AinvT=AinvT: e.matmul(pC[:, 256:384], lhsT=AinvT, rhs=Rb[d_][:], start=True, stop=True),
                         reads=[kAinv, "Rb" + dk_], writes=[kC])
                    K.op("dve", lambda e, i=i, r=r, d_=d_, pC=pC: e.tensor_scalar(Xb[d_][:], pC[:, 256:384], beta[:, i, r:r + 1], None, op0=ALU.mult),
                         reads=[kC, "beta"], writes=["Xb" + dk_])
                    K.op("pool", lambda e, i=i, r=r, d_=d_: e.tensor_scalar(Xs[d_][:], Xb[d_][:], eTG[:, i, r:r + 1], None, op0=ALU.mult),
                         reads=["Xb" + dk_, "eTG"], writes=["Xs" + dk_])
                    if want_o:
                        stop_at("s_o1")
                        K.op("pe", lambda e, d_=d_, pC=pC: e.matmul(pC[:, 384:512], lhsT=Mqk[d_][:], rhs=Xb[d_][:], start=True, stop=True),
                             reads=["Mqk" + dk_, "Xb" + dk_], writes=[kC])
                        stop_at("s_o2")
                        K.op("dve", lambda e, i=i, d_=d_, pC=pC: e.tensor_tensor(obuf[d_][:, i, :], pC[:, 384:512], QSe[d_][:], op=ALU.add),
                             reads=[kC, "QSe" + dk_], writes=["obuf" + dk_])
                        stop_at("s_o3")
                    stop_at("x@%d.%d" % (step, d_))
                    pD = ps[d_ * 4 + 2]; kD = "ps%d" % (d_ * 4 + 2)
                    K.op("pe", lambda e, i=i, d_=d_, pD=pD: e.matmul(pD[:, 0:128], lhsT=ktok[:, i, :], rhs=Xs[d_][:], start=True, stop=True),
                         reads=["ktok", "Xs" + dk_], writes=[kD])
                    K.op("dve", lambda e, i=i, r=r, d_=d_, pD=pD: e.scalar_tensor_tensor(
                        S[d_][:], S[d_][:], eTot[:, i, r:r + 1], pD[:, 0:128], op0=ALU.mult, op1=ALU.add),
                        reads=["S" + dk_, "eTot", kD], writes=["S" + dk_])
                    K.op("act", lambda e, d_=d_: e.copy(Sb[d_][:], S[d_][:]), reads=["S" + dk_], writes=["Sb" + dk_])
                    stop_at("s@%d.%d" % (step, d_))
                    if d_ == 1:
                        stop_at("s_step%d" % step)
            stop_at("gdn_scan")
            for i in range(2, NT):
                K.op("dve", lambda e, i=i: e.tensor_tensor(on_[:], obuf[0][:, i, :], obuf[1][:, i, :], op=ALU.add),
                     reads=["obuf0", "obuf1"], writes=["on_"])
                K.op("act", lambda e: e.activation(oj[:], on_[:], AF.Square), reads=["on_"], writes=["oj"])
                K.op("dve", lambda e: e.reduce_sum(oss[:, 0:1], oj[:], axis=AX.X), reads=["oj"], writes=["oss"])
                K.op("act", lambda e: e.activation(oss[:, 1:2], oss[:, 0:1], AF.Sqrt, bias=eps_t[:, 0:1], scale=1.0 / 128), reads=["oss", "eps"], writes=["oss1"])
                K.op("dve", lambda e: e.reciprocal(oss[:, 2:3], oss[:, 1:2]), reads=["oss1"], writes=["oss2"])
                K.op("dve", lambda e: e.tensor_scalar(onn[:], on_[:], oss[:, 2:3], None, op0=ALU.mult), reads=["on_", "oss2"], writes=["onn"])
                pt = ps[i % 2]; pk = "ps%d" % (i % 2)
                K.op("pe", lambda e, pt=pt: e.transpose(pt[:, 0:128], onn[:], ident[:]), reads=["onn", "ident"], writes=[pk])
                gcol = pk2[:, PK2["GNORM"]:PK2["GNORM"] + 1]
                K.op("dve", lambda e, i=i, pt=pt, gcol=gcol: e.scalar_tensor_tensor(
                    mst[:, (i - 2) * 128:(i - 1) * 128], pt[:, 0:128], gcol, zs[:, i * 128:(i + 1) * 128], op0=ALU.mult, op1=ALU.mult),
                    reads=[pk, "pk2", "zs"], writes=["mst"])
            K.dma(mT_d[h, :, :], mst[:, :], reads=["mst"], writes=[("mT", h)], key="st_mst")

    K.barrier()
    pm_d = nc.dram_tensor("pm_s", [12, 128, T], F32).ap()
    lora_d = nc.dram_tensor("lora_s", [3, 128, T], BF16).ap()
    CW_ = float(np.exp(-0.5))
    NRW = n_rows
    with ExitStack() as pr:
        cidx_i = sb("cidx_i", [128, 15], I32, stack=pr)
        cidx = sb("cidx", [128, 15], stack=pr)
        mum = {nm: sb("mum_" + nm, [128, 15], stack=pr) for nm in ("om", "L", "R", "U", "D", "P", "N")}
        tmpm = sb("tmpm", [128, 15], stack=pr)
        K.op("pool", lambda e: e.iota(cidx_i[:], pattern=[[128, 15]], base=0, channel_multiplier=1), writes=["cidx_i"])
        K.op("dve", lambda e: e.tensor_copy(cidx[:], cidx_i[:]), reads=["cidx_i"], writes=["cidx"])
        mu_ap = pk2[:, PK2["MU"]:PK2["MU"] + 15]
        K.op("dve", lambda e: e.tensor_scalar(mum["om"][:], mu_ap, -1.0, 1.0, op0=ALU.mult, op1=ALU.add), reads=["pk2"], writes=["mum"])

        def band(nm, lo, hi):
            K.op("dve", lambda e: e.tensor_scalar(tmpm[:], cidx[:], float(lo), None, op0=ALU.is_ge), reads=["cidx"], writes=["tmpm"])
            K.op("dve", lambda e: e.scalar_tensor_tensor(tmpm[:], cidx[:], float(hi), tmpm[:], op0=ALU.is_lt, op1=ALU.mult), reads=["cidx", "tmpm"], writes=["tmpm"])
            K.op("dve", lambda e: e.tensor_tensor(mum[nm][:], tmpm[:], mu_ap, op=ALU.mult), reads=["tmpm", "pk2"], writes=["mum"])

        band("L", 0, 480); band("R", 480, 960); band("U", 960, 1440); band("D", 1440, 1920)
        band("P", 0, 960); band("N", 960, 1920)
        pins = [sb("rpin%d" % i, [128, T], stack=pr) for i in range(2)]
        pmx = [sb("pmx%d" % i, [128, T], stack=pr) for i in range(2)]
        lob = sb("lob", [128, T], BF16, stack=pr)
        for j in range(15):
            pin_ = pins[j % 2]; pk_ = "rpin%d" % (j % 2)
            po = pmx[j % 2]; ok_ = "pmx%d" % (j % 2)
            K.dma(pin_[:, :], pT_d[17 + j, :, :], reads=[("pT", 17 + j)], writes=[pk_], key=pk_)
            K.op("dve", lambda e, j=j, pin_=pin_, po=po: e.tensor_scalar(po[:], pin_[:], mum["om"][:, j:j + 1], None, op0=ALU.mult),
                 reads=[pk_, "mum"], writes=[ok_])
            c0, c1 = j * 128, j * 128 + 128

            def has(lo, hi):
                return c0 < hi and c1 > lo

            def acc(dst, src, nm, j=j, pin_=pin_, po=po, pk_=pk_, ok_=ok_, eng="dve"):
                K.op("dve", lambda e: e.scalar_tensor_tensor(dst(po), src(pin_), mum[nm][:, j:j + 1], dst(po), op0=ALU.mult, op1=ALU.add),
                     reads=[pk_, "mum", ok_], writes=[ok_])

            lat = lambda t: t[:, TC:T].rearrange("p (r w) -> p r w", w=64)
            if has(0, 960):
                acc(lambda t: t[:, 1:TC], lambda t: t[:, 0:TC - 1], "P")
            if has(960, 1920):
                acc(lambda t: t[:, 0:TC - 1], lambda t: t[:, 1:TC], "N")
            if has(0, 480):
                acc(lambda t: lat(t)[:, :, 1:64], lambda t: lat(t)[:, :, 0:63], "L")
            if has(480, 960):
                acc(lambda t: lat(t)[:, :, 0:63], lambda t: lat(t)[:, :, 1:64], "R")
            if has(960, 1440) and NRW > 1:
                acc(lambda t: lat(t)[:, 1:NRW, :], lambda t: lat(t)[:, 0:NRW - 1, :], "U")
            if has(1440, 1920) and NRW > 1:
                acc(lambda t: lat(t)[:, 0:NRW - 1, :], lambda t: lat(t)[:, 1:NRW, :], "D")
            if j < 12:
                K.dma(pm_d[j, :, :], po[:, :], reads=[ok_], writes=[("pm", j)], key="st_" + ok_)
            else:
                fn_ = {12: AF.Tanh, 13: AF.Identity, 14: AF.Sigmoid}[j]
                K.op("act", lambda e, po=po, fn_=fn_: e.activation(lob[:], po[:], fn_), reads=[ok_], writes=["lob"])
                K.dma(lora_d[j - 12, :, :], lob[:, :], reads=["lob"], writes=[("lora", j - 12)], key="st_lob")
    K.barrier()
    stop_at("rw_mix")
    if "pm" in dbg:
        d_o = dbgt("pm", [12, 128, T])
        K.dma(d_o[:, :, :], pm_d[:, :, :], reads=[("pm", j) for j in range(12)], writes=["dbgpm"], key="dbg")

    with ExitStack() as pw:
        nm_tiles.clear()
        RW_NDT = BF16 if RW_NM[0] == "bf16" else F32
        for tag in ("n0", "n1"):
            nm_tiles[tag] = dict(PR=[sb(tag + "rPR%d" % i, [128, 256], NDT, stack=pw) for i in range(2)],
                                 Pt=[sb(tag + "rPt%d" % i, [128, 128], NDT, stack=pw) for i in range(2)],
                                 fin=sb(tag + "rfin", [128, 128], BF16, stack=pw))
        BL = min(256, T)
        blocks = [(b0, min(BL, T - b0)) for b0 in range(0, T, BL)]
        wst_ = sb("rw_wst", [128, 3, 512], stack=pw)
        wlb = sb("rw_wlb", [128, 3, 512], BF16, stack=pw)
        for q_, src in enumerate((w2_d, a2_d, g2w_d)):
            K.dma(wst_[:, q_, :], src[:, :], writes=["rw_wst"], key="rw_wst")
        K.op("dve", lambda e: e.tensor_copy(wlb[:], wst_[:]), reads=["rw_wst"], writes=["wlb"])
        bones = sb("bones", [128, 128], stack=pw)
        K.op("pool", lambda e: e.memset(bones[:], 0.0), writes=["bones"])
        K.op("pool", lambda e: e.memset(bones[0:64, 0:64], 1.0), writes=["bones"])
        K.op("pool", lambda e: e.memset(bones[64:128, 64:128], 1.0), writes=["bones"])
        rmask = sb("rmask", [128, BL], stack=pw)
        K.op("pool", lambda e: e.memset(rmask[:], 1.0), writes=["rmask"])
        K.op("pool", lambda e: e.memset(rmask[:].rearrange("p (n t) -> p n t", t=128)[:, :, 0:1], 0.0), writes=["rmask"])
        mask4 = [sb("mask4_%d" % d_, [128, 512], stack=pw) for d_ in range(2)]
        for d_ in range(2):
            ms_, mi_ = (m_lt, m_le) if d_ == 0 else (m_gt, m_ge)
            for q_ in range(4):
                src = ms_ if q_ % 2 == 0 else mi_
                K.op("dve", lambda e, d_=d_, q_=q_, src=src: e.tensor_copy(mask4[d_][:, q_ * 128:(q_ + 1) * 128], src[:]),
                     reads=["m_lt", "m_le", "m_gt", "m_ge"], writes=["mask4"])
        rT = [sb("rT%d" % d_, [128, T], BF16, stack=pw) for d_ in range(2)]
        kpT = [sb("kpT%d" % d_, [128, T], BF16, stack=pw) for d_ in range(2)]
        ktT = [sb("ktT%d" % d_, [128, T], BF16, stack=pw) for d_ in range(2)]
        nbT = [sb("nbT%d" % d_, [128, T], BF16, stack=pw) for d_ in range(2)]
        Lam = [sb("Lam%d" % d_, [128, NT], stack=pw) for d_ in range(2)]
        vtk = sb("rvtok", [128, NT, 128], BF16, stack=pw)
        bonus = sb("bonus", [128, T], BF16, stack=pw)
        gateT = sb("gateT", [128, T], BF16, stack=pw)
        ybuf = [sb("ybuf%d" % d_, [128, NT, 128], BF16, stack=pw) for d_ in range(2)]
        rmst = sb("rmst", [128, TL], BF16, stack=pw)
        bt = {nm: sb("b_" + nm, [128, BL], stack=pw) for nm in
              ("r", "k", "v", "kap", "sig", "cum", "w", "iw", "wp", "a", "t1", "t2", "rk")}
        lbt = sb("b_lora", [128, 3, BL], BF16, stack=pw)
        vb16 = sb("b_vb16", [128, BL], BF16, stack=pw)
        Z = [sb("Z%d" % d_, [128, 128], stack=pw) for d_ in range(2)]
        Zb = [sb("Zb%d" % d_, [128, 128], BF16, stack=pw) for d_ in range(2)]
        AK = [sb("AK%d" % d_, [128, 512], BF16, stack=pw) for d_ in range(2)]
        ANB = [sb("ANB%d" % d_, [128, 512], BF16, stack=pw) for d_ in range(2)]
        YY = [[sb("YY%d%d" % (d_, hh), [128, 128], NDT, stack=pw) for hh in range(2)] for d_ in range(2)]
        YYt = [[sb("YYt%d%d" % (d_, hh), [128, 128], NDT, stack=pw) for hh in range(2)] for d_ in range(2)]
        AinvS = [[sb("Ainv%d%d" % (d_, hh), [128, 128], BF16, stack=pw) for hh in range(2)] for d_ in range(2)]
        ktok_ = [sb("rktok%d" % d_, [128, 128], BF16, stack=pw) for d_ in range(2)]
        nbtok_ = [sb("rnbtok%d" % d_, [128, 128], BF16, stack=pw) for d_ in range(2)]
        P1b = [sb("P1b%d" % d_, [128, 128], BF16, stack=pw) for d_ in range(2)]
        Ub = [sb("Ub%d" % d_, [128, 128], BF16, stack=pw) for d_ in range(2)]
        zt = [sb("zt%d" % d_, [128, 128], stack=pw) for d_ in range(2)]
        yo = sb("yo", [128, 128], stack=pw)
        yc = sb("yc", [128, 128], stack=pw)
        ysq = sb("ysq", [128, 128], stack=pw)
        yst = sb("yst", [128, 8], stack=pw)
        yt2 = sb("yt2", [128, 128], stack=pw)

        for P in range(4):
            ch = slice(P * 128, (P + 1) * 128)
            for (b0, bn) in blocks:
                bs = slice(b0, b0 + bn)
                ntb = bn // 128
                for nm, jj in (("r", P), ("k", 4 + P), ("v", 8 + P)):
                    K.dma(bt[nm][:, 0:bn], pm_d[jj, :, bs], reads=[("pm", jj)], writes=["b_" + nm], key="b_" + nm)
                K.dma(lbt[:, :, 0:bn], lora_d[:, :, bs].rearrange("q p t -> p q t"), reads=[("lora", 0), ("lora", 1), ("lora", 2)], writes=["b_lora"], key="b_lora")
                K.op("pool", lambda e, bn=bn: e.tensor_copy(vb16[:, 0:bn], bt["v"][:, 0:bn]), reads=["b_v"], writes=["vb16"])
                for ii in range(ntb):
                    gi = b0 // 128 + ii
                    pt = psb[6 + (ii % 2)]; pk = "ps%d" % (6 + (ii % 2))
                    K.op("pe", lambda e, ii=ii, pt=pt: e.transpose(pt[:, 0:128], vb16[:, ii * 128:(ii + 1) * 128], identb[:]), reads=["vb16", "identb"], writes=[pk])
                    K.op("act", lambda e, gi=gi, pt=pt: e.copy(vtk[:, gi, :], pt[:, 0:128]), reads=[pk], writes=["rvtok"])
                kkc = pk2[:, PK2["KK"] + P:PK2["KK"] + P + 1]
                K.op("dve", lambda e, bn=bn, kkc=kkc: e.tensor_scalar(bt["kap"][:, 0:bn], bt["k"][:, 0:bn], kkc, None, op0=ALU.mult), reads=["b_k", "pk2"], writes=["b_kap"])
                K.op("pool", lambda e, bn=bn: e.tensor_tensor(bt["t1"][:, 0:bn], bt["kap"][:, 0:bn], bt["kap"][:, 0:bn], op=ALU.mult), reads=["b_kap"], writes=["b_t1"])
                for n in range((bn + 511) // 512):
                    t0 = n * 512; tn = min(512, bn - t0)
                    K.op("pe", lambda e, t0=t0, tn=tn: e.matmul(ps[0][:, 0:tn], lhsT=bones[:], rhs=bt["t1"][:, t0:t0 + tn], start=True, stop=True), reads=["bones", "b_t1"], writes=["ps0"])
                    K.op("act", lambda e, t0=t0, tn=tn: e.activation(bt["t2"][:, t0:t0 + tn], ps[0][:, 0:tn], AF.Sqrt, bias=eps_t[:, 1:2]), reads=["ps0", "eps"], writes=["b_t2"])
                K.op("dve", lambda e, bn=bn: e.reciprocal(bt["t2"][:, 0:bn], bt["t2"][:, 0:bn]), reads=["b_t2"], writes=["b_t2"])
                K.op("dve", lambda e, bn=bn: e.tensor_tensor(bt["kap"][:, 0:bn], bt["kap"][:, 0:bn], bt["t2"][:, 0:bn], op=ALU.mult), reads=["b_kap", "b_t2"], writes=["b_kap"])
                for n in range((bn + 511) // 512):
                    t0 = n * 512; tn = min(512, bn - t0)
                    K.op("pe", lambda e, t0=t0, tn=tn: e.matmul(ps[1][:, 0:tn], lhsT=wlb[:, 2, ch], rhs=lbt[:, 2, t0:t0 + tn], start=True, stop=True), reads=["wlb", "b_lora"], writes=["ps1"])
                    K.op("act", lambda e, t0=t0, tn=tn: e.copy(gateT[:, b0 + t0:b0 + t0 + tn], ps[1][:, 0:tn]), reads=["ps1"], writes=["gateT"])
                first_rk = True
                for d_ in range(2):
                    ds = slice(d_ * 64, (d_ + 1) * 64)
                    w0c = pk2[:, PK2["W0"] + d_ * 4 + P:PK2["W0"] + d_ * 4 + P + 1]
                    a0c = pk2[:, PK2["A0"] + d_ * 4 + P:PK2["A0"] + d_ * 4 + P + 1]
                    for n in range((bn + 511) // 512):
                        t0 = n * 512; tn = min(512, bn - t0)
                        K.op("pe", lambda e, t0=t0, tn=tn, ds=ds: e.matmul(ps[2][:, 0:tn], lhsT=wlb[ds, 0, ch], rhs=lbt[ds, 0, t0:t0 + tn], start=True, stop=True), reads=["wlb", "b_lora"], writes=["ps2"])
                        K.op("act", lambda e, t0=t0, tn=tn, w0c=w0c: e.activation(bt["sig"][:, t0:t0 + tn], ps[2][:, 0:tn], AF.Sigmoid, bias=w0c), reads=["ps2", "pk2"], writes=["b_sig"])
                        K.op("pe", lambda e, t0=t0, tn=tn, ds=ds: e.matmul(ps[3][:, 0:tn], lhsT=wlb[ds, 1, ch], rhs=lbt[ds, 1, t0:t0 + tn], start=True, stop=True), reads=["wlb", "b_lora"], writes=["ps3"])
                        K.op("act", lambda e, t0=t0, tn=tn, a0c=a0c: e.activation(bt["a"][:, t0:t0 + tn], ps[3][:, 0:tn], AF.Sigmoid, bias=a0c), reads=["ps3", "pk2"], writes=["b_a"])
                    K.op("dve", lambda e, bn=bn: e.tensor_tensor_scan(bt["cum"][:, 0:bn], rmask[:, 0:bn], bt["sig"][:, 0:bn], 0.0, op0=ALU.mult, op1=ALU.add),
                         reads=["rmask", "b_sig"], writes=["b_cum"])
                    c3 = bt["cum"][:, 0:bn].rearrange("p (n t) -> p n t", t=128)
                    tot_b = c3[:, :, 127:128].to_broadcast([128, ntb, 128])
                    K.op("act", lambda e, d_=d_, c3=c3, ntb=ntb: e.activation(Lam[d_][:, b0 // 128:b0 // 128 + ntb], c3[:, :, 127], AF.Exp, scale=-CW_),
                         reads=["b_cum"], writes=["Lam%d" % d_])
                    if d_ == 1:
                        K.op("dve", lambda e, bn=bn, c3=c3, tot_b=tot_b, ntb=ntb: e.tensor_tensor(
                            bt["t1"][:, 0:bn].rearrange("p (n t) -> p n t", t=128), tot_b, c3, op=ALU.subtract), reads=["b_cum"], writes=["b_t1"])
                        K.op("dve", lambda e, bn=bn: e.tensor_tensor(bt["cum"][:, 0:bn], bt["t1"][:, 0:bn], bt["sig"][:, 0:bn], op=ALU.add),
                             reads=["b_t1", "b_sig"], writes=["b_cum"])
                    K.op("act", lambda e, bn=bn: e.activation(bt["w"][:, 0:bn], bt["cum"][:, 0:bn], AF.Exp, scale=-CW_), reads=["b_cum"], writes=["b_w"])
                    K.op("act", lambda e, bn=bn: e.activation(bt["iw"][:, 0:bn], bt["cum"][:, 0:bn], AF.Exp, scale=CW_), reads=["b_cum"], writes=["b_iw"])
                    K.op("pool", lambda e, bn=bn: e.tensor_tensor(bt["t1"][:, 0:bn], bt["cum"][:, 0:bn], bt["sig"][:, 0:bn], op=ALU.subtract), reads=["b_cum", "b_sig"], writes=["b_t1"])
                    K.op("act", lambda e, bn=bn: e.activation(bt["wp"][:, 0:bn], bt["t1"][:, 0:bn], AF.Exp, scale=-CW_), reads=["b_t1"], writes=["b_wp"])
                    K.op("dve", lambda e, d_=d_, bn=bn: e.tensor_tensor(rT[d_][:, bs], bt["r"][:, 0:bn], bt["w"][:, 0:bn], op=ALU.mult), reads=["b_r", "b_w"], writes=["rT%d" % d_])
                    K.op("pool", lambda e, d_=d_, bn=bn: e.tensor_tensor(kpT[d_][:, bs], bt["kap"][:, 0:bn], bt["wp"][:, 0:bn], op=ALU.mult), reads=["b_kap", "b_wp"], writes=["kpT%d" % d_])
                    kac = pk2[:, PK2["KA"] + P:PK2["KA"] + P + 1]
                    K.op("dve", lambda e, bn=bn, kac=kac: e.tensor_scalar(bt["t1"][:, 0:bn], bt["a"][:, 0:bn], -1.0, kac, op0=ALU.add, op1=ALU.mult), reads=["b_a", "pk2"], writes=["b_t1"])
                    K.op("dve", lambda e, bn=bn: e.scalar_tensor_tensor(bt["t1"][:, 0:bn], bt["t1"][:, 0:bn], 1.0, bt["k"][:, 0:bn], op0=ALU.add, op1=ALU.mult), reads=["b_t1", "b_k"], writes=["b_t1"])
                    K.op("dve", lambda e, d_=d_, bn=bn: e.tensor_tensor(ktT[d_][:, bs], bt["t1"][:, 0:bn], bt["iw"][:, 0:bn], op=ALU.mult), reads=["b_t1", "b_iw"], writes=["ktT%d" % d_])
                    if first_rk:
                        K.op("pool", lambda e, bn=bn: e.tensor_tensor(bt["rk"][:, 0:bn], bt["t1"][:, 0:bn], bt["r"][:, 0:bn], op=ALU.mult), reads=["b_t1", "b_r"], writes=["b_rk"])
                        first_rk = False
                    else:
                        K.op("pool", lambda e, bn=bn: e.tensor_tensor(bt["t2"][:, 0:bn], bt["t1"][:, 0:bn], bt["r"][:, 0:bn], op=ALU.mult), reads=["b_t1", "b_r"], writes=["b_t2"])
                        K.op("pool", lambda e, bn=bn: e.tensor_tensor(bt["rk"][:, 0:bn], bt["rk"][:, 0:bn], bt["t2"][:, 0:bn], op=ALU.add), reads=["b_rk", "b_t2"], writes=["b_rk"])
                    K.op("dve", lambda e, bn=bn: e.scalar_tensor_tensor(bt["t2"][:, 0:bn], bt["kap"][:, 0:bn], -1.0, bt["a"][:, 0:bn], op0=ALU.mult, op1=ALU.mult), reads=["b_kap", "b_a"], writes=["b_t2"])
                    K.op("dve", lambda e, d_=d_, bn=bn: e.tensor_tensor(nbT[d_][:, bs], bt["t2"][:, 0:bn], bt["iw"][:, 0:bn], op=ALU.mult), reads=["b_t2", "b_iw"], writes=["nbT%d" % d_])
                rkc = pk2[:, PK2["RK"] + P:PK2["RK"] + P + 1]
                K.op("dve", lambda e, bn=bn, rkc=rkc: e.tensor_scalar(bt["rk"][:, 0:bn], bt["rk"][:, 0:bn], rkc, None, op0=ALU.mult), reads=["b_rk", "pk2"], writes=["b_rk"])
                for n in range((bn + 511) // 512):
                    t0 = n * 512; tn = min(512, bn - t0)
                    K.op("pe", lambda e, t0=t0, tn=tn: e.matmul(ps[4][:, 0:tn], lhsT=bones[:], rhs=bt["rk"][:, t0:t0 + tn], start=True, stop=True), reads=["bones", "b_rk"], writes=["ps4"])
                    K.op("dve", lambda e, t0=t0, tn=tn: e.tensor_tensor(bonus[:, b0 + t0:b0 + t0 + tn], ps[4][:, 0:tn], bt["v"][:, t0:t0 + tn], op=ALU.mult), reads=["ps4", "b_v"], writes=["bonus"])
            stop_at("rw_prep")
            for d_ in range(2):
                K.op("pool", lambda e, d_=d_: e.memset(Z[d_][:], 0.0), writes=["Z%d" % d_])
                K.op("pool", lambda e, d_=d_: e.memset(Zb[d_][:], 0.0), writes=["Zb%d" % d_])
            for step in range(NT):
                for d_ in K.streams(2):
                    i = fwd_order[step] if d_ == 0 else bwd_order[step]
                    sl = slice(i * 128, (i + 1) * 128)
                    want_o = i >= 2
                    dk_ = "%d" % d_
                    tag = "n%d" % d_
                    b_ = d_ * 4
                    for hh in range(2):
                        hs = slice(hh * 64, (hh + 1) * 64)
                        pt = ps[b_ + hh]; pk = "ps%d" % (b_ + hh)
                        for qq, src in enumerate((ktT, nbT)):
                            K.op("pe", lambda e, src=src, pt=pt, qq=qq, hs=hs, d_=d_, sl=sl: e.matmul(pt[:, qq * 256:qq * 256 + 128], lhsT=src[d_][hs, sl], rhs=kpT[d_][hs, sl], start=True, stop=True),
                                 reads=["ktT" + dk_, "nbT" + dk_, "kpT" + dk_], writes=[pk])
                            K.op("pe", lambda e, src=src, pt=pt, qq=qq, hs=hs, d_=d_, sl=sl: e.matmul(pt[:, qq * 256 + 128:qq * 256 + 256], lhsT=src[d_][hs, sl], rhs=rT[d_][hs, sl], start=True, stop=True),
                                 reads=["ktT" + dk_, "nbT" + dk_, "rT" + dk_], writes=[pk])
                    for hh in range(2):
                        K.op("dve", lambda e, d_=d_, hh=hh: e.tensor_tensor(AK[d_][:, hh * 256:(hh + 1) * 256], ps[d_ * 4 + hh][:, 0:256], mask4[d_][:, 0:256], op=ALU.mult),
                             reads=["ps%d" % (b_ + hh), "mask4"], writes=["AK" + dk_])
                        K.op("dve", lambda e, d_=d_, hh=hh: e.tensor_tensor(ANB[d_][:, hh * 256:(hh + 1) * 256], ps[d_ * 4 + hh][:, 256:512], mask4[d_][:, 0:256], op=ALU.mult),
                             reads=["ps%d" % (b_ + hh), "mask4"], writes=["ANB" + dk_])
                    pT2 = psb[b_ + 2]; kT2 = "ps%d" % (b_ + 2)
                    K.op("pe", lambda e, d_=d_, sl=sl, pT2=pT2: e.transpose(pT2[:, 0:128], ktT[d_][:, sl], identb[:]), reads=["ktT" + dk_, "identb"], writes=[kT2])
                    K.op("pe", lambda e, d_=d_, sl=sl, pT2=pT2: e.transpose(pT2[:, 128:256], nbT[d_][:, sl], identb[:]), reads=["nbT" + dk_, "identb"], writes=[kT2])
                    K.op("act", lambda e, d_=d_, pT2=pT2: e.copy(ktok_[d_][:], pT2[:, 0:128]), reads=[kT2], writes=["rktok" + dk_])
                    K.op("act", lambda e, d_=d_, pT2=pT2: e.copy(nbtok_[d_][:], pT2[:, 128:256]), reads=[kT2], writes=["rnbtok" + dk_])
                    for hh in range(2):
                        K.op("pool", lambda e, d_=d_, hh=hh: e.tensor_copy(YY[d_][hh][:], ANB[d_][:, hh * 256:hh * 256 + 128]), reads=["ANB" + dk_], writes=[tag + "Y"])
                        pB = (psb if NMODE == "bf16" else ps)[b_ + 2]
                        K.op("pe", lambda e, d_=d_, hh=hh, pB=pB: e.transpose(pB[:, 0:128], YY[d_][hh][:], identn[:]), reads=[tag + "Y", "identb", "ident"], writes=[kT2])
                        K.op("act", lambda e, d_=d_, hh=hh, pB=pB: e.copy(YYt[d_][hh][:], pB[:, 0:128]), reads=[kT2], writes=[tag + "Yt"])
                        Ai, kAi = neumann(YY[d_][hh], YYt[d_][hh], tag, b_ + 1)
                        K.op("dve", lambda e, d_=d_, hh=hh, Ai=Ai: e.tensor_copy(AinvS[d_][hh][:], Ai), reads=[kAi], writes=["Ainv%d%d" % (d_, hh)])
                    pC = ps[b_ + 3]; kC = "ps%d" % (b_ + 3)
                    for hh in range(2):
                        K.op("pe", lambda e, d_=d_, hh=hh, i=i, pC=pC: e.matmul(pC[:, hh * 64:(hh + 1) * 64], lhsT=AK[d_][:, hh * 256:hh * 256 + 128], rhs=vtk[:, i, hh * 64:(hh + 1) * 64], start=(hh == 0), stop=False),
                             reads=["AK" + dk_, "rvtok"], writes=[kC])
                    K.op("pe", lambda e, d_=d_, sl=sl, pC=pC: e.matmul(pC[:, 0:128], lhsT=kpT[d_][:, sl], rhs=Zb[d_][:], start=False, stop=True), reads=["kpT" + dk_, "Zb" + dk_], writes=[kC])
                    K.op("act", lambda e, d_=d_, pC=pC: e.copy(P1b[d_][:], pC[:, 0:128]), reads=[kC], writes=["P1b" + dk_])
                    for hh in range(2):
                        K.op("pe", lambda e, d_=d_, hh=hh, pC=pC: e.matmul(pC[:, 128 + hh * 64:128 + (hh + 1) * 64], lhsT=AinvS[d_][hh][:], rhs=P1b[d_][:, hh * 64:(hh + 1) * 64], start=True, stop=True),
                             reads=["Ainv%d%d" % (d_, hh), "P1b" + dk_], writes=[kC])
                    K.op("dve", lambda e, d_=d_, pC=pC: e.tensor_copy(Ub[d_][:], pC[:, 128:256]), reads=[kC], writes=["Ub" + dk_])
                    if want_o:
                        for hh in range(2):
                            K.op("pe", lambda e, d_=d_, hh=hh, i=i, pC=pC: e.matmul(pC[:, 256 + hh * 64:256 + (hh + 1) * 64], lhsT=AK[d_][:, hh * 256 + 128:hh * 256 + 256], rhs=vtk[:, i, hh * 64:(hh + 1) * 64], start=(hh == 0), stop=False),
                                 reads=["AK" + dk_, "rvtok"], writes=[kC])
                        K.op("pe", lambda e, d_=d_, sl=sl, pC=pC: e.matmul(pC[:, 256:384], lhsT=rT[d_][:, sl], rhs=Zb[d_][:], start=False, stop=False), reads=["rT" + dk_, "Zb" + dk_], writes=[kC])
                        for hh in range(2):
                            K.op("pe", lambda e, d_=d_, hh=hh, pC=pC: e.matmul(pC[:, 256 + hh * 64:256 + (hh + 1) * 64], lhsT=ANB[d_][:, hh * 256 + 128:hh * 256 + 256], rhs=Ub[d_][:, hh * 64:(hh + 1) * 64], start=False, stop=(hh == 1)),
                                 reads=["ANB" + dk_, "Ub" + dk_], writes=[kC])
                        K.op("act", lambda e, d_=d_, i=i, pC=pC: e.copy(ybuf[d_][:, i, :], pC[:, 256:384]), reads=[kC], writes=["ybuf" + dk_])
                    pD = ps[b_ + 2]; kD = "ps%d" % (b_ + 2)
                    K.op("pe", lambda e, d_=d_, i=i, pD=pD: e.matmul(pD[:, 256:384], lhsT=ktok_[d_][:], rhs=vtk[:, i, :], start=True, stop=False), reads=["rktok" + dk_, "rvtok"], writes=[kD])
                    K.op("pe", lambda e, d_=d_, pD=pD: e.matmul(pD[:, 256:384], lhsT=nbtok_[d_][:], rhs=Ub[d_][:], start=False, stop=True), reads=["rnbtok" + dk_, "Ub" + dk_], writes=[kD])
                    K.op("dve", lambda e, d_=d_, pD=pD: e.tensor_tensor(zt[d_][:], pD[:, 256:384], Z[d_][:], op=ALU.add), reads=[kD, "Z" + dk_], writes=["zt" + dk_])
                    K.op("dve", lambda e, d_=d_, i=i: e.scalar_tensor_tensor(Z[d_][:], zt[d_][:], Lam[d_][:, i:i + 1], bones[:], op0=ALU.mult, op1=ALU.mult),
                         reads=["zt" + dk_, "Lam" + dk_, "bones"], writes=["Z" + dk_])
                    K.op("act", lambda e, d_=d_: e.copy(Zb[d_][:], Z[d_][:]), reads=["Z" + dk_], writes=["Zb" + dk_])
            stop_at("rw_scan")
            gwc = pk2[:, PK2["GNW"] + P:PK2["GNW"] + P + 1]
            gbc = pk2[:, PK2["GNB"] + P:PK2["GNB"] + P + 1]
            for i in range(2, NT):
                K.op("dve", lambda e, i=i: e.tensor_tensor(yo[:], ybuf[0][:, i, :], ybuf[1][:, i, :], op=ALU.add), reads=["ybuf0", "ybuf1"], writes=["yo"])
                y3 = yo[:].rearrange("p (h c) -> p h c", c=64)
                K.op("dve", lambda e, y3=y3: e.reduce_sum(yst[:, 0:2], y3, axis=AX.X), reads=["yo"], writes=["yst0"])
                K.op("dve", lambda e: e.tensor_scalar(yst[:, 2:4], yst[:, 0:2], 1.0 / 64, None, op0=ALU.mult), reads=["yst0"], writes=["yst1"])
                K.op("dve", lambda e, y3=y3: e.tensor_tensor(yc[:].rearrange("p (h c) -> p h c", c=64), y3, yst[:, 2:4].unsqueeze(2).to_broadcast([128, 2, 64]), op=ALU.subtract),
                     reads=["yo", "yst1"], writes=["yc"])
                K.op("act", lambda e: e.activation(ysq[:], yc[:], AF.Square), reads=["yc"], writes=["ysq"])
                K.op("dve", lambda e: e.reduce_sum(yst[:, 4:6], ysq[:].rearrange("p (h c) -> p h c", c=64), axis=AX.X), reads=["ysq"], writes=["yst2"])
                K.op("act", lambda e: e.activation(yst[:, 6:8], yst[:, 4:6], AF.Sqrt, bias=eps_t[:, 2:3], scale=1.0 / 64), reads=["yst2", "eps"], writes=["yst3"])
                K.op("dve", lambda e: e.reciprocal(yst[:, 6:8], yst[:, 6:8]), reads=["yst3"], writes=["yst3"])
                K.op("dve", lambda e: e.tensor_tensor(yt2[:].rearrange("p (h c) -> p h c", c=64), yc[:].rearrange("p (h c) -> p h c", c=64),
                                                      yst[:, 6:8].unsqueeze(2).to_broadcast([128, 2, 64]), op=ALU.mult), reads=["yc", "yst3"], writes=["yt2"])
                pt = ps[i % 2]; pk = "ps%d" % (i % 2)
                K.op("pe", lambda e, pt=pt: e.transpose(pt[:, 0:128], yt2[:], ident[:]), reads=["yt2", "ident"], writes=[pk])
                K.op("act", lambda e, pt=pt: e.activation(yc[:], pt[:, 0:128], AF.Identity, bias=gbc, scale=gwc), reads=[pk, "pk2", "yc"], writes=["yc"])
                K.op("dve", lambda e, i=i: e.tensor_tensor(yc[:], yc[:], bonus[:, i * 128:(i + 1) * 128], op=ALU.add), reads=["yc", "bonus"], writes=["yc"])
                K.op("dve", lambda e, i=i: e.tensor_tensor(rmst[:, (i - 2) * 128:(i - 1) * 128], yc[:], gateT[:, i * 128:(i + 1) * 128], op=ALU.mult), reads=["yc", "gateT"], writes=["rmst"])
            K.dma(mT_d[4 + P, :, :], rmst[:, :], reads=["rmst"], writes=[("mT", 4 + P)], key="st_rmst")
    K.barrier()

    stop_at("mix_done")
    with ExitStack() as pp_:
        mTs = [sb("mTs%d" % i_, [128, 8, 128], BF16, stack=pp_) for i_ in range(2)]
        woutb = sb("woutb", [128, 8, D], BF16, stack=pp_)
        wqb = sb("wqb", [128, 8, 2048], BF16, stack=pp_)
        skT = sb("skT", [128, 16, 128], BF16, stack=pp_)
        with ExitStack() as pset:
            wstg = sb("wstg", [128, 8, 512], stack=pset)
            skst = sb("skst", [128, 16, 128], stack=pset)
            for hf in range(2):
                K.dma(wstg[:, :, :], wout_d[:, hf * 512:(hf + 1) * 512].rearrange("(k p) c -> p k c", p=128), writes=["wstg"], key="wstg")
                K.op("pool", lambda e, hf=hf: e.tensor_copy(woutb[:, :, hf * 512:(hf + 1) * 512], wstg[:]), reads=["wstg"], writes=["woutb"])
            for hf in range(4):
                K.dma(wstg[:, :, :], wq_d[:, hf * 512:(hf + 1) * 512].rearrange("(k p) c -> p k c", p=128), writes=["wstg"], key="wstg")
                K.op("pool", lambda e, hf=hf: e.tensor_copy(wqb[:, :, hf * 512:(hf + 1) * 512], wstg[:]), reads=["wstg"], writes=["wqb"])
            K.dma(skst[:, :, :], sk_d[:, :, :].rearrange("g k d -> k g d"), writes=["skst"], key="skst")
            for g in range(16):
                pt = ps[g % 2]; pk = "ps%d" % (g % 2)
                K.op("pe", lambda e, g=g, pt=pt: e.transpose(pt[:, 0:128], skst[:, g, :], ident[:]), reads=["skst", "ident"], writes=[pk])
                K.op("act", lambda e, g=g, pt=pt: e.copy(skT[:, g, :], pt[:, 0:128]), reads=[pk], writes=["skT"])
            K.barrier()
        gtB = sb("gtB2", [128, 4, D], stack=pp_)
        K.dma(gtB[:].rearrange("p q d -> p (q d)"), gt_d[:, :], reads=["gt_d"], writes=["gtB"], key="gtB2")
        comb_d = nc.dram_tensor("comb_s", [16384, 2 * D], BF16).ap()
        with ExitStack() as pcv:
            cst = [sb("cst%d" % i_, [128, 2, D], stack=pcv) for i_ in range(2)]
            cbf = [sb("cbf%d" % i_, [128, 2 * D], BF16, stack=pcv) for i_ in range(2)]
            for c_ in range(128):
                st_ = cst[c_ % 2]; bf_ = cbf[c_ % 2]
                sk_ = "cst%d" % (c_ % 2); bk_ = "cbf%d" % (c_ % 2)
                K.dma(st_[:, 0, :], down_d[c_ * 128:(c_ + 1) * 128, :], writes=[sk_], key=sk_)
                K.dma(st_[:, 1, :], up_d[c_ * 128:(c_ + 1) * 128, :], writes=[sk_], key=sk_)
                K.op("act", lambda e, st_=st_, bf_=bf_: e.copy(bf_[:, 0:D], st_[:, 0, :]), reads=[sk_], writes=[bk_])
                K.op("dve", lambda e, st_=st_, bf_=bf_: e.tensor_copy(bf_[:, D:2 * D], st_[:, 1, :]), reads=[sk_], writes=[bk_])
                K.dma(comb_d[c_ * 128:(c_ + 1) * 128, :], bf_[:, :], reads=[bk_], writes=["comb"], key="st_" + bk_)
            K.barrier()
        A2row = sb("A2row", [128, D], stack=pp_)
        fgB = sb("fgB", [128, D], stack=pp_)
        K.dma(A2row[:, :], g2row_d.partition_broadcast(128), writes=["A2row"], key="A2row")
        K.dma(fgB[:, :], fng_d.partition_broadcast(128), writes=["fgB"], key="fgB")
        K.op("dve", lambda e: e.scalar_tensor_tensor(A2row[:], gtB[:, 2, :], 1.0, A2row[:], op0=ALU.add, op1=ALU.mult), reads=["gtB", "A2row"], writes=["A2row"])
        iota16i = sb("iota16i", [128, 16], I32, stack=pp_)
        iota16 = sb("iota16", [128, 16], stack=pp_)
        K.op("pool", lambda e: e.iota(iota16i[:], pattern=[[1, 16]], base=0, channel_multiplier=0), writes=["iota16i"])
        K.op("dve", lambda e: e.tensor_copy(iota16[:], iota16i[:]), reads=["iota16i"], writes=["iota16"])

        xt_ = sb("p_xt", [128, D], stack=pp_)
        x1s = [sb("p_x1_%d" % i_, [128, D], stack=pp_) for i_ in range(2)]
        h2bs = [sb("p_h2b_%d" % i_, [128, D], BF16, stack=pp_) for i_ in range(2)]
        eidxs = [sb("p_eidx_%d" % i_, [128, 128], U32, stack=pp_) for i_ in range(2)]
        gflats = [sb("p_gflat_%d" % i_, [128, 128], stack=pp_) for i_ in range(2)]
        pj2 = sb("p_junk2", [128, D], stack=pp_)
        pss2 = sb("p_ss2", [128, 1], stack=pp_)
        prs2 = sb("p_rs2b", [128, 2], stack=pp_)
        pxs2 = sb("p_xs2", [128, D], stack=pp_)
        h2 = sb("p_h2", [128, D], stack=pp_)
        yacc = sb("p_y", [128, D], stack=pp_)
        pj = sb("p_junk", [128, D], stack=pp_)
        pss = sb("p_ss", [128, 1], stack=pp_)
        prs = sb("p_rs", [128, 2], stack=pp_)
        pxs = sb("p_xs", [128, D], stack=pp_)
        h2T = sb("p_h2T", [128, 8, 128], BF16, stack=pp_)
        qTs = sb("p_qT", [128, 16, 128], BF16, stack=pp_)
        scs = sb("p_sc", [128, 16, 128], stack=pp_)
        tmp1 = sb("p_tmp1", [128, 16, 128], stack=pp_)
        tv = sb("p_tv", [128, 16, 16], stack=pp_)
        tiu = sb("p_tiu", [128, 16, 16], U32, stack=pp_)
        tif = sb("p_tif", [128, 16, 16], stack=pp_)
        cand = sb("p_cand", [128, 8, 256], stack=pp_)
        tmp2 = sb("p_tmp2", [128, 8, 256], stack=pp_)
        eq = tmp2
        bsv = sb("p_bs", [128, 8, 16], stack=pp_)
        posu = sb("p_posu", [128, 8, 16], U32, stack=pp_)
        pau = sb("p_pau", [128, 8, 16], U32, stack=pp_)
        pbu = sb("p_pbu", [128, 8, 16], U32, stack=pp_)
        paf = sb("p_paf", [128, 8, 16], stack=pp_)
        pbf = sb("p_pbf", [128, 8, 16], stack=pp_)
        i0f = sb("p_i0f", [128, 8, 16], stack=pp_)
        i1f = sb("p_i1f", [128, 8, 16], stack=pp_)
        gat = sb("p_gate", [128, 8, 16], stack=pp_)
        gsum = sb("p_gsum", [128, 8], stack=pp_)
        apre = sb("p_apre", [128, 128], stack=pp_)
        coef = sb("p_coef", [128, 128], stack=pp_)
        NRB = 6
        rowc = [sb("p_rowc%d" % i, [128, 2 * D], BF16, stack=pp_) for i in range(NRB)]
        pjb = sb("p_junkb", [128, D], BF16, stack=pp_)
        dgs = [sb("p_dg%d" % i, [128, 128], BF16, stack=pp_) for i in range(4)]
        NEG = -1.0e30

        def front(i):
            par = i % 2
            tsl = slice(i * 128, (i + 1) * 128)
            K.dma(xt_[:, :], x_d[tsl, :], writes=["p_xt"], key="p_xt")
            mTt = mTs[i % 2]; mk_ = "mTs%d" % (i % 2)
            K.dma(mTt[:, :, :], mT_d[:, :, tsl].rearrange("k p t -> p k t"), reads=[("mT", j) for j in range(8)], writes=[mk_], key=mk_)
            for hf in range(2):
                pt = ps[2 + hf]; pk = "ps%d" % (2 + hf)
                for k in range(8):
                    K.op("pe", lambda e, k=k, hf=hf, pt=pt, mTt=mTt: e.matmul(pt[:, :], lhsT=mTt[:, k, :], rhs=woutb[:, k, hf * 512:(hf + 1) * 512], start=(k == 0), stop=(k == 7)),
                         reads=[mk_, "woutb"], writes=[pk])
                K.op("dve", lambda e, hf=hf, pt=pt: e.tensor_tensor(x1s[par][:, hf * 512:(hf + 1) * 512], pt[:, :], gtB[:, 0, hf * 512:(hf + 1) * 512], op=ALU.mult),
                     reads=[pk, "gtB"], writes=["p_x1_%d" % par])
            K.op("pool", lambda e: e.tensor_tensor(x1s[par][:], x1s[par][:], xt_[:], op=ALU.add), reads=["p_x1_%d" % par, "p_xt"], writes=["p_x1_%d" % par])
            K.op("act", lambda e: e.activation(pj[:], x1s[par][:], AF.Square), reads=["p_x1_%d" % par], writes=["p_junk"])
            K.op("dve", lambda e: e.reduce_sum(pss[:, 0:1], pj[:], axis=AX.X), reads=["p_junk"], writes=["p_ss"])
            K.op("act", lambda e: e.activation(prs[:, 0:1], pss[:, 0:1], AF.Sqrt, bias=eps_t[:, 0:1], scale=1.0 / D), reads=["p_ss", "eps"], writes=["p_rs"])
            K.op("dve", lambda e: e.reciprocal(prs[:, 1:2], prs[:, 0:1]), reads=["p_rs"], writes=["p_rs2"])
            K.op("dve", lambda e: e.tensor_scalar(pxs[:], x1s[par][:], prs[:, 1:2], None, op0=ALU.mult), reads=["p_x1_%d" % par, "p_rs2"], writes=["p_xs"])
            for hf in range(2):
                pt = ps[2 + hf]; pk = "ps%d" % (2 + hf)
                for kk in range(4):
                    k = hf * 4 + kk
                    K.op("pe", lambda e, k=k, kk=kk, pt=pt: e.transpose(pt[:, kk * 128:(kk + 1) * 128], pxs[:, k * 128:(k + 1) * 128], ident[:]), reads=["p_xs", "ident"], writes=[pk])
                for kk in range(4):
                    k = hf * 4 + kk
                    K.op("act", lambda e, k=k, kk=kk, pt=pt: e.activation(h2T[:, k, :], pt[:, kk * 128:(kk + 1) * 128], AF.Identity, bias=B2[:, k:k + 1], scale=A2[:, k:k + 1]),
                         reads=[pk, "mods"], writes=["p_h2T"])
            K.op("dve", lambda e: e.tensor_tensor(h2[:], pxs[:], A2row[:], op=ALU.mult), reads=["p_xs", "A2row"], writes=["p_h2"])
            K.op("pool", lambda e: e.tensor_tensor(h2[:], h2[:], gtB[:, 1, :], op=ALU.add), reads=["p_h2", "gtB"], writes=["p_h2"])
            for g in range(16):
                pt = ps[4 + (g % 2)]; pk = "ps%d" % (4 + (g % 2))
                for k in range(8):
                    K.op("pe", lambda e, g=g, k=k, pt=pt: e.matmul(pt[:, 0:128], lhsT=wqb[:, k, g * 128:(g + 1) * 128], rhs=h2T[:, k, :], start=(k == 0), stop=(k == 7)),
                         reads=["wqb", "p_h2T"], writes=[pk])
                K.op("act", lambda e, g=g, pt=pt: e.copy(qTs[:, g, :], pt[:, 0:128]), reads=[pk], writes=[("p_qT", g)])
            for g in range(16):
                pt = ps[6 + (g // 4) % 2]; pk = "ps%d" % (6 + (g // 4) % 2)
                K.op("pe", lambda e, g=g, pt=pt: e.matmul(pt[:, (g % 4) * 128:(g % 4 + 1) * 128], lhsT=qTs[:, g, :], rhs=skT[:, g, :], start=True, stop=True),
                     reads=[("p_qT", g), "skT"], writes=[pk])
                if g % 4 == 3:
                    K.op("dve", lambda e, g=g, pt=pt: e.tensor_copy(scs[:, g - 3:g + 1, :].rearrange("p g k -> p (g k)"), pt[:, :]), reads=[pk], writes=[("p_sc", g // 4)])
            for g in range(16):
                K.op("dve", lambda e, g=g: e.max(tv[:, g, 0:8], scs[:, g, :]), reads=[("p_sc", g // 4)], writes=[("tv", g)])
            for g in range(16):
                K.op("dve", lambda e, g=g: e.max_index(tiu[:, g, 0:8], tv[:, g, 0:8], scs[:, g, :]), reads=[("p_sc", g // 4), ("tv", g)], writes=[("tiu", g)])
            for g in range(16):
                K.op("dve", lambda e, g=g: e.match_replace(tmp1[:, g, :], tv[:, g, 0:8], scs[:, g, :], NEG), reads=[("p_sc", g // 4), ("tv", g)], writes=[("tmp1", g)])
            for g in range(16):
                K.op("dve", lambda e, g=g: e.max(tv[:, g, 8:16], tmp1[:, g, :]), reads=[("tmp1", g)], writes=[("tv2", g)])
            for g in range(16):
                K.op("dve", lambda e, g=g: e.max_index(tiu[:, g, 8:16], tv[:, g, 8:16], tmp1[:, g, :]), reads=[("tmp1", g), ("tv2", g)], writes=[("tiu2", g)])
            allg = [("tv", g) for g in range(16)] + [("tv2", g) for g in range(16)]
            alli = [("tiu", g) for g in range(16)] + [("tiu2", g) for g in range(16)]
            K.op("dve", lambda e: e.tensor_copy(tif[:], tiu[:]), reads=alli, writes=["p_tif"])
            tvv = tv[:].rearrange("p (h q) a -> p h q a", q=2)
            tfv = tif[:].rearrange("p (h q) a -> p h q a", q=2)
            c4 = cand[:].rearrange("p h (a b) -> p h a b", b=16)
            K.op("dve", lambda e: e.tensor_tensor(c4, tvv[:, :, 0, :].unsqueeze(3).to_broadcast([128, 8, 16, 16]),
                                                  tvv[:, :, 1, :].unsqueeze(2).to_broadcast([128, 8, 16, 16]), op=ALU.add), reads=allg, writes=["p_cand"])
            for hh in range(8):
                K.op("dve", lambda e, hh=hh: e.max(bsv[:, hh, 0:8], cand[:, hh, :]), reads=["p_cand"], writes=[("bs", hh)])
            for hh in range(8):
                K.op("dve", lambda e, hh=hh: e.max_index(posu[:, hh, 0:8], bsv[:, hh, 0:8], cand[:, hh, :]), reads=["p_cand", ("bs", hh)], writes=[("pos", hh)])
            for hh in range(8):
                K.op("dve", lambda e, hh=hh: e.match_replace(tmp2[:, hh, :], bsv[:, hh, 0:8], cand[:, hh, :], NEG), reads=["p_cand", ("bs", hh), "p_eq"], writes=[("tmp2", hh)])
            for hh in range(8):
                K.op("dve", lambda e, hh=hh: e.max(bsv[:, hh, 8:16], tmp2[:, hh, :]), reads=[("tmp2", hh)], writes=[("bs2", hh)])
            for hh in range(8):
                K.op("dve", lambda e, hh=hh: e.max_index(posu[:, hh, 8:16], bsv[:, hh, 8:16], tmp2[:, hh, :]), reads=[("tmp2", hh), ("bs2", hh)], writes=[("pos2", hh)])
            allb = [("bs", hh) for hh in range(8)] + [("bs2", hh) for hh in range(8)]
            allp = [("pos", hh) for hh in range(8)] + [("pos2", hh) for hh in range(8)]
            K.op("dve", lambda e: e.tensor_single_scalar(pau[:], posu[:], 4, op=ALU.logical_shift_right), reads=allp, writes=["p_pau"])
            K.op("dve", lambda e: e.tensor_single_scalar(pbu[:], posu[:], 15, op=ALU.bitwise_and), reads=allp, writes=["p_pbu"])
            K.op("dve", lambda e: e.tensor_copy(paf[:], pau[:]), reads=["p_pau"], writes=["p_paf"])
            K.op("dve", lambda e: e.tensor_copy(pbf[:], pbu[:]), reads=["p_pbu"], writes=["p_pbf"])
            e4 = eq[:].rearrange("p h (k a) -> p h k a", a=16)
            io4 = iota16[:].unsqueeze(1).unsqueeze(1).to_broadcast([128, 8, 16, 16])
            for (pf, q_, dst, nm) in ((paf, 0, i0f, "i0f"), (pbf, 1, i1f, "i1f")):
                K.op("dve", lambda e, pf=pf: e.tensor_tensor(e4, pf[:].unsqueeze(3).to_broadcast([128, 8, 16, 16]), io4, op=ALU.is_equal),
                     reads=["p_paf", "p_pbf", "iota16"], writes=["p_eq"] + [("tmp2", hh_) for hh_ in range(8)])
                K.op("dve", lambda e, q_=q_: e.tensor_tensor(e4, e4, tfv[:, :, q_, :].unsqueeze(2).to_broadcast([128, 8, 16, 16]), op=ALU.mult),
                     reads=["p_eq", "p_tif"], writes=["p_eq"])
                K.op("dve", lambda e, dst=dst: e.reduce_sum(dst[:], e4, axis=AX.X), reads=["p_eq"], writes=["p_" + nm])
            K.op("dve", lambda e: e.scalar_tensor_tensor(i0f[:], i0f[:], 128.0, i1f[:], op0=ALU.mult, op1=ALU.add), reads=["p_i0f", "p_i1f"], writes=["p_i0f"])
            K.op("dve", lambda e: e.tensor_copy(eidxs[par][:], i0f[:].rearrange("p h k -> p (h k)")), reads=["p_i0f"], writes=["p_eidx_%d" % par])
            K.op("dve", lambda e: e.tensor_tensor(gat[:], bsv[:], bsv[:, :, 0:1].to_broadcast([128, 8, 16]), op=ALU.subtract), reads=allb, writes=["p_gate"])
            K.op("act", lambda e: e.activation(gat[:], gat[:], AF.Exp), reads=["p_gate"], writes=["p_gate"])
            K.op("dve", lambda e: e.reduce_sum(gsum[:], gat[:], axis=AX.X), reads=["p_gate"], writes=["p_gsum"])
            K.op("dve", lambda e: e.reciprocal(gsum[:], gsum[:]), reads=["p_gsum"], writes=["p_gsum"])
            K.op("dve", lambda e: e.tensor_tensor(gat[:], gat[:], gsum[:].unsqueeze(2).to_broadcast([128, 8, 16]), op=ALU.mult), reads=["p_gate", "p_gsum"], writes=["p_gate"])
            K.op("act", lambda e: e.copy(h2bs[par][:], h2[:]), reads=["p_h2"], writes=["p_h2b_%d" % par])
            K.op("dve", lambda e: e.tensor_copy(gflats[par][:], gat[:].rearrange("p h k -> p (h k)")), reads=["p_gate"], writes=["p_gflat_%d" % par])

        def back(i):
            par = i % 2
            tsl = slice(i * 128, (i + 1) * 128)
            GRP = 4
            for g0 in range(0, 128, GRP):
                for kslot in range(g0, g0 + GRP):
                    rb = rowc[kslot % NRB]; rk_ = "p_rowc%d" % (kslot % NRB)
                    K.gather(rb[:, :], comb_d[:, :], eidxs[par][:, kslot:kslot + 1], reads=["p_eidx_%d" % par, "comb"], writes=[rk_], key=rk_)
                    K.op("dve", lambda e, rb=rb, kslot=kslot: e.scalar_tensor_tensor(pjb[:], rb[:, 0:D], 1.0, h2bs[par][:], op0=ALU.mult, op1=ALU.mult, accum_out=apre[:, kslot:kslot + 1]),
                         reads=[rk_, "p_h2b_%d" % par], writes=["p_junkb", ("apre", g0 // GRP)])
                K.op("dve", lambda e, g0=g0: e.tensor_copy(coef[:, g0:g0 + GRP], apre[:, g0:g0 + GRP]), reads=[("apre", g0 // GRP)], writes=[("cf0", g0 // GRP)])
                K.op("act", lambda e, g0=g0: e.activation(coef[:, g0:g0 + GRP], coef[:, g0:g0 + GRP], AF.Gelu), reads=[("cf0", g0 // GRP)], writes=[("cf1", g0 // GRP)])
                K.op("dve", lambda e, g0=g0: e.tensor_tensor(coef[:, g0:g0 + GRP], coef[:, g0:g0 + GRP], gflats[par][:, g0:g0 + GRP], op=ALU.mult),
                     reads=[("cf1", g0 // GRP), "p_gflat_%d" % par], writes=[("cf2", g0 // GRP)])
                for kslot in range(g0, g0 + GRP):
                    rb = rowc[kslot % NRB]; rk_ = "p_rowc%d" % (kslot % NRB)
                    dg = dgs[kslot % 4]; dk__ = "p_dg%d" % (kslot % 4)
                    K.op("act", lambda e, dg=dg, kslot=kslot: e.activation(dg[:], identb[:], AF.Identity, scale=coef[:, kslot:kslot + 1]),
                         reads=["identb", ("cf2", g0 // GRP)], writes=[dk__])
                    for hf in range(2):
                        K.op("pe", lambda e, dg=dg, rb=rb, hf=hf, kslot=kslot: e.matmul(ps[hf][:, :], lhsT=dg[:], rhs=rb[:, D + hf * 512:D + (hf + 1) * 512],
                                                                                       start=(kslot == 0), stop=(kslot == 127)),
                             reads=[dk__, rk_], writes=["ps%d" % hf])
            for hf in range(2):
                K.op("act", lambda e, hf=hf: e.copy(yacc[:, hf * 512:(hf + 1) * 512], ps[hf][:, :]), reads=["ps%d" % hf], writes=["p_y"])
            K.op("dve", lambda e: e.tensor_tensor(yacc[:], yacc[:], gtB[:, 3, :], op=ALU.mult), reads=["p_y", "gtB"], writes=["p_y"])
            K.op("pool", lambda e: e.tensor_tensor(yacc[:], yacc[:], x1s[par][:], op=ALU.add), reads=["p_y", "p_x1_%d" % par], writes=["p_y"])
            K.op("act", lambda e: e.activation(pj2[:], yacc[:], AF.Square), reads=["p_y"], writes=["p_junk2"])
            K.op("dve", lambda e: e.reduce_sum(pss2[:, 0:1], pj2[:], axis=AX.X), reads=["p_junk2"], writes=["p_ss2"])
            K.op("act", lambda e: e.activation(prs2[:, 0:1], pss2[:, 0:1], AF.Sqrt, bias=eps_t[:, 0:1], scale=1.0 / D), reads=["p_ss2", "eps"], writes=["p_rs_b"])
            K.op("dve", lambda e: e.reciprocal(prs2[:, 1:2], prs2[:, 0:1]), reads=["p_rs_b"], writes=["p_rs2_b"])
            K.op("dve", lambda e: e.scalar_tensor_tensor(pxs2[:], yacc[:], prs2[:, 1:2], fgB[:], op0=ALU.mult, op1=ALU.mult), reads=["p_y", "p_rs2_b", "fgB"], writes=["p_xs2"])
            K.dma(out_d[tsl, :], pxs2[:, :], reads=["p_xs2"], writes=["outdone"], key="st_out")

        front(0)
        for i in range(NTL):
            for d_ in K.streams(2):
                if d_ == 0:
                    back(i)
                elif i + 1 < NTL:
                    front(i + 1)
    K.barrier()
    if "mT" in dbg:
        d_o = dbgt("mT", [8, 128, TL], BF16)
        K.dma(d_o[:, :, :], mT_d[:, :, :], reads=[("mT", j) for j in range(8)], writes=["dbgmT"], key="dbg")

    K.finish([k for k in K.st.keys() if (isinstance(k, str) and k.startswith("dbg")) or k == "outdone"])
    return dbg_out


def _inputs_for_core(inp, b, n_rows):
    TL = 64 * n_rows
    f = lambda a: np.ascontiguousarray(np.asarray(a, dtype=np.float32))
    m = {
        "x": f(inp["x"][b, :TL]),
        "c": f(inp["c"][b:b + 1]),
        "ctx": f(inp["ctx"][b]),
        "c_ctx": f(inp["c_ctx"][None, :]),
        "ada_w": f(inp["ada_w"][0]),
        "ada_b": f(inp["ada_b"][0].reshape(48, 128)),
        "ada_b_row": f(inp["ada_b"][0].reshape(1, 6144)),
        "norm1_g": f(inp["norm1_g"][0].reshape(8, 128)),
        "w_in": f(inp["w_in"][0]),
        "gdn_conv_w": f(inp["gdn_conv_w"][0].reshape(60, 128)),
        "gdn_a_log": f(inp["gdn_a_log"][0].reshape(1, 8)),
        "gdn_dt_bias": f(inp["gdn_dt_bias"][0].reshape(1, 8)),
        "gdn_norm_w": f(inp["gdn_norm_w"][0].reshape(1, 128)),
        "rwkv_mu": f(inp["rwkv_mu"][0].reshape(15, 128)),
        "rwkv_w0": f(inp["rwkv_w0"][0].reshape(8, 128)),
        "rwkv_w2": f(inp["rwkv_w2"][0].reshape(128, 512)),
        "rwkv_a0": f(inp["rwkv_a0"][0].reshape(8, 128)),
        "rwkv_a2": f(inp["rwkv_a2"][0].reshape(128, 512)),
        "rwkv_g2": f(inp["rwkv_g2"][0]),
        "rwkv_k_k": f(inp["rwkv_k_k"][0].reshape(4, 128)),
        "rwkv_k_a": f(inp["rwkv_k_a"][0].reshape(4, 128)),
        "rwkv_r_k": f(inp["rwkv_r_k"][0].reshape(4, 128)),
        "rwkv_gn_w": f(inp["rwkv_gn_w"][0].reshape(4, 128)),
        "rwkv_gn_b": f(inp["rwkv_gn_b"][0].reshape(4, 128)),
        "w_out": f(inp["w_out"][0]),
        "norm2_g": f(inp["norm2_g"][0].reshape(8, 128)),
        "peer_w_query": f(inp["peer_w_query"][0]),
        "peer_sub_keys": f(inp["peer_sub_keys"][0].reshape(16, 128, 128)),
        "peer_down": f(inp["peer_down"][0]),
        "peer_up": f(inp["peer_up"][0]),
        "final_norm_g": f(inp["final_norm_g"][None, :]),
        "norm2_g_row": f(inp["norm2_g"][0].reshape(1, 1024)),
    }
    return m


def run(inp, n_rows=64, cores=None, dbg=(), stop=None):
    nb = inp["x"].shape[0]
    cores = list(range(nb)) if cores is None else cores
    nc = bass.Bass("TRN2", target_bir_lowering=False)
    build(nc, n_rows=n_rows, dbg=dbg, stop=stop)
    in_maps = [_inputs_for_core(inp, b, n_rows) for b in cores]
    res = run_bass_kernel_spmd(nc, in_maps, core_ids=list(range(len(cores))))
    return res.results


def kernel(**inputs):
    res = run(inputs, n_rows=64)
    return np.stack([np.asarray(r["out"], dtype=np.float32) for r in res], axis=0)
```

```python
from contextlib import ExitStack
import numpy as np
import concourse.bass as bass
import concourse.mybir as mybir
from concourse.bass_utils import run_bass_kernel_spmd

F32 = mybir.dt.float32
BF16 = mybir.dt.bfloat16
I32 = mybir.dt.int32
U32 = mybir.dt.uint32
AF = mybir.ActivationFunctionType
ALU = mybir.AluOpType
AX = mybir.AxisListType

D = 1024
TC = 256
IN_COLS = 3984
GDN_COLS = 2064
NORM_EPS = 1e-6
L2_EPS = 1e-6
GN_EPS = 64e-5


class Ctx:
    def __init__(self, nc):
        self.nc = nc
        self.es = ExitStack()
        self.eng = dict(pe=nc.tensor, act=nc.scalar, dve=nc.vector, pool=nc.gpsimd, sp=nc.sync)
        self.csem = {}
        self.cnt = {}
        for e in ("pe", "act", "dve", "pool"):
            self.csem[e] = self.es.enter_context(nc.semaphore("cs_" + e))
            self.cnt[e] = 0
        self.dsem = {}
        self.seen = {e: {} for e in self.eng}
        self.st = {}
        self.ninst = 0
        self._rec = None

    def _sem(self, sk):
        if sk[0] == "c":
            return self.csem[sk[1]]
        return self.dsem[sk[1]][0]

    def _deps(self, reads, writes, e=None):
        need = {}

        def add(m):
            if m is None:
                return
            sk, v = m
            if need.get(sk, 0) < v:
                need[sk] = v

        for r in reads:
            s = self.st.get(r)
            if s is not None:
                add(s[0])
                if isinstance(r, str) and r.startswith("ps") and r[2:].isdigit():
                    for sk, v in s[1].items():
                        if sk != ("c", e):
                            add((sk, v))
        for w in writes:
            s = self.st.get(w)
            if s is not None:
                add(s[0])
                for sk, v in s[1].items():
                    add((sk, v))
        return need

    def _wait(self, e, need):
        eng = self.eng[e]
        seen = self.seen[e]
        for sk, v in need.items():
            if e == "pe" and sk == ("c", "pe"):
                continue
            if sk[0] == "d":
                v = max(v, self.dsem[sk[1]][1])
            if seen.get(sk, 0) >= v:
                continue
            eng.wait_ge(self._sem(sk), v)
            seen[sk] = v

    def _mark(self, mark, reads, writes):
        for w in writes:
            self.st[w] = [mark, {}]
        for r in reads:
            s = self.st.get(r)
            if s is None:
                s = self.st[r] = [None, {}]
            sk, v = mark
            if s[1].get(sk, 0) < v:
                s[1][sk] = v

    def streams(self, n):
        lists = []
        for d in range(n):
            self._rec = []
            yield d
            lists.append(self._rec)
            self._rec = None
        idx = [0] * n
        left = sum(len(l) for l in lists)
        while left:
            for d in range(n):
                if idx[d] < len(lists[d]):
                    kind, args, kw = lists[d][idx[d]]
                    idx[d] += 1
                    left -= 1
                    getattr(self, kind)(*args, **kw)

    def op(self, e, fn, reads=(), writes=()):
        if self._rec is not None:
            self._rec.append(("op", (e, fn, tuple(reads), tuple(writes)), {}))
            return
        need = self._deps(reads, writes, e)
        self._wait(e, need)
        ins = fn(self.eng[e])
        ins.then_inc(self.csem[e], 1)
        self.cnt[e] += 1
        self.ninst += 1
        self._mark((("c", e), self.cnt[e]), reads, writes)

    def dma(self, out, in_, reads=(), writes=(), key=None, q="sp", **kw):
        if self._rec is not None:
            self._rec.append(("dma", (out, in_, tuple(reads), tuple(writes), key, q), kw))
            return
        if key not in self.dsem:
            self.dsem[key] = [self.es.enter_context(self.nc.semaphore("ds_%d" % len(self.dsem))), 0]
        need = self._deps(reads, writes)
        self._wait(q, need)
        d = self.dsem[key]
        self.eng[q].dma_start(out=out, in_=in_, **kw).then_inc(d[0], 16)
        d[1] += 16
        self.ninst += 1
        self._mark((("d", key), d[1]), reads, writes)

    def gather(self, out, in_, idx_ap, reads=(), writes=(), key=None):
        if key not in self.dsem:
            self.dsem[key] = [self.es.enter_context(self.nc.semaphore("ds_%d" % len(self.dsem))), 0]
        need = self._deps(reads, writes)
        self._wait("pool", need)
        d = self.dsem[key]
        self.nc.gpsimd.indirect_dma_start(
            out=out, out_offset=None, in_=in_,
            in_offset=bass.IndirectOffsetOnAxis(ap=idx_ap, axis=0)).then_inc(d[0], 16)
        d[1] += 16
        self.ninst += 1
        self._mark((("d", key), d[1]), reads, writes)

    def barrier(self):
        need = {("c", e): v for e, v in self.cnt.items() if v > 0}
        for k, d in self.dsem.items():
            if d[1] > 0:
                need[("d", k)] = d[1]
        for e in self.eng:
            self._wait(e, need)

    def finish(self, keys):
        need = self._deps(keys, ())
        self._wait("sp", need)


NM_MODE = ["f32"]
RW_NM = ["f32"]


class _Stop(Exception):
    pass


def build(nc, n_rows=64, dbg=(), stop=None):
    K = Ctx(nc)
    try:
        return _build(nc, K, n_rows, dbg, stop)
    except _Stop:
        K.barrier()
        return None


def _build(nc, K, n_rows, dbg, stop):
    def stop_at(name):
        if stop == name:
            raise _Stop()

    TL = 64 * n_rows
    T = TC + TL
    NT = T // 128
    NTL = TL // 128
    es = K.es

    def din(name, shape, dt=F32):
        return nc.dram_tensor(name, list(shape), dt, kind="ExternalInput").ap()

    x_d = din("x", [TL, D])
    c_d = din("c", [1, D])
    ctx_d = din("ctx", [TC, D])
    cctx_d = din("c_ctx", [1, D])
    adaw_d = din("ada_w", [D, 6144])
    adab_d = din("ada_b", [48, 128])
    adabr_d = din("ada_b_row", [1, 6144])
    g1_d = din("norm1_g", [8, 128])
    win_d = din("w_in", [D, IN_COLS])
    convw_d = din("gdn_conv_w", [60, 128])
    alog_d = din("gdn_a_log", [1, 8])
    dtb_d = din("gdn_dt_bias", [1, 8])
    gnorm_d = din("gdn_norm_w", [1, 128])
    mu_d = din("rwkv_mu", [15, 128])
    w0_d = din("rwkv_w0", [8, 128])
    w2_d = din("rwkv_w2", [128, 512])
    a0_d = din("rwkv_a0", [8, 128])
    a2_d = din("rwkv_a2", [128, 512])
    g2w_d = din("rwkv_g2", [128, 512])
    kk_d = din("rwkv_k_k", [4, 128])
    ka_d = din("rwkv_k_a", [4, 128])
    rk_d = din("rwkv_r_k", [4, 128])
    gnw_d = din("rwkv_gn_w", [4, 128])
    gnb_d = din("rwkv_gn_b", [4, 128])
    wout_d = din("w_out", [D, D])
    g2_d = din("norm2_g", [8, 128])
    wq_d = din("peer_w_query", [D, 2048])
    sk_d = din("peer_sub_keys", [16, 128, 128])
    down_d = din("peer_down", [16384, D])
    up_d = din("peer_up", [16384, D])
    fng_d = din("final_norm_g", [1, D])
    g2row_d = din("norm2_g_row", [1, D])
    out_d = nc.dram_tensor("out", [TL, D], F32, kind="ExternalOutput").ap()

    dbg_out = {}

    def dbgt(name, shape, dt=F32):
        dbg_out[name] = nc.dram_tensor("dbg_" + name, list(shape), dt, kind="ExternalOutput").ap()
        return dbg_out[name]

    pT_d = nc.dram_tensor("pT_s", [32, 128, T], F32).ap()

    def sb(name, shape, dt=F32, stack=es):
        return stack.enter_context(nc.sbuf_tensor(name, list(shape), dt))

    ps = [es.enter_context(nc.psum_tensor("ps%d" % i, [128, 512], F32)) for i in range(8)]

    dI = sb("dI", [128, 128], I32)
    ident = sb("ident", [128, 128])
    identb = sb("identb", [128, 128], BF16)
    ones = sb("ones", [128, 128])
    onesb = sb("onesb", [128, 128], BF16)
    m_lt = sb("m_lt", [128, 128])
    m_le = sb("m_le", [128, 128])
    m_gt = sb("m_gt", [128, 128])
    m_ge = sb("m_ge", [128, 128])
    e0 = sb("e0", [2, 128])
    K.op("pool", lambda e: e.iota(dI[:], pattern=[[1, 128]], base=0, channel_multiplier=-1), writes=["dI"])
    for t, op_, nm in ((ident, ALU.is_equal, "ident"), (m_lt, ALU.is_gt, "m_lt"), (m_le, ALU.is_ge, "m_le"),
                       (m_gt, ALU.is_lt, "m_gt"), (m_ge, ALU.is_le, "m_ge")):
        K.op("dve", lambda e, t=t, op_=op_: e.tensor_scalar(t[:], dI[:], 0.0, None, op0=op_), reads=["dI"], writes=[nm])
    K.op("dve", lambda e: e.tensor_copy(identb[:], ident[:]), reads=["ident"], writes=["identb"])
    K.op("pool", lambda e: e.memset(ones[:], 1.0), writes=["ones"])
    K.op("pool", lambda e: e.memset(onesb[:], 1.0), writes=["onesb"])
    e0i = sb("e0i", [2, 128], I32)
    K.op("pool", lambda e: e.iota(e0i[:], pattern=[[0, 128]], base=1, channel_multiplier=-1), writes=["e0i"])
    K.op("dve", lambda e: e.tensor_copy(e0[:], e0i[:]), reads=["e0i"], writes=["e0"])

    PK1 = dict(ADAB=0, G1=48, G2=56, CW=64)
    PK2 = dict(MU=0, W0=15, A0=23, KK=31, KA=35, RK=39, GNW=43, GNB=47, GNORM=51)
    pk1s = sb("pk1s", [128, 128])
    pk2s = sb("pk2s", [128, 128])
    pk1 = sb("pk1", [128, 128])
    pk2 = sb("pk2", [128, 128])
    K.op("pool", lambda e: e.memset(pk1s[:], 0.0), writes=["pk1s"])
    K.op("pool", lambda e: e.memset(pk2s[:], 0.0), writes=["pk2s"])
    for src, off, n in ((adab_d, 0, 48), (g1_d, 48, 8), (g2_d, 56, 8), (convw_d, 64, 60)):
        K.dma(pk1s[off:off + n, :], src[:, :], writes=["pk1s"], key="pk1s")
    for src, off, n in ((mu_d, 0, 15), (w0_d, 15, 8), (a0_d, 23, 8), (kk_d, 31, 4), (ka_d, 35, 4),
                        (rk_d, 39, 4), (gnw_d, 43, 4), (gnb_d, 47, 4), (gnorm_d, 51, 1)):
        K.dma(pk2s[off:off + n, :], src[:, :], writes=["pk2s"], key="pk2s")
    K.op("pe", lambda e: e.transpose(ps[0][:, 0:128], pk1s[:], ident[:]), reads=["pk1s", "ident"], writes=["ps0"])
    K.op("pe", lambda e: e.transpose(ps[0][:, 128:256], pk2s[:], ident[:]), reads=["pk2s", "ident"], writes=["ps0"])
    K.op("dve", lambda e: e.tensor_copy(pk1[:], ps[0][:, 0:128]), reads=["ps0"], writes=["pk1"])
    K.op("dve", lambda e: e.tensor_copy(pk2[:], ps[0][:, 128:256]), reads=["ps0"], writes=["pk2"])

    modT = sb("modT", [128, 48, 2])
    gt_d = nc.dram_tensor("gt_s", [128, 4 * D], F32).ap()
    A1 = sb("A1", [128, 8]); B1 = sb("B1", [128, 8])
    A1c = sb("A1c", [128, 8]); B1c = sb("B1c", [128, 8])
    A2 = sb("A2", [128, 8]); B2 = sb("B2", [128, 8])
    with ExitStack() as pa:
        cc = sb("cc", [2, D], stack=pa)
        scT = sb("scT", [128, 8, 2], stack=pa)
        aw = [sb("aw%d" % i, [128, 8, 1024], stack=pa) for i in range(2)]
        gtrow = sb("gtrow", [2, 4, D], stack=pa)
        gtB = sb("gtB", [128, 4, D], stack=pa)
        K.dma(cc[0:1, :], c_d[:, :], writes=["cc"], key="cc")
        K.dma(cc[1:2, :], cctx_d[:, :], writes=["cc"], key="cc")
        K.op("act", lambda e: e.activation(cc[:], cc[:], AF.Silu), reads=["cc"], writes=["cc"])
        for k in range(8):
            K.op("pe", lambda e, k=k: e.transpose(ps[1][:, 2 * k:2 * k + 2], cc[0:2, k * 128:(k + 1) * 128], ident[0:2, 0:2]),
                 reads=["cc", "ident"], writes=["ps1"])
        K.op("dve", lambda e: e.tensor_copy(scT[:].rearrange("p k c -> p (k c)"), ps[1][:, 0:16]), reads=["ps1"], writes=["scT"])
        K.op("pool", lambda e: e.memset(gtrow[:], 0.0), writes=["gtrow"])
        for q_ in range(4):
            K.dma(gtrow[0:1, q_, :], adabr_d[:, 2048 + q_ * 1024:3072 + q_ * 1024], writes=["gtrow"], key="gtrow")
        for g in range(6):
            a = aw[g % 2]
            an = "aw%d" % (g % 2)
            K.dma(a[:], adaw_d[:, g * 1024:(g + 1) * 1024].rearrange("(k p) c -> p k c", p=128), writes=[an], key=an)
            for j in range(8):
                col = (g * 8 + j) * 2
                for k in range(8):
                    K.op("pe", lambda e, a=a, j=j, k=k, col=col: e.matmul(
                        ps[2][:, col:col + 2], lhsT=a[:, k, j * 128:(j + 1) * 128], rhs=scT[:, k, :],
                        start=(k == 0), stop=(k == 7)), reads=[an, "scT"], writes=["ps2"])
            if g >= 2:
                q = g - 2
                for half in range(2):
                    for k in range(8):
                        K.op("pe", lambda e, a=a, k=k, half=half: e.matmul(
                            ps[3][0:2, :], lhsT=scT[:, k, :], rhs=a[:, k, half * 512:(half + 1) * 512],
                            start=(k == 0), stop=(k == 7)), reads=[an, "scT"], writes=["ps3"])
                    K.op("dve", lambda e, q=q, half=half: e.tensor_tensor(
                        gtrow[:, q, half * 512:(half + 1) * 512], ps[3][0:2, :], gtrow[:, q, half * 512:(half + 1) * 512], op=ALU.add),
                        reads=["ps3", "gtrow"], writes=["gtrow"])
                    K.op("pe", lambda e, q=q, half=half: e.matmul(
                        ps[4][:, :], lhsT=e0[:, :], rhs=gtrow[:, q, half * 512:(half + 1) * 512], start=True, stop=True),
                        reads=["e0", "gtrow"], writes=["ps4"])
                    K.op("act", lambda e, q=q, half=half: e.copy(gtB[:, q, half * 512:(half + 1) * 512], ps[4][:, :]),
                         reads=["ps4"], writes=["gtB"])
        K.op("dve", lambda e: e.tensor_tensor(
            modT[:], ps[2][:, 0:96].rearrange("p (j c) -> p j c", c=2),
            pk1[:, 0:48].unsqueeze(2).to_broadcast([128, 48, 2]), op=ALU.add), reads=["ps2", "pk1"], writes=["modT"])
        for (A, B, gname, sc0, sh0, col, nm) in ((A1, B1, "G1", 8, 0, 0, "1"), (A1c, B1c, "G1", 8, 0, 1, "1c"),
                                                 (A2, B2, "G2", 32, 24, 0, "2")):
            g0 = PK1[gname]
            K.op("dve", lambda e, A=A, g0=g0, sc0=sc0, col=col: e.scalar_tensor_tensor(
                A[:], modT[:, sc0:sc0 + 8, col], 1.0, pk1[:, g0:g0 + 8], op0=ALU.add, op1=ALU.mult),
                reads=["modT", "pk1"], writes=["A" + nm])
            K.op("dve", lambda e, B=B, sh0=sh0, col=col: e.tensor_copy(B[:], modT[:, sh0:sh0 + 8, col]),
                 reads=["modT"], writes=["B" + nm])

        K.dma(gt_d[:, :], gtB[:].rearrange("p q d -> p (q d)"), reads=["gtB"], writes=["gt_d"], key="st_gt")
        K.barrier()
    if "mod" in dbg:
        d_ = dbgt("mod", [128, 96])
        K.dma(d_[:, :], modT[:].rearrange("p j c -> p (j c)"), reads=["modT"], writes=["dbgmod"], key="dbg")

    def norm_tile(xt, xkey, A, B, hT, hkey, col0, pp, stage):
        junk, ss, rs, xs = stage
        K.op("act", lambda e: e.activation(junk[:], xt[:], AF.Square), reads=[xkey], writes=["n_junk"])
        K.op("dve", lambda e: e.reduce_sum(ss[:, 0:1], junk[:], axis=AX.X), reads=["n_junk"], writes=["n_ss"])
        K.op("act", lambda e: e.activation(rs[:, 0:1], ss[:, 0:1], AF.Sqrt, bias=eps_t[:, 0:1], scale=1.0 / D),
             reads=["n_ss", "eps"], writes=["n_rs"])
        K.op("dve", lambda e: e.reciprocal(rs[:, 1:2], rs[:, 0:1]), reads=["n_rs"], writes=["n_rs2"])
        K.op("dve", lambda e: e.tensor_scalar(xs[:], xt[:], rs[:, 1:2], None, op0=ALU.mult),
             reads=[xkey, "n_rs2"], writes=["n_xs"])
        for half in range(2):
            pt = ps[pp + half]
            pk = "ps%d" % (pp + half)
            for kk in range(4):
                k = half * 4 + kk
                K.op("pe", lambda e, k=k, kk=kk, pt=pt: e.transpose(pt[:, kk * 128:(kk + 1) * 128], xs[:, k * 128:(k + 1) * 128], ident[:]),
                     reads=["n_xs", "ident"], writes=[pk])
            for kk in range(4):
                k = half * 4 + kk
                eng = "act" if kk % 2 == 0 else "dve"
                if eng == "act":
                    K.op("act", lambda e, k=k, kk=kk, pt=pt: e.activation(
                        hT[:, k, col0:col0 + 128], pt[:, kk * 128:(kk + 1) * 128], AF.Identity,
                        bias=B[:, k:k + 1], scale=A[:, k:k + 1]), reads=[pk, "mods"], writes=[hkey])
                else:
                    K.op("dve", lambda e, k=k, kk=kk, pt=pt: e.tensor_scalar(
                        hT[:, k, col0:col0 + 128], pt[:, kk * 128:(kk + 1) * 128], A[:, k:k + 1], B[:, k:k + 1],
                        op0=ALU.mult, op1=ALU.add), reads=[pk, "mods"], writes=[hkey])

    eps_t = sb("eps_t", [128, 4])
    K.op("pool", lambda e: e.memset(eps_t[:, 0:1], NORM_EPS), writes=["eps"])
    K.op("pool", lambda e: e.memset(eps_t[:, 1:2], L2_EPS), writes=["eps"])
    K.op("pool", lambda e: e.memset(eps_t[:, 2:3], GN_EPS), writes=["eps"])
    K.op("pool", lambda e: e.memset(eps_t[:, 3:4], 1.0), writes=["eps"])
    K.op("dve", lambda e: e.tensor_copy(A1[:, 0:1], A1[:, 0:1]), reads=["A1", "B1", "A1c", "B1c", "A2", "B2"], writes=["mods"])

    with ExitStack() as pb:
        hT = sb("hT", [128, 8, T], BF16, stack=pb)
        junk = sb("junk", [128, D], stack=pb)
        ss = sb("ss", [128, 1], stack=pb)
        rs = sb("rs", [128, 2], stack=pb)
        xs = sb("xs", [128, D], stack=pb)
        xts = [sb("xt%d" % i, [128, D], stack=pb) for i in range(3)]
        for i in range(NT):
            xt = xts[i % 3]
            xk = "xt%d" % (i % 3)
            src = ctx_d[i * 128:(i + 1) * 128, :] if i < 2 else x_d[(i - 2) * 128:(i - 1) * 128, :]
            K.dma(xt[:], src, writes=[xk], key=xk)
            A, B = (A1c, B1c) if i < 2 else (A1, B1)
            norm_tile(xt, xk, A, B, hT, "hT", i * 128, 0, (junk, ss, rs, xs))
        if "hT" in dbg:
            d_ = dbgt("ss", [128, 1])
            K.dma(d_[:, :], ss[:, :], reads=["n_ss"], writes=["dbgss"], key="dbg")
            d_ = dbgt("rs", [128, 2])
            K.dma(d_[:, :], rs[:, :], reads=["n_rs", "n_rs2"], writes=["dbgrs"], key="dbg")
            d_ = dbgt("hT", [128, 8 * T], BF16)
            K.dma(d_[:, :], hT[:].rearrange("p k t -> p (k t)"), reads=["hT"], writes=["dbghT"], key="dbg")
        wst = [sb("wst%d" % i, [128, 8, 128], stack=pb) for i in range(2)]
        wbf = [sb("wbf%d" % i, [128, 8, 128], BF16, stack=pb) for i in range(2)]
        pcs = [sb("pc%d" % i, [128, T], stack=pb) for i in range(2)]
        nblk = (T + 511) // 512
        chunks = [(j, j * 128, 128) for j in range(16)] + [(16, 2048, 16)] + \
                 [(17 + j, GDN_COLS + j * 128, 128) for j in range(15)]
        ev = 0
        for ci, (dst, c0, ncol) in enumerate(chunks):
            w_s = wst[ci % 2]; w_b = wbf[ci % 2]; pc = pcs[ci % 2]
            ws_k = "wst%d" % (ci % 2); wb_k = "wbf%d" % (ci % 2); pc_k = "pc%d" % (ci % 2)
            K.dma(w_s[:, :, 0:ncol], win_d[:, c0:c0 + ncol].rearrange("(k p) c -> p k c", p=128), writes=[ws_k], key=ws_k)
            K.op("pool", lambda e, w_s=w_s, w_b=w_b, ncol=ncol: e.tensor_copy(w_b[:, :, 0:ncol], w_s[:, :, 0:ncol]),
                 reads=[ws_k], writes=[wb_k])
            for n in range(nblk):
                t0 = n * 512
                tn = min(512, T - t0)
                pt = ps[2 + (n % 4)]
                pk = "ps%d" % (2 + (n % 4))
                for k in range(8):
                    K.op("pe", lambda e, k=k, pt=pt, w_b=w_b, ncol=ncol, t0=t0, tn=tn: e.matmul(
                        pt[0:ncol, 0:tn], lhsT=w_b[:, k, 0:ncol], rhs=hT[:, k, t0:t0 + tn], start=(k == 0), stop=(k == 7)),
                        reads=[wb_k, "hT"], writes=[pk])
                eng = "act" if ev % 2 == 0 else "dve"
                ev += 1
                if eng == "act":
                    K.op("act", lambda e, pt=pt, pc=pc, ncol=ncol, t0=t0, tn=tn: e.copy(pc[0:ncol, t0:t0 + tn], pt[0:ncol, 0:tn]),
                         reads=[pk], writes=[pc_k])
                else:
                    K.op("dve", lambda e, pt=pt, pc=pc, ncol=ncol, t0=t0, tn=tn: e.tensor_copy(pc[0:ncol, t0:t0 + tn], pt[0:ncol, 0:tn]),
                         reads=[pk], writes=[pc_k])
            K.dma(pT_d[dst, 0:ncol, :], pc[0:ncol, :], reads=[pc_k], writes=[("pT", dst)], key="st_" + pc_k)

    K.barrier()
    if "pT" in dbg:
        d_ = dbgt("pT", [32, 128, T])
        K.dma(d_[:, :, :], pT_d[:, :, :], reads=[("pT", i) for i in range(32)], writes=["dbgpT"], key="dbg")


    mT_d = nc.dram_tensor("mT_s", [8, 128, TL], BF16).ap()
    psb = [p.bitcast(BF16) for p in ps]
    negm_le = sb("negm_le", [128, 128])
    negm_ge = sb("negm_ge", [128, 128])
    K.op("dve", lambda e: e.tensor_scalar(negm_le[:], m_le[:], 30000.0, -30000.0, op0=ALU.mult, op1=ALU.add), reads=["m_le"], writes=["negm_le"])
    K.op("dve", lambda e: e.tensor_scalar(negm_ge[:], m_ge[:], 30000.0, -30000.0, op0=ALU.mult, op1=ALU.add), reads=["m_ge"], writes=["negm_ge"])
    fwd_order = list(range(NT))
    bwd_order = [1, 0] + list(range(NT - 1, 1, -1))

    NMODE = NM_MODE[0]
    NDT = BF16 if NMODE == "bf16" else F32

    def mmv(ap):
        return ap

    identn = identb if NMODE == "bf16" else ident

    def neumann(Y, Yt, tag, pbank, pb=None):
        PR = nm_tiles[tag]["PR"]; Pt = nm_tiles[tag]["Pt"]
        kPR = [tag + "PR0", tag + "PR1"]; kPt = [tag + "Pt0", tag + "Pt1"]
        pa_ = ps[pbank]
        ka = "ps%d" % pbank
        if pb is None:
            pb_, kb = ps[pbank + 1], "ps%d" % (pbank + 1)
        else:
            pb_, kb = ps[pb[0]][:, pb[1]:pb[1] + 128], "ps%d" % pb[0]
        K.op("pe", lambda e: e.matmul(pa_[:, 0:128], lhsT=mmv(Yt[:]), rhs=mmv(Y[:]), start=True, stop=True), reads=[tag + "Y", tag + "Yt"], writes=[ka])
        K.op("pe", lambda e: e.matmul(pb_[:, 0:128], lhsT=mmv(Y[:]), rhs=mmv(Yt[:]), start=True, stop=True), reads=[tag + "Y", tag + "Yt"], writes=[kb])
        K.op("act", lambda e: e.copy(PR[0][:, 0:128], pa_[:, 0:128]), reads=[ka], writes=[kPR[0]])
        K.op("dve", lambda e: e.tensor_tensor(PR[0][:, 128:256], Y[:], identn[:], op=ALU.add), reads=[tag + "Y", "identb", "ident"], writes=[kPR[0]])
        K.op("dve", lambda e: e.tensor_copy(Pt[0][:], pb_[:, 0:128]), reads=[kb], writes=[kPt[0]])
        cur = 0
        for l in range(1, 7):
            nxt = 1 - cur
            last = (l == 6)
            n0 = 128 if last else 0
            K.op("pe", lambda e, cur=cur, n0=n0: e.matmul(pa_[:, n0:256], lhsT=mmv(Pt[cur][:]), rhs=mmv(PR[cur][:, n0:256]), start=True, stop=False),
                 reads=[kPt[cur], kPR[cur]], writes=[ka])
            K.op("pe", lambda e, cur=cur: e.matmul(pa_[:, 128:256], lhsT=mmv(identn[:]), rhs=mmv(PR[cur][:, 128:256]), start=False, stop=True),
                 reads=["identb", "ident", kPR[cur]], writes=[ka])
            if not last:
                K.op("pe", lambda e, cur=cur: e.matmul(pb_[:, 0:128], lhsT=mmv(PR[cur][:, 0:128]), rhs=mmv(Pt[cur][:]), start=True, stop=True),
                     reads=[kPt[cur], kPR[cur]], writes=[kb])
            K.op("act", lambda e, nxt=nxt, n0=n0: e.copy(PR[nxt][:, n0:256], pa_[:, n0:256]), reads=[ka], writes=[kPR[nxt]])
            if not last:
                K.op("dve", lambda e, nxt=nxt: e.tensor_copy(Pt[nxt][:], pb_[:, 0:128]), reads=[kb], writes=[kPt[nxt]])
            cur = nxt
        if NMODE == "bf16":
            return PR[cur][:, 128:256], kPR[cur]
        fin = nm_tiles[tag]["fin"]
        K.op("act", lambda e, cur=cur: e.copy(fin[:], PR[cur][:, 128:256]), reads=[kPR[cur]], writes=[tag + "fin"])
        return fin[:], tag + "fin"

    nm_tiles = {}
    with ExitStack() as pg:
        for tag in ("n0", "n1", "n2", "n3"):
            nm_tiles[tag] = dict(PR=[sb(tag + "PR%d" % i, [128, 256], NDT, stack=pg) for i in range(2)],
                                 Pt=[sb(tag + "Pt%d" % i, [128, 128], NDT, stack=pg) for i in range(2)],
                                 fin=sb(tag + "fin", [128, 128], BF16, stack=pg))
        ab = sb("ab", [128, NT, 16], stack=pg)
        dtb_b = sb("dtb_b", [128, 8], stack=pg)
        nA_b = sb("nA_b", [128, 8], stack=pg)
        gg = sb("gg", [128, NT, 8], stack=pg)
        Gc = sb("Gc", [128, NT, 8], stack=pg)
        nbeta = sb("nbeta", [128, NT, 8], stack=pg)
        beta = sb("beta", [128, NT, 8], stack=pg)
        negeG = sb("negeG", [128, NT, 8], stack=pg)
        eG = sb("eG", [128, NT, 8], stack=pg)
        eTG = sb("eTG", [128, NT, 8], stack=pg)
        eTot = sb("eTot", [128, NT, 8], stack=pg)
        pg_ab = ExitStack()
        abT = sb("abT", [16, T], stack=pg_ab)
        K.dma(abT[:, :], pT_d[16, 0:16, :], reads=[("pT", 16)], writes=["abT"], key="abT")
        K.dma(dtb_b[:, :], dtb_d.partition_broadcast(128), writes=["dtb_b"], key="dtb_b")
        K.dma(nA_b[:, :], alog_d.partition_broadcast(128), writes=["nA_b"], key="nA_b")
        for i in range(NT):
            K.op("pe", lambda e, i=i: e.transpose(ps[i // 32][:, (i % 32) * 16:(i % 32) * 16 + 16], abT[0:16, i * 128:(i + 1) * 128], ident[0:16, 0:16]),
                 reads=["abT", "ident"], writes=["ps%d" % (i // 32)])
        for b0 in range(0, NT, 32):
            nb = min(32, NT - b0)
            K.op("dve", lambda e, b0=b0, nb=nb: e.tensor_copy(ab[:, b0:b0 + nb, :].rearrange("p n c -> p (n c)"), ps[b0 // 32][:, 0:nb * 16]),
                 reads=["ps%d" % (b0 // 32)], writes=["ab"])
        K.op("act", lambda e: e.activation(nA_b[:], nA_b[:], AF.Exp), reads=["nA_b"], writes=["nA_b"])
        K.op("dve", lambda e: e.tensor_scalar(nA_b[:], nA_b[:], -1.0, None, op0=ALU.mult), reads=["nA_b"], writes=["nA_b"])
        K.op("dve", lambda e: e.tensor_tensor(gg[:], ab[:, :, 0:8], dtb_b[:].unsqueeze(1).to_broadcast([128, NT, 8]), op=ALU.add),
             reads=["ab", "dtb_b"], writes=["gg"])
        K.op("act", lambda e: e.activation(gg[:], gg[:], AF.Exp), reads=["gg"], writes=["gg"])
        K.op("act", lambda e: e.activation(gg[:], gg[:], AF.Ln, bias=eps_t[:, 3:4]), reads=["gg", "eps"], writes=["gg"])
        K.op("dve", lambda e: e.tensor_tensor(gg[:], gg[:], nA_b[:].unsqueeze(1).to_broadcast([128, NT, 8]), op=ALU.mult),
             reads=["gg", "nA_b"], writes=["gg"])
        K.op("act", lambda e: e.activation(beta[:], ab[:, :, 8:16], AF.Sigmoid), reads=["ab"], writes=["beta"])
        K.op("dve", lambda e: e.tensor_scalar(nbeta[:], beta[:], -1.0, None, op0=ALU.mult), reads=["beta"], writes=["nbeta"])
        ggf = gg[:].rearrange("p n c -> p (n c)")
        K.op("pe", lambda e: e.matmul(ps[2][:, 0:NT * 8], lhsT=m_le[:], rhs=ggf, start=True, stop=True), reads=["m_le", "gg"], writes=["ps2"])
        K.op("pe", lambda e: e.matmul(ps[3][:, 0:NT * 8], lhsT=m_ge[:], rhs=ggf, start=True, stop=True), reads=["m_ge", "gg"], writes=["ps3"])
        K.op("pe", lambda e: e.matmul(ps[4][:, 0:NT * 8], lhsT=ones[:], rhs=ggf, start=True, stop=True), reads=["ones", "gg"], writes=["ps4"])
        K.op("dve", lambda e: e.tensor_copy(Gc[:, :, 0:4], ps[2][:, 0:NT * 8].rearrange("p (n c) -> p n c", c=8)[:, :, 0:4]), reads=["ps2"], writes=["Gc"])
        K.op("dve", lambda e: e.tensor_copy(Gc[:, :, 4:8], ps[3][:, 0:NT * 8].rearrange("p (n c) -> p n c", c=8)[:, :, 4:8]), reads=["ps3"], writes=["Gc"])
        K.op("act", lambda e: e.activation(eG[:], Gc[:], AF.Exp), reads=["Gc"], writes=["eG"])
        K.op("dve", lambda e: e.tensor_scalar(negeG[:], eG[:], -1.0, None, op0=ALU.mult), reads=["eG"], writes=["negeG"])
        K.op("act", lambda e: e.activation(eTot[:].rearrange("p n c -> p (n c)"), ps[4][:, 0:NT * 8], AF.Exp), reads=["ps4"], writes=["eTot"])
        K.op("dve", lambda e: e.tensor_tensor(eTG[:].rearrange("p n c -> p (n c)"), ps[4][:, 0:NT * 8], Gc[:].rearrange("p n c -> p (n c)"), op=ALU.subtract),
             reads=["ps4", "Gc"], writes=["eTG"])
        K.op("act", lambda e: e.activation(eTG[:], eTG[:], AF.Exp), reads=["eTG"], writes=["eTG"])

        K.barrier()
        pg_ab.close()
        stop_at("gdn_scal")
        qT = sb("qT", [128, T], BF16, stack=pg)
        kT = sb("kT", [128, T], BF16, stack=pg)
        vT = sb("vT", [128, T], BF16, stack=pg)
        zs = sb("zs", [128, T], BF16, stack=pg)
        vtok = sb("vtok", [128, NT, 128], BF16, stack=pg)
        ktok = sb("ktok", [128, NT, 128], BF16, stack=pg)
        obuf = [sb("obuf%d" % d_, [128, NT, 128], BF16, stack=pg) for d_ in range(2)]
        AinvAll = [sb("AinvAll%d" % d_, [128, NT, 128], BF16, stack=pg) for d_ in range(2)]
        MqkAll = [sb("MqkAll%d" % d_, [128, NT, 128], BF16, stack=pg) for d_ in range(2)]
        pin = sb("pin", [128, T], stack=pg)
        cv = sb("cv", [128, T], stack=pg)
        sq = pin
        rn = sb("rn", [128, 512], stack=pg)
        S = [sb("S%d" % d_, [128, 128], stack=pg) for d_ in range(2)]
        Sb = [sb("Sb%d" % d_, [128, 128], BF16, stack=pg) for d_ in range(2)]
        mst = sb("mst", [128, TL], BF16, stack=pg)
        dgl = [sb("dgl%d" % d_, [128, 128], stack=pg) for d_ in range(4)]
        arg = dgl
        DTi = [sb("DTi%d" % d_, [128, 128], stack=pg) for d_ in range(4)]
        DTs = dgl
        Yb = [sb("Yb%d" % d_, [128, 128], NDT, stack=pg) for d_ in range(4)]
        Ytb = [sb("Ytb%d" % d_, [128, 128], NDT, stack=pg) for d_ in range(4)]
        Rb = [sb("Rb%d" % d_, [128, 128], BF16, stack=pg) for d_ in range(2)]
        Xb = [sb("Xb%d" % d_, [128, 128], BF16, stack=pg) for d_ in range(2)]
        Xs = [sb("Xs%d" % d_, [128, 128], BF16, stack=pg) for d_ in range(2)]
        QSe = [sb("QSe%d" % d_, [128, 128], stack=pg) for d_ in range(2)]
        on_ = sb("on_", [128, 128], stack=pg)
        oj = sb("oj", [128, 128], stack=pg)
        onn = sb("onn", [128, 128], stack=pg)
        oss = sb("oss", [128, 4], stack=pg)

        def conv_silu(cidx, dst, dkey, final_silu_to):
            cw0 = PK1["CW"]
            K.op("dve", lambda e: e.tensor_scalar(cv[:], pin[:], pk1[:, cw0 + 2 * 12 + cidx:cw0 + 2 * 12 + cidx + 1], None, op0=ALU.mult),
                 reads=["pin", "pk1"], writes=["cv"])
            for j in (0, 1, 3, 4):
                sh = j - 2
                wcol = pk1[:, cw0 + j * 12 + cidx:cw0 + j * 12 + cidx + 1]
                for (s0, s1) in ((0, TC), (TC, T)):
                    lo = max(s0, s0 - sh); hi = min(s1, s1 - sh)
                    K.op("dve", lambda e, lo=lo, hi=hi, sh=sh, wcol=wcol: e.scalar_tensor_tensor(
                        cv[:, lo:hi], pin[:, lo + sh:hi + sh], wcol, cv[:, lo:hi], op0=ALU.mult, op1=ALU.add),
                        reads=["pin", "pk1", "cv"], writes=["cv"])
            K.op("act", lambda e: e.activation(final_silu_to[:], cv[:], AF.Silu), reads=["cv"], writes=[dkey])

        def l2n(src, skey, dst, dkey, scale):
            K.op("pool", lambda e: e.tensor_tensor(sq[:], src[:], src[:], op=ALU.mult), reads=[skey], writes=["pin"])
            for n in range((T + 511) // 512):
                t0 = n * 512; tn = min(512, T - t0)
                pt = ps[5 + (n % 2)]; pk = "ps%d" % (5 + (n % 2))
                K.op("pe", lambda e, pt=pt, t0=t0, tn=tn: e.matmul(pt[:, 0:tn], lhsT=ones[:], rhs=sq[:, t0:t0 + tn], start=True, stop=True),
                     reads=["ones", "pin"], writes=[pk])
                K.op("act", lambda e, pt=pt, tn=tn: e.activation(rn[:, 0:tn], pt[:, 0:tn], AF.Sqrt, bias=eps_t[:, 1:2]), reads=[pk, "eps"], writes=["rn"])
                K.op("dve", lambda e, tn=tn: e.reciprocal(rn[:, 0:tn], rn[:, 0:tn]), reads=["rn"], writes=["rn"])
                K.op("dve", lambda e, t0=t0, tn=tn: e.scalar_tensor_tensor(dst[:, t0:t0 + tn], src[:, t0:t0 + tn], scale, rn[:, 0:tn], op0=ALU.mult, op1=ALU.mult),
                     reads=[skey, "rn"], writes=[dkey])

        def to_tok(src, skey, dst, dkey):
            for i in range(NT):
                pt = psb[5 + (i % 2)]; pk = "ps%d" % (5 + (i % 2))
                K.op("pe", lambda e, i=i, pt=pt: e.transpose(pt[:, 0:128], src[:, i * 128:(i + 1) * 128], identb[:]), reads=[skey, "identb"], writes=[pk])
                eng = "act" if i % 2 == 0 else "dve"
                if eng == "act":
                    K.op("act", lambda e, i=i, pt=pt: e.copy(dst[:, i, :], pt[:, 0:128]), reads=[pk], writes=[dkey])
                else:
                    K.op("dve", lambda e, i=i, pt=pt: e.tensor_copy(dst[:, i, :], pt[:, 0:128]), reads=[pk], writes=[dkey])

        for h in range(4):
            K.dma(pin[:, :], pT_d[h, :, :], reads=[("pT", h)], writes=["pin"], key="pin")
            conv_silu(h, cv, "cv", cv)
            l2n(cv, "cv", qT, "qT", float(128 ** -0.5))
            K.dma(pin[:, :], pT_d[4 + h, :, :], reads=[("pT", 4 + h)], writes=["pin"], key="pin")
            conv_silu(4 + h, cv, "cv", cv)
            l2n(cv, "cv", kT, "kT", 1.0)
            to_tok(kT, "kT", ktok, "ktok")
            K.dma(pin[:, :], pT_d[8 + h, :, :], reads=[("pT", 8 + h)], writes=["pin"], key="pin")
            conv_silu(8 + h, vT, "vT", vT)
            to_tok(vT, "vT", vtok, "vtok")
            K.dma(pin[:, :], pT_d[12 + h, :, :], reads=[("pT", 12 + h)], writes=["pin"], key="pin")
            K.op("act", lambda e: e.activation(zs[:], pin[:], AF.Silu), reads=["pin"], writes=["zs"])
            stop_at("gdn_prep")
            for d_ in range(2):
                K.op("pool", lambda e, d_=d_: e.memset(S[d_][:], 0.0), writes=["S%d" % d_])
                K.op("pool", lambda e, d_=d_: e.memset(Sb[d_][:], 0.0), writes=["Sb%d" % d_])
            def g_pre(step, d_, sl_):
                i = fwd_order[step] if d_ == 0 else bwd_order[step]
                par = step % 2
                r = d_ * 4 + h
                tag = "n%d" % sl_
                sl = slice(i * 128, (i + 1) * 128)
                want_o = i >= 2
                pA = ps[2 * sl_]; kA = "ps%d" % (2 * sl_)
                dk_ = "%d" % sl_
                K.op("dve", lambda e: e.tensor_scalar(dgl[sl_][:], ident[:], Gc[:, i, r:r + 1], None, op0=ALU.mult),
                     reads=["ident", "Gc"], writes=["dgl" + dk_])
                K.op("pe", lambda e: e.matmul(pA[:, 256:384], lhsT=ones[:], rhs=dgl[sl_][:], start=True, stop=True),
                     reads=["ones", "dgl" + dk_], writes=[kA])
                negm = negm_le if d_ == 0 else negm_ge
                mstrict = m_lt if d_ == 0 else m_gt
                K.op("dve", lambda e: e.scalar_tensor_tensor(
                    arg[sl_][:], pA[:, 256:384], Gc[:, i, r:r + 1], negm[:], op0=ALU.subtract, op1=ALU.add),
                    reads=[kA, "Gc", "negm_le", "negm_ge"], writes=["dgl" + dk_])
                K.op("act", lambda e: e.activation(DTi[sl_][:], arg[sl_][:], AF.Exp), reads=["dgl" + dk_], writes=["DTi" + dk_])
                K.op("pool", lambda e: e.tensor_tensor(DTs[sl_][:], DTi[sl_][:], mstrict[:], op=ALU.mult),
                     reads=["DTi" + dk_, "m_lt", "m_gt"], writes=["dgl" + dk_])
                K.op("pe", lambda e: e.matmul(pA[:, 0:128], lhsT=kT[:, sl], rhs=kT[:, sl], start=True, stop=True),
                     reads=["kT"], writes=[kA])
                K.op("dve", lambda e: e.scalar_tensor_tensor(
                    Yb[sl_][:], pA[:, 0:128], nbeta[:, i, r:r + 1], DTs[sl_][:], op0=ALU.mult, op1=ALU.mult),
                    reads=[kA, "nbeta", "dgl" + dk_], writes=[tag + "Y"])
                if want_o:
                    K.op("pe", lambda e: e.matmul(pA[:, 128:256], lhsT=kT[:, sl], rhs=qT[:, sl], start=True, stop=True),
                         reads=["kT", "qT"], writes=[kA])
                    K.op("dve", lambda e: e.tensor_tensor(MqkAll[d_][:, step, :], pA[:, 128:256], DTi[sl_][:], op=ALU.mult),
                         reads=[kA, "DTi" + dk_], writes=[("Mqk", d_, step)])
                pB = (psb if NMODE == "bf16" else ps)[2 * sl_ + 1]; kB = "ps%d" % (2 * sl_ + 1)
                K.op("pe", lambda e: e.transpose(pB[:, 0:128], Yb[sl_][:], identn[:]), reads=[tag + "Y", "identb", "ident"], writes=[kB])
                K.op("act", lambda e: e.copy(Ytb[sl_][:], pB[:, 0:128]), reads=[kB], writes=[tag + "Yt"])
                AinvT, kAinv = neumann(Yb[sl_], Ytb[sl_], tag, 2 * sl_ + 1, pb=(2 * sl_, 384))
                K.op("pool", lambda e: e.tensor_copy(AinvAll[d_][:, step, :], AinvT), reads=[kAinv], writes=[("gAinv", d_, step)])

            def g_chain(step, d_):
                i = fwd_order[step] if d_ == 0 else bwd_order[step]
                par = step % 2
                r = d_ * 4 + h
                sl = slice(i * 128, (i + 1) * 128)
                want_o = i >= 2
                pC = ps[d_ * 4 + 3]; kC = "ps%d" % (d_ * 4 + 3)
                dk_ = "%d" % d_
                kAinv = ("gAinv", d_, step)
                kMqk = ("Mqk", d_, step)
                K.op("pe", lambda e: e.matmul(pC[:, 0:128], lhsT=kT[:, sl], rhs=Sb[d_][:], start=True, stop=True),
                     reads=["kT", "Sb" + dk_], writes=[kC])
                if want_o:
                    K.op("pe", lambda e: e.matmul(pC[:, 128:256], lhsT=qT[:, sl], rhs=Sb[d_][:], start=True, stop=True),
                         reads=["qT", "Sb" + dk_], writes=[kC])
                K.op("dve", lambda e: e.scalar_tensor_tensor(
                    Rb[d_][:], pC[:, 0:128], negeG[:, i, r:r + 1], vtok[:, i, :], op0=ALU.mult, op1=ALU.add),
                    reads=[kC, "negeG", "vtok"], writes=["Rb" + dk_])
                if want_o:
                    K.op("act", lambda e: e.activation(QSe[d_][:], pC[:, 128:256], AF.Identity, scale=eG[:, i, r:r + 1]),
                         reads=[kC, "eG"], writes=["QSe" + dk_])
                K.op("pe", lambda e: e.matmul(pC[:, 0:128], lhsT=AinvAll[d_][:, step, :], rhs=Rb[d_][:], start=True, stop=True),
                     reads=[kAinv, "Rb" + dk_], writes=[kC])
                K.op("dve", lambda e: e.tensor_scalar(Xb[d_][:], pC[:, 0:128], beta[:, i, r:r + 1], None, op0=ALU.mult),
                     reads=[kC, "beta"], writes=["Xb" + dk_])
                K.op("pool", lambda e: e.tensor_scalar(Xs[d_][:], Xb[d_][:], eTG[:, i, r:r + 1], None, op0=ALU.mult),
                     reads=["Xb" + dk_, "eTG"], writes=["Xs" + dk_])
                if want_o:
                    K.op("pe", lambda e: e.matmul(pC[:, 384:512], lhsT=MqkAll[d_][:, step, :], rhs=Xb[d_][:], start=True, stop=True),
                         reads=[kMqk, "Xb" + dk_], writes=[kC])
                    K.op("dve", lambda e: e.tensor_tensor(obuf[d_][:, i, :], pC[:, 384:512], QSe[d_][:], op=ALU.add),
                         reads=[kC, "QSe" + dk_], writes=["obuf" + dk_])
                K.op("pe", lambda e: e.matmul(pC[:, 256:384], lhsT=ktok[:, i, :], rhs=Xs[d_][:], start=True, stop=True),
                     reads=["ktok", "Xs" + dk_], writes=[kC])
                K.op("dve", lambda e: e.scalar_tensor_tensor(
                    S[d_][:], S[d_][:], eTot[:, i, r:r + 1], pC[:, 256:384], op0=ALU.mult, op1=ALU.add),
                    reads=["S" + dk_, "eTot", kC], writes=["S" + dk_])
                K.op("act", lambda e: e.copy(Sb[d_][:], S[d_][:]), reads=["S" + dk_], writes=["Sb" + dk_])

            inst = [(st_, dd_) for st_ in range(NT) for dd_ in range(2)]
            for g0 in range(0, len(inst), 4):
                grp = inst[g0:g0 + 4]
                for q_ in K.streams(len(grp)):
                    g_pre(grp[q_][0], grp[q_][1], q_)
            for step in range(NT):
                for d_ in K.streams(2):
                    g_chain(step, d_)
            stop_at("gdn_scan")
            for i in range(2, NT):
                K.op("dve", lambda e, i=i: e.tensor_tensor(on_[:], obuf[0][:, i, :], obuf[1][:, i, :], op=ALU.add),
                     reads=["obuf0", "obuf1"], writes=["on_"])
                K.op("act", lambda e: e.activation(oj[:], on_[:], AF.Square), reads=["on_"], writes=["oj"])
                K.op("dve", lambda e: e.reduce_sum(oss[:, 0:1], oj[:], axis=AX.X), reads=["oj"], writes=["oss"])
                K.op("act", lambda e: e.activation(oss[:, 1:2], oss[:, 0:1], AF.Sqrt, bias=eps_t[:, 0:1], scale=1.0 / 128), reads=["oss", "eps"], writes=["oss1"])
                K.op("dve", lambda e: e.reciprocal(oss[:, 2:3], oss[:, 1:2]), reads=["oss1"], writes=["oss2"])
                K.op("dve", lambda e: e.tensor_scalar(onn[:], on_[:], oss[:, 2:3], None, op0=ALU.mult), reads=["on_", "oss2"], writes=["onn"])
                pt = ps[i % 2]; pk = "ps%d" % (i % 2)
                K.op("pe", lambda e, pt=pt: e.transpose(pt[:, 0:128], onn[:], ident[:]), reads=["onn", "ident"], writes=[pk])
                gcol = pk2[:, PK2["GNORM"]:PK2["GNORM"] + 1]
                K.op("dve", lambda e, i=i, pt=pt, gcol=gcol: e.scalar_tensor_tensor(
                    mst[:, (i - 2) * 128:(i - 1) * 128], pt[:, 0:128], gcol, zs[:, i * 128:(i + 1) * 128], op0=ALU.mult, op1=ALU.mult),
                    reads=[pk, "pk2", "zs"], writes=["mst"])
            K.dma(mT_d[h, :, :], mst[:, :], reads=["mst"], writes=[("mT", h)], key="st_mst")

    K.barrier()
    pm_d = nc.dram_tensor("pm_s", [12, 128, T], F32).ap()
    lora_d = nc.dram_tensor("lora_s", [3, 128, T], BF16).ap()
    CW_ = float(np.exp(-0.5))
    NRW = n_rows
    with ExitStack() as pr:
        cidx_i = sb("cidx_i", [128, 15], I32, stack=pr)
        cidx = sb("cidx", [128, 15], stack=pr)
        mum = {nm: sb("mum_" + nm, [128, 15], stack=pr) for nm in ("om", "L", "R", "U", "D", "P", "N")}
        tmpm = sb("tmpm", [128, 15], stack=pr)
        K.op("pool", lambda e: e.iota(cidx_i[:], pattern=[[128, 15]], base=0, channel_multiplier=1), writes=["cidx_i"])
        K.op("dve", lambda e: e.tensor_copy(cidx[:], cidx_i[:]), reads=["cidx_i"], writes=["cidx"])
        mu_ap = pk2[:, PK2["MU"]:PK2["MU"] + 15]
        K.op("dve", lambda e: e.tensor_scalar(mum["om"][:], mu_ap, -1.0, 1.0, op0=ALU.mult, op1=ALU.add), reads=["pk2"], writes=["mum"])

        def band(nm, lo, hi):
            K.op("dve", lambda e: e.tensor_scalar(tmpm[:], cidx[:], float(lo), None, op0=ALU.is_ge), reads=["cidx"], writes=["tmpm"])
            K.op("dve", lambda e: e.scalar_tensor_tensor(tmpm[:], cidx[:], float(hi), tmpm[:], op0=ALU.is_lt, op1=ALU.mult), reads=["cidx", "tmpm"], writes=["tmpm"])
            K.op("dve", lambda e: e.tensor_tensor(mum[nm][:], tmpm[:], mu_ap, op=ALU.mult), reads=["tmpm", "pk2"], writes=["mum"])

        band("L", 0, 480); band("R", 480, 960); band("U", 960, 1440); band("D", 1440, 1920)
        band("P", 0, 960); band("N", 960, 1920)
        pins = [sb("rpin%d" % i, [128, T], stack=pr) for i in range(2)]
        pmx = [sb("pmx%d" % i, [128, T], stack=pr) for i in range(2)]
        lob = sb("lob", [128, T], BF16, stack=pr)
        for j in range(15):
            pin_ = pins[j % 2]; pk_ = "rpin%d" % (j % 2)
            po = pmx[j % 2]; ok_ = "pmx%d" % (j % 2)
            K.dma(pin_[:, :], pT_d[17 + j, :, :], reads=[("pT", 17 + j)], writes=[pk_], key=pk_)
            K.op("dve", lambda e, j=j, pin_=pin_, po=po: e.tensor_scalar(po[:], pin_[:], mum["om"][:, j:j + 1], None, op0=ALU.mult),
                 reads=[pk_, "mum"], writes=[ok_])
            c0, c1 = j * 128, j * 128 + 128

            def has(lo, hi):
                return c0 < hi and c1 > lo

            def acc(dst, src, nm, j=j, pin_=pin_, po=po, pk_=pk_, ok_=ok_, eng="dve"):
                K.op("dve", lambda e: e.scalar_tensor_tensor(dst(po), src(pin_), mum[nm][:, j:j + 1], dst(po), op0=ALU.mult, op1=ALU.add),
                     reads=[pk_, "mum", ok_], writes=[ok_])

            lat = lambda t: t[:, TC:T].rearrange("p (r w) -> p r w", w=64)
            if has(0, 960):
                acc(lambda t: t[:, 1:TC], lambda t: t[:, 0:TC - 1], "P")
            if has(960, 1920):
                acc(lambda t: t[:, 0:TC - 1], lambda t: t[:, 1:TC], "N")
            if has(0, 480):
                acc(lambda t: lat(t)[:, :, 1:64], lambda t: lat(t)[:, :, 0:63], "L")
            if has(480, 960):
                acc(lambda t: lat(t)[:, :, 0:63], lambda t: lat(t)[:, :, 1:64], "R")
            if has(960, 1440) and NRW > 1:
                acc(lambda t: lat(t)[:, 1:NRW, :], lambda t: lat(t)[:, 0:NRW - 1, :], "U")
            if has(1440, 1920) and NRW > 1:
                acc(lambda t: lat(t)[:, 0:NRW - 1, :], lambda t: lat(t)[:, 1:NRW, :], "D")
            if j < 12:
                K.dma(pm_d[j, :, :], po[:, :], reads=[ok_], writes=[("pm", j)], key="st_" + ok_)
            else:
                fn_ = {12: AF.Tanh, 13: AF.Identity, 14: AF.Sigmoid}[j]
                K.op("act", lambda e, po=po, fn_=fn_: e.activation(lob[:], po[:], fn_), reads=[ok_], writes=["lob"])
                K.dma(lora_d[j - 12, :, :], lob[:, :], reads=["lob"], writes=[("lora", j - 12)], key="st_lob")
    K.barrier()
    stop_at("rw_mix")
    if "pm" in dbg:
        d_o = dbgt("pm", [12, 128, T])
        K.dma(d_o[:, :, :], pm_d[:, :, :], reads=[("pm", j) for j in range(12)], writes=["dbgpm"], key="dbg")

    with ExitStack() as pw:
        nm_tiles.clear()
        RW_NDT = BF16 if RW_NM[0] == "bf16" else F32
        for tag in ("n0", "n1"):
            nm_tiles[tag] = dict(PR=[sb(tag + "rPR%d" % i, [128, 256], NDT, stack=pw) for i in range(2)],
                                 Pt=[sb(tag + "rPt%d" % i, [128, 128], NDT, stack=pw) for i in range(2)],
                                 fin=sb(tag + "rfin", [128, 128], BF16, stack=pw))
        BL = min(256, T)
        blocks = [(b0, min(BL, T - b0)) for b0 in range(0, T, BL)]
        wst_ = sb("rw_wst", [128, 3, 512], stack=pw)
        wlb = sb("rw_wlb", [128, 3, 512], BF16, stack=pw)
        for q_, src in enumerate((w2_d, a2_d, g2w_d)):
            K.dma(wst_[:, q_, :], src[:, :], writes=["rw_wst"], key="rw_wst")
        K.op("dve", lambda e: e.tensor_copy(wlb[:], wst_[:]), reads=["rw_wst"], writes=["wlb"])
        bones = sb("bones", [128, 128], stack=pw)
        K.op("pool", lambda e: e.memset(bones[:], 0.0), writes=["bones"])
        K.op("pool", lambda e: e.memset(bones[0:64, 0:64], 1.0), writes=["bones"])
        K.op("pool", lambda e: e.memset(bones[64:128, 64:128], 1.0), writes=["bones"])
        rmask = sb("rmask", [128, BL], stack=pw)
        K.op("pool", lambda e: e.memset(rmask[:], 1.0), writes=["rmask"])
        K.op("pool", lambda e: e.memset(rmask[:].rearrange("p (n t) -> p n t", t=128)[:, :, 0:1], 0.0), writes=["rmask"])
        mask4 = [sb("mask4_%d" % d_, [128, 512], stack=pw) for d_ in range(2)]
        for d_ in range(2):
            ms_, mi_ = (m_lt, m_le) if d_ == 0 else (m_gt, m_ge)
            for q_ in range(4):
                src = ms_ if q_ % 2 == 0 else mi_
                K.op("dve", lambda e, d_=d_, q_=q_, src=src: e.tensor_copy(mask4[d_][:, q_ * 128:(q_ + 1) * 128], src[:]),
                     reads=["m_lt", "m_le", "m_gt", "m_ge"], writes=["mask4"])
        rT = [sb("rT%d" % d_, [128, T], BF16, stack=pw) for d_ in range(2)]
        kpT = [sb("kpT%d" % d_, [128, T], BF16, stack=pw) for d_ in range(2)]
        ktT = [sb("ktT%d" % d_, [128, T], BF16, stack=pw) for d_ in range(2)]
        nbT = [sb("nbT%d" % d_, [128, T], BF16, stack=pw) for d_ in range(2)]
        Lam = [sb("Lam%d" % d_, [128, NT], stack=pw) for d_ in range(2)]
        vtk = sb("rvtok", [128, NT, 128], BF16, stack=pw)
        bonus = sb("bonus", [128, T], BF16, stack=pw)
        gateT = sb("gateT", [128, T], BF16, stack=pw)
        ybuf = [sb("ybuf%d" % d_, [128, NT, 128], BF16, stack=pw) for d_ in range(2)]
        rmst = sb("rmst", [128, TL], BF16, stack=pw)
        bt = {nm: sb("b_" + nm, [128, BL], stack=pw) for nm in
              ("r", "k", "v", "kap", "sig", "cum", "w", "iw", "wp", "a", "t1", "t2", "rk")}
        lbt = sb("b_lora", [128, 3, BL], BF16, stack=pw)
        vb16 = sb("b_vb16", [128, BL], BF16, stack=pw)
        Z = [sb("Z%d" % d_, [128, 128], stack=pw) for d_ in range(2)]
        Zb = [sb("Zb%d" % d_, [128, 128], BF16, stack=pw) for d_ in range(2)]
        AK = [sb("AK%d" % d_, [128, 512], BF16, stack=pw) for d_ in range(2)]
        ANB = [sb("ANB%d" % d_, [128, 512], BF16, stack=pw) for d_ in range(2)]
        YY = [[sb("YY%d%d" % (d_, hh), [128, 128], NDT, stack=pw) for hh in range(2)] for d_ in range(2)]
        YYt = [[sb("YYt%d%d" % (d_, hh), [128, 128], NDT, stack=pw) for hh in range(2)] for d_ in range(2)]
        AinvS = [[sb("Ainv%d%d" % (d_, hh), [128, 128], BF16, stack=pw) for hh in range(2)] for d_ in range(2)]
        ktok_ = [sb("rktok%d" % d_, [128, 128], BF16, stack=pw) for d_ in range(2)]
        nbtok_ = [sb("rnbtok%d" % d_, [128, 128], BF16, stack=pw) for d_ in range(2)]
        P1b = [sb("P1b%d" % d_, [128, 128], BF16, stack=pw) for d_ in range(2)]
        Ub = [sb("Ub%d" % d_, [128, 128], BF16, stack=pw) for d_ in range(2)]
        zt = [sb("zt%d" % d_, [128, 128], stack=pw) for d_ in range(2)]
        yo = sb("yo", [128, 128], stack=pw)
        yc = sb("yc", [128, 128], stack=pw)
        ysq = sb("ysq", [128, 128], stack=pw)
        yst = sb("yst", [128, 8], stack=pw)
        yt2 = sb("yt2", [128, 128], stack=pw)

        for P in range(4):
            ch = slice(P * 128, (P + 1) * 128)
            for (b0, bn) in blocks:
                bs = slice(b0, b0 + bn)
                ntb = bn // 128
                for nm, jj in (("r", P), ("k", 4 + P), ("v", 8 + P)):
                    K.dma(bt[nm][:, 0:bn], pm_d[jj, :, bs], reads=[("pm", jj)], writes=["b_" + nm], key="b_" + nm)
                K.dma(lbt[:, :, 0:bn], lora_d[:, :, bs].rearrange("q p t -> p q t"), reads=[("lora", 0), ("lora", 1), ("lora", 2)], writes=["b_lora"], key="b_lora")
                K.op("pool", lambda e, bn=bn: e.tensor_copy(vb16[:, 0:bn], bt["v"][:, 0:bn]), reads=["b_v"], writes=["vb16"])
                for ii in range(ntb):
                    gi = b0 // 128 + ii
                    pt = psb[6 + (ii % 2)]; pk = "ps%d" % (6 + (ii % 2))
                    K.op("pe", lambda e, ii=ii, pt=pt: e.transpose(pt[:, 0:128], vb16[:, ii * 128:(ii + 1) * 128], identb[:]), reads=["vb16", "identb"], writes=[pk])
                    K.op("act", lambda e, gi=gi, pt=pt: e.copy(vtk[:, gi, :], pt[:, 0:128]), reads=[pk], writes=["rvtok"])
                kkc = pk2[:, PK2["KK"] + P:PK2["KK"] + P + 1]
                K.op("dve", lambda e, bn=bn, kkc=kkc: e.tensor_scalar(bt["kap"][:, 0:bn], bt["k"][:, 0:bn], kkc, None, op0=ALU.mult), reads=["b_k", "pk2"], writes=["b_kap"])
                K.op("pool", lambda e, bn=bn: e.tensor_tensor(bt["t1"][:, 0:bn], bt["kap"][:, 0:bn], bt["kap"][:, 0:bn], op=ALU.mult), reads=["b_kap"], writes=["b_t1"])
                for n in range((bn + 511) // 512):
                    t0 = n * 512; tn = min(512, bn - t0)
                    K.op("pe", lambda e, t0=t0, tn=tn: e.matmul(ps[0][:, 0:tn], lhsT=bones[:], rhs=bt["t1"][:, t0:t0 + tn], start=True, stop=True), reads=["bones", "b_t1"], writes=["ps0"])
                    K.op("act", lambda e, t0=t0, tn=tn: e.activation(bt["t2"][:, t0:t0 + tn], ps[0][:, 0:tn], AF.Sqrt, bias=eps_t[:, 1:2]), reads=["ps0", "eps"], writes=["b_t2"])
                K.op("dve", lambda e, bn=bn: e.reciprocal(bt["t2"][:, 0:bn], bt["t2"][:, 0:bn]), reads=["b_t2"], writes=["b_t2"])
                K.op("dve", lambda e, bn=bn: e.tensor_tensor(bt["kap"][:, 0:bn], bt["kap"][:, 0:bn], bt["t2"][:, 0:bn], op=ALU.mult), reads=["b_kap", "b_t2"], writes=["b_kap"])
                for n in range((bn + 511) // 512):
                    t0 = n * 512; tn = min(512, bn - t0)
                    K.op("pe", lambda e, t0=t0, tn=tn: e.matmul(ps[1][:, 0:tn], lhsT=wlb[:, 2, ch], rhs=lbt[:, 2, t0:t0 + tn], start=True, stop=True), reads=["wlb", "b_lora"], writes=["ps1"])
                    K.op("act", lambda e, t0=t0, tn=tn: e.copy(gateT[:, b0 + t0:b0 + t0 + tn], ps[1][:, 0:tn]), reads=["ps1"], writes=["gateT"])
                first_rk = True
                for d_ in range(2):
                    ds = slice(d_ * 64, (d_ + 1) * 64)
                    w0c = pk2[:, PK2["W0"] + d_ * 4 + P:PK2["W0"] + d_ * 4 + P + 1]
                    a0c = pk2[:, PK2["A0"] + d_ * 4 + P:PK2["A0"] + d_ * 4 + P + 1]
                    for n in range((bn + 511) // 512):
                        t0 = n * 512; tn = min(512, bn - t0)
                        K.op("pe", lambda e, t0=t0, tn=tn, ds=ds: e.matmul(ps[2][:, 0:tn], lhsT=wlb[ds, 0, ch], rhs=lbt[ds, 0, t0:t0 + tn], start=True, stop=True), reads=["wlb", "b_lora"], writes=["ps2"])
                        K.op("act", lambda e, t0=t0, tn=tn, w0c=w0c: e.activation(bt["sig"][:, t0:t0 + tn], ps[2][:, 0:tn], AF.Sigmoid, bias=w0c), reads=["ps2", "pk2"], writes=["b_sig"])
                        K.op("pe", lambda e, t0=t0, tn=tn, ds=ds: e.matmul(ps[3][:, 0:tn], lhsT=wlb[ds, 1, ch], rhs=lbt[ds, 1, t0:t0 + tn], start=True, stop=True), reads=["wlb", "b_lora"], writes=["ps3"])
                        K.op("act", lambda e, t0=t0, tn=tn, a0c=a0c: e.activation(bt["a"][:, t0:t0 + tn], ps[3][:, 0:tn], AF.Sigmoid, bias=a0c), reads=["ps3", "pk2"], writes=["b_a"])
                    K.op("dve", lambda e, bn=bn: e.tensor_tensor_scan(bt["cum"][:, 0:bn], rmask[:, 0:bn], bt["sig"][:, 0:bn], 0.0, op0=ALU.mult, op1=ALU.add),
                         reads=["rmask", "b_sig"], writes=["b_cum"])
                    c3 = bt["cum"][:, 0:bn].rearrange("p (n t) -> p n t", t=128)
                    tot_b = c3[:, :, 127:128].to_broadcast([128, ntb, 128])
                    K.op("act", lambda e, d_=d_, c3=c3, ntb=ntb: e.activation(Lam[d_][:, b0 // 128:b0 // 128 + ntb], c3[:, :, 127], AF.Exp, scale=-CW_),
                         reads=["b_cum"], writes=["Lam%d" % d_])
                    if d_ == 1:
                        K.op("dve", lambda e, bn=bn, c3=c3, tot_b=tot_b, ntb=ntb: e.tensor_tensor(
                            bt["t1"][:, 0:bn].rearrange("p (n t) -> p n t", t=128), tot_b, c3, op=ALU.subtract), reads=["b_cum"], writes=["b_t1"])
                        K.op("dve", lambda e, bn=bn: e.tensor_tensor(bt["cum"][:, 0:bn], bt["t1"][:, 0:bn], bt["sig"][:, 0:bn], op=ALU.add),
                             reads=["b_t1", "b_sig"], writes=["b_cum"])
                    K.op("act", lambda e, bn=bn: e.activation(bt["w"][:, 0:bn], bt["cum"][:, 0:bn], AF.Exp, scale=-CW_), reads=["b_cum"], writes=["b_w"])
                    K.op("act", lambda e, bn=bn: e.activation(bt["iw"][:, 0:bn], bt["cum"][:, 0:bn], AF.Exp, scale=CW_), reads=["b_cum"], writes=["b_iw"])
                    K.op("pool", lambda e, bn=bn: e.tensor_tensor(bt["t1"][:, 0:bn], bt["cum"][:, 0:bn], bt["sig"][:, 0:bn], op=ALU.subtract), reads=["b_cum", "b_sig"], writes=["b_t1"])
                    K.op("act", lambda e, bn=bn: e.activation(bt["wp"][:, 0:bn], bt["t1"][:, 0:bn], AF.Exp, scale=-CW_), reads=["b_t1"], writes=["b_wp"])
                    K.op("dve", lambda e, d_=d_, bn=bn: e.tensor_tensor(rT[d_][:, bs], bt["r"][:, 0:bn], bt["w"][:, 0:bn], op=ALU.mult), reads=["b_r", "b_w"], writes=["rT%d" % d_])
                    K.op("pool", lambda e, d_=d_, bn=bn: e.tensor_tensor(kpT[d_][:, bs], bt["kap"][:, 0:bn], bt["wp"][:, 0:bn], op=ALU.mult), reads=["b_kap", "b_wp"], writes=["kpT%d" % d_])
                    kac = pk2[:, PK2["KA"] + P:PK2["KA"] + P + 1]
                    K.op("dve", lambda e, bn=bn, kac=kac: e.tensor_scalar(bt["t1"][:, 0:bn], bt["a"][:, 0:bn], -1.0, kac, op0=ALU.add, op1=ALU.mult), reads=["b_a", "pk2"], writes=["b_t1"])
                    K.op("dve", lambda e, bn=bn: e.scalar_tensor_tensor(bt["t1"][:, 0:bn], bt["t1"][:, 0:bn], 1.0, bt["k"][:, 0:bn], op0=ALU.add, op1=ALU.mult), reads=["b_t1", "b_k"], writes=["b_t1"])
                    K.op("dve", lambda e, d_=d_, bn=bn: e.tensor_tensor(ktT[d_][:, bs], bt["t1"][:, 0:bn], bt["iw"][:, 0:bn], op=ALU.mult), reads=["b_t1", "b_iw"], writes=["ktT%d" % d_])
                    if first_rk:
                        K.op("pool", lambda e, bn=bn: e.tensor_tensor(bt["rk"][:, 0:bn], bt["t1"][:, 0:bn], bt["r"][:, 0:bn], op=ALU.mult), reads=["b_t1", "b_r"], writes=["b_rk"])
                        first_rk = False
                    else:
                        K.op("pool", lambda e, bn=bn: e.tensor_tensor(bt["t2"][:, 0:bn], bt["t1"][:, 0:bn], bt["r"][:, 0:bn], op=ALU.mult), reads=["b_t1", "b_r"], writes=["b_t2"])
                        K.op("pool", lambda e, bn=bn: e.tensor_tensor(bt["rk"][:, 0:bn], bt["rk"][:, 0:bn], bt["t2"][:, 0:bn], op=ALU.add), reads=["b_rk", "b_t2"], writes=["b_rk"])
                    K.op("dve", lambda e, bn=bn: e.scalar_tensor_tensor(bt["t2"][:, 0:bn], bt["kap"][:, 0:bn], -1.0, bt["a"][:, 0:bn], op0=ALU.mult, op1=ALU.mult), reads=["b_kap", "b_a"], writes=["b_t2"])
                    K.op("dve", lambda e, d_=d_, bn=bn: e.tensor_tensor(nbT[d_][:, bs], bt["t2"][:, 0:bn], bt["iw"][:, 0:bn], op=ALU.mult), reads=["b_t2", "b_iw"], writes=["nbT%d" % d_])
                rkc = pk2[:, PK2["RK"] + P:PK2["RK"] + P + 1]
                K.op("dve", lambda e, bn=bn, rkc=rkc: e.tensor_scalar(bt["rk"][:, 0:bn], bt["rk"][:, 0:bn], rkc, None, op0=ALU.mult), reads=["b_rk", "pk2"], writes=["b_rk"])
                for n in range((bn + 511) // 512):
                    t0 = n * 512; tn = min(512, bn - t0)
                    K.op("pe", lambda e, t0=t0, tn=tn: e.matmul(ps[4][:, 0:tn], lhsT=bones[:], rhs=bt["rk"][:, t0:t0 + tn], start=True, stop=True), reads=["bones", "b_rk"], writes=["ps4"])
                    K.op("dve", lambda e, t0=t0, tn=tn: e.tensor_tensor(bonus[:, b0 + t0:b0 + t0 + tn], ps[4][:, 0:tn], bt["v"][:, t0:t0 + tn], op=ALU.mult), reads=["ps4", "b_v"], writes=["bonus"])
            stop_at("rw_prep")
            for d_ in range(2):
                K.op("pool", lambda e, d_=d_: e.memset(Z[d_][:], 0.0), writes=["Z%d" % d_])
                K.op("pool", lambda e, d_=d_: e.memset(Zb[d_][:], 0.0), writes=["Zb%d" % d_])
            for step in range(NT):
                for d_ in K.streams(2):
                    i = fwd_order[step] if d_ == 0 else bwd_order[step]
                    sl = slice(i * 128, (i + 1) * 128)
                    want_o = i >= 2
                    dk_ = "%d" % d_
                    tag = "n%d" % d_
                    b_ = d_ * 4
                    for hh in range(2):
                        hs = slice(hh * 64, (hh + 1) * 64)
                        pt = ps[b_ + hh]; pk = "ps%d" % (b_ + hh)
                        for qq, src in enumerate((ktT, nbT)):
                            K.op("pe", lambda e, src=src, pt=pt, qq=qq, hs=hs, d_=d_, sl=sl: e.matmul(pt[:, qq * 256:qq * 256 + 128], lhsT=src[d_][hs, sl], rhs=kpT[d_][hs, sl], start=True, stop=True),
                                 reads=["ktT" + dk_, "nbT" + dk_, "kpT" + dk_], writes=[pk])
                            K.op("pe", lambda e, src=src, pt=pt, qq=qq, hs=hs, d_=d_, sl=sl: e.matmul(pt[:, qq * 256 + 128:qq * 256 + 256], lhsT=src[d_][hs, sl], rhs=rT[d_][hs, sl], start=True, stop=True),
                                 reads=["ktT" + dk_, "nbT" + dk_, "rT" + dk_], writes=[pk])
                    for hh in range(2):
                        K.op("dve", lambda e, d_=d_, hh=hh: e.tensor_tensor(AK[d_][:, hh * 256:(hh + 1) * 256], ps[d_ * 4 + hh][:, 0:256], mask4[d_][:, 0:256], op=ALU.mult),
                             reads=["ps%d" % (b_ + hh), "mask4"], writes=["AK" + dk_])
                        K.op("dve", lambda e, d_=d_, hh=hh: e.tensor_tensor(ANB[d_][:, hh * 256:(hh + 1) * 256], ps[d_ * 4 + hh][:, 256:512], mask4[d_][:, 0:256], op=ALU.mult),
                             reads=["ps%d" % (b_ + hh), "mask4"], writes=["ANB" + dk_])
                    pT2 = psb[b_ + 2]; kT2 = "ps%d" % (b_ + 2)
                    K.op("pe", lambda e, d_=d_, sl=sl, pT2=pT2: e.transpose(pT2[:, 0:128], ktT[d_][:, sl], identb[:]), reads=["ktT" + dk_, "identb"], writes=[kT2])
                    K.op("pe", lambda e, d_=d_, sl=sl, pT2=pT2: e.transpose(pT2[:, 128:256], nbT[d_][:, sl], identb[:]), reads=["nbT" + dk_, "identb"], writes=[kT2])
                    K.op("act", lambda e, d_=d_, pT2=pT2: e.copy(ktok_[d_][:], pT2[:, 0:128]), reads=[kT2], writes=["rktok" + dk_])
                    K.op("act", lambda e, d_=d_, pT2=pT2: e.copy(nbtok_[d_][:], pT2[:, 128:256]), reads=[kT2], writes=["rnbtok" + dk_])
                    for hh in range(2):
                        K.op("pool", lambda e, d_=d_, hh=hh: e.tensor_copy(YY[d_][hh][:], ANB[d_][:, hh * 256:hh * 256 + 128]), reads=["ANB" + dk_], writes=[tag + "Y"])
                        pB = (psb if NMODE == "bf16" else ps)[b_ + 2]
                        K.op("pe", lambda e, d_=d_, hh=hh, pB=pB: e.transpose(pB[:, 0:128], YY[d_][hh][:], identn[:]), reads=[tag + "Y", "identb", "ident"], writes=[kT2])
                        K.op("act", lambda e, d_=d_, hh=hh, pB=pB: e.copy(YYt[d_][hh][:], pB[:, 0:128]), reads=[kT2], writes=[tag + "Yt"])
                        Ai, kAi = neumann(YY[d_][hh], YYt[d_][hh], tag, b_ + 1)
                        K.op("dve", lambda e, d_=d_, hh=hh, Ai=Ai: e.tensor_copy(AinvS[d_][hh][:], Ai), reads=[kAi], writes=["Ainv%d%d" % (d_, hh)])
                    pC = ps[b_ + 3]; kC = "ps%d" % (b_ + 3)
                    for hh in range(2):
                        K.op("pe", lambda e, d_=d_, hh=hh, i=i, pC=pC: e.matmul(pC[:, hh * 64:(hh + 1) * 64], lhsT=AK[d_][:, hh * 256:hh * 256 + 128], rhs=vtk[:, i, hh * 64:(hh + 1) * 64], start=(hh == 0), stop=False),
                             reads=["AK" + dk_, "rvtok"], writes=[kC])
                    K.op("pe", lambda e, d_=d_, sl=sl, pC=pC: e.matmul(pC[:, 0:128], lhsT=kpT[d_][:, sl], rhs=Zb[d_][:], start=False, stop=True), reads=["kpT" + dk_, "Zb" + dk_], writes=[kC])
                    K.op("act", lambda e, d_=d_, pC=pC: e.copy(P1b[d_][:], pC[:, 0:128]), reads=[kC], writes=["P1b" + dk_])
                    for hh in range(2):
                        K.op("pe", lambda e, d_=d_, hh=hh, pC=pC: e.matmul(pC[:, 128 + hh * 64:128 + (hh + 1) * 64], lhsT=AinvS[d_][hh][:], rhs=P1b[d_][:, hh * 64:(hh + 1) * 64], start=True, stop=True),
                             reads=["Ainv%d%d" % (d_, hh), "P1b" + dk_], writes=[kC])
                    K.op("dve", lambda e, d_=d_, pC=pC: e.tensor_copy(Ub[d_][:], pC[:, 128:256]), reads=[kC], writes=["Ub" + dk_])
                    if want_o:
                        for hh in range(2):
                            K.op("pe", lambda e, d_=d_, hh=hh, i=i, pC=pC: e.matmul(pC[:, 256 + hh * 64:256 + (hh + 1) * 64], lhsT=AK[d_][:, hh * 256 + 128:hh * 256 + 256], rhs=vtk[:, i, hh * 64:(hh + 1) * 64], start=(hh == 0), stop=False),
                                 reads=["AK" + dk_, "rvtok"], writes=[kC])
                        K.op("pe", lambda e, d_=d_, sl=sl, pC=pC: e.matmul(pC[:, 256:384], lhsT=rT[d_][:, sl], rhs=Zb[d_][:], start=False, stop=False), reads=["rT" + dk_, "Zb" + dk_], writes=[kC])
                        for hh in range(2):
                            K.op("pe", lambda e, d_=d_, hh=hh, pC=pC: e.matmul(pC[:, 256 + hh * 64:256 + (hh + 1) * 64], lhsT=ANB[d_][:, hh * 256 + 128:hh * 256 + 256], rhs=Ub[d_][:, hh * 64:(hh + 1) * 64], start=False, stop=(hh == 1)),
                                 reads=["ANB" + dk_, "Ub" + dk_], writes=[kC])
                        K.op("act", lambda e, d_=d_, i=i, pC=pC: e.copy(ybuf[d_][:, i, :], pC[:, 256:384]), reads=[kC], writes=["ybuf" + dk_])
                    pD = ps[b_ + 2]; kD = "ps%d" % (b_ + 2)
                    K.op("pe", lambda e, d_=d_, i=i, pD=pD: e.matmul(pD[:, 256:384], lhsT=ktok_[d_][:], rhs=vtk[:, i, :], start=True, stop=False), reads=["rktok" + dk_, "rvtok"], writes=[kD])
                    K.op("pe", lambda e, d_=d_, pD=pD: e.matmul(pD[:, 256:384], lhsT=nbtok_[d_][:], rhs=Ub[d_][:], start=False, stop=True), reads=["rnbtok" + dk_, "Ub" + dk_], writes=[kD])
                    K.op("dve", lambda e, d_=d_, pD=pD: e.tensor_tensor(zt[d_][:], pD[:, 256:384], Z[d_][:], op=ALU.add), reads=[kD, "Z" + dk_], writes=["zt" + dk_])
                    K.op("dve", lambda e, d_=d_, i=i: e.scalar_tensor_tensor(Z[d_][:], zt[d_][:], Lam[d_][:, i:i + 1], bones[:], op0=ALU.mult, op1=ALU.mult),
                         reads=["zt" + dk_, "Lam" + dk_, "bones"], writes=["Z" + dk_])
                    K.op("act", lambda e, d_=d_: e.copy(Zb[d_][:], Z[d_][:]), reads=["Z" + dk_], writes=["Zb" + dk_])
            stop_at("rw_scan")
            gwc = pk2[:, PK2["GNW"] + P:PK2["GNW"] + P + 1]
            gbc = pk2[:, PK2["GNB"] + P:PK2["GNB"] + P + 1]
            for i in range(2, NT):
                K.op("dve", lambda e, i=i: e.tensor_tensor(yo[:], ybuf[0][:, i, :], ybuf[1][:, i, :], op=ALU.add), reads=["ybuf0", "ybuf1"], writes=["yo"])
                y3 = yo[:].rearrange("p (h c) -> p h c", c=64)
                K.op("dve", lambda e, y3=y3: e.reduce_sum(yst[:, 0:2], y3, axis=AX.X), reads=["yo"], writes=["yst0"])
                K.op("dve", lambda e: e.tensor_scalar(yst[:, 2:4], yst[:, 0:2], 1.0 / 64, None, op0=ALU.mult), reads=["yst0"], writes=["yst1"])
                K.op("dve", lambda e, y3=y3: e.tensor_tensor(yc[:].rearrange("p (h c) -> p h c", c=64), y3, yst[:, 2:4].unsqueeze(2).to_broadcast([128, 2, 64]), op=ALU.subtract),
                     reads=["yo", "yst1"], writes=["yc"])
                K.op("act", lambda e: e.activation(ysq[:], yc[:], AF.Square), reads=["yc"], writes=["ysq"])
                K.op("dve", lambda e: e.reduce_sum(yst[:, 4:6], ysq[:].rearrange("p (h c) -> p h c", c=64), axis=AX.X), reads=["ysq"], writes=["yst2"])
                K.op("act", lambda e: e.activation(yst[:, 6:8], yst[:, 4:6], AF.Sqrt, bias=eps_t[:, 2:3], scale=1.0 / 64), reads=["yst2", "eps"], writes=["yst3"])
                K.op("dve", lambda e: e.reciprocal(yst[:, 6:8], yst[:, 6:8]), reads=["yst3"], writes=["yst3"])
                K.op("dve", lambda e: e.tensor_tensor(yt2[:].rearrange("p (h c) -> p h c", c=64), yc[:].rearrange("p (h c) -> p h c", c=64),
                                                      yst[:, 6:8].unsqueeze(2).to_broadcast([128, 2, 64]), op=ALU.mult), reads=["yc", "yst3"], writes=["yt2"])
                pt = ps[i % 2]; pk = "ps%d" % (i % 2)
                K.op("pe", lambda e, pt=pt: e.transpose(pt[:, 0:128], yt2[:], ident[:]), reads=["yt2", "ident"], writes=[pk])
                K.op("act", lambda e, pt=pt: e.activation(yc[:], pt[:, 0:128], AF.Identity, bias=gbc, scale=gwc), reads=[pk, "pk2", "yc"], writes=["yc"])
                K.op("dve", lambda e, i=i: e.tensor_tensor(yc[:], yc[:], bonus[:, i * 128:(i + 1) * 128], op=ALU.add), reads=["yc", "bonus"], writes=["yc"])
                K.op("dve", lambda e, i=i: e.tensor_tensor(rmst[:, (i - 2) * 128:(i - 1) * 128], yc[:], gateT[:, i * 128:(i + 1) * 128], op=ALU.mult), reads=["yc", "gateT"], writes=["rmst"])
            K.dma(mT_d[4 + P, :, :], rmst[:, :], reads=["rmst"], writes=[("mT", 4 + P)], key="st_rmst")
    K.barrier()

    stop_at("mix_done")
    with ExitStack() as pp_:
        mTs = [sb("mTs%d" % i_, [128, 8, 128], BF16, stack=pp_) for i_ in range(2)]
        woutb = sb("woutb", [128, 8, D], BF16, stack=pp_)
        wqb = sb("wqb", [128, 8, 2048], BF16, stack=pp_)
        skT = sb("skT", [128, 16, 128], BF16, stack=pp_)
        with ExitStack() as pset:
            wstg = sb("wstg", [128, 8, 512], stack=pset)
            skst = sb("skst", [128, 16, 128], stack=pset)
            for hf in range(2):
                K.dma(wstg[:, :, :], wout_d[:, hf * 512:(hf + 1) * 512].rearrange("(k p) c -> p k c", p=128), writes=["wstg"], key="wstg")
                K.op("pool", lambda e, hf=hf: e.tensor_copy(woutb[:, :, hf * 512:(hf + 1) * 512], wstg[:]), reads=["wstg"], writes=["woutb"])
            for hf in range(4):
                K.dma(wstg[:, :, :], wq_d[:, hf * 512:(hf + 1) * 512].rearrange("(k p) c -> p k c", p=128), writes=["wstg"], key="wstg")
                K.op("pool", lambda e, hf=hf: e.tensor_copy(wqb[:, :, hf * 512:(hf + 1) * 512], wstg[:]), reads=["wstg"], writes=["wqb"])
            K.dma(skst[:, :, :], sk_d[:, :, :].rearrange("g k d -> k g d"), writes=["skst"], key="skst")
            for g in range(16):
                pt = ps[g % 2]; pk = "ps%d" % (g % 2)
                K.op("pe", lambda e, g=g, pt=pt: e.transpose(pt[:, 0:128], skst[:, g, :], ident[:]), reads=["skst", "ident"], writes=[pk])
                K.op("act", lambda e, g=g, pt=pt: e.copy(skT[:, g, :], pt[:, 0:128]), reads=[pk], writes=["skT"])
            K.barrier()
        gtB = sb("gtB2", [128, 4, D], stack=pp_)
        K.dma(gtB[:].rearrange("p q d -> p (q d)"), gt_d[:, :], reads=["gt_d"], writes=["gtB"], key="gtB2")
        comb_d = nc.dram_tensor("comb_s", [16384, 2 * D], BF16).ap()
        with ExitStack() as pcv:
            cst = [sb("cst%d" % i_, [128, 2, D], stack=pcv) for i_ in range(2)]
            cbf = [sb("cbf%d" % i_, [128, 2 * D], BF16, stack=pcv) for i_ in range(2)]
            for c_ in range(128):
                st_ = cst[c_ % 2]; bf_ = cbf[c_ % 2]
                sk_ = "cst%d" % (c_ % 2); bk_ = "cbf%d" % (c_ % 2)
                K.dma(st_[:, 0, :], down_d[c_ * 128:(c_ + 1) * 128, :], writes=[sk_], key=sk_)
                K.dma(st_[:, 1, :], up_d[c_ * 128:(c_ + 1) * 128, :], writes=[sk_], key=sk_)
                K.op("act", lambda e, st_=st_, bf_=bf_: e.copy(bf_[:, 0:D], st_[:, 0, :]), reads=[sk_], writes=[bk_])
                K.op("dve", lambda e, st_=st_, bf_=bf_: e.tensor_copy(bf_[:, D:2 * D], st_[:, 1, :]), reads=[sk_], writes=[bk_])
                K.dma(comb_d[c_ * 128:(c_ + 1) * 128, :], bf_[:, :], reads=[bk_], writes=["comb"], key="st_" + bk_)
            K.barrier()
        A2row = sb("A2row", [128, D], stack=pp_)
        fgB = sb("fgB", [128, D], stack=pp_)
        K.dma(A2row[:, :], g2row_d.partition_broadcast(128), writes=["A2row"], key="A2row")
        K.dma(fgB[:, :], fng_d.partition_broadcast(128), writes=["fgB"], key="fgB")
        K.op("dve", lambda e: e.scalar_tensor_tensor(A2row[:], gtB[:, 2, :], 1.0, A2row[:], op0=ALU.add, op1=ALU.mult), reads=["gtB", "A2row"], writes=["A2row"])
        iota16i = sb("iota16i", [128, 16], I32, stack=pp_)
        iota16 = sb("iota16", [128, 16], stack=pp_)
        K.op("pool", lambda e: e.iota(iota16i[:], pattern=[[1, 16]], base=0, channel_multiplier=0), writes=["iota16i"])
        K.op("dve", lambda e: e.tensor_copy(iota16[:], iota16i[:]), reads=["iota16i"], writes=["iota16"])

        xt_ = sb("p_xt", [128, D], stack=pp_)
        x1 = sb("p_x1", [128, D], stack=pp_)
        h2 = sb("p_h2", [128, D], stack=pp_)
        yacc = sb("p_y", [128, D], stack=pp_)
        pj = sb("p_junk", [128, D], stack=pp_)
        pss = sb("p_ss", [128, 1], stack=pp_)
        prs = sb("p_rs", [128, 2], stack=pp_)
        pxs = sb("p_xs", [128, D], stack=pp_)
        h2T = sb("p_h2T", [128, 8, 128], BF16, stack=pp_)
        qTs = sb("p_qT", [128, 16, 128], BF16, stack=pp_)
        scs = sb("p_sc", [128, 16, 128], stack=pp_)
        tmp1 = sb("p_tmp1", [128, 16, 128], stack=pp_)
        tv = sb("p_tv", [128, 16, 16], stack=pp_)
        tiu = sb("p_tiu", [128, 16, 16], U32, stack=pp_)
        tif = sb("p_tif", [128, 16, 16], stack=pp_)
        cand = sb("p_cand", [128, 8, 256], stack=pp_)
        tmp2 = sb("p_tmp2", [128, 8, 256], stack=pp_)
        eq = tmp2
        bsv = sb("p_bs", [128, 8, 16], stack=pp_)
        posu = sb("p_posu", [128, 8, 16], U32, stack=pp_)
        pau = sb("p_pau", [128, 8, 16], U32, stack=pp_)
        pbu = sb("p_pbu", [128, 8, 16], U32, stack=pp_)
        paf = sb("p_paf", [128, 8, 16], stack=pp_)
        pbf = sb("p_pbf", [128, 8, 16], stack=pp_)
        i0f = sb("p_i0f", [128, 8, 16], stack=pp_)
        i1f = sb("p_i1f", [128, 8, 16], stack=pp_)
        eidx = sb("p_eidx", [128, 128], U32, stack=pp_)
        gat = sb("p_gate", [128, 8, 16], stack=pp_)
        gsum = sb("p_gsum", [128, 8], stack=pp_)
        apre = sb("p_apre", [128, 128], stack=pp_)
        coef = sb("p_coef", [128, 128], stack=pp_)
        NRB = 8
        rowc = [sb("p_rowc%d" % i, [128, 2 * D], BF16, stack=pp_) for i in range(NRB)]
        pjb = sb("p_junkb", [128, D], BF16, stack=pp_)
        h2b = sb("p_h2b", [128, D], BF16, stack=pp_)
        dgs = [sb("p_dg%d" % i, [128, 128], BF16, stack=pp_) for i in range(4)]
        gflat = sb("p_gflat", [128, 128], stack=pp_)
        NEG = -1.0e30

        for i in range(NTL):
            tsl = slice(i * 128, (i + 1) * 128)
            K.dma(xt_[:, :], x_d[tsl, :], writes=["p_xt"], key="p_xt")
            mTt = mTs[i % 2]; mk_ = "mTs%d" % (i % 2)
            K.dma(mTt[:, :, :], mT_d[:, :, tsl].rearrange("k p t -> p k t"), reads=[("mT", j) for j in range(8)], writes=[mk_], key=mk_)
            for hf in range(2):
                pt = ps[hf]; pk = "ps%d" % hf
                for k in range(8):
                    K.op("pe", lambda e, k=k, hf=hf, pt=pt, mTt=mTt: e.matmul(pt[:, :], lhsT=mTt[:, k, :], rhs=woutb[:, k, hf * 512:(hf + 1) * 512], start=(k == 0), stop=(k == 7)),
                         reads=[mk_, "woutb"], writes=[pk])
                K.op("dve", lambda e, hf=hf, pt=pt: e.tensor_tensor(x1[:, hf * 512:(hf + 1) * 512], pt[:, :], gtB[:, 0, hf * 512:(hf + 1) * 512], op=ALU.mult),
                     reads=[pk, "gtB"], writes=["p_x1"])
            K.op("pool", lambda e: e.tensor_tensor(x1[:], x1[:], xt_[:], op=ALU.add), reads=["p_x1", "p_xt"], writes=["p_x1"])
            K.op("act", lambda e: e.activation(pj[:], x1[:], AF.Square), reads=["p_x1"], writes=["p_junk"])
            K.op("dve", lambda e: e.reduce_sum(pss[:, 0:1], pj[:], axis=AX.X), reads=["p_junk"], writes=["p_ss"])
            K.op("act", lambda e: e.activation(prs[:, 0:1], pss[:, 0:1], AF.Sqrt, bias=eps_t[:, 0:1], scale=1.0 / D), reads=["p_ss", "eps"], writes=["p_rs"])
            K.op("dve", lambda e: e.reciprocal(prs[:, 1:2], prs[:, 0:1]), reads=["p_rs"], writes=["p_rs2"])
            K.op("dve", lambda e: e.tensor_scalar(pxs[:], x1[:], prs[:, 1:2], None, op0=ALU.mult), reads=["p_x1", "p_rs2"], writes=["p_xs"])
            for hf in range(2):
                pt = ps[2 + hf]; pk = "ps%d" % (2 + hf)
                for kk in range(4):
                    k = hf * 4 + kk
                    K.op("pe", lambda e, k=k, kk=kk, pt=pt: e.transpose(pt[:, kk * 128:(kk + 1) * 128], pxs[:, k * 128:(k + 1) * 128], ident[:]), reads=["p_xs", "ident"], writes=[pk])
                for kk in range(4):
                    k = hf * 4 + kk
                    K.op("act", lambda e, k=k, kk=kk, pt=pt: e.activation(h2T[:, k, :], pt[:, kk * 128:(kk + 1) * 128], AF.Identity, bias=B2[:, k:k + 1], scale=A2[:, k:k + 1]),
                         reads=[pk, "mods"], writes=["p_h2T"])
            K.op("dve", lambda e: e.tensor_tensor(h2[:], pxs[:], A2row[:], op=ALU.mult), reads=["p_xs", "A2row"], writes=["p_h2"])
            K.op("pool", lambda e: e.tensor_tensor(h2[:], h2[:], gtB[:, 1, :], op=ALU.add), reads=["p_h2", "gtB"], writes=["p_h2"])
            for g in range(16):
                pt = ps[4 + (g % 2)]; pk = "ps%d" % (4 + (g % 2))
                for k in range(8):
                    K.op("pe", lambda e, g=g, k=k, pt=pt: e.matmul(pt[:, 0:128], lhsT=wqb[:, k, g * 128:(g + 1) * 128], rhs=h2T[:, k, :], start=(k == 0), stop=(k == 7)),
                         reads=["wqb", "p_h2T"], writes=[pk])
                K.op("act", lambda e, g=g, pt=pt: e.copy(qTs[:, g, :], pt[:, 0:128]), reads=[pk], writes=[("p_qT", g)])
            for g in range(16):
                pt = ps[6 + (g // 4) % 2]; pk = "ps%d" % (6 + (g // 4) % 2)
                K.op("pe", lambda e, g=g, pt=pt: e.matmul(pt[:, (g % 4) * 128:(g % 4 + 1) * 128], lhsT=qTs[:, g, :], rhs=skT[:, g, :], start=True, stop=True),
                     reads=[("p_qT", g), "skT"], writes=[pk])
                if g % 4 == 3:
                    K.op("dve", lambda e, g=g, pt=pt: e.tensor_copy(scs[:, g - 3:g + 1, :].rearrange("p g k -> p (g k)"), pt[:, :]), reads=[pk], writes=[("p_sc", g // 4)])
            for g in range(16):
                K.op("dve", lambda e, g=g: e.max(tv[:, g, 0:8], scs[:, g, :]), reads=[("p_sc", g // 4)], writes=[("tv", g)])
            for g in range(16):
                K.op("dve", lambda e, g=g: e.max_index(tiu[:, g, 0:8], tv[:, g, 0:8], scs[:, g, :]), reads=[("p_sc", g // 4), ("tv", g)], writes=[("tiu", g)])
            for g in range(16):
                K.op("dve", lambda e, g=g: e.match_replace(tmp1[:, g, :], tv[:, g, 0:8], scs[:, g, :], NEG), reads=[("p_sc", g // 4), ("tv", g)], writes=[("tmp1", g)])
            for g in range(16):
                K.op("dve", lambda e, g=g: e.max(tv[:, g, 8:16], tmp1[:, g, :]), reads=[("tmp1", g)], writes=[("tv2", g)])
            for g in range(16):
                K.op("dve", lambda e, g=g: e.max_index(tiu[:, g, 8:16], tv[:, g, 8:16], tmp1[:, g, :]), reads=[("tmp1", g), ("tv2", g)], writes=[("tiu2", g)])
            allg = [("tv", g) for g in range(16)] + [("tv2", g) for g in range(16)]
            alli = [("tiu", g) for g in range(16)] + [("tiu2", g) for g in range(16)]
            K.op("dve", lambda e: e.tensor_copy(tif[:], tiu[:]), reads=alli, writes=["p_tif"])
            tvv = tv[:].rearrange("p (h q) a -> p h q a", q=2)
            tfv = tif[:].rearrange("p (h q) a -> p h q a", q=2)
            c4 = cand[:].rearrange("p h (a b) -> p h a b", b=16)
            K.op("dve", lambda e: e.tensor_tensor(c4, tvv[:, :, 0, :].unsqueeze(3).to_broadcast([128, 8, 16, 16]),
                                                  tvv[:, :, 1, :].unsqueeze(2).to_broadcast([128, 8, 16, 16]), op=ALU.add), reads=allg, writes=["p_cand"])
            for hh in range(8):
                K.op("dve", lambda e, hh=hh: e.max(bsv[:, hh, 0:8], cand[:, hh, :]), reads=["p_cand"], writes=[("bs", hh)])
            for hh in range(8):
                K.op("dve", lambda e, hh=hh: e.max_index(posu[:, hh, 0:8], bsv[:, hh, 0:8], cand[:, hh, :]), reads=["p_cand", ("bs", hh)], writes=[("pos", hh)])
            for hh in range(8):
                K.op("dve", lambda e, hh=hh: e.match_replace(tmp2[:, hh, :], bsv[:, hh, 0:8], cand[:, hh, :], NEG), reads=["p_cand", ("bs", hh), "p_eq"], writes=[("tmp2", hh)])
            for hh in range(8):
                K.op("dve", lambda e, hh=hh: e.max(bsv[:, hh, 8:16], tmp2[:, hh, :]), reads=[("tmp2", hh)], writes=[("bs2", hh)])
            for hh in range(8):
                K.op("dve", lambda e, hh=hh: e.max_index(posu[:, hh, 8:16], bsv[:, hh, 8:16], tmp2[:, hh, :]), reads=[("tmp2", hh), ("bs2", hh)], writes=[("pos2", hh)])
            allb = [("bs", hh) for hh in range(8)] + [("bs2", hh) for hh in range(8)]
            allp = [("pos", hh) for hh in range(8)] + [("pos2", hh) for hh in range(8)]
            K.op("dve", lambda e: e.tensor_single_scalar(pau[:], posu[:], 4, op=ALU.logical_shift_right), reads=allp, writes=["p_pau"])
            K.op("dve", lambda e: e.tensor_single_scalar(pbu[:], posu[:], 15, op=ALU.bitwise_and), reads=allp, writes=["p_pbu"])
            K.op("dve", lambda e: e.tensor_copy(paf[:], pau[:]), reads=["p_pau"], writes=["p_paf"])
            K.op("dve", lambda e: e.tensor_copy(pbf[:], pbu[:]), reads=["p_pbu"], writes=["p_pbf"])
            e4 = eq[:].rearrange("p h (k a) -> p h k a", a=16)
            io4 = iota16[:].unsqueeze(1).unsqueeze(1).to_broadcast([128, 8, 16, 16])
            for (pf, q_, dst, nm) in ((paf, 0, i0f, "i0f"), (pbf, 1, i1f, "i1f")):
                K.op("dve", lambda e, pf=pf: e.tensor_tensor(e4, pf[:].unsqueeze(3).to_broadcast([128, 8, 16, 16]), io4, op=ALU.is_equal),
                     reads=["p_paf", "p_pbf", "iota16"], writes=["p_eq"] + [("tmp2", hh_) for hh_ in range(8)])
                K.op("dve", lambda e, q_=q_: e.tensor_tensor(e4, e4, tfv[:, :, q_, :].unsqueeze(2).to_broadcast([128, 8, 16, 16]), op=ALU.mult),
                     reads=["p_eq", "p_tif"], writes=["p_eq"])
                K.op("dve", lambda e, dst=dst: e.reduce_sum(dst[:], e4, axis=AX.X), reads=["p_eq"], writes=["p_" + nm])
            K.op("dve", lambda e: e.scalar_tensor_tensor(i0f[:], i0f[:], 128.0, i1f[:], op0=ALU.mult, op1=ALU.add), reads=["p_i0f", "p_i1f"], writes=["p_i0f"])
            K.op("dve", lambda e: e.tensor_copy(eidx[:], i0f[:].rearrange("p h k -> p (h k)")), reads=["p_i0f"], writes=["p_eidx"])
            K.op("dve", lambda e: e.tensor_tensor(gat[:], bsv[:], bsv[:, :, 0:1].to_broadcast([128, 8, 16]), op=ALU.subtract), reads=allb, writes=["p_gate"])
            K.op("act", lambda e: e.activation(gat[:], gat[:], AF.Exp), reads=["p_gate"], writes=["p_gate"])
            K.op("dve", lambda e: e.reduce_sum(gsum[:], gat[:], axis=AX.X), reads=["p_gate"], writes=["p_gsum"])
            K.op("dve", lambda e: e.reciprocal(gsum[:], gsum[:]), reads=["p_gsum"], writes=["p_gsum"])
            K.op("dve", lambda e: e.tensor_tensor(gat[:], gat[:], gsum[:].unsqueeze(2).to_broadcast([128, 8, 16]), op=ALU.mult), reads=["p_gate", "p_gsum"], writes=["p_gate"])
            K.op("act", lambda e: e.copy(h2b[:], h2[:]), reads=["p_h2"], writes=["p_h2b"])
            K.op("dve", lambda e: e.tensor_copy(gflat[:], gat[:].rearrange("p h k -> p (h k)")), reads=["p_gate"], writes=["p_gflat"])
            GRP = 4
            for g0 in range(0, 128, GRP):
                for kslot in range(g0, g0 + GRP):
                    rb = rowc[kslot % NRB]; rk_ = "p_rowc%d" % (kslot % NRB)
                    K.gather(rb[:, :], comb_d[:, :], eidx[:, kslot:kslot + 1], reads=["p_eidx", "comb"], writes=[rk_], key=rk_)
                    K.op("dve", lambda e, rb=rb, kslot=kslot: e.scalar_tensor_tensor(pjb[:], rb[:, 0:D], 1.0, h2b[:], op0=ALU.mult, op1=ALU.mult, accum_out=apre[:, kslot:kslot + 1]),
                         reads=[rk_, "p_h2b"], writes=["p_junkb", ("apre", g0 // GRP)])
                K.op("dve", lambda e, g0=g0: e.tensor_copy(coef[:, g0:g0 + GRP], apre[:, g0:g0 + GRP]), reads=[("apre", g0 // GRP)], writes=[("cf0", g0 // GRP)])
                K.op("act", lambda e, g0=g0: e.activation(coef[:, g0:g0 + GRP], coef[:, g0:g0 + GRP], AF.Gelu), reads=[("cf0", g0 // GRP)], writes=[("cf1", g0 // GRP)])
                K.op("dve", lambda e, g0=g0: e.tensor_tensor(coef[:, g0:g0 + GRP], coef[:, g0:g0 + GRP], gflat[:, g0:g0 + GRP], op=ALU.mult),
                     reads=[("cf1", g0 // GRP), "p_gflat"], writes=[("cf2", g0 // GRP)])
                for kslot in range(g0, g0 + GRP):
                    rb = rowc[kslot % NRB]; rk_ = "p_rowc%d" % (kslot % NRB)
                    dg = dgs[kslot % 4]; dk__ = "p_dg%d" % (kslot % 4)
                    K.op("act", lambda e, dg=dg, kslot=kslot: e.activation(dg[:], identb[:], AF.Identity, scale=coef[:, kslot:kslot + 1]),
                         reads=["identb", ("cf2", g0 // GRP)], writes=[dk__])
                    for hf in range(2):
                        K.op("pe", lambda e, dg=dg, rb=rb, hf=hf, kslot=kslot: e.matmul(ps[hf][:, :], lhsT=dg[:], rhs=rb[:, D + hf * 512:D + (hf + 1) * 512],
                                                                                       start=(kslot == 0), stop=(kslot == 127)),
                             reads=[dk__, rk_], writes=["ps%d" % hf])
            for hf in range(2):
                K.op("act", lambda e, hf=hf: e.copy(yacc[:, hf * 512:(hf + 1) * 512], ps[hf][:, :]), reads=["ps%d" % hf], writes=["p_y"])
            K.op("dve", lambda e: e.tensor_tensor(yacc[:], yacc[:], gtB[:, 3, :], op=ALU.mult), reads=["p_y", "gtB"], writes=["p_y"])
            K.op("pool", lambda e: e.tensor_tensor(yacc[:], yacc[:], x1[:], op=ALU.add), reads=["p_y", "p_x1"], writes=["p_y"])
            K.op("act", lambda e: e.activation(pj[:], yacc[:], AF.Square), reads=["p_y"], writes=["p_junk"])
            K.op("dve", lambda e: e.reduce_sum(pss[:, 0:1], pj[:], axis=AX.X), reads=["p_junk"], writes=["p_ss"])
            K.op("act", lambda e: e.activation(prs[:, 0:1], pss[:, 0:1], AF.Sqrt, bias=eps_t[:, 0:1], scale=1.0 / D), reads=["p_ss", "eps"], writes=["p_rs"])
            K.op("dve", lambda e: e.reciprocal(prs[:, 1:2], prs[:, 0:1]), reads=["p_rs"], writes=["p_rs2"])
            K.op("dve", lambda e: e.scalar_tensor_tensor(pxs[:], yacc[:], prs[:, 1:2], fgB[:], op0=ALU.mult, op1=ALU.mult), reads=["p_y", "p_rs2", "fgB"], writes=["p_xs"])
            K.dma(out_d[tsl, :], pxs[:, :], reads=["p_xs"], writes=["outdone"], key="st_out")
            if i == 0:
                stop_at("peer_t0")
    K.barrier()
    if "mT" in dbg:
        d_o = dbgt("mT", [8, 128, TL], BF16)
        K.dma(d_o[:, :, :], mT_d[:, :, :], reads=[("mT", j) for j in range(8)], writes=["dbgmT"], key="dbg")

    K.finish([k for k in K.st.keys() if (isinstance(k, str) and k.startswith("dbg")) or k == "outdone"])
    return dbg_out


def _inputs_for_core(inp, b, n_rows):
    TL = 64 * n_rows
    f = lambda a: np.ascontiguousarray(np.asarray(a, dtype=np.float32))
    m = {
        "x": f(inp["x"][b, :TL]),
        "c": f(inp["c"][b:b + 1]),
        "ctx": f(inp["ctx"][b]),
        "c_ctx": f(inp["c_ctx"][None, :]),
        "ada_w": f(inp["ada_w"][0]),
        "ada_b": f(inp["ada_b"][0].reshape(48, 128)),
        "ada_b_row": f(inp["ada_b"][0].reshape(1, 6144)),
        "norm1_g": f(inp["norm1_g"][0].reshape(8, 128)),
        "w_in": f(inp["w_in"][0]),
        "gdn_conv_w": f(inp["gdn_conv_w"][0].reshape(60, 128)),
        "gdn_a_log": f(inp["gdn_a_log"][0].reshape(1, 8)),
        "gdn_dt_bias": f(inp["gdn_dt_bias"][0].reshape(1, 8)),
        "gdn_norm_w": f(inp["gdn_norm_w"][0].reshape(1, 128)),
        "rwkv_mu": f(inp["rwkv_mu"][0].reshape(15, 128)),
        "rwkv_w0": f(inp["rwkv_w0"][0].reshape(8, 128)),
        "rwkv_w2": f(inp["rwkv_w2"][0].reshape(128, 512)),
        "rwkv_a0": f(inp["rwkv_a0"][0].reshape(8, 128)),
        "rwkv_a2": f(inp["rwkv_a2"][0].reshape(128, 512)),
        "rwkv_g2": f(inp["rwkv_g2"][0]),
        "rwkv_k_k": f(inp["rwkv_k_k"][0].reshape(4, 128)),
        "rwkv_k_a": f(inp["rwkv_k_a"][0].reshape(4, 128)),
        "rwkv_r_k": f(inp["rwkv_r_k"][0].reshape(4, 128)),
        "rwkv_gn_w": f(inp["rwkv_gn_w"][0].reshape(4, 128)),
        "rwkv_gn_b": f(inp["rwkv_gn_b"][0].reshape(4, 128)),
        "w_out": f(inp["w_out"][0]),
        "norm2_g": f(inp["norm2_g"][0].reshape(8, 128)),
        "peer_w_query": f(inp["peer_w_query"][0]),
        "peer_sub_keys": f(inp["peer_sub_keys"][0].reshape(16, 128, 128)),
        "peer_down": f(inp["peer_down"][0]),
        "peer_up": f(inp["peer_up"][0]),
        "final_norm_g": f(inp["final_norm_g"][None, :]),
        "norm2_g_row": f(inp["norm2_g"][0].reshape(1, 1024)),
    }
    return m


def run(inp, n_rows=64, cores=None, dbg=(), stop=None):
    nb = inp["x"].shape[0]
    cores = list(range(nb)) if cores is None else cores
    nc = bass.Bass("TRN2", target_bir_lowering=False)
    build(nc, n_rows=n_rows, dbg=dbg, stop=stop)
    in_maps = [_inputs_for_core(inp, b, n_rows) for b in cores]
    res = run_bass_kernel_spmd(nc, in_maps, core_ids=list(range(len(cores))))
    return res.results


def kernel(**inputs):
    res = run(inputs, n_rows=64)
    return np.stack([np.asarray(r["out"], dtype=np.float32) for r in res], axis=0)
```

```python
from contextlib import ExitStack
import numpy as np
import concourse.bass as bass
import concourse.mybir as mybir
from concourse.bass_utils import run_bass_kernel_spmd

F32 = mybir.dt.float32
BF16 = mybir.dt.bfloat16
I32 = mybir.dt.int32
U32 = mybir.dt.uint32
AF = mybir.ActivationFunctionType
ALU = mybir.AluOpType
AX = mybir.AxisListType

D = 1024
TC = 256
IN_COLS = 3984
GDN_COLS = 2064
NORM_EPS = 1e-6
L2_EPS = 1e-6
GN_EPS = 64e-5


class Ctx:
    def __init__(self, nc):
        self.nc = nc
        self.es = ExitStack()
        self.eng = dict(pe=nc.tensor, act=nc.scalar, dve=nc.vector, pool=nc.gpsimd, sp=nc.sync)
        self.csem = {}
        self.cnt = {}
        for e in ("pe", "act", "dve", "pool"):
            self.csem[e] = self.es.enter_context(nc.semaphore("cs_" + e))
            self.cnt[e] = 0
        self.dsem = {}
        self.seen = {e: {} for e in self.eng}
        self.st = {}
        self.ninst = 0
        self._rec = None

    def _sem(self, sk):
        if sk[0] == "c":
            return self.csem[sk[1]]
        return self.dsem[sk[1]][0]

    def _deps(self, reads, writes, e=None):
        need = {}

        def add(m):
            if m is None:
                return
            sk, v = m
            if need.get(sk, 0) < v:
                need[sk] = v

        for r in reads:
            s = self.st.get(r)
            if s is not None:
                add(s[0])
                if isinstance(r, str) and r.startswith("ps") and r[2:].isdigit():
                    for sk, v in s[1].items():
                        if sk != ("c", e):
                            add((sk, v))
        for w in writes:
            s = self.st.get(w)
            if s is not None:
                add(s[0])
                for sk, v in s[1].items():
                    add((sk, v))
        return need

    def _wait(self, e, need):
        eng = self.eng[e]
        seen = self.seen[e]
        for sk, v in need.items():
            if e == "pe" and sk == ("c", "pe"):
                continue
            if sk[0] == "d":
                v = max(v, self.dsem[sk[1]][1])
            if seen.get(sk, 0) >= v:
                continue
            eng.wait_ge(self._sem(sk), v)
            seen[sk] = v

    def _mark(self, mark, reads, writes):
        for w in writes:
            self.st[w] = [mark, {}]
        for r in reads:
            s = self.st.get(r)
            if s is None:
                s = self.st[r] = [None, {}]
            sk, v = mark
            if s[1].get(sk, 0) < v:
                s[1][sk] = v

    def streams(self, n):
        lists = []
        for d in range(n):
            self._rec = []
            yield d
            lists.append(self._rec)
            self._rec = None
        idx = [0] * n
        left = sum(len(l) for l in lists)
        while left:
            for d in range(n):
                if idx[d] < len(lists[d]):
                    kind, args, kw = lists[d][idx[d]]
                    idx[d] += 1
                    left -= 1
                    getattr(self, kind)(*args, **kw)

    def op(self, e, fn, reads=(), writes=()):
        if self._rec is not None:
            self._rec.append(("op", (e, fn, tuple(reads), tuple(writes)), {}))
            return
        need = self._deps(reads, writes, e)
        self._wait(e, need)
        ins = fn(self.eng[e])
        ins.then_inc(self.csem[e], 1)
        self.cnt[e] += 1
        self.ninst += 1
        self._mark((("c", e), self.cnt[e]), reads, writes)

    def dma(self, out, in_, reads=(), writes=(), key=None, q="sp", **kw):
        if self._rec is not None:
            self._rec.append(("dma", (out, in_, tuple(reads), tuple(writes), key, q), kw))
            return
        if key not in self.dsem:
            self.dsem[key] = [self.es.enter_context(self.nc.semaphore("ds_%d" % len(self.dsem))), 0]
        need = self._deps(reads, writes)
        self._wait(q, need)
        d = self.dsem[key]
        self.eng[q].dma_start(out=out, in_=in_, **kw).then_inc(d[0], 16)
        d[1] += 16
        self.ninst += 1
        self._mark((("d", key), d[1]), reads, writes)

    def gather(self, out, in_, idx_ap, reads=(), writes=(), key=None):
        if key not in self.dsem:
            self.dsem[key] = [self.es.enter_context(self.nc.semaphore("ds_%d" % len(self.dsem))), 0]
        need = self._deps(reads, writes)
        self._wait("pool", need)
        d = self.dsem[key]
        self.nc.gpsimd.indirect_dma_start(
            out=out, out_offset=None, in_=in_,
            in_offset=bass.IndirectOffsetOnAxis(ap=idx_ap, axis=0)).then_inc(d[0], 16)
        d[1] += 16
        self.ninst += 1
        self._mark((("d", key), d[1]), reads, writes)

    def barrier(self):
        need = {("c", e): v for e, v in self.cnt.items() if v > 0}
        for k, d in self.dsem.items():
            if d[1] > 0:
                need[("d", k)] = d[1]
        for e in self.eng:
            self._wait(e, need)

    def finish(self, keys):
        need = self._deps(keys, ())
        self._wait("sp", need)


NM_MODE = ["f32"]
RW_NM = ["bf16"]


class _Stop(Exception):
    pass


def build(nc, n_rows=64, dbg=(), stop=None):
    K = Ctx(nc)
    try:
        return _build(nc, K, n_rows, dbg, stop)
    except _Stop:
        K.barrier()
        return None


def _build(nc, K, n_rows, dbg, stop):
    def stop_at(name):
        if stop == name:
            raise _Stop()

    TL = 64 * n_rows
    T = TC + TL
    NT = T // 128
    NTL = TL // 128
    es = K.es

    def din(name, shape, dt=F32):
        return nc.dram_tensor(name, list(shape), dt, kind="ExternalInput").ap()

    x_d = din("x", [TL, D])
    c_d = din("c", [1, D])
    ctx_d = din("ctx", [TC, D])
    cctx_d = din("c_ctx", [1, D])
    adaw_d = din("ada_w", [D, 6144])
    adab_d = din("ada_b", [48, 128])
    adabr_d = din("ada_b_row", [1, 6144])
    g1_d = din("norm1_g", [8, 128])
    win_d = din("w_in", [D, IN_COLS])
    convw_d = din("gdn_conv_w", [60, 128])
    alog_d = din("gdn_a_log", [1, 8])
    dtb_d = din("gdn_dt_bias", [1, 8])
    gnorm_d = din("gdn_norm_w", [1, 128])
    mu_d = din("rwkv_mu", [15, 128])
    w0_d = din("rwkv_w0", [8, 128])
    w2_d = din("rwkv_w2", [128, 512])
    a0_d = din("rwkv_a0", [8, 128])
    a2_d = din("rwkv_a2", [128, 512])
    g2w_d = din("rwkv_g2", [128, 512])
    kk_d = din("rwkv_k_k", [4, 128])
    ka_d = din("rwkv_k_a", [4, 128])
    rk_d = din("rwkv_r_k", [4, 128])
    gnw_d = din("rwkv_gn_w", [4, 128])
    gnb_d = din("rwkv_gn_b", [4, 128])
    wout_d = din("w_out", [D, D])
    g2_d = din("norm2_g", [8, 128])
    wq_d = din("peer_w_query", [D, 2048])
    sk_d = din("peer_sub_keys", [16, 128, 128])
    down_d = din("peer_down", [16384, D])
    up_d = din("peer_up", [16384, D])
    fng_d = din("final_norm_g", [1, D])
    g2row_d = din("norm2_g_row", [1, D])
    out_d = nc.dram_tensor("out", [TL, D], F32, kind="ExternalOutput").ap()

    dbg_out = {}

    def dbgt(name, shape, dt=F32):
        dbg_out[name] = nc.dram_tensor("dbg_" + name, list(shape), dt, kind="ExternalOutput").ap()
        return dbg_out[name]

    pT_d = nc.dram_tensor("pT_s", [32, 128, T], F32).ap()

    def sb(name, shape, dt=F32, stack=es):
        return stack.enter_context(nc.sbuf_tensor(name, list(shape), dt))

    ps = [es.enter_context(nc.psum_tensor("ps%d" % i, [128, 512], F32)) for i in range(8)]

    dI = sb("dI", [128, 128], I32)
    ident = sb("ident", [128, 128])
    identb = sb("identb", [128, 128], BF16)
    ones = sb("ones", [128, 128])
    onesb = sb("onesb", [128, 128], BF16)
    m_lt = sb("m_lt", [128, 128])
    m_le = sb("m_le", [128, 128])
    m_gt = sb("m_gt", [128, 128])
    m_ge = sb("m_ge", [128, 128])
    e0 = sb("e0", [2, 128])
    K.op("pool", lambda e: e.iota(dI[:], pattern=[[1, 128]], base=0, channel_multiplier=-1), writes=["dI"])
    for t, op_, nm in ((ident, ALU.is_equal, "ident"), (m_lt, ALU.is_gt, "m_lt"), (m_le, ALU.is_ge, "m_le"),
                       (m_gt, ALU.is_lt, "m_gt"), (m_ge, ALU.is_le, "m_ge")):
        K.op("dve", lambda e, t=t, op_=op_: e.tensor_scalar(t[:], dI[:], 0.0, None, op0=op_), reads=["dI"], writes=[nm])
    K.op("dve", lambda e: e.tensor_copy(identb[:], ident[:]), reads=["ident"], writes=["identb"])
    K.op("pool", lambda e: e.memset(ones[:], 1.0), writes=["ones"])
    K.op("pool", lambda e: e.memset(onesb[:], 1.0), writes=["onesb"])
    e0i = sb("e0i", [2, 128], I32)
    K.op("pool", lambda e: e.iota(e0i[:], pattern=[[0, 128]], base=1, channel_multiplier=-1), writes=["e0i"])
    K.op("dve", lambda e: e.tensor_copy(e0[:], e0i[:]), reads=["e0i"], writes=["e0"])

    PK1 = dict(ADAB=0, G1=48, G2=56, CW=64)
    PK2 = dict(MU=0, W0=15, A0=23, KK=31, KA=35, RK=39, GNW=43, GNB=47, GNORM=51)
    pk1s = sb("pk1s", [128, 128])
    pk2s = sb("pk2s", [128, 128])
    pk1 = sb("pk1", [128, 128])
    pk2 = sb("pk2", [128, 128])
    K.op("pool", lambda e: e.memset(pk1s[:], 0.0), writes=["pk1s"])
    K.op("pool", lambda e: e.memset(pk2s[:], 0.0), writes=["pk2s"])
    for src, off, n in ((adab_d, 0, 48), (g1_d, 48, 8), (g2_d, 56, 8), (convw_d, 64, 60)):
        K.dma(pk1s[off:off + n, :], src[:, :], writes=["pk1s"], key="pk1s")
    for src, off, n in ((mu_d, 0, 15), (w0_d, 15, 8), (a0_d, 23, 8), (kk_d, 31, 4), (ka_d, 35, 4),
                        (rk_d, 39, 4), (gnw_d, 43, 4), (gnb_d, 47, 4), (gnorm_d, 51, 1)):
        K.dma(pk2s[off:off + n, :], src[:, :], writes=["pk2s"], key="pk2s")
    K.op("pe", lambda e: e.transpose(ps[0][:, 0:128], pk1s[:], ident[:]), reads=["pk1s", "ident"], writes=["ps0"])
    K.op("pe", lambda e: e.transpose(ps[0][:, 128:256], pk2s[:], ident[:]), reads=["pk2s", "ident"], writes=["ps0"])
    K.op("dve", lambda e: e.tensor_copy(pk1[:], ps[0][:, 0:128]), reads=["ps0"], writes=["pk1"])
    K.op("dve", lambda e: e.tensor_copy(pk2[:], ps[0][:, 128:256]), reads=["ps0"], writes=["pk2"])

    modT = sb("modT", [128, 48, 2])
    gt_d = nc.dram_tensor("gt_s", [128, 4 * D], F32).ap()
    A1 = sb("A1", [128, 8]); B1 = sb("B1", [128, 8])
    A1c = sb("A1c", [128, 8]); B1c = sb("B1c", [128, 8])
    A2 = sb("A2", [128, 8]); B2 = sb("B2", [128, 8])
    with ExitStack() as pa:
        cc = sb("cc", [2, D], stack=pa)
        scT = sb("scT", [128, 8, 2], stack=pa)
        aw = [sb("aw%d" % i, [128, 8, 1024], stack=pa) for i in range(2)]
        gtrow = sb("gtrow", [2, 4, D], stack=pa)
        gtB = sb("gtB", [128, 4, D], stack=pa)
        K.dma(cc[0:1, :], c_d[:, :], writes=["cc"], key="cc")
        K.dma(cc[1:2, :], cctx_d[:, :], writes=["cc"], key="cc")
        K.op("act", lambda e: e.activation(cc[:], cc[:], AF.Silu), reads=["cc"], writes=["cc"])
        for k in range(8):
            K.op("pe", lambda e, k=k: e.transpose(ps[1][:, 2 * k:2 * k + 2], cc[0:2, k * 128:(k + 1) * 128], ident[0:2, 0:2]),
                 reads=["cc", "ident"], writes=["ps1"])
        K.op("dve", lambda e: e.tensor_copy(scT[:].rearrange("p k c -> p (k c)"), ps[1][:, 0:16]), reads=["ps1"], writes=["scT"])
        K.op("pool", lambda e: e.memset(gtrow[:], 0.0), writes=["gtrow"])
        for q_ in range(4):
            K.dma(gtrow[0:1, q_, :], adabr_d[:, 2048 + q_ * 1024:3072 + q_ * 1024], writes=["gtrow"], key="gtrow")
        for g in range(6):
            a = aw[g % 2]
            an = "aw%d" % (g % 2)
            K.dma(a[:], adaw_d[:, g * 1024:(g + 1) * 1024].rearrange("(k p) c -> p k c", p=128), writes=[an], key=an)
            for j in range(8):
                col = (g * 8 + j) * 2
                for k in range(8):
                    K.op("pe", lambda e, a=a, j=j, k=k, col=col: e.matmul(
                        ps[2][:, col:col + 2], lhsT=a[:, k, j * 128:(j + 1) * 128], rhs=scT[:, k, :],
                        start=(k == 0), stop=(k == 7)), reads=[an, "scT"], writes=["ps2"])
            if g >= 2:
                q = g - 2
                for half in range(2):
                    for k in range(8):
                        K.op("pe", lambda e, a=a, k=k, half=half: e.matmul(
                            ps[3][0:2, :], lhsT=scT[:, k, :], rhs=a[:, k, half * 512:(half + 1) * 512],
                            start=(k == 0), stop=(k == 7)), reads=[an, "scT"], writes=["ps3"])
                    K.op("dve", lambda e, q=q, half=half: e.tensor_tensor(
                        gtrow[:, q, half * 512:(half + 1) * 512], ps[3][0:2, :], gtrow[:, q, half * 512:(half + 1) * 512], op=ALU.add),
                        reads=["ps3", "gtrow"], writes=["gtrow"])
                    K.op("pe", lambda e, q=q, half=half: e.matmul(
                        ps[4][:, :], lhsT=e0[:, :], rhs=gtrow[:, q, half * 512:(half + 1) * 512], start=True, stop=True),
                        reads=["e0", "gtrow"], writes=["ps4"])
                    K.op("act", lambda e, q=q, half=half: e.copy(gtB[:, q, half * 512:(half + 1) * 512], ps[4][:, :]),
                         reads=["ps4"], writes=["gtB"])
        K.op("dve", lambda e: e.tensor_tensor(
            modT[:], ps[2][:, 0:96].rearrange("p (j c) -> p j c", c=2),
            pk1[:, 0:48].unsqueeze(2).to_broadcast([128, 48, 2]), op=ALU.add), reads=["ps2", "pk1"], writes=["modT"])
        for (A, B, gname, sc0, sh0, col, nm) in ((A1, B1, "G1", 8, 0, 0, "1"), (A1c, B1c, "G1", 8, 0, 1, "1c"),
                                                 (A2, B2, "G2", 32, 24, 0, "2")):
            g0 = PK1[gname]
            K.op("dve", lambda e, A=A, g0=g0, sc0=sc0, col=col: e.scalar_tensor_tensor(
                A[:], modT[:, sc0:sc0 + 8, col], 1.0, pk1[:, g0:g0 + 8], op0=ALU.add, op1=ALU.mult),
                reads=["modT", "pk1"], writes=["A" + nm])
            K.op("dve", lambda e, B=B, sh0=sh0, col=col: e.tensor_copy(B[:], modT[:, sh0:sh0 + 8, col]),
                 reads=["modT"], writes=["B" + nm])

        K.dma(gt_d[:, :], gtB[:].rearrange("p q d -> p (q d)"), reads=["gtB"], writes=["gt_d"], key="st_gt")
        K.barrier()
    if "mod" in dbg:
        d_ = dbgt("mod", [128, 96])
        K.dma(d_[:, :], modT[:].rearrange("p j c -> p (j c)"), reads=["modT"], writes=["dbgmod"], key="dbg")

    def norm_tile(xt, xkey, A, B, hT, hkey, col0, pp, stage):
        junk, ss, rs, xs = stage
        K.op("act", lambda e: e.activation(junk[:], xt[:], AF.Square), reads=[xkey], writes=["n_junk"])
        K.op("dve", lambda e: e.reduce_sum(ss[:, 0:1], junk[:], axis=AX.X), reads=["n_junk"], writes=["n_ss"])
        K.op("act", lambda e: e.activation(rs[:, 0:1], ss[:, 0:1], AF.Sqrt, bias=eps_t[:, 0:1], scale=1.0 / D),
             reads=["n_ss", "eps"], writes=["n_rs"])
        K.op("dve", lambda e: e.reciprocal(rs[:, 1:2], rs[:, 0:1]), reads=["n_rs"], writes=["n_rs2"])
        K.op("dve", lambda e: e.tensor_scalar(xs[:], xt[:], rs[:, 1:2], None, op0=ALU.mult),
             reads=[xkey, "n_rs2"], writes=["n_xs"])
        for half in range(2):
            pt = ps[pp + half]
            pk = "ps%d" % (pp + half)
            for kk in range(4):
                k = half * 4 + kk
                K.op("pe", lambda e, k=k, kk=kk, pt=pt: e.transpose(pt[:, kk * 128:(kk + 1) * 128], xs[:, k * 128:(k + 1) * 128], ident[:]),
                     reads=["n_xs", "ident"], writes=[pk])
            for kk in range(4):
                k = half * 4 + kk
                eng = "act" if kk % 2 == 0 else "dve"
                if eng == "act":
                    K.op("act", lambda e, k=k, kk=kk, pt=pt: e.activation(
                        hT[:, k, col0:col0 + 128], pt[:, kk * 128:(kk + 1) * 128], AF.Identity,
                        bias=B[:, k:k + 1], scale=A[:, k:k + 1]), reads=[pk, "mods"], writes=[hkey])
                else:
                    K.op("dve", lambda e, k=k, kk=kk, pt=pt: e.tensor_scalar(
                        hT[:, k, col0:col0 + 128], pt[:, kk * 128:(kk + 1) * 128], A[:, k:k + 1], B[:, k:k + 1],
                        op0=ALU.mult, op1=ALU.add), reads=[pk, "mods"], writes=[hkey])

    eps_t = sb("eps_t", [128, 4])
    K.op("pool", lambda e: e.memset(eps_t[:, 0:1], NORM_EPS), writes=["eps"])
    K.op("pool", lambda e: e.memset(eps_t[:, 1:2], L2_EPS), writes=["eps"])
    K.op("pool", lambda e: e.memset(eps_t[:, 2:3], GN_EPS), writes=["eps"])
    K.op("pool", lambda e: e.memset(eps_t[:, 3:4], 1.0), writes=["eps"])
    K.op("dve", lambda e: e.tensor_copy(A1[:, 0:1], A1[:, 0:1]), reads=["A1", "B1", "A1c", "B1c", "A2", "B2"], writes=["mods"])

    with ExitStack() as pb:
        hT = sb("hT", [128, 8, T], BF16, stack=pb)
        junk = sb("junk", [128, D], stack=pb)
        ss = sb("ss", [128, 1], stack=pb)
        rs = sb("rs", [128, 2], stack=pb)
        xs = sb("xs", [128, D], stack=pb)
        xts = [sb("xt%d" % i, [128, D], stack=pb) for i in range(3)]
        for i in range(NT):
            xt = xts[i % 3]
            xk = "xt%d" % (i % 3)
            src = ctx_d[i * 128:(i + 1) * 128, :] if i < 2 else x_d[(i - 2) * 128:(i - 1) * 128, :]
            K.dma(xt[:], src, writes=[xk], key=xk)
            A, B = (A1c, B1c) if i < 2 else (A1, B1)
            norm_tile(xt, xk, A, B, hT, "hT", i * 128, 0, (junk, ss, rs, xs))
        if "hT" in dbg:
            d_ = dbgt("ss", [128, 1])
            K.dma(d_[:, :], ss[:, :], reads=["n_ss"], writes=["dbgss"], key="dbg")
            d_ = dbgt("rs", [128, 2])
            K.dma(d_[:, :], rs[:, :], reads=["n_rs", "n_rs2"], writes=["dbgrs"], key="dbg")
            d_ = dbgt("hT", [128, 8 * T], BF16)
            K.dma(d_[:, :], hT[:].rearrange("p k t -> p (k t)"), reads=["hT"], writes=["dbghT"], key="dbg")
        wst = [sb("wst%d" % i, [128, 8, 128], stack=pb) for i in range(2)]
        wbf = [sb("wbf%d" % i, [128, 8, 128], BF16, stack=pb) for i in range(2)]
        pcs = [sb("pc%d" % i, [128, T], stack=pb) for i in range(2)]
        nblk = (T + 511) // 512
        chunks = [(j, j * 128, 128) for j in range(16)] + [(16, 2048, 16)] + \
                 [(17 + j, GDN_COLS + j * 128, 128) for j in range(15)]
        ev = 0
        for ci, (dst, c0, ncol) in enumerate(chunks):
            w_s = wst[ci % 2]; w_b = wbf[ci % 2]; pc = pcs[ci % 2]
            ws_k = "wst%d" % (ci % 2); wb_k = "wbf%d" % (ci % 2); pc_k = "pc%d" % (ci % 2)
            K.dma(w_s[:, :, 0:ncol], win_d[:, c0:c0 + ncol].rearrange("(k p) c -> p k c", p=128), writes=[ws_k], key=ws_k)
            K.op("pool", lambda e, w_s=w_s, w_b=w_b, ncol=ncol: e.tensor_copy(w_b[:, :, 0:ncol], w_s[:, :, 0:ncol]),
                 reads=[ws_k], writes=[wb_k])
            for n in range(nblk):
                t0 = n * 512
                tn = min(512, T - t0)
                pt = ps[2 + (n % 4)]
                pk = "ps%d" % (2 + (n % 4))
                for k in range(8):
                    K.op("pe", lambda e, k=k, pt=pt, w_b=w_b, ncol=ncol, t0=t0, tn=tn: e.matmul(
                        pt[0:ncol, 0:tn], lhsT=w_b[:, k, 0:ncol], rhs=hT[:, k, t0:t0 + tn], start=(k == 0), stop=(k == 7)),
                        reads=[wb_k, "hT"], writes=[pk])
                eng = "act" if ev % 2 == 0 else "dve"
                ev += 1
                if eng == "act":
                    K.op("act", lambda e, pt=pt, pc=pc, ncol=ncol, t0=t0, tn=tn: e.copy(pc[0:ncol, t0:t0 + tn], pt[0:ncol, 0:tn]),
                         reads=[pk], writes=[pc_k])
                else:
                    K.op("dve", lambda e, pt=pt, pc=pc, ncol=ncol, t0=t0, tn=tn: e.tensor_copy(pc[0:ncol, t0:t0 + tn], pt[0:ncol, 0:tn]),
                         reads=[pk], writes=[pc_k])
            K.dma(pT_d[dst, 0:ncol, :], pc[0:ncol, :], reads=[pc_k], writes=[("pT", dst)], key="st_" + pc_k)

    K.barrier()
    if "pT" in dbg:
        d_ = dbgt("pT", [32, 128, T])
        K.dma(d_[:, :, :], pT_d[:, :, :], reads=[("pT", i) for i in range(32)], writes=["dbgpT"], key="dbg")


    mT_d = nc.dram_tensor("mT_s", [8, 128, TL], BF16).ap()
    psb = [p.bitcast(BF16) for p in ps]
    negm_le = sb("negm_le", [128, 128])
    negm_ge = sb("negm_ge", [128, 128])
    K.op("dve", lambda e: e.tensor_scalar(negm_le[:], m_le[:], 30000.0, -30000.0, op0=ALU.mult, op1=ALU.add), reads=["m_le"], writes=["negm_le"])
    K.op("dve", lambda e: e.tensor_scalar(negm_ge[:], m_ge[:], 30000.0, -30000.0, op0=ALU.mult, op1=ALU.add), reads=["m_ge"], writes=["negm_ge"])
    fwd_order = list(range(NT))
    bwd_order = [1, 0] + list(range(NT - 1, 1, -1))

    NMODE = NM_MODE[0]
    NDT = BF16 if NMODE == "bf16" else F32

    def mmv(ap):
        return ap

    identn = identb if NMODE == "bf16" else ident

    nmode = {'cur': NMODE}

    def neumann(Y, Yt, tag, pbank, pb=None):
        PR = nm_tiles[tag]["PR"]; Pt = nm_tiles[tag]["Pt"]
        kPR = [tag + "PR0", tag + "PR1"]; kPt = [tag + "Pt0", tag + "Pt1"]
        pa_ = ps[pbank]
        ka = "ps%d" % pbank
        if pb is None:
            pb_, kb = ps[pbank + 1], "ps%d" % (pbank + 1)
        else:
            pb_, kb = ps[pb[0]][:, pb[1]:pb[1] + 128], "ps%d" % pb[0]
        K.op("pe", lambda e: e.matmul(pa_[:, 0:128], lhsT=mmv(Yt[:]), rhs=mmv(Y[:]), start=True, stop=True), reads=[tag + "Y", tag + "Yt"], writes=[ka])
        K.op("pe", lambda e: e.matmul(pb_[:, 0:128], lhsT=mmv(Y[:]), rhs=mmv(Yt[:]), start=True, stop=True), reads=[tag + "Y", tag + "Yt"], writes=[kb])
        K.op("act", lambda e: e.copy(PR[0][:, 0:128], pa_[:, 0:128]), reads=[ka], writes=[kPR[0]])
        K.op("dve", lambda e: e.tensor_tensor(PR[0][:, 128:256], Y[:], (identb if nmode['cur'] == 'bf16' else ident)[:], op=ALU.add), reads=[tag + "Y", "identb", "ident"], writes=[kPR[0]])
        K.op("dve", lambda e: e.tensor_copy(Pt[0][:], pb_[:, 0:128]), reads=[kb], writes=[kPt[0]])
        cur = 0
        for l in range(1, 7):
            nxt = 1 - cur
            last = (l == 6)
            n0 = 128 if last else 0
            K.op("pe", lambda e, cur=cur, n0=n0: e.matmul(pa_[:, n0:256], lhsT=mmv(Pt[cur][:]), rhs=mmv(PR[cur][:, n0:256]), start=True, stop=False),
                 reads=[kPt[cur], kPR[cur]], writes=[ka])
            K.op("pe", lambda e, cur=cur: e.matmul(pa_[:, 128:256], lhsT=mmv((identb if nmode['cur'] == 'bf16' else ident)[:]), rhs=mmv(PR[cur][:, 128:256]), start=False, stop=True),
                 reads=["identb", "ident", kPR[cur]], writes=[ka])
            if not last:
                K.op("pe", lambda e, cur=cur: e.matmul(pb_[:, 0:128], lhsT=mmv(PR[cur][:, 0:128]), rhs=mmv(Pt[cur][:]), start=True, stop=True),
                     reads=[kPt[cur], kPR[cur]], writes=[kb])
            K.op("act", lambda e, nxt=nxt, n0=n0: e.copy(PR[nxt][:, n0:256], pa_[:, n0:256]), reads=[ka], writes=[kPR[nxt]])
            if not last:
                K.op("dve", lambda e, nxt=nxt: e.tensor_copy(Pt[nxt][:], pb_[:, 0:128]), reads=[kb], writes=[kPt[nxt]])
            cur = nxt
        if nmode['cur'] == 'bf16':
            return PR[cur][:, 128:256], kPR[cur]
        fin = nm_tiles[tag]["fin"]
        K.op("act", lambda e, cur=cur: e.copy(fin[:], PR[cur][:, 128:256]), reads=[kPR[cur]], writes=[tag + "fin"])
        return fin[:], tag + "fin"

    nm_tiles = {}
    with ExitStack() as pg:
        for tag in ("n0", "n1", "n2", "n3"):
            nm_tiles[tag] = dict(PR=[sb(tag + "PR%d" % i, [128, 256], NDT, stack=pg) for i in range(2)],
                                 Pt=[sb(tag + "Pt%d" % i, [128, 128], NDT, stack=pg) for i in range(2)],
                                 fin=sb(tag + "fin", [128, 128], BF16, stack=pg))
        ab = sb("ab", [128, NT, 16], stack=pg)
        dtb_b = sb("dtb_b", [128, 8], stack=pg)
        nA_b = sb("nA_b", [128, 8], stack=pg)
        gg = sb("gg", [128, NT, 8], stack=pg)
        Gc = sb("Gc", [128, NT, 8], stack=pg)
        nbeta = sb("nbeta", [128, NT, 8], stack=pg)
        beta = sb("beta", [128, NT, 8], stack=pg)
        negeG = sb("negeG", [128, NT, 8], stack=pg)
        eG = sb("eG", [128, NT, 8], stack=pg)
        eTG = sb("eTG", [128, NT, 8], stack=pg)
        eTot = sb("eTot", [128, NT, 8], stack=pg)
        pg_ab = ExitStack()
        abT = sb("abT", [16, T], stack=pg_ab)
        K.dma(abT[:, :], pT_d[16, 0:16, :], reads=[("pT", 16)], writes=["abT"], key="abT")
        K.dma(dtb_b[:, :], dtb_d.partition_broadcast(128), writes=["dtb_b"], key="dtb_b")
        K.dma(nA_b[:, :], alog_d.partition_broadcast(128), writes=["nA_b"], key="nA_b")
        for i in range(NT):
            K.op("pe", lambda e, i=i: e.transpose(ps[i // 32][:, (i % 32) * 16:(i % 32) * 16 + 16], abT[0:16, i * 128:(i + 1) * 128], ident[0:16, 0:16]),
                 reads=["abT", "ident"], writes=["ps%d" % (i // 32)])
        for b0 in range(0, NT, 32):
            nb = min(32, NT - b0)
            K.op("dve", lambda e, b0=b0, nb=nb: e.tensor_copy(ab[:, b0:b0 + nb, :].rearrange("p n c -> p (n c)"), ps[b0 // 32][:, 0:nb * 16]),
                 reads=["ps%d" % (b0 // 32)], writes=["ab"])
        K.op("act", lambda e: e.activation(nA_b[:], nA_b[:], AF.Exp), reads=["nA_b"], writes=["nA_b"])
        K.op("dve", lambda e: e.tensor_scalar(nA_b[:], nA_b[:], -1.0, None, op0=ALU.mult), reads=["nA_b"], writes=["nA_b"])
        K.op("dve", lambda e: e.tensor_tensor(gg[:], ab[:, :, 0:8], dtb_b[:].unsqueeze(1).to_broadcast([128, NT, 8]), op=ALU.add),
             reads=["ab", "dtb_b"], writes=["gg"])
        K.op("act", lambda e: e.activation(gg[:], gg[:], AF.Exp), reads=["gg"], writes=["gg"])
        K.op("act", lambda e: e.activation(gg[:], gg[:], AF.Ln, bias=eps_t[:, 3:4]), reads=["gg", "eps"], writes=["gg"])
        K.op("dve", lambda e: e.tensor_tensor(gg[:], gg[:], nA_b[:].unsqueeze(1).to_broadcast([128, NT, 8]), op=ALU.mult),
             reads=["gg", "nA_b"], writes=["gg"])
        K.op("act", lambda e: e.activation(beta[:], ab[:, :, 8:16], AF.Sigmoid), reads=["ab"], writes=["beta"])
        K.op("dve", lambda e: e.tensor_scalar(nbeta[:], beta[:], -1.0, None, op0=ALU.mult), reads=["beta"], writes=["nbeta"])
        ggf = gg[:].rearrange("p n c -> p (n c)")
        K.op("pe", lambda e: e.matmul(ps[2][:, 0:NT * 8], lhsT=m_le[:], rhs=ggf, start=True, stop=True), reads=["m_le", "gg"], writes=["ps2"])
        K.op("pe", lambda e: e.matmul(ps[3][:, 0:NT * 8], lhsT=m_ge[:], rhs=ggf, start=True, stop=True), reads=["m_ge", "gg"], writes=["ps3"])
        K.op("pe", lambda e: e.matmul(ps[4][:, 0:NT * 8], lhsT=ones[:], rhs=ggf, start=True, stop=True), reads=["ones", "gg"], writes=["ps4"])
        K.op("dve", lambda e: e.tensor_copy(Gc[:, :, 0:4], ps[2][:, 0:NT * 8].rearrange("p (n c) -> p n c", c=8)[:, :, 0:4]), reads=["ps2"], writes=["Gc"])
        K.op("dve", lambda e: e.tensor_copy(Gc[:, :, 4:8], ps[3][:, 0:NT * 8].rearrange("p (n c) -> p n c", c=8)[:, :, 4:8]), reads=["ps3"], writes=["Gc"])
        K.op("act", lambda e: e.activation(eG[:], Gc[:], AF.Exp), reads=["Gc"], writes=["eG"])
        K.op("dve", lambda e: e.tensor_scalar(negeG[:], eG[:], -1.0, None, op0=ALU.mult), reads=["eG"], writes=["negeG"])
        K.op("act", lambda e: e.activation(eTot[:].rearrange("p n c -> p (n c)"), ps[4][:, 0:NT * 8], AF.Exp), reads=["ps4"], writes=["eTot"])
        K.op("dve", lambda e: e.tensor_tensor(eTG[:].rearrange("p n c -> p (n c)"), ps[4][:, 0:NT * 8], Gc[:].rearrange("p n c -> p (n c)"), op=ALU.subtract),
             reads=["ps4", "Gc"], writes=["eTG"])
        K.op("act", lambda e: e.activation(eTG[:], eTG[:], AF.Exp), reads=["eTG"], writes=["eTG"])

        K.barrier()
        pg_ab.close()
        stop_at("gdn_scal")
        qT = sb("qT", [128, T], BF16, stack=pg)
        kT = sb("kT", [128, T], BF16, stack=pg)
        vT = sb("vT", [128, T], BF16, stack=pg)
        zs = sb("zs", [128, T], BF16, stack=pg)
        vtok = sb("vtok", [128, NT, 128], BF16, stack=pg)
        ktok = sb("ktok", [128, NT, 128], BF16, stack=pg)
        obuf = [sb("obuf%d" % d_, [128, NT, 128], BF16, stack=pg) for d_ in range(2)]
        AinvAll = [sb("AinvAll%d" % d_, [128, NT, 128], BF16, stack=pg) for d_ in range(2)]
        MqkAll = [sb("MqkAll%d" % d_, [128, NT, 128], BF16, stack=pg) for d_ in range(2)]
        pin = sb("pin", [128, T], stack=pg)
        cv = sb("cv", [128, T], stack=pg)
        sq = pin
        rn = sb("rn", [128, 512], stack=pg)
        S = [sb("S%d" % d_, [128, 128], stack=pg) for d_ in range(2)]
        Sb = [sb("Sb%d" % d_, [128, 128], BF16, stack=pg) for d_ in range(2)]
        mst = sb("mst", [128, TL], BF16, stack=pg)
        dgl = [sb("dgl%d" % d_, [128, 128], stack=pg) for d_ in range(4)]
        arg = dgl
        DTi = [sb("DTi%d" % d_, [128, 128], stack=pg) for d_ in range(4)]
        DTs = dgl
        Yb = [sb("Yb%d" % d_, [128, 128], NDT, stack=pg) for d_ in range(4)]
        Ytb = [sb("Ytb%d" % d_, [128, 128], NDT, stack=pg) for d_ in range(4)]
        Rb = [sb("Rb%d" % d_, [128, 128], BF16, stack=pg) for d_ in range(2)]
        Xb = [sb("Xb%d" % d_, [128, 128], BF16, stack=pg) for d_ in range(2)]
        Xs = [sb("Xs%d" % d_, [128, 128], BF16, stack=pg) for d_ in range(2)]
        QSe = [sb("QSe%d" % d_, [128, 128], stack=pg) for d_ in range(2)]
        on_ = sb("on_", [128, 128], stack=pg)
        oj = sb("oj", [128, 128], stack=pg)
        onn = sb("onn", [128, 128], stack=pg)
        oss = sb("oss", [128, 4], stack=pg)

        def conv_silu(cidx, dst, dkey, final_silu_to):
            cw0 = PK1["CW"]
            K.op("dve", lambda e: e.tensor_scalar(cv[:], pin[:], pk1[:, cw0 + 2 * 12 + cidx:cw0 + 2 * 12 + cidx + 1], None, op0=ALU.mult),
                 reads=["pin", "pk1"], writes=["cv"])
            for j in (0, 1, 3, 4):
                sh = j - 2
                wcol = pk1[:, cw0 + j * 12 + cidx:cw0 + j * 12 + cidx + 1]
                for (s0, s1) in ((0, TC), (TC, T)):
                    lo = max(s0, s0 - sh); hi = min(s1, s1 - sh)
                    K.op("dve", lambda e, lo=lo, hi=hi, sh=sh, wcol=wcol: e.scalar_tensor_tensor(
                        cv[:, lo:hi], pin[:, lo + sh:hi + sh], wcol, cv[:, lo:hi], op0=ALU.mult, op1=ALU.add),
                        reads=["pin", "pk1", "cv"], writes=["cv"])
            K.op("act", lambda e: e.activation(final_silu_to[:], cv[:], AF.Silu), reads=["cv"], writes=[dkey])

        def l2n(src, skey, dst, dkey, scale):
            K.op("pool", lambda e: e.tensor_tensor(sq[:], src[:], src[:], op=ALU.mult), reads=[skey], writes=["pin"])
            for n in range((T + 511) // 512):
                t0 = n * 512; tn = min(512, T - t0)
                pt = ps[5 + (n % 2)]; pk = "ps%d" % (5 + (n % 2))
                K.op("pe", lambda e, pt=pt, t0=t0, tn=tn: e.matmul(pt[:, 0:tn], lhsT=ones[:], rhs=sq[:, t0:t0 + tn], start=True, stop=True),
                     reads=["ones", "pin"], writes=[pk])
                K.op("act", lambda e, pt=pt, tn=tn: e.activation(rn[:, 0:tn], pt[:, 0:tn], AF.Sqrt, bias=eps_t[:, 1:2]), reads=[pk, "eps"], writes=["rn"])
                K.op("dve", lambda e, tn=tn: e.reciprocal(rn[:, 0:tn], rn[:, 0:tn]), reads=["rn"], writes=["rn"])
                K.op("dve", lambda e, t0=t0, tn=tn: e.scalar_tensor_tensor(dst[:, t0:t0 + tn], src[:, t0:t0 + tn], scale, rn[:, 0:tn], op0=ALU.mult, op1=ALU.mult),
                     reads=[skey, "rn"], writes=[dkey])

        def to_tok(src, skey, dst, dkey):
            for i in range(NT):
                pt = psb[5 + (i % 2)]; pk = "ps%d" % (5 + (i % 2))
                K.op("pe", lambda e, i=i, pt=pt: e.transpose(pt[:, 0:128], src[:, i * 128:(i + 1) * 128], identb[:]), reads=[skey, "identb"], writes=[pk])
                eng = "act" if i % 2 == 0 else "dve"
                if eng == "act":
                    K.op("act", lambda e, i=i, pt=pt: e.copy(dst[:, i, :], pt[:, 0:128]), reads=[pk], writes=[dkey])
                else:
                    K.op("dve", lambda e, i=i, pt=pt: e.tensor_copy(dst[:, i, :], pt[:, 0:128]), reads=[pk], writes=[dkey])

        for h in range(4):
            K.dma(pin[:, :], pT_d[h, :, :], reads=[("pT", h)], writes=["pin"], key="pin")
            conv_silu(h, cv, "cv", cv)
            l2n(cv, "cv", qT, "qT", float(128 ** -0.5))
            K.dma(pin[:, :], pT_d[4 + h, :, :], reads=[("pT", 4 + h)], writes=["pin"], key="pin")
            conv_silu(4 + h, cv, "cv", cv)
            l2n(cv, "cv", kT, "kT", 1.0)
            to_tok(kT, "kT", ktok, "ktok")
            K.dma(pin[:, :], pT_d[8 + h, :, :], reads=[("pT", 8 + h)], writes=["pin"], key="pin")
            conv_silu(8 + h, vT, "vT", vT)
            to_tok(vT, "vT", vtok, "vtok")
            K.dma(pin[:, :], pT_d[12 + h, :, :], reads=[("pT", 12 + h)], writes=["pin"], key="pin")
            K.op("act", lambda e: e.activation(zs[:], pin[:], AF.Silu), reads=["pin"], writes=["zs"])
            stop_at("gdn_prep")
            for d_ in range(2):
                K.op("pool", lambda e, d_=d_: e.memset(S[d_][:], 0.0), writes=["S%d" % d_])
                K.op("pool", lambda e, d_=d_: e.memset(Sb[d_][:], 0.0), writes=["Sb%d" % d_])
            def g_pre(step, d_, sl_):
                i = fwd_order[step] if d_ == 0 else bwd_order[step]
                par = step % 2
                r = d_ * 4 + h
                tag = "n%d" % sl_
                sl = slice(i * 128, (i + 1) * 128)
                want_o = i >= 2
                pA = ps[2 * sl_]; kA = "ps%d" % (2 * sl_)
                dk_ = "%d" % sl_
                K.op("dve", lambda e: e.tensor_scalar(dgl[sl_][:], ident[:], Gc[:, i, r:r + 1], None, op0=ALU.mult),
                     reads=["ident", "Gc"], writes=["dgl" + dk_])
                K.op("pe", lambda e: e.matmul(pA[:, 256:384], lhsT=ones[:], rhs=dgl[sl_][:], start=True, stop=True),
                     reads=["ones", "dgl" + dk_], writes=[kA])
                negm = negm_le if d_ == 0 else negm_ge
                mstrict = m_lt if d_ == 0 else m_gt
                K.op("dve", lambda e: e.scalar_tensor_tensor(
                    arg[sl_][:], pA[:, 256:384], Gc[:, i, r:r + 1], negm[:], op0=ALU.subtract, op1=ALU.add),
                    reads=[kA, "Gc", "negm_le", "negm_ge"], writes=["dgl" + dk_])
                K.op("act", lambda e: e.activation(DTi[sl_][:], arg[sl_][:], AF.Exp), reads=["dgl" + dk_], writes=["DTi" + dk_])
                K.op("pool", lambda e: e.tensor_tensor(DTs[sl_][:], DTi[sl_][:], mstrict[:], op=ALU.mult),
                     reads=["DTi" + dk_, "m_lt", "m_gt"], writes=["dgl" + dk_])
                K.op("pe", lambda e: e.matmul(pA[:, 0:128], lhsT=kT[:, sl], rhs=kT[:, sl], start=True, stop=True),
                     reads=["kT"], writes=[kA])
                K.op("dve", lambda e: e.scalar_tensor_tensor(
                    Yb[sl_][:], pA[:, 0:128], nbeta[:, i, r:r + 1], DTs[sl_][:], op0=ALU.mult, op1=ALU.mult),
                    reads=[kA, "nbeta", "dgl" + dk_], writes=[tag + "Y"])
                if want_o:
                    K.op("pe", lambda e: e.matmul(pA[:, 128:256], lhsT=kT[:, sl], rhs=qT[:, sl], start=True, stop=True),
                         reads=["kT", "qT"], writes=[kA])
                    K.op("dve", lambda e: e.tensor_tensor(MqkAll[d_][:, step, :], pA[:, 128:256], DTi[sl_][:], op=ALU.mult),
                         reads=[kA, "DTi" + dk_], writes=[("Mqk", d_, step)])
                pB = (psb if NMODE == "bf16" else ps)[2 * sl_ + 1]; kB = "ps%d" % (2 * sl_ + 1)
                K.op("pe", lambda e: e.transpose(pB[:, 0:128], Yb[sl_][:], identn[:]), reads=[tag + "Y", "identb", "ident"], writes=[kB])
                K.op("act", lambda e: e.copy(Ytb[sl_][:], pB[:, 0:128]), reads=[kB], writes=[tag + "Yt"])
                AinvT, kAinv = neumann(Yb[sl_], Ytb[sl_], tag, 2 * sl_ + 1, pb=(2 * sl_, 384))
                K.op("pool", lambda e: e.tensor_copy(AinvAll[d_][:, step, :], AinvT), reads=[kAinv], writes=[("gAinv", d_, step)])

            def g_chain(step, d_):
                i = fwd_order[step] if d_ == 0 else bwd_order[step]
                par = step % 2
                r = d_ * 4 + h
                sl = slice(i * 128, (i + 1) * 128)
                want_o = i >= 2
                pC = ps[d_ * 4 + 3]; kC = "ps%d" % (d_ * 4 + 3)
                dk_ = "%d" % d_
                kAinv = ("gAinv", d_, step)
                kMqk = ("Mqk", d_, step)
                K.op("pe", lambda e: e.matmul(pC[:, 0:128], lhsT=kT[:, sl], rhs=Sb[d_][:], start=True, stop=True),
                     reads=["kT", "Sb" + dk_], writes=[kC])
                if want_o:
                    K.op("pe", lambda e: e.matmul(pC[:, 128:256], lhsT=qT[:, sl], rhs=Sb[d_][:], start=True, stop=True),
                         reads=["qT", "Sb" + dk_], writes=[kC])
                K.op("dve", lambda e: e.scalar_tensor_tensor(
                    Rb[d_][:], pC[:, 0:128], negeG[:, i, r:r + 1], vtok[:, i, :], op0=ALU.mult, op1=ALU.add),
                    reads=[kC, "negeG", "vtok"], writes=["Rb" + dk_])
                if want_o:
                    K.op("act", lambda e: e.activation(QSe[d_][:], pC[:, 128:256], AF.Identity, scale=eG[:, i, r:r + 1]),
                         reads=[kC, "eG"], writes=["QSe" + dk_])
                K.op("pe", lambda e: e.matmul(pC[:, 0:128], lhsT=AinvAll[d_][:, step, :], rhs=Rb[d_][:], start=True, stop=True),
                     reads=[kAinv, "Rb" + dk_], writes=[kC])
                K.op("dve", lambda e: e.tensor_scalar(Xb[d_][:], pC[:, 0:128], beta[:, i, r:r + 1], None, op0=ALU.mult),
                     reads=[kC, "beta"], writes=["Xb" + dk_])
                K.op("pool", lambda e: e.tensor_scalar(Xs[d_][:], Xb[d_][:], eTG[:, i, r:r + 1], None, op0=ALU.mult),
                     reads=["Xb" + dk_, "eTG"], writes=["Xs" + dk_])
                if want_o:
                    K.op("pe", lambda e: e.matmul(pC[:, 384:512], lhsT=MqkAll[d_][:, step, :], rhs=Xb[d_][:], start=True, stop=True),
                         reads=[kMqk, "Xb" + dk_], writes=[kC])
                    K.op("dve", lambda e: e.tensor_tensor(obuf[d_][:, i, :], pC[:, 384:512], QSe[d_][:], op=ALU.add),
                         reads=[kC, "QSe" + dk_], writes=["obuf" + dk_])
                K.op("pe", lambda e: e.matmul(pC[:, 256:384], lhsT=ktok[:, i, :], rhs=Xs[d_][:], start=True, stop=True),
                     reads=["ktok", "Xs" + dk_], writes=[kC])
                K.op("dve", lambda e: e.scalar_tensor_tensor(
                    S[d_][:], S[d_][:], eTot[:, i, r:r + 1], pC[:, 256:384], op0=ALU.mult, op1=ALU.add),
                    reads=["S" + dk_, "eTot", kC], writes=["S" + dk_])
                K.op("act", lambda e: e.copy(Sb[d_][:], S[d_][:]), reads=["S" + dk_], writes=["Sb" + dk_])

            inst = [(st_, dd_) for st_ in range(NT) for dd_ in range(2)]
            for g0 in range(0, len(inst), 4):
                grp = inst[g0:g0 + 4]
                for q_ in K.streams(len(grp)):
                    g_pre(grp[q_][0], grp[q_][1], q_)
            for step in range(NT):
                for d_ in K.streams(2):
                    g_chain(step, d_)
            stop_at("gdn_scan")
            for i in range(2, NT):
                K.op("dve", lambda e, i=i: e.tensor_tensor(on_[:], obuf[0][:, i, :], obuf[1][:, i, :], op=ALU.add),
                     reads=["obuf0", "obuf1"], writes=["on_"])
                K.op("act", lambda e: e.activation(oj[:], on_[:], AF.Square), reads=["on_"], writes=["oj"])
                K.op("dve", lambda e: e.reduce_sum(oss[:, 0:1], oj[:], axis=AX.X), reads=["oj"], writes=["oss"])
                K.op("act", lambda e: e.activation(oss[:, 1:2], oss[:, 0:1], AF.Sqrt, bias=eps_t[:, 0:1], scale=1.0 / 128), reads=["oss", "eps"], writes=["oss1"])
                K.op("dve", lambda e: e.reciprocal(oss[:, 2:3], oss[:, 1:2]), reads=["oss1"], writes=["oss2"])
                K.op("dve", lambda e: e.tensor_scalar(onn[:], on_[:], oss[:, 2:3], None, op0=ALU.mult), reads=["on_", "oss2"], writes=["onn"])
                pt = ps[i % 2]; pk = "ps%d" % (i % 2)
                K.op("pe", lambda e, pt=pt: e.transpose(pt[:, 0:128], onn[:], ident[:]), reads=["onn", "ident"], writes=[pk])
                gcol = pk2[:, PK2["GNORM"]:PK2["GNORM"] + 1]
                K.op("dve", lambda e, i=i, pt=pt, gcol=gcol: e.scalar_tensor_tensor(
                    mst[:, (i - 2) * 128:(i - 1) * 128], pt[:, 0:128], gcol, zs[:, i * 128:(i + 1) * 128], op0=ALU.mult, op1=ALU.mult),
                    reads=[pk, "pk2", "zs"], writes=["mst"])
            K.dma(mT_d[h, :, :], mst[:, :], reads=["mst"], writes=[("mT", h)], key="st_mst")

    K.barrier()
    pm_d = nc.dram_tensor("pm_s", [12, 128, T], F32).ap()
    lora_d = nc.dram_tensor("lora_s", [3, 128, T], BF16).ap()
    CW_ = float(np.exp(-0.5))
    NRW = n_rows
    with ExitStack() as pr:
        cidx_i = sb("cidx_i", [128, 15], I32, stack=pr)
        cidx = sb("cidx", [128, 15], stack=pr)
        mum = {nm: sb("mum_" + nm, [128, 15], stack=pr) for nm in ("om", "L", "R", "U", "D", "P", "N")}
        tmpm = sb("tmpm", [128, 15], stack=pr)
        K.op("pool", lambda e: e.iota(cidx_i[:], pattern=[[128, 15]], base=0, channel_multiplier=1), writes=["cidx_i"])
        K.op("dve", lambda e: e.tensor_copy(cidx[:], cidx_i[:]), reads=["cidx_i"], writes=["cidx"])
        mu_ap = pk2[:, PK2["MU"]:PK2["MU"] + 15]
        K.op("dve", lambda e: e.tensor_scalar(mum["om"][:], mu_ap, -1.0, 1.0, op0=ALU.mult, op1=ALU.add), reads=["pk2"], writes=["mum"])

        def band(nm, lo, hi):
            K.op("dve", lambda e: e.tensor_scalar(tmpm[:], cidx[:], float(lo), None, op0=ALU.is_ge), reads=["cidx"], writes=["tmpm"])
            K.op("dve", lambda e: e.scalar_tensor_tensor(tmpm[:], cidx[:], float(hi), tmpm[:], op0=ALU.is_lt, op1=ALU.mult), reads=["cidx", "tmpm"], writes=["tmpm"])
            K.op("dve", lambda e: e.tensor_tensor(mum[nm][:], tmpm[:], mu_ap, op=ALU.mult), reads=["tmpm", "pk2"], writes=["mum"])

        band("L", 0, 480); band("R", 480, 960); band("U", 960, 1440); band("D", 1440, 1920)
        band("P", 0, 960); band("N", 960, 1920)
        pins = [sb("rpin%d" % i, [128, T], stack=pr) for i in range(2)]
        pmx = [sb("pmx%d" % i, [128, T], stack=pr) for i in range(2)]
        lob = sb("lob", [128, T], BF16, stack=pr)
        for j in range(15):
            pin_ = pins[j % 2]; pk_ = "rpin%d" % (j % 2)
            po = pmx[j % 2]; ok_ = "pmx%d" % (j % 2)
            K.dma(pin_[:, :], pT_d[17 + j, :, :], reads=[("pT", 17 + j)], writes=[pk_], key=pk_)
            K.op("dve", lambda e, j=j, pin_=pin_, po=po: e.tensor_scalar(po[:], pin_[:], mum["om"][:, j:j + 1], None, op0=ALU.mult),
                 reads=[pk_, "mum"], writes=[ok_])
            c0, c1 = j * 128, j * 128 + 128

            def has(lo, hi):
                return c0 < hi and c1 > lo

            def acc(dst, src, nm, j=j, pin_=pin_, po=po, pk_=pk_, ok_=ok_, eng="dve"):
                K.op("dve", lambda e: e.scalar_tensor_tensor(dst(po), src(pin_), mum[nm][:, j:j + 1], dst(po), op0=ALU.mult, op1=ALU.add),
                     reads=[pk_, "mum", ok_], writes=[ok_])

            lat = lambda t: t[:, TC:T].rearrange("p (r w) -> p r w", w=64)
            if has(0, 960):
                acc(lambda t: t[:, 1:TC], lambda t: t[:, 0:TC - 1], "P")
            if has(960, 1920):
                acc(lambda t: t[:, 0:TC - 1], lambda t: t[:, 1:TC], "N")
            if has(0, 480):
                acc(lambda t: lat(t)[:, :, 1:64], lambda t: lat(t)[:, :, 0:63], "L")
            if has(480, 960):
                acc(lambda t: lat(t)[:, :, 0:63], lambda t: lat(t)[:, :, 1:64], "R")
            if has(960, 1440) and NRW > 1:
                acc(lambda t: lat(t)[:, 1:NRW, :], lambda t: lat(t)[:, 0:NRW - 1, :], "U")
            if has(1440, 1920) and NRW > 1:
                acc(lambda t: lat(t)[:, 0:NRW - 1, :], lambda t: lat(t)[:, 1:NRW, :], "D")
            if j < 12:
                K.dma(pm_d[j, :, :], po[:, :], reads=[ok_], writes=[("pm", j)], key="st_" + ok_)
            else:
                fn_ = {12: AF.Tanh, 13: AF.Identity, 14: AF.Sigmoid}[j]
                K.op("act", lambda e, po=po, fn_=fn_: e.activation(lob[:], po[:], fn_), reads=[ok_], writes=["lob"])
                K.dma(lora_d[j - 12, :, :], lob[:, :], reads=["lob"], writes=[("lora", j - 12)], key="st_lob")
    K.barrier()
    stop_at("rw_mix")
    if "pm" in dbg:
        d_o = dbgt("pm", [12, 128, T])
        K.dma(d_o[:, :, :], pm_d[:, :, :], reads=[("pm", j) for j in range(12)], writes=["dbgpm"], key="dbg")

    with ExitStack() as pw:
        nm_tiles.clear()
        RW_NDT = BF16 if RW_NM[0] == "bf16" else F32
        nmode['cur'] = RW_NM[0]
        for tag in ("n0", "n1", "n2", "n3"):
            nm_tiles[tag] = dict(PR=[sb(tag + "rPR%d" % i, [128, 256], RW_NDT, stack=pw) for i in range(2)],
                                 Pt=[sb(tag + "rPt%d" % i, [128, 128], RW_NDT, stack=pw) for i in range(2)],
                                 fin=sb(tag + "rfin", [128, 128], BF16, stack=pw))
        BL = min(256, T)
        blocks = [(b0, min(BL, T - b0)) for b0 in range(0, T, BL)]
        wst_ = sb("rw_wst", [128, 3, 512], stack=pw)
        wlb = sb("rw_wlb", [128, 3, 512], BF16, stack=pw)
        for q_, src in enumerate((w2_d, a2_d, g2w_d)):
            K.dma(wst_[:, q_, :], src[:, :], writes=["rw_wst"], key="rw_wst")
        K.op("dve", lambda e: e.tensor_copy(wlb[:], wst_[:]), reads=["rw_wst"], writes=["wlb"])
        bones = sb("bones", [128, 128], stack=pw)
        K.op("pool", lambda e: e.memset(bones[:], 0.0), writes=["bones"])
        K.op("pool", lambda e: e.memset(bones[0:64, 0:64], 1.0), writes=["bones"])
        K.op("pool", lambda e: e.memset(bones[64:128, 64:128], 1.0), writes=["bones"])
        rmask = sb("rmask", [128, BL], stack=pw)
        K.op("pool", lambda e: e.memset(rmask[:], 1.0), writes=["rmask"])
        K.op("pool", lambda e: e.memset(rmask[:].rearrange("p (n t) -> p n t", t=128)[:, :, 0:1], 0.0), writes=["rmask"])
        mask4 = [sb("mask4_%d" % d_, [128, 512], stack=pw) for d_ in range(2)]
        for d_ in range(2):
            ms_, mi_ = (m_lt, m_le) if d_ == 0 else (m_gt, m_ge)
            for q_ in range(4):
                src = ms_ if q_ % 2 == 0 else mi_
                K.op("dve", lambda e, d_=d_, q_=q_, src=src: e.tensor_copy(mask4[d_][:, q_ * 128:(q_ + 1) * 128], src[:]),
                     reads=["m_lt", "m_le", "m_gt", "m_ge"], writes=["mask4"])
        rT = [sb("rT%d" % d_, [128, T], BF16, stack=pw) for d_ in range(2)]
        kpT = [sb("kpT%d" % d_, [128, T], BF16, stack=pw) for d_ in range(2)]
        ktT = [sb("ktT%d" % d_, [128, T], BF16, stack=pw) for d_ in range(2)]
        nbT = [sb("nbT%d" % d_, [128, T], BF16, stack=pw) for d_ in range(2)]
        Lam = [sb("Lam%d" % d_, [128, NT], stack=pw) for d_ in range(2)]
        vtk = sb("rvtok", [128, NT, 128], BF16, stack=pw)
        bonus = sb("bonus", [128, T], BF16, stack=pw)
        gateT = sb("gateT", [128, T], BF16, stack=pw)
        ybuf = [sb("ybuf%d" % d_, [128, NT, 128], BF16, stack=pw) for d_ in range(2)]
        rmst = sb("rmst", [128, TL], BF16, stack=pw)
        bt = {nm: sb("b_" + nm, [128, BL], stack=pw) for nm in
              ("r", "k", "v", "kap", "sig", "cum", "w", "iw", "wp", "a", "t1", "t2", "rk")}
        lbt = sb("b_lora", [128, 3, BL], BF16, stack=pw)
        vb16 = sb("b_vb16", [128, BL], BF16, stack=pw)
        Z = [sb("Z%d" % d_, [128, 128], stack=pw) for d_ in range(2)]
        Zb = [sb("Zb%d" % d_, [128, 128], BF16, stack=pw) for d_ in range(2)]
        AK = [sb("AK%d" % d_, [128, 512], BF16, stack=pw) for d_ in range(2)]
        ANB = [sb("ANB%d" % d_, [128, 512], BF16, stack=pw) for d_ in range(2)]
        YY = [[sb("YY%d%d" % (d_, hh), [128, 128], RW_NDT, stack=pw) for hh in range(2)] for d_ in range(2)]
        YYt = [[sb("YYt%d%d" % (d_, hh), [128, 128], RW_NDT, stack=pw) for hh in range(2)] for d_ in range(2)]
        AinvS = [[sb("Ainv%d%d" % (d_, hh), [128, 128], BF16, stack=pw) for hh in range(2)] for d_ in range(2)]
        ktok_ = [sb("rktok%d" % d_, [128, 128], BF16, stack=pw) for d_ in range(2)]
        nbtok_ = [sb("rnbtok%d" % d_, [128, 128], BF16, stack=pw) for d_ in range(2)]
        P1b = [sb("P1b%d" % d_, [128, 128], BF16, stack=pw) for d_ in range(2)]
        Ub = [sb("Ub%d" % d_, [128, 128], BF16, stack=pw) for d_ in range(2)]
        zt = [sb("zt%d" % d_, [128, 128], stack=pw) for d_ in range(2)]
        yo = sb("yo", [128, 128], stack=pw)
        yc = sb("yc", [128, 128], stack=pw)
        ysq = sb("ysq", [128, 128], stack=pw)
        yst = sb("yst", [128, 8], stack=pw)
        yt2 = sb("yt2", [128, 128], stack=pw)

        for P in range(4):
            ch = slice(P * 128, (P + 1) * 128)
            for (b0, bn) in blocks:
                bs = slice(b0, b0 + bn)
                ntb = bn // 128
                for nm, jj in (("r", P), ("k", 4 + P), ("v", 8 + P)):
                    K.dma(bt[nm][:, 0:bn], pm_d[jj, :, bs], reads=[("pm", jj)], writes=["b_" + nm], key="b_" + nm)
                K.dma(lbt[:, :, 0:bn], lora_d[:, :, bs].rearrange("q p t -> p q t"), reads=[("lora", 0), ("lora", 1), ("lora", 2)], writes=["b_lora"], key="b_lora")
                K.op("pool", lambda e, bn=bn: e.tensor_copy(vb16[:, 0:bn], bt["v"][:, 0:bn]), reads=["b_v"], writes=["vb16"])
                for ii in range(ntb):
                    gi = b0 // 128 + ii
                    pt = psb[6 + (ii % 2)]; pk = "ps%d" % (6 + (ii % 2))
                    K.op("pe", lambda e, ii=ii, pt=pt: e.transpose(pt[:, 0:128], vb16[:, ii * 128:(ii + 1) * 128], identb[:]), reads=["vb16", "identb"], writes=[pk])
                    K.op("act", lambda e, gi=gi, pt=pt: e.copy(vtk[:, gi, :], pt[:, 0:128]), reads=[pk], writes=["rvtok"])
                kkc = pk2[:, PK2["KK"] + P:PK2["KK"] + P + 1]
                K.op("dve", lambda e, bn=bn, kkc=kkc: e.tensor_scalar(bt["kap"][:, 0:bn], bt["k"][:, 0:bn], kkc, None, op0=ALU.mult), reads=["b_k", "pk2"], writes=["b_kap"])
                K.op("pool", lambda e, bn=bn: e.tensor_tensor(bt["t1"][:, 0:bn], bt["kap"][:, 0:bn], bt["kap"][:, 0:bn], op=ALU.mult), reads=["b_kap"], writes=["b_t1"])
                for n in range((bn + 511) // 512):
                    t0 = n * 512; tn = min(512, bn - t0)
                    K.op("pe", lambda e, t0=t0, tn=tn: e.matmul(ps[0][:, 0:tn], lhsT=bones[:], rhs=bt["t1"][:, t0:t0 + tn], start=True, stop=True), reads=["bones", "b_t1"], writes=["ps0"])
                    K.op("act", lambda e, t0=t0, tn=tn: e.activation(bt["t2"][:, t0:t0 + tn], ps[0][:, 0:tn], AF.Sqrt, bias=eps_t[:, 1:2]), reads=["ps0", "eps"], writes=["b_t2"])
                K.op("dve", lambda e, bn=bn: e.reciprocal(bt["t2"][:, 0:bn], bt["t2"][:, 0:bn]), reads=["b_t2"], writes=["b_t2"])
                K.op("dve", lambda e, bn=bn: e.tensor_tensor(bt["kap"][:, 0:bn], bt["kap"][:, 0:bn], bt["t2"][:, 0:bn], op=ALU.mult), reads=["b_kap", "b_t2"], writes=["b_kap"])
                for n in range((bn + 511) // 512):
                    t0 = n * 512; tn = min(512, bn - t0)
                    K.op("pe", lambda e, t0=t0, tn=tn: e.matmul(ps[1][:, 0:tn], lhsT=wlb[:, 2, ch], rhs=lbt[:, 2, t0:t0 + tn], start=True, stop=True), reads=["wlb", "b_lora"], writes=["ps1"])
                    K.op("act", lambda e, t0=t0, tn=tn: e.copy(gateT[:, b0 + t0:b0 + t0 + tn], ps[1][:, 0:tn]), reads=["ps1"], writes=["gateT"])
                first_rk = True
                for d_ in range(2):
                    ds = slice(d_ * 64, (d_ + 1) * 64)
                    w0c = pk2[:, PK2["W0"] + d_ * 4 + P:PK2["W0"] + d_ * 4 + P + 1]
                    a0c = pk2[:, PK2["A0"] + d_ * 4 + P:PK2["A0"] + d_ * 4 + P + 1]
                    for n in range((bn + 511) // 512):
                        t0 = n * 512; tn = min(512, bn - t0)
                        K.op("pe", lambda e, t0=t0, tn=tn, ds=ds: e.matmul(ps[2][:, 0:tn], lhsT=wlb[ds, 0, ch], rhs=lbt[ds, 0, t0:t0 + tn], start=True, stop=True), reads=["wlb", "b_lora"], writes=["ps2"])
                        K.op("act", lambda e, t0=t0, tn=tn, w0c=w0c: e.activation(bt["sig"][:, t0:t0 + tn], ps[2][:, 0:tn], AF.Sigmoid, bias=w0c), reads=["ps2", "pk2"], writes=["b_sig"])
                        K.op("pe", lambda e, t0=t0, tn=tn, ds=ds: e.matmul(ps[3][:, 0:tn], lhsT=wlb[ds, 1, ch], rhs=lbt[ds, 1, t0:t0 + tn], start=True, stop=True), reads=["wlb", "b_lora"], writes=["ps3"])
                        K.op("act", lambda e, t0=t0, tn=tn, a0c=a0c: e.activation(bt["a"][:, t0:t0 + tn], ps[3][:, 0:tn], AF.Sigmoid, bias=a0c), reads=["ps3", "pk2"], writes=["b_a"])
                    K.op("dve", lambda e, bn=bn: e.tensor_tensor_scan(bt["cum"][:, 0:bn], rmask[:, 0:bn], bt["sig"][:, 0:bn], 0.0, op0=ALU.mult, op1=ALU.add),
                         reads=["rmask", "b_sig"], writes=["b_cum"])
                    c3 = bt["cum"][:, 0:bn].rearrange("p (n t) -> p n t", t=128)
                    tot_b = c3[:, :, 127:128].to_broadcast([128, ntb, 128])
                    K.op("act", lambda e, d_=d_, c3=c3, ntb=ntb: e.activation(Lam[d_][:, b0 // 128:b0 // 128 + ntb], c3[:, :, 127], AF.Exp, scale=-CW_),
                         reads=["b_cum"], writes=["Lam%d" % d_])
                    if d_ == 1:
                        K.op("dve", lambda e, bn=bn, c3=c3, tot_b=tot_b, ntb=ntb: e.tensor_tensor(
                            bt["t1"][:, 0:bn].rearrange("p (n t) -> p n t", t=128), tot_b, c3, op=ALU.subtract), reads=["b_cum"], writes=["b_t1"])
                        K.op("dve", lambda e, bn=bn: e.tensor_tensor(bt["cum"][:, 0:bn], bt["t1"][:, 0:bn], bt["sig"][:, 0:bn], op=ALU.add),
                             reads=["b_t1", "b_sig"], writes=["b_cum"])
                    K.op("act", lambda e, bn=bn: e.activation(bt["w"][:, 0:bn], bt["cum"][:, 0:bn], AF.Exp, scale=-CW_), reads=["b_cum"], writes=["b_w"])
                    K.op("act", lambda e, bn=bn: e.activation(bt["iw"][:, 0:bn], bt["cum"][:, 0:bn], AF.Exp, scale=CW_), reads=["b_cum"], writes=["b_iw"])
                    K.op("pool", lambda e, bn=bn: e.tensor_tensor(bt["t1"][:, 0:bn], bt["cum"][:, 0:bn], bt["sig"][:, 0:bn], op=ALU.subtract), reads=["b_cum", "b_sig"], writes=["b_t1"])
                    K.op("act", lambda e, bn=bn: e.activation(bt["wp"][:, 0:bn], bt["t1"][:, 0:bn], AF.Exp, scale=-CW_), reads=["b_t1"], writes=["b_wp"])
                    K.op("dve", lambda e, d_=d_, bn=bn: e.tensor_tensor(rT[d_][:, bs], bt["r"][:, 0:bn], bt["w"][:, 0:bn], op=ALU.mult), reads=["b_r", "b_w"], writes=["rT%d" % d_])
                    K.op("pool", lambda e, d_=d_, bn=bn: e.tensor_tensor(kpT[d_][:, bs], bt["kap"][:, 0:bn], bt["wp"][:, 0:bn], op=ALU.mult), reads=["b_kap", "b_wp"], writes=["kpT%d" % d_])
                    kac = pk2[:, PK2["KA"] + P:PK2["KA"] + P + 1]
                    K.op("dve", lambda e, bn=bn, kac=kac: e.tensor_scalar(bt["t1"][:, 0:bn], bt["a"][:, 0:bn], -1.0, kac, op0=ALU.add, op1=ALU.mult), reads=["b_a", "pk2"], writes=["b_t1"])
                    K.op("dve", lambda e, bn=bn: e.scalar_tensor_tensor(bt["t1"][:, 0:bn], bt["t1"][:, 0:bn], 1.0, bt["k"][:, 0:bn], op0=ALU.add, op1=ALU.mult), reads=["b_t1", "b_k"], writes=["b_t1"])
                    K.op("dve", lambda e, d_=d_, bn=bn: e.tensor_tensor(ktT[d_][:, bs], bt["t1"][:, 0:bn], bt["iw"][:, 0:bn], op=ALU.mult), reads=["b_t1", "b_iw"], writes=["ktT%d" % d_])
                    if first_rk:
                        K.op("pool", lambda e, bn=bn: e.tensor_tensor(bt["rk"][:, 0:bn], bt["t1"][:, 0:bn], bt["r"][:, 0:bn], op=ALU.mult), reads=["b_t1", "b_r"], writes=["b_rk"])
                        first_rk = False
                    else:
                        K.op("pool", lambda e, bn=bn: e.tensor_tensor(bt["t2"][:, 0:bn], bt["t1"][:, 0:bn], bt["r"][:, 0:bn], op=ALU.mult), reads=["b_t1", "b_r"], writes=["b_t2"])
                        K.op("pool", lambda e, bn=bn: e.tensor_tensor(bt["rk"][:, 0:bn], bt["rk"][:, 0:bn], bt["t2"][:, 0:bn], op=ALU.add), reads=["b_rk", "b_t2"], writes=["b_rk"])
                    K.op("dve", lambda e, bn=bn: e.scalar_tensor_tensor(bt["t2"][:, 0:bn], bt["kap"][:, 0:bn], -1.0, bt["a"][:, 0:bn], op0=ALU.mult, op1=ALU.mult), reads=["b_kap", "b_a"], writes=["b_t2"])
                    K.op("dve", lambda e, d_=d_, bn=bn: e.tensor_tensor(nbT[d_][:, bs], bt["t2"][:, 0:bn], bt["iw"][:, 0:bn], op=ALU.mult), reads=["b_t2", "b_iw"], writes=["nbT%d" % d_])
                rkc = pk2[:, PK2["RK"] + P:PK2["RK"] + P + 1]
                K.op("dve", lambda e, bn=bn, rkc=rkc: e.tensor_scalar(bt["rk"][:, 0:bn], bt["rk"][:, 0:bn], rkc, None, op0=ALU.mult), reads=["b_rk", "pk2"], writes=["b_rk"])
                for n in range((bn + 511) // 512):
                    t0 = n * 512; tn = min(512, bn - t0)
                    K.op("pe", lambda e, t0=t0, tn=tn: e.matmul(ps[4][:, 0:tn], lhsT=bones[:], rhs=bt["rk"][:, t0:t0 + tn], start=True, stop=True), reads=["bones", "b_rk"], writes=["ps4"])
                    K.op("dve", lambda e, t0=t0, tn=tn: e.tensor_tensor(bonus[:, b0 + t0:b0 + t0 + tn], ps[4][:, 0:tn], bt["v"][:, t0:t0 + tn], op=ALU.mult), reads=["ps4", "b_v"], writes=["bonus"])
            stop_at("rw_prep")
            for d_ in range(2):
                K.op("pool", lambda e, d_=d_: e.memset(Z[d_][:], 0.0), writes=["Z%d" % d_])
                K.op("pool", lambda e, d_=d_: e.memset(Zb[d_][:], 0.0), writes=["Zb%d" % d_])
            def r1(step, d_):
                i = fwd_order[step] if d_ == 0 else bwd_order[step]
                sl = slice(i * 128, (i + 1) * 128)
                want_o = i >= 2
                dk_ = "%d" % d_
                b_ = d_ * 4
                for hh in range(2):
                    hs = slice(hh * 64, (hh + 1) * 64)
                    pt = ps[b_ + hh]; pk = "ps%d" % (b_ + hh)
                    for qq, src in enumerate((ktT, nbT)):
                        K.op("pe", lambda e, src=src, pt=pt, qq=qq, hs=hs, d_=d_, sl=sl: e.matmul(pt[:, qq * 256:qq * 256 + 128], lhsT=src[d_][hs, sl], rhs=kpT[d_][hs, sl], start=True, stop=True),
                             reads=["ktT" + dk_, "nbT" + dk_, "kpT" + dk_], writes=[pk])
                        K.op("pe", lambda e, src=src, pt=pt, qq=qq, hs=hs, d_=d_, sl=sl: e.matmul(pt[:, qq * 256 + 128:qq * 256 + 256], lhsT=src[d_][hs, sl], rhs=rT[d_][hs, sl], start=True, stop=True),
                             reads=["ktT" + dk_, "nbT" + dk_, "rT" + dk_], writes=[pk])
                for hh in range(2):
                    K.op("dve", lambda e, d_=d_, hh=hh: e.tensor_tensor(AK[d_][:, hh * 256:(hh + 1) * 256], ps[d_ * 4 + hh][:, 0:256], mask4[d_][:, 0:256], op=ALU.mult),
                         reads=["ps%d" % (b_ + hh), "mask4"], writes=["AK" + dk_])
                    K.op("dve", lambda e, d_=d_, hh=hh: e.tensor_tensor(ANB[d_][:, hh * 256:(hh + 1) * 256], ps[d_ * 4 + hh][:, 256:512], mask4[d_][:, 0:256], op=ALU.mult),
                         reads=["ps%d" % (b_ + hh), "mask4"], writes=["ANB" + dk_])
                pT2 = psb[b_ + 2]; kT2 = "ps%d" % (b_ + 2)
                K.op("pe", lambda e, d_=d_, sl=sl, pT2=pT2: e.transpose(pT2[:, 0:128], ktT[d_][:, sl], identb[:]), reads=["ktT" + dk_, "identb"], writes=[kT2])
                K.op("pe", lambda e, d_=d_, sl=sl, pT2=pT2: e.transpose(pT2[:, 128:256], nbT[d_][:, sl], identb[:]), reads=["nbT" + dk_, "identb"], writes=[kT2])
                K.op("act", lambda e, d_=d_, pT2=pT2: e.copy(ktok_[d_][:], pT2[:, 0:128]), reads=[kT2], writes=["rktok" + dk_])
                K.op("act", lambda e, d_=d_, pT2=pT2: e.copy(nbtok_[d_][:], pT2[:, 128:256]), reads=[kT2], writes=["rnbtok" + dk_])
            def r2(step, d_, hh):
                dk_ = "%d" % d_
                tag = "n%d" % (d_ * 2 + hh)
                bank = d_ * 4 + hh
                kB2 = "ps%d" % bank
                K.op("pool", lambda e: e.tensor_copy(YY[d_][hh][:], ANB[d_][:, hh * 256:hh * 256 + 128]), reads=["ANB" + dk_], writes=[tag + "Y"])
                pB = (psb if RW_NM[0] == "bf16" else ps)[bank]
                idn_ = identb if RW_NM[0] == "bf16" else ident
                K.op("pe", lambda e: e.transpose(pB[:, 0:128], YY[d_][hh][:], idn_[:]), reads=[tag + "Y", "identb", "ident"], writes=[kB2])
                K.op("act", lambda e: e.copy(YYt[d_][hh][:], pB[:, 0:128]), reads=[kB2], writes=[tag + "Yt"])
                Ai, kAi = neumann(YY[d_][hh], YYt[d_][hh], tag, bank, pb=(bank, 384))
                K.op("dve", lambda e: e.tensor_copy(AinvS[d_][hh][:], Ai), reads=[kAi], writes=["Ainv%d%d" % (d_, hh)])

            def r3(step, d_):
                i = fwd_order[step] if d_ == 0 else bwd_order[step]
                sl = slice(i * 128, (i + 1) * 128)
                want_o = i >= 2
                dk_ = "%d" % d_
                b_ = d_ * 4
                pC = ps[b_ + 3]; kC = "ps%d" % (b_ + 3)
                for hh in range(2):
                    K.op("pe", lambda e, d_=d_, hh=hh, i=i, pC=pC: e.matmul(pC[:, hh * 64:(hh + 1) * 64], lhsT=AK[d_][:, hh * 256:hh * 256 + 128], rhs=vtk[:, i, hh * 64:(hh + 1) * 64], start=(hh == 0), stop=False),
                         reads=["AK" + dk_, "rvtok"], writes=[kC])
                K.op("pe", lambda e, d_=d_, sl=sl, pC=pC: e.matmul(pC[:, 0:128], lhsT=kpT[d_][:, sl], rhs=Zb[d_][:], start=False, stop=True), reads=["kpT" + dk_, "Zb" + dk_], writes=[kC])
                K.op("act", lambda e, d_=d_, pC=pC: e.copy(P1b[d_][:], pC[:, 0:128]), reads=[kC], writes=["P1b" + dk_])
                for hh in range(2):
                    K.op("pe", lambda e, d_=d_, hh=hh, pC=pC: e.matmul(pC[:, 128 + hh * 64:128 + (hh + 1) * 64], lhsT=AinvS[d_][hh][:], rhs=P1b[d_][:, hh * 64:(hh + 1) * 64], start=True, stop=True),
                         reads=["Ainv%d%d" % (d_, hh), "P1b" + dk_], writes=[kC])
                K.op("dve", lambda e, d_=d_, pC=pC: e.tensor_copy(Ub[d_][:], pC[:, 128:256]), reads=[kC], writes=["Ub" + dk_])
                if want_o:
                    for hh in range(2):
                        K.op("pe", lambda e, d_=d_, hh=hh, i=i, pC=pC: e.matmul(pC[:, 256 + hh * 64:256 + (hh + 1) * 64], lhsT=AK[d_][:, hh * 256 + 128:hh * 256 + 256], rhs=vtk[:, i, hh * 64:(hh + 1) * 64], start=(hh == 0), stop=False),
                             reads=["AK" + dk_, "rvtok"], writes=[kC])
                    K.op("pe", lambda e, d_=d_, sl=sl, pC=pC: e.matmul(pC[:, 256:384], lhsT=rT[d_][:, sl], rhs=Zb[d_][:], start=False, stop=False), reads=["rT" + dk_, "Zb" + dk_], writes=[kC])
                    for hh in range(2):
                        K.op("pe", lambda e, d_=d_, hh=hh, pC=pC: e.matmul(pC[:, 256 + hh * 64:256 + (hh + 1) * 64], lhsT=ANB[d_][:, hh * 256 + 128:hh * 256 + 256], rhs=Ub[d_][:, hh * 64:(hh + 1) * 64], start=False, stop=(hh == 1)),
                             reads=["ANB" + dk_, "Ub" + dk_], writes=[kC])
                    K.op("act", lambda e, d_=d_, i=i, pC=pC: e.copy(ybuf[d_][:, i, :], pC[:, 256:384]), reads=[kC], writes=["ybuf" + dk_])
                pD = ps[b_ + 2]; kD = "ps%d" % (b_ + 2)
                K.op("pe", lambda e, d_=d_, i=i, pD=pD: e.matmul(pD[:, 256:384], lhsT=ktok_[d_][:], rhs=vtk[:, i, :], start=True, stop=False), reads=["rktok" + dk_, "rvtok"], writes=[kD])
                K.op("pe", lambda e, d_=d_, pD=pD: e.matmul(pD[:, 256:384], lhsT=nbtok_[d_][:], rhs=Ub[d_][:], start=False, stop=True), reads=["rnbtok" + dk_, "Ub" + dk_], writes=[kD])
                K.op("dve", lambda e, d_=d_, pD=pD: e.tensor_tensor(zt[d_][:], pD[:, 256:384], Z[d_][:], op=ALU.add), reads=[kD, "Z" + dk_], writes=["zt" + dk_])
                K.op("dve", lambda e, d_=d_, i=i: e.scalar_tensor_tensor(Z[d_][:], zt[d_][:], Lam[d_][:, i:i + 1], bones[:], op0=ALU.mult, op1=ALU.mult),
                     reads=["zt" + dk_, "Lam" + dk_, "bones"], writes=["Z" + dk_])
                K.op("act", lambda e, d_=d_: e.copy(Zb[d_][:], Z[d_][:]), reads=["Z" + dk_], writes=["Zb" + dk_])
            for step in range(NT):
                for d_ in K.streams(2):
                    r1(step, d_)
                for q_ in K.streams(4):
                    r2(step, q_ // 2, q_ % 2)
                for d_ in K.streams(2):
                    r3(step, d_)
            stop_at("rw_scan")
            gwc = pk2[:, PK2["GNW"] + P:PK2["GNW"] + P + 1]
            gbc = pk2[:, PK2["GNB"] + P:PK2["GNB"] + P + 1]
            for i in range(2, NT):
                K.op("dve", lambda e, i=i: e.tensor_tensor(yo[:], ybuf[0][:, i, :], ybuf[1][:, i, :], op=ALU.add), reads=["ybuf0", "ybuf1"], writes=["yo"])
                y3 = yo[:].rearrange("p (h c) -> p h c", c=64)
                K.op("dve", lambda e, y3=y3: e.reduce_sum(yst[:, 0:2], y3, axis=AX.X), reads=["yo"], writes=["yst0"])
                K.op("dve", lambda e: e.tensor_scalar(yst[:, 2:4], yst[:, 0:2], 1.0 / 64, None, op0=ALU.mult), reads=["yst0"], writes=["yst1"])
                K.op("dve", lambda e, y3=y3: e.tensor_tensor(yc[:].rearrange("p (h c) -> p h c", c=64), y3, yst[:, 2:4].unsqueeze(2).to_broadcast([128, 2, 64]), op=ALU.subtract),
                     reads=["yo", "yst1"], writes=["yc"])
                K.op("act", lambda e: e.activation(ysq[:], yc[:], AF.Square), reads=["yc"], writes=["ysq"])
                K.op("dve", lambda e: e.reduce_sum(yst[:, 4:6], ysq[:].rearrange("p (h c) -> p h c", c=64), axis=AX.X), reads=["ysq"], writes=["yst2"])
                K.op("act", lambda e: e.activation(yst[:, 6:8], yst[:, 4:6], AF.Sqrt, bias=eps_t[:, 2:3], scale=1.0 / 64), reads=["yst2", "eps"], writes=["yst3"])
                K.op("dve", lambda e: e.reciprocal(yst[:, 6:8], yst[:, 6:8]), reads=["yst3"], writes=["yst3"])
                K.op("dve", lambda e: e.tensor_tensor(yt2[:].rearrange("p (h c) -> p h c", c=64), yc[:].rearrange("p (h c) -> p h c", c=64),
                                                      yst[:, 6:8].unsqueeze(2).to_broadcast([128, 2, 64]), op=ALU.mult), reads=["yc", "yst3"], writes=["yt2"])
                pt = ps[i % 2]; pk = "ps%d" % (i % 2)
                K.op("pe", lambda e, pt=pt: e.transpose(pt[:, 0:128], yt2[:], ident[:]), reads=["yt2", "ident"], writes=[pk])
                K.op("act", lambda e, pt=pt: e.activation(yc[:], pt[:, 0:128], AF.Identity, bias=gbc, scale=gwc), reads=[pk, "pk2", "yc"], writes=["yc"])
                K.op("dve", lambda e, i=i: e.tensor_tensor(yc[:], yc[:], bonus[:, i * 128:(i + 1) * 128], op=ALU.add), reads=["yc", "bonus"], writes=["yc"])
                K.op("dve", lambda e, i=i: e.tensor_tensor(rmst[:, (i - 2) * 128:(i - 1) * 128], yc[:], gateT[:, i * 128:(i + 1) * 128], op=ALU.mult), reads=["yc", "gateT"], writes=["rmst"])
            K.dma(mT_d[4 + P, :, :], rmst[:, :], reads=["rmst"], writes=[("mT", 4 + P)], key="st_rmst")
    K.barrier()

    stop_at("mix_done")
    with ExitStack() as pp_:
        mTs = [sb("mTs%d" % i_, [128, 8, 128], BF16, stack=pp_) for i_ in range(2)]
        woutb = sb("woutb", [128, 8, D], BF16, stack=pp_)
        wqb = sb("wqb", [128, 8, 2048], BF16, stack=pp_)
        skT = sb("skT", [128, 16, 128], BF16, stack=pp_)
        with ExitStack() as pset:
            wstg = sb("wstg", [128, 8, 512], stack=pset)
            skst = sb("skst", [128, 16, 128], stack=pset)
            for hf in range(2):
                K.dma(wstg[:, :, :], wout_d[:, hf * 512:(hf + 1) * 512].rearrange("(k p) c -> p k c", p=128), writes=["wstg"], key="wstg")
                K.op("pool", lambda e, hf=hf: e.tensor_copy(woutb[:, :, hf * 512:(hf + 1) * 512], wstg[:]), reads=["wstg"], writes=["woutb"])
            for hf in range(4):
                K.dma(wstg[:, :, :], wq_d[:, hf * 512:(hf + 1) * 512].rearrange("(k p) c -> p k c", p=128), writes=["wstg"], key="wstg")
                K.op("pool", lambda e, hf=hf: e.tensor_copy(wqb[:, :, hf * 512:(hf + 1) * 512], wstg[:]), reads=["wstg"], writes=["wqb"])
            K.dma(skst[:, :, :], sk_d[:, :, :].rearrange("g k d -> k g d"), writes=["skst"], key="skst")
            for g in range(16):
                pt = ps[g % 2]; pk = "ps%d" % (g % 2)
                K.op("pe", lambda e, g=g, pt=pt: e.transpose(pt[:, 0:128], skst[:, g, :], ident[:]), reads=["skst", "ident"], writes=[pk])
                K.op("act", lambda e, g=g, pt=pt: e.copy(skT[:, g, :], pt[:, 0:128]), reads=[pk], writes=["skT"])
            K.barrier()
        gtB = sb("gtB2", [128, 4, D], stack=pp_)
        K.dma(gtB[:].rearrange("p q d -> p (q d)"), gt_d[:, :], reads=["gt_d"], writes=["gtB"], key="gtB2")
        comb_d = nc.dram_tensor("comb_s", [16384, 2 * D], BF16).ap()
        with ExitStack() as pcv:
            cst = [sb("cst%d" % i_, [128, 2, D], stack=pcv) for i_ in range(2)]
            cbf = [sb("cbf%d" % i_, [128, 2 * D], BF16, stack=pcv) for i_ in range(2)]
            for c_ in range(128):
                st_ = cst[c_ % 2]; bf_ = cbf[c_ % 2]
                sk_ = "cst%d" % (c_ % 2); bk_ = "cbf%d" % (c_ % 2)
                K.dma(st_[:, 0, :], down_d[c_ * 128:(c_ + 1) * 128, :], writes=[sk_], key=sk_)
                K.dma(st_[:, 1, :], up_d[c_ * 128:(c_ + 1) * 128, :], writes=[sk_], key=sk_)
                K.op("act", lambda e, st_=st_, bf_=bf_: e.copy(bf_[:, 0:D], st_[:, 0, :]), reads=[sk_], writes=[bk_])
                K.op("dve", lambda e, st_=st_, bf_=bf_: e.tensor_copy(bf_[:, D:2 * D], st_[:, 1, :]), reads=[sk_], writes=[bk_])
                K.dma(comb_d[c_ * 128:(c_ + 1) * 128, :], bf_[:, :], reads=[bk_], writes=["comb"], key="st_" + bk_)
            K.barrier()
        A2row = sb("A2row", [128, D], stack=pp_)
        fgB = sb("fgB", [128, D], stack=pp_)
        K.dma(A2row[:, :], g2row_d.partition_broadcast(128), writes=["A2row"], key="A2row")
        K.dma(fgB[:, :], fng_d.partition_broadcast(128), writes=["fgB"], key="fgB")
        K.op("dve", lambda e: e.scalar_tensor_tensor(A2row[:], gtB[:, 2, :], 1.0, A2row[:], op0=ALU.add, op1=ALU.mult), reads=["gtB", "A2row"], writes=["A2row"])
        iota16i = sb("iota16i", [128, 16], I32, stack=pp_)
        iota16 = sb("iota16", [128, 16], stack=pp_)
        K.op("pool", lambda e: e.iota(iota16i[:], pattern=[[1, 16]], base=0, channel_multiplier=0), writes=["iota16i"])
        K.op("dve", lambda e: e.tensor_copy(iota16[:], iota16i[:]), reads=["iota16i"], writes=["iota16"])

        xt_ = sb("p_xt", [128, D], stack=pp_)
        x1 = sb("p_x1", [128, D], stack=pp_)
        h2 = sb("p_h2", [128, D], stack=pp_)
        yacc = sb("p_y", [128, D], stack=pp_)
        pj = sb("p_junk", [128, D], stack=pp_)
        pss = sb("p_ss", [128, 1], stack=pp_)
        prs = sb("p_rs", [128, 2], stack=pp_)
        pxs = sb("p_xs", [128, D], stack=pp_)
        h2T = sb("p_h2T", [128, 8, 128], BF16, stack=pp_)
        qTs = sb("p_qT", [128, 16, 128], BF16, stack=pp_)
        scs = sb("p_sc", [128, 16, 128], stack=pp_)
        tmp1 = sb("p_tmp1", [128, 16, 128], stack=pp_)
        tv = sb("p_tv", [128, 16, 16], stack=pp_)
        tiu = sb("p_tiu", [128, 16, 16], U32, stack=pp_)
        tif = sb("p_tif", [128, 16, 16], stack=pp_)
        cand = sb("p_cand", [128, 8, 256], stack=pp_)
        tmp2 = sb("p_tmp2", [128, 8, 256], stack=pp_)
        eq = tmp2
        bsv = sb("p_bs", [128, 8, 16], stack=pp_)
        posu = sb("p_posu", [128, 8, 16], U32, stack=pp_)
        pau = sb("p_pau", [128, 8, 16], U32, stack=pp_)
        pbu = sb("p_pbu", [128, 8, 16], U32, stack=pp_)
        paf = sb("p_paf", [128, 8, 16], stack=pp_)
        pbf = sb("p_pbf", [128, 8, 16], stack=pp_)
        i0f = sb("p_i0f", [128, 8, 16], stack=pp_)
        i1f = sb("p_i1f", [128, 8, 16], stack=pp_)
        eidx = sb("p_eidx", [128, 128], U32, stack=pp_)
        gat = sb("p_gate", [128, 8, 16], stack=pp_)
        gsum = sb("p_gsum", [128, 8], stack=pp_)
        apre = sb("p_apre", [128, 128], stack=pp_)
        coef = sb("p_coef", [128, 128], stack=pp_)
        NRB = 8
        rowc = [sb("p_rowc%d" % i, [128, 2 * D], BF16, stack=pp_) for i in range(NRB)]
        pjb = sb("p_junkb", [128, D], BF16, stack=pp_)
        h2b = sb("p_h2b", [128, D], BF16, stack=pp_)
        dgs = [sb("p_dg%d" % i, [128, 128], BF16, stack=pp_) for i in range(4)]
        gflat = sb("p_gflat", [128, 128], stack=pp_)
        NEG = -1.0e30

        for i in range(NTL):
            tsl = slice(i * 128, (i + 1) * 128)
            K.dma(xt_[:, :], x_d[tsl, :], writes=["p_xt"], key="p_xt")
            mTt = mTs[i % 2]; mk_ = "mTs%d" % (i % 2)
            K.dma(mTt[:, :, :], mT_d[:, :, tsl].rearrange("k p t -> p k t"), reads=[("mT", j) for j in range(8)], writes=[mk_], key=mk_)
            for hf in range(2):
                pt = ps[hf]; pk = "ps%d" % hf
                for k in range(8):
                    K.op("pe", lambda e, k=k, hf=hf, pt=pt, mTt=mTt: e.matmul(pt[:, :], lhsT=mTt[:, k, :], rhs=woutb[:, k, hf * 512:(hf + 1) * 512], start=(k == 0), stop=(k == 7)),
                         reads=[mk_, "woutb"], writes=[pk])
                K.op("dve", lambda e, hf=hf, pt=pt: e.tensor_tensor(x1[:, hf * 512:(hf + 1) * 512], pt[:, :], gtB[:, 0, hf * 512:(hf + 1) * 512], op=ALU.mult),
                     reads=[pk, "gtB"], writes=["p_x1"])
            K.op("pool", lambda e: e.tensor_tensor(x1[:], x1[:], xt_[:], op=ALU.add), reads=["p_x1", "p_xt"], writes=["p_x1"])
            K.op("act", lambda e: e.activation(pj[:], x1[:], AF.Square), reads=["p_x1"], writes=["p_junk"])
            K.op("dve", lambda e: e.reduce_sum(pss[:, 0:1], pj[:], axis=AX.X), reads=["p_junk"], writes=["p_ss"])
            K.op("act", lambda e: e.activation(prs[:, 0:1], pss[:, 0:1], AF.Sqrt, bias=eps_t[:, 0:1], scale=1.0 / D), reads=["p_ss", "eps"], writes=["p_rs"])
            K.op("dve", lambda e: e.reciprocal(prs[:, 1:2], prs[:, 0:1]), reads=["p_rs"], writes=["p_rs2"])
            K.op("dve", lambda e: e.tensor_scalar(pxs[:], x1[:], prs[:, 1:2], None, op0=ALU.mult), reads=["p_x1", "p_rs2"], writes=["p_xs"])
            for hf in range(2):
                pt = ps[2 + hf]; pk = "ps%d" % (2 + hf)
                for kk in range(4):
                    k = hf * 4 + kk
                    K.op("pe", lambda e, k=k, kk=kk, pt=pt: e.transpose(pt[:, kk * 128:(kk + 1) * 128], pxs[:, k * 128:(k + 1) * 128], ident[:]), reads=["p_xs", "ident"], writes=[pk])
                for kk in range(4):
                    k = hf * 4 + kk
                    K.op("act", lambda e, k=k, kk=kk, pt=pt: e.activation(h2T[:, k, :], pt[:, kk * 128:(kk + 1) * 128], AF.Identity, bias=B2[:, k:k + 1], scale=A2[:, k:k + 1]),
                         reads=[pk, "mods"], writes=["p_h2T"])
            K.op("dve", lambda e: e.tensor_tensor(h2[:], pxs[:], A2row[:], op=ALU.mult), reads=["p_xs", "A2row"], writes=["p_h2"])
            K.op("pool", lambda e: e.tensor_tensor(h2[:], h2[:], gtB[:, 1, :], op=ALU.add), reads=["p_h2", "gtB"], writes=["p_h2"])
            for g in range(16):
                pt = ps[4 + (g % 2)]; pk = "ps%d" % (4 + (g % 2))
                for k in range(8):
                    K.op("pe", lambda e, g=g, k=k, pt=pt: e.matmul(pt[:, 0:128], lhsT=wqb[:, k, g * 128:(g + 1) * 128], rhs=h2T[:, k, :], start=(k == 0), stop=(k == 7)),
                         reads=["wqb", "p_h2T"], writes=[pk])
                K.op("act", lambda e, g=g, pt=pt: e.copy(qTs[:, g, :], pt[:, 0:128]), reads=[pk], writes=[("p_qT", g)])
            for g in range(16):
                pt = ps[6 + (g // 4) % 2]; pk = "ps%d" % (6 + (g // 4) % 2)
                K.op("pe", lambda e, g=g, pt=pt: e.matmul(pt[:, (g % 4) * 128:(g % 4 + 1) * 128], lhsT=qTs[:, g, :], rhs=skT[:, g, :], start=True, stop=True),
                     reads=[("p_qT", g), "skT"], writes=[pk])
                if g % 4 == 3:
                    K.op("dve", lambda e, g=g, pt=pt: e.tensor_copy(scs[:, g - 3:g + 1, :].rearrange("p g k -> p (g k)"), pt[:, :]), reads=[pk], writes=[("p_sc", g // 4)])
            for g in range(16):
                K.op("dve", lambda e, g=g: e.max(tv[:, g, 0:8], scs[:, g, :]), reads=[("p_sc", g // 4)], writes=[("tv", g)])
            for g in range(16):
                K.op("dve", lambda e, g=g: e.max_index(tiu[:, g, 0:8], tv[:, g, 0:8], scs[:, g, :]), reads=[("p_sc", g // 4), ("tv", g)], writes=[("tiu", g)])
            for g in range(16):
                K.op("dve", lambda e, g=g: e.match_replace(tmp1[:, g, :], tv[:, g, 0:8], scs[:, g, :], NEG), reads=[("p_sc", g // 4), ("tv", g)], writes=[("tmp1", g)])
            for g in range(16):
                K.op("dve", lambda e, g=g: e.max(tv[:, g, 8:16], tmp1[:, g, :]), reads=[("tmp1", g)], writes=[("tv2", g)])
            for g in range(16):
                K.op("dve", lambda e, g=g: e.max_index(tiu[:, g, 8:16], tv[:, g, 8:16], tmp1[:, g, :]), reads=[("tmp1", g), ("tv2", g)], writes=[("tiu2", g)])
            allg = [("tv", g) for g in range(16)] + [("tv2", g) for g in range(16)]
            alli = [("tiu", g) for g in range(16)] + [("tiu2", g) for g in range(16)]
            K.op("dve", lambda e: e.tensor_copy(tif[:], tiu[:]), reads=alli, writes=["p_tif"])
            tvv = tv[:].rearrange("p (h q) a -> p h q a", q=2)
            tfv = tif[:].rearrange("p (h q) a -> p h q a", q=2)
            c4 = cand[:].rearrange("p h (a b) -> p h a b", b=16)
            K.op("dve", lambda e: e.tensor_tensor(c4, tvv[:, :, 0, :].unsqueeze(3).to_broadcast([128, 8, 16, 16]),
                                                  tvv[:, :, 1, :].unsqueeze(2).to_broadcast([128, 8, 16, 16]), op=ALU.add), reads=allg, writes=["p_cand"])
            for hh in range(8):
                K.op("dve", lambda e, hh=hh: e.max(bsv[:, hh, 0:8], cand[:, hh, :]), reads=["p_cand"], writes=[("bs", hh)])
            for hh in range(8):
                K.op("dve", lambda e, hh=hh: e.max_index(posu[:, hh, 0:8], bsv[:, hh, 0:8], cand[:, hh, :]), reads=["p_cand", ("bs", hh)], writes=[("pos", hh)])
            for hh in range(8):
                K.op("dve", lambda e, hh=hh: e.match_replace(tmp2[:, hh, :], bsv[:, hh, 0:8], cand[:, hh, :], NEG), reads=["p_cand", ("bs", hh), "p_eq"], writes=[("tmp2", hh)])
            for hh in range(8):
                K.op("dve", lambda e, hh=hh: e.max(bsv[:, hh, 8:16], tmp2[:, hh, :]), reads=[("tmp2", hh)], writes=[("bs2", hh)])
            for hh in range(8):
                K.op("dve", lambda e, hh=hh: e.max_index(posu[:, hh, 8:16], bsv[:, hh, 8:16], tmp2[:, hh, :]), reads=[("tmp2", hh), ("bs2", hh)], writes=[("pos2", hh)])
            allb = [("bs", hh) for hh in range(8)] + [("bs2", hh) for hh in range(8)]
            allp = [("pos", hh) for hh in range(8)] + [("pos2", hh) for hh in range(8)]
            K.op("dve", lambda e: e.tensor_single_scalar(pau[:], posu[:], 4, op=ALU.logical_shift_right), reads=allp, writes=["p_pau"])
            K.op("dve", lambda e: e.tensor_single_scalar(pbu[:], posu[:], 15, op=ALU.bitwise_and), reads=allp, writes=["p_pbu"])
            K.op("dve", lambda e: e.tensor_copy(paf[:], pau[:]), reads=["p_pau"], writes=["p_paf"])
            K.op("dve", lambda e: e.tensor_copy(pbf[:], pbu[:]), reads=["p_pbu"], writes=["p_pbf"])
            e4 = eq[:].rearrange("p h (k a) -> p h k a", a=16)
            io4 = iota16[:].unsqueeze(1).unsqueeze(1).to_broadcast([128, 8, 16, 16])
            for (pf, q_, dst, nm) in ((paf, 0, i0f, "i0f"), (pbf, 1, i1f, "i1f")):
                K.op("dve", lambda e, pf=pf: e.tensor_tensor(e4, pf[:].unsqueeze(3).to_broadcast([128, 8, 16, 16]), io4, op=ALU.is_equal),
                     reads=["p_paf", "p_pbf", "iota16"], writes=["p_eq"] + [("tmp2", hh_) for hh_ in range(8)])
                K.op("dve", lambda e, q_=q_: e.tensor_tensor(e4, e4, tfv[:, :, q_, :].unsqueeze(2).to_broadcast([128, 8, 16, 16]), op=ALU.mult),
                     reads=["p_eq", "p_tif"], writes=["p_eq"])
                K.op("dve", lambda e, dst=dst: e.reduce_sum(dst[:], e4, axis=AX.X), reads=["p_eq"], writes=["p_" + nm])
            K.op("dve", lambda e: e.scalar_tensor_tensor(i0f[:], i0f[:], 128.0, i1f[:], op0=ALU.mult, op1=ALU.add), reads=["p_i0f", "p_i1f"], writes=["p_i0f"])
            K.op("dve", lambda e: e.tensor_copy(eidx[:], i0f[:].rearrange("p h k -> p (h k)")), reads=["p_i0f"], writes=["p_eidx"])
            K.op("dve", lambda e: e.tensor_tensor(gat[:], bsv[:], bsv[:, :, 0:1].to_broadcast([128, 8, 16]), op=ALU.subtract), reads=allb, writes=["p_gate"])
            K.op("act", lambda e: e.activation(gat[:], gat[:], AF.Exp), reads=["p_gate"], writes=["p_gate"])
            K.op("dve", lambda e: e.reduce_sum(gsum[:], gat[:], axis=AX.X), reads=["p_gate"], writes=["p_gsum"])
            K.op("dve", lambda e: e.reciprocal(gsum[:], gsum[:]), reads=["p_gsum"], writes=["p_gsum"])
            K.op("dve", lambda e: e.tensor_tensor(gat[:], gat[:], gsum[:].unsqueeze(2).to_broadcast([128, 8, 16]), op=ALU.mult), reads=["p_gate", "p_gsum"], writes=["p_gate"])
            K.op("act", lambda e: e.copy(h2b[:], h2[:]), reads=["p_h2"], writes=["p_h2b"])
            K.op("dve", lambda e: e.tensor_copy(gflat[:], gat[:].rearrange("p h k -> p (h k)")), reads=["p_gate"], writes=["p_gflat"])
            GRP = 4
            for g0 in range(0, 128, GRP):
                for kslot in range(g0, g0 + GRP):
                    rb = rowc[kslot % NRB]; rk_ = "p_rowc%d" % (kslot % NRB)
                    K.gather(rb[:, :], comb_d[:, :], eidx[:, kslot:kslot + 1], reads=["p_eidx", "comb"], writes=[rk_], key=rk_)
                    K.op("dve", lambda e, rb=rb, kslot=kslot: e.scalar_tensor_tensor(pjb[:], rb[:, 0:D], 1.0, h2b[:], op0=ALU.mult, op1=ALU.mult, accum_out=apre[:, kslot:kslot + 1]),
                         reads=[rk_, "p_h2b"], writes=["p_junkb", ("apre", g0 // GRP)])
                K.op("dve", lambda e, g0=g0: e.tensor_copy(coef[:, g0:g0 + GRP], apre[:, g0:g0 + GRP]), reads=[("apre", g0 // GRP)], writes=[("cf0", g0 // GRP)])
                K.op("act", lambda e, g0=g0: e.activation(coef[:, g0:g0 + GRP], coef[:, g0:g0 + GRP], AF.Gelu), reads=[("cf0", g0 // GRP)], writes=[("cf1", g0 // GRP)])
                K.op("dve", lambda e, g0=g0: e.tensor_tensor(coef[:, g0:g0 + GRP], coef[:, g0:g0 + GRP], gflat[:, g0:g0 + GRP], op=ALU.mult),
                     reads=[("cf1", g0 // GRP), "p_gflat"], writes=[("cf2", g0 // GRP)])
                for kslot in range(g0, g0 + GRP):
                    rb = rowc[kslot % NRB]; rk_ = "p_rowc%d" % (kslot % NRB)
                    dg = dgs[kslot % 4]; dk__ = "p_dg%d" % (kslot % 4)
                    K.op("act", lambda e, dg=dg, kslot=kslot: e.activation(dg[:], identb[:], AF.Identity, scale=coef[:, kslot:kslot + 1]),
                         reads=["identb", ("cf2", g0 // GRP)], writes=[dk__])
                    for hf in range(2):
                        K.op("pe", lambda e, dg=dg, rb=rb, hf=hf, kslot=kslot: e.matmul(ps[hf][:, :], lhsT=dg[:], rhs=rb[:, D + hf * 512:D + (hf + 1) * 512],
                                                                                       start=(kslot == 0), stop=(kslot == 127)),
                             reads=[dk__, rk_], writes=["ps%d" % hf])
            for hf in range(2):
                K.op("act", lambda e, hf=hf: e.copy(yacc[:, hf * 512:(hf + 1) * 512], ps[hf][:, :]), reads=["ps%d" % hf], writes=["p_y"])
            K.op("dve", lambda e: e.tensor_tensor(yacc[:], yacc[:], gtB[:, 3, :], op=ALU.mult), reads=["p_y", "gtB"], writes=["p_y"])
            K.op("pool", lambda e: e.tensor_tensor(yacc[:], yacc[:], x1[:], op=ALU.add), reads=["p_y", "p_x1"], writes=["p_y"])
            K.op("act", lambda e: e.activation(pj[:], yacc[:], AF.Square), reads=["p_y"], writes=["p_junk"])
            K.op("dve", lambda e: e.reduce_sum(pss[:, 0:1], pj[:], axis=AX.X), reads=["p_junk"], writes=["p_ss"])
            K.op("act", lambda e: e.activation(prs[:, 0:1], pss[:, 0:1], AF.Sqrt, bias=eps_t[:, 0:1], scale=1.0 / D), reads=["p_ss", "eps"], writes=["p_rs"])
            K.op("dve", lambda e: e.reciprocal(prs[:, 1:2], prs[:, 0:1]), reads=["p_rs"], writes=["p_rs2"])
            K.op("dve", lambda e: e.scalar_tensor_tensor(pxs[:], yacc[:], prs[:, 1:2], fgB[:], op0=ALU.mult, op1=ALU.mult), reads=["p_y", "p_rs2", "fgB"], writes=["p_xs"])
            K.dma(out_d[tsl, :], pxs[:, :], reads=["p_xs"], writes=["outdone"], key="st_out")
            if i == 0:
                stop_at("peer_t0")
    K.barrier()
    if "mT" in dbg:
        d_o = dbgt("mT", [8, 128, TL], BF16)
        K.dma(d_o[:, :, :], mT_d[:, :, :], reads=[("mT", j) for j in range(8)], writes=["dbgmT"], key="dbg")

    K.finish([k for k in K.st.keys() if (isinstance(k, str) and k.startswith("dbg")) or k == "outdone"])
    return dbg_out


def _inputs_for_core(inp, b, n_rows):
    TL = 64 * n_rows
    f = lambda a: np.ascontiguousarray(np.asarray(a, dtype=np.float32))
    m = {
        "x": f(inp["x"][b, :TL]),
        "c": f(inp["c"][b:b + 1]),
        "ctx": f(inp["ctx"][b]),
        "c_ctx": f(inp["c_ctx"][None, :]),
        "ada_w": f(inp["ada_w"][0]),
        "ada_b": f(inp["ada_b"][0].reshape(48, 128)),
        "ada_b_row": f(inp["ada_b"][0].reshape(1, 6144)),
        "norm1_g": f(inp["norm1_g"][0].reshape(8, 128)),
        "w_in": f(inp["w_in"][0]),
        "gdn_conv_w": f(inp["gdn_conv_w"][0].reshape(60, 128)),
        "gdn_a_log": f(inp["gdn_a_log"][0].reshape(1, 8)),
        "gdn_dt_bias": f(inp["gdn_dt_bias"][0].reshape(1, 8)),
        "gdn_norm_w": f(inp["gdn_norm_w"][0].reshape(1, 128)),
        "rwkv_mu": f(inp["rwkv_mu"][0].reshape(15, 128)),
        "rwkv_w0": f(inp["rwkv_w0"][0].reshape(8, 128)),
        "rwkv_w2": f(inp["rwkv_w2"][0].reshape(128, 512)),
        "rwkv_a0": f(inp["rwkv_a0"][0].reshape(8, 128)),
        "rwkv_a2": f(inp["rwkv_a2"][0].reshape(128, 512)),
        "rwkv_g2": f(inp["rwkv_g2"][0]),
        "rwkv_k_k": f(inp["rwkv_k_k"][0].reshape(4, 128)),
        "rwkv_k_a": f(inp["rwkv_k_a"][0].reshape(4, 128)),
        "rwkv_r_k": f(inp["rwkv_r_k"][0].reshape(4, 128)),
        "rwkv_gn_w": f(inp["rwkv_gn_w"][0].reshape(4, 128)),
        "rwkv_gn_b": f(inp["rwkv_gn_b"][0].reshape(4, 128)),
        "w_out": f(inp["w_out"][0]),
        "norm2_g": f(inp["norm2_g"][0].reshape(8, 128)),
        "peer_w_query": f(inp["peer_w_query"][0]),
        "peer_sub_keys": f(inp["peer_sub_keys"][0].reshape(16, 128, 128)),
        "peer_down": f(inp["peer_down"][0]),
        "peer_up": f(inp["peer_up"][0]),
        "final_norm_g": f(inp["final_norm_g"][None, :]),
        "norm2_g_row": f(inp["norm2_g"][0].reshape(1, 1024)),
    }
    return m


def run(inp, n_rows=64, cores=None, dbg=(), stop=None):
    nb = inp["x"].shape[0]
    cores = list(range(nb)) if cores is None else cores
    nc = bass.Bass("TRN2", target_bir_lowering=False)
    build(nc, n_rows=n_rows, dbg=dbg, stop=stop)
    in_maps = [_inputs_for_core(inp, b, n_rows) for b in cores]
    res = run_bass_kernel_spmd(nc, in_maps, core_ids=list(range(len(cores))))
    return res.results


def kernel(**inputs):
    res = run(inputs, n_rows=64)
    return np.stack([np.asarray(r["out"], dtype=np.float32) for r in res], axis=0)
```

```python
from contextlib import ExitStack
import numpy as np
import concourse.bass as bass
import concourse.mybir as mybir
from concourse.bass_utils import run_bass_kernel_spmd

F32 = mybir.dt.float32
BF16 = mybir.dt.bfloat16
I32 = mybir.dt.int32
U32 = mybir.dt.uint32
AF = mybir.ActivationFunctionType
ALU = mybir.AluOpType
AX = mybir.AxisListType

D = 1024
TC = 256
IN_COLS = 3984
GDN_COLS = 2064
NORM_EPS = 1e-6
L2_EPS = 1e-6
GN_EPS = 64e-5


class Ctx:
    def __init__(self, nc):
        self.nc = nc
        self.es = ExitStack()
        self.eng = dict(pe=nc.tensor, act=nc.scalar, dve=nc.vector, pool=nc.gpsimd, sp=nc.sync)
        self.csem = {}
        self.cnt = {}
        for e in ("pe", "act", "dve", "pool"):
            self.csem[e] = self.es.enter_context(nc.semaphore("cs_" + e))
            self.cnt[e] = 0
        self.dsem = {}
        self.seen = {e: {} for e in self.eng}
        self.st = {}
        self.ninst = 0
        self._rec = None

    def _sem(self, sk):
        if sk[0] == "c":
            return self.csem[sk[1]]
        return self.dsem[sk[1]][0]

    def _deps(self, reads, writes, e=None):
        need = {}

        def add(m):
            if m is None:
                return
            sk, v = m
            if need.get(sk, 0) < v:
                need[sk] = v

        for r in reads:
            s = self.st.get(r)
            if s is not None:
                add(s[0])
                if isinstance(r, str) and r.startswith("ps") and r[2:].isdigit():
                    for sk, v in s[1].items():
                        if sk != ("c", e):
                            add((sk, v))
        for w in writes:
            s = self.st.get(w)
            if s is not None:
                add(s[0])
                for sk, v in s[1].items():
                    add((sk, v))
        return need

    def _wait(self, e, need):
        eng = self.eng[e]
        seen = self.seen[e]
        for sk, v in need.items():
            if e == "pe" and sk == ("c", "pe"):
                continue
            if sk[0] == "d":
                v = max(v, self.dsem[sk[1]][1])
            if seen.get(sk, 0) >= v:
                continue
            eng.wait_ge(self._sem(sk), v)
            seen[sk] = v

    def _mark(self, mark, reads, writes):
        for w in writes:
            self.st[w] = [mark, {}]
        for r in reads:
            s = self.st.get(r)
            if s is None:
                s = self.st[r] = [None, {}]
            sk, v = mark
            if s[1].get(sk, 0) < v:
                s[1][sk] = v

    def streams(self, n):
        lists = []
        for d in range(n):
            self._rec = []
            yield d
            lists.append(self._rec)
            self._rec = None
        idx = [0] * n
        left = sum(len(l) for l in lists)
        while left:
            for d in range(n):
                if idx[d] < len(lists[d]):
                    kind, args, kw = lists[d][idx[d]]
                    idx[d] += 1
                    left -= 1
                    getattr(self, kind)(*args, **kw)

    def op(self, e, fn, reads=(), writes=()):
        if self._rec is not None:
            self._rec.append(("op", (e, fn, tuple(reads), tuple(writes)), {}))
            return
        need = self._deps(reads, writes, e)
        self._wait(e, need)
        ins = fn(self.eng[e])
        ins.then_inc(self.csem[e], 1)
        self.cnt[e] += 1
        self.ninst += 1
        self._mark((("c", e), self.cnt[e]), reads, writes)

    def dma(self, out, in_, reads=(), writes=(), key=None, q="sp", **kw):
        if self._rec is not None:
            self._rec.append(("dma", (out, in_, tuple(reads), tuple(writes), key, q), kw))
            return
        if key not in self.dsem:
            self.dsem[key] = [self.es.enter_context(self.nc.semaphore("ds_%d" % len(self.dsem))), 0]
        need = self._deps(reads, writes)
        self._wait(q, need)
        d = self.dsem[key]
        self.eng[q].dma_start(out=out, in_=in_, **kw).then_inc(d[0], 16)
        d[1] += 16
        self.ninst += 1
        self._mark((("d", key), d[1]), reads, writes)

    def gather(self, out, in_, idx_ap, reads=(), writes=(), key=None):
        if key not in self.dsem:
            self.dsem[key] = [self.es.enter_context(self.nc.semaphore("ds_%d" % len(self.dsem))), 0]
        need = self._deps(reads, writes)
        self._wait("pool", need)
        d = self.dsem[key]
        self.nc.gpsimd.indirect_dma_start(
            out=out, out_offset=None, in_=in_,
            in_offset=bass.IndirectOffsetOnAxis(ap=idx_ap, axis=0)).then_inc(d[0], 16)
        d[1] += 16
        self.ninst += 1
        self._mark((("d", key), d[1]), reads, writes)

    def barrier(self):
        need = {("c", e): v for e, v in self.cnt.items() if v > 0}
        for k, d in self.dsem.items():
            if d[1] > 0:
                need[("d", k)] = d[1]
        for e in self.eng:
            self._wait(e, need)

    def finish(self, keys):
        need = self._deps(keys, ())
        self._wait("sp", need)


NM_MODE = ["f32"]
RW_NM = ["bf16"]


class _Stop(Exception):
    pass


def build(nc, n_rows=64, dbg=(), stop=None):
    K = Ctx(nc)
    try:
        return _build(nc, K, n_rows, dbg, stop)
    except _Stop:
        K.barrier()
        return None


def _build(nc, K, n_rows, dbg, stop):
    def stop_at(name):
        if stop == name:
            raise _Stop()

    TL = 64 * n_rows
    T = TC + TL
    NT = T // 128
    NTL = TL // 128
    es = K.es

    def din(name, shape, dt=F32):
        return nc.dram_tensor(name, list(shape), dt, kind="ExternalInput").ap()

    x_d = din("x", [TL, D])
    c_d = din("c", [1, D])
    ctx_d = din("ctx", [TC, D])
    cctx_d = din("c_ctx", [1, D])
    adaw_d = din("ada_w", [D, 6144])
    adab_d = din("ada_b", [48, 128])
    adabr_d = din("ada_b_row", [1, 6144])
    g1_d = din("norm1_g", [8, 128])
    win_d = din("w_in", [D, IN_COLS])
    convw_d = din("gdn_conv_w", [60, 128])
    alog_d = din("gdn_a_log", [1, 8])
    dtb_d = din("gdn_dt_bias", [1, 8])
    gnorm_d = din("gdn_norm_w", [1, 128])
    mu_d = din("rwkv_mu", [15, 128])
    w0_d = din("rwkv_w0", [8, 128])
    w2_d = din("rwkv_w2", [128, 512])
    a0_d = din("rwkv_a0", [8, 128])
    a2_d = din("rwkv_a2", [128, 512])
    g2w_d = din("rwkv_g2", [128, 512])
    kk_d = din("rwkv_k_k", [4, 128])
    ka_d = din("rwkv_k_a", [4, 128])
    rk_d = din("rwkv_r_k", [4, 128])
    gnw_d = din("rwkv_gn_w", [4, 128])
    gnb_d = din("rwkv_gn_b", [4, 128])
    wout_d = din("w_out", [D, D])
    g2_d = din("norm2_g", [8, 128])
    wq_d = din("peer_w_query", [D, 2048])
    sk_d = din("peer_sub_keys", [16, 128, 128])
    down_d = din("peer_down", [16384, D])
    up_d = din("peer_up", [16384, D])
    fng_d = din("final_norm_g", [1, D])
    g2row_d = din("norm2_g_row", [1, D])
    out_d = nc.dram_tensor("out", [TL, D], F32, kind="ExternalOutput").ap()

    dbg_out = {}

    def dbgt(name, shape, dt=F32):
        dbg_out[name] = nc.dram_tensor("dbg_" + name, list(shape), dt, kind="ExternalOutput").ap()
        return dbg_out[name]

    pT_d = nc.dram_tensor("pT_s", [32, 128, T], F32).ap()

    def sb(name, shape, dt=F32, stack=es):
        return stack.enter_context(nc.sbuf_tensor(name, list(shape), dt))

    ps = [es.enter_context(nc.psum_tensor("ps%d" % i, [128, 512], F32)) for i in range(8)]

    dI = sb("dI", [128, 128], I32)
    ident = sb("ident", [128, 128])
    identb = sb("identb", [128, 128], BF16)
    ones = sb("ones", [128, 128])
    onesb = sb("onesb", [128, 128], BF16)
    m_lt = sb("m_lt", [128, 128])
    m_le = sb("m_le", [128, 128])
    m_gt = sb("m_gt", [128, 128])
    m_ge = sb("m_ge", [128, 128])
    e0 = sb("e0", [2, 128])
    K.op("pool", lambda e: e.iota(dI[:], pattern=[[1, 128]], base=0, channel_multiplier=-1), writes=["dI"])
    for t, op_, nm in ((ident, ALU.is_equal, "ident"), (m_lt, ALU.is_gt, "m_lt"), (m_le, ALU.is_ge, "m_le"),
                       (m_gt, ALU.is_lt, "m_gt"), (m_ge, ALU.is_le, "m_ge")):
        K.op("dve", lambda e, t=t, op_=op_: e.tensor_scalar(t[:], dI[:], 0.0, None, op0=op_), reads=["dI"], writes=[nm])
    K.op("dve", lambda e: e.tensor_copy(identb[:], ident[:]), reads=["ident"], writes=["identb"])
    K.op("pool", lambda e: e.memset(ones[:], 1.0), writes=["ones"])
    K.op("pool", lambda e: e.memset(onesb[:], 1.0), writes=["onesb"])
    e0i = sb("e0i", [2, 128], I32)
    K.op("pool", lambda e: e.iota(e0i[:], pattern=[[0, 128]], base=1, channel_multiplier=-1), writes=["e0i"])
    K.op("dve", lambda e: e.tensor_copy(e0[:], e0i[:]), reads=["e0i"], writes=["e0"])

    PK1 = dict(ADAB=0, G1=48, G2=56, CW=64)
    PK2 = dict(MU=0, W0=15, A0=23, KK=31, KA=35, RK=39, GNW=43, GNB=47, GNORM=51)
    pk1s = sb("pk1s", [128, 128])
    pk2s = sb("pk2s", [128, 128])
    pk1 = sb("pk1", [128, 128])
    pk2 = sb("pk2", [128, 128])
    K.op("pool", lambda e: e.memset(pk1s[:], 0.0), writes=["pk1s"])
    K.op("pool", lambda e: e.memset(pk2s[:], 0.0), writes=["pk2s"])
    for src, off, n in ((adab_d, 0, 48), (g1_d, 48, 8), (g2_d, 56, 8), (convw_d, 64, 60)):
        K.dma(pk1s[off:off + n, :], src[:, :], writes=["pk1s"], key="pk1s")
    for src, off, n in ((mu_d, 0, 15), (w0_d, 15, 8), (a0_d, 23, 8), (kk_d, 31, 4), (ka_d, 35, 4),
                        (rk_d, 39, 4), (gnw_d, 43, 4), (gnb_d, 47, 4), (gnorm_d, 51, 1)):
        K.dma(pk2s[off:off + n, :], src[:, :], writes=["pk2s"], key="pk2s")
    K.op("pe", lambda e: e.transpose(ps[0][:, 0:128], pk1s[:], ident[:]), reads=["pk1s", "ident"], writes=["ps0"])
    K.op("pe", lambda e: e.transpose(ps[0][:, 128:256], pk2s[:], ident[:]), reads=["pk2s", "ident"], writes=["ps0"])
    K.op("dve", lambda e: e.tensor_copy(pk1[:], ps[0][:, 0:128]), reads=["ps0"], writes=["pk1"])
    K.op("dve", lambda e: e.tensor_copy(pk2[:], ps[0][:, 128:256]), reads=["ps0"], writes=["pk2"])

    modT = sb("modT", [128, 48, 2])
    gt_d = nc.dram_tensor("gt_s", [128, 4 * D], F32).ap()
    A1 = sb("A1", [128, 8]); B1 = sb("B1", [128, 8])
    A1c = sb("A1c", [128, 8]); B1c = sb("B1c", [128, 8])
    A2 = sb("A2", [128, 8]); B2 = sb("B2", [128, 8])
    with ExitStack() as pa:
        cc = sb("cc", [2, D], stack=pa)
        scT = sb("scT", [128, 8, 2], stack=pa)
        aw = [sb("aw%d" % i, [128, 8, 1024], stack=pa) for i in range(2)]
        gtrow = sb("gtrow", [2, 4, D], stack=pa)
        gtB = sb("gtB", [128, 4, D], stack=pa)
        K.dma(cc[0:1, :], c_d[:, :], writes=["cc"], key="cc")
        K.dma(cc[1:2, :], cctx_d[:, :], writes=["cc"], key="cc")
        K.op("act", lambda e: e.activation(cc[:], cc[:], AF.Silu), reads=["cc"], writes=["cc"])
        for k in range(8):
            K.op("pe", lambda e, k=k: e.transpose(ps[1][:, 2 * k:2 * k + 2], cc[0:2, k * 128:(k + 1) * 128], ident[0:2, 0:2]),
                 reads=["cc", "ident"], writes=["ps1"])
        K.op("dve", lambda e: e.tensor_copy(scT[:].rearrange("p k c -> p (k c)"), ps[1][:, 0:16]), reads=["ps1"], writes=["scT"])
        K.op("pool", lambda e: e.memset(gtrow[:], 0.0), writes=["gtrow"])
        for q_ in range(4):
            K.dma(gtrow[0:1, q_, :], adabr_d[:, 2048 + q_ * 1024:3072 + q_ * 1024], writes=["gtrow"], key="gtrow")
        for g in range(6):
            a = aw[g % 2]
            an = "aw%d" % (g % 2)
            K.dma(a[:], adaw_d[:, g * 1024:(g + 1) * 1024].rearrange("(k p) c -> p k c", p=128), writes=[an], key=an)
            for j in range(8):
                col = (g * 8 + j) * 2
                for k in range(8):
                    K.op("pe", lambda e, a=a, j=j, k=k, col=col: e.matmul(
                        ps[2][:, col:col + 2], lhsT=a[:, k, j * 128:(j + 1) * 128], rhs=scT[:, k, :],
                        start=(k == 0), stop=(k == 7)), reads=[an, "scT"], writes=["ps2"])
            if g >= 2:
                q = g - 2
                for half in range(2):
                    for k in range(8):
                        K.op("pe", lambda e, a=a, k=k, half=half: e.matmul(
                            ps[3][0:2, :], lhsT=scT[:, k, :], rhs=a[:, k, half * 512:(half + 1) * 512],
                            start=(k == 0), stop=(k == 7)), reads=[an, "scT"], writes=["ps3"])
                    K.op("dve", lambda e, q=q, half=half: e.tensor_tensor(
                        gtrow[:, q, half * 512:(half + 1) * 512], ps[3][0:2, :], gtrow[:, q, half * 512:(half + 1) * 512], op=ALU.add),
                        reads=["ps3", "gtrow"], writes=["gtrow"])
                    K.op("pe", lambda e, q=q, half=half: e.matmul(
                        ps[4][:, :], lhsT=e0[:, :], rhs=gtrow[:, q, half * 512:(half + 1) * 512], start=True, stop=True),
                        reads=["e0", "gtrow"], writes=["ps4"])
                    K.op("act", lambda e, q=q, half=half: e.copy(gtB[:, q, half * 512:(half + 1) * 512], ps[4][:, :]),
                         reads=["ps4"], writes=["gtB"])
        K.op("dve", lambda e: e.tensor_tensor(
            modT[:], ps[2][:, 0:96].rearrange("p (j c) -> p j c", c=2),
            pk1[:, 0:48].unsqueeze(2).to_broadcast([128, 48, 2]), op=ALU.add), reads=["ps2", "pk1"], writes=["modT"])
        for (A, B, gname, sc0, sh0, col, nm) in ((A1, B1, "G1", 8, 0, 0, "1"), (A1c, B1c, "G1", 8, 0, 1, "1c"),
                                                 (A2, B2, "G2", 32, 24, 0, "2")):
            g0 = PK1[gname]
            K.op("dve", lambda e, A=A, g0=g0, sc0=sc0, col=col: e.scalar_tensor_tensor(
                A[:], modT[:, sc0:sc0 + 8, col], 1.0, pk1[:, g0:g0 + 8], op0=ALU.add, op1=ALU.mult),
                reads=["modT", "pk1"], writes=["A" + nm])
            K.op("dve", lambda e, B=B, sh0=sh0, col=col: e.tensor_copy(B[:], modT[:, sh0:sh0 + 8, col]),
                 reads=["modT"], writes=["B" + nm])

        K.dma(gt_d[:, :], gtB[:].rearrange("p q d -> p (q d)"), reads=["gtB"], writes=["gt_d"], key="st_gt")
        K.barrier()
    if "mod" in dbg:
        d_ = dbgt("mod", [128, 96])
        K.dma(d_[:, :], modT[:].rearrange("p j c -> p (j c)"), reads=["modT"], writes=["dbgmod"], key="dbg")

    def norm_tile(xt, xkey, A, B, hT, hkey, col0, pp, stage):
        junk, ss, rs, xs = stage
        K.op("act", lambda e: e.activation(junk[:], xt[:], AF.Square), reads=[xkey], writes=["n_junk"])
        K.op("dve", lambda e: e.reduce_sum(ss[:, 0:1], junk[:], axis=AX.X), reads=["n_junk"], writes=["n_ss"])
        K.op("act", lambda e: e.activation(rs[:, 0:1], ss[:, 0:1], AF.Sqrt, bias=eps_t[:, 0:1], scale=1.0 / D),
             reads=["n_ss", "eps"], writes=["n_rs"])
        K.op("dve", lambda e: e.reciprocal(rs[:, 1:2], rs[:, 0:1]), reads=["n_rs"], writes=["n_rs2"])
        K.op("dve", lambda e: e.tensor_scalar(xs[:], xt[:], rs[:, 1:2], None, op0=ALU.mult),
             reads=[xkey, "n_rs2"], writes=["n_xs"])
        for half in range(2):
            pt = ps[pp + half]
            pk = "ps%d" % (pp + half)
            for kk in range(4):
                k = half * 4 + kk
                K.op("pe", lambda e, k=k, kk=kk, pt=pt: e.transpose(pt[:, kk * 128:(kk + 1) * 128], xs[:, k * 128:(k + 1) * 128], ident[:]),
                     reads=["n_xs", "ident"], writes=[pk])
            for kk in range(4):
                k = half * 4 + kk
                eng = "act" if kk % 2 == 0 else "dve"
                if eng == "act":
                    K.op("act", lambda e, k=k, kk=kk, pt=pt: e.activation(
                        hT[:, k, col0:col0 + 128], pt[:, kk * 128:(kk + 1) * 128], AF.Identity,
                        bias=B[:, k:k + 1], scale=A[:, k:k + 1]), reads=[pk, "mods"], writes=[hkey])
                else:
                    K.op("dve", lambda e, k=k, kk=kk, pt=pt: e.tensor_scalar(
                        hT[:, k, col0:col0 + 128], pt[:, kk * 128:(kk + 1) * 128], A[:, k:k + 1], B[:, k:k + 1],
                        op0=ALU.mult, op1=ALU.add), reads=[pk, "mods"], writes=[hkey])

    eps_t = sb("eps_t", [128, 4])
    K.op("pool", lambda e: e.memset(eps_t[:, 0:1], NORM_EPS), writes=["eps"])
    K.op("pool", lambda e: e.memset(eps_t[:, 1:2], L2_EPS), writes=["eps"])
    K.op("pool", lambda e: e.memset(eps_t[:, 2:3], GN_EPS), writes=["eps"])
    K.op("pool", lambda e: e.memset(eps_t[:, 3:4], 1.0), writes=["eps"])
    K.op("dve", lambda e: e.tensor_copy(A1[:, 0:1], A1[:, 0:1]), reads=["A1", "B1", "A1c", "B1c", "A2", "B2"], writes=["mods"])

    with ExitStack() as pb:
        hT = sb("hT", [128, 8, T], BF16, stack=pb)
        junk = sb("junk", [128, D], stack=pb)
        ss = sb("ss", [128, 1], stack=pb)
        rs = sb("rs", [128, 2], stack=pb)
        xs = sb("xs", [128, D], stack=pb)
        xts = [sb("xt%d" % i, [128, D], stack=pb) for i in range(3)]
        for i in range(NT):
            xt = xts[i % 3]
            xk = "xt%d" % (i % 3)
            src = ctx_d[i * 128:(i + 1) * 128, :] if i < 2 else x_d[(i - 2) * 128:(i - 1) * 128, :]
            K.dma(xt[:], src, writes=[xk], key=xk)
            A, B = (A1c, B1c) if i < 2 else (A1, B1)
            norm_tile(xt, xk, A, B, hT, "hT", i * 128, 0, (junk, ss, rs, xs))
        if "hT" in dbg:
            d_ = dbgt("ss", [128, 1])
            K.dma(d_[:, :], ss[:, :], reads=["n_ss"], writes=["dbgss"], key="dbg")
            d_ = dbgt("rs", [128, 2])
            K.dma(d_[:, :], rs[:, :], reads=["n_rs", "n_rs2"], writes=["dbgrs"], key="dbg")
            d_ = dbgt("hT", [128, 8 * T], BF16)
            K.dma(d_[:, :], hT[:].rearrange("p k t -> p (k t)"), reads=["hT"], writes=["dbghT"], key="dbg")
        wst = [sb("wst%d" % i, [128, 8, 128], stack=pb) for i in range(2)]
        wbf = [sb("wbf%d" % i, [128, 8, 128], BF16, stack=pb) for i in range(2)]
        pcs = [sb("pc%d" % i, [128, T], stack=pb) for i in range(2)]
        nblk = (T + 511) // 512
        chunks = [(j, j * 128, 128) for j in range(16)] + [(16, 2048, 16)] + \
                 [(17 + j, GDN_COLS + j * 128, 128) for j in range(15)]
        ev = 0
        for ci, (dst, c0, ncol) in enumerate(chunks):
            w_s = wst[ci % 2]; w_b = wbf[ci % 2]; pc = pcs[ci % 2]
            ws_k = "wst%d" % (ci % 2); wb_k = "wbf%d" % (ci % 2); pc_k = "pc%d" % (ci % 2)
            K.dma(w_s[:, :, 0:ncol], win_d[:, c0:c0 + ncol].rearrange("(k p) c -> p k c", p=128), writes=[ws_k], key=ws_k)
            K.op("pool", lambda e, w_s=w_s, w_b=w_b, ncol=ncol: e.tensor_copy(w_b[:, :, 0:ncol], w_s[:, :, 0:ncol]),
                 reads=[ws_k], writes=[wb_k])
            for n in range(nblk):
                t0 = n * 512
                tn = min(512, T - t0)
                pt = ps[2 + (n % 4)]
                pk = "ps%d" % (2 + (n % 4))
                for k in range(8):
                    K.op("pe", lambda e, k=k, pt=pt, w_b=w_b, ncol=ncol, t0=t0, tn=tn: e.matmul(
                        pt[0:ncol, 0:tn], lhsT=w_b[:, k, 0:ncol], rhs=hT[:, k, t0:t0 + tn], start=(k == 0), stop=(k == 7)),
                        reads=[wb_k, "hT"], writes=[pk])
                eng = "act" if ev % 2 == 0 else "dve"
                ev += 1
                if eng == "act":
                    K.op("act", lambda e, pt=pt, pc=pc, ncol=ncol, t0=t0, tn=tn: e.copy(pc[0:ncol, t0:t0 + tn], pt[0:ncol, 0:tn]),
                         reads=[pk], writes=[pc_k])
                else:
                    K.op("dve", lambda e, pt=pt, pc=pc, ncol=ncol, t0=t0, tn=tn: e.tensor_copy(pc[0:ncol, t0:t0 + tn], pt[0:ncol, 0:tn]),
                         reads=[pk], writes=[pc_k])
            K.dma(pT_d[dst, 0:ncol, :], pc[0:ncol, :], reads=[pc_k], writes=[("pT", dst)], key="st_" + pc_k)

    K.barrier()
    if "pT" in dbg:
        d_ = dbgt("pT", [32, 128, T])
        K.dma(d_[:, :, :], pT_d[:, :, :], reads=[("pT", i) for i in range(32)], writes=["dbgpT"], key="dbg")


    mT_d = nc.dram_tensor("mT_s", [8, 128, TL], BF16).ap()
    psb = [p.bitcast(BF16) for p in ps]
    negm_le = sb("negm_le", [128, 128])
    negm_ge = sb("negm_ge", [128, 128])
    K.op("dve", lambda e: e.tensor_scalar(negm_le[:], m_le[:], 30000.0, -30000.0, op0=ALU.mult, op1=ALU.add), reads=["m_le"], writes=["negm_le"])
    K.op("dve", lambda e: e.tensor_scalar(negm_ge[:], m_ge[:], 30000.0, -30000.0, op0=ALU.mult, op1=ALU.add), reads=["m_ge"], writes=["negm_ge"])
    fwd_order = list(range(NT))
    bwd_order = [1, 0] + list(range(NT - 1, 1, -1))

    NMODE = NM_MODE[0]
    NDT = BF16 if NMODE == "bf16" else F32

    def mmv(ap):
        return ap

    identn = identb if NMODE == "bf16" else ident

    nmode = {'cur': NMODE}

    def neumann(Y, Yt, tag, pbank, pb=None):
        PR = nm_tiles[tag]["PR"]; Pt = nm_tiles[tag]["Pt"]
        kPR = [tag + "PR0", tag + "PR1"]; kPt = [tag + "Pt0", tag + "Pt1"]
        pa_ = ps[pbank]
        ka = "ps%d" % pbank
        if pb is None:
            pb_, kb = ps[pbank + 1], "ps%d" % (pbank + 1)
        else:
            pb_, kb = ps[pb[0]][:, pb[1]:pb[1] + 128], "ps%d" % pb[0]
        K.op("pe", lambda e: e.matmul(pa_[:, 0:128], lhsT=mmv(Yt[:]), rhs=mmv(Y[:]), start=True, stop=True), reads=[tag + "Y", tag + "Yt"], writes=[ka])
        K.op("pe", lambda e: e.matmul(pb_[:, 0:128], lhsT=mmv(Y[:]), rhs=mmv(Yt[:]), start=True, stop=True), reads=[tag + "Y", tag + "Yt"], writes=[kb])
        K.op("act", lambda e: e.copy(PR[0][:, 0:128], pa_[:, 0:128]), reads=[ka], writes=[kPR[0]])
        K.op("dve", lambda e: e.tensor_tensor(PR[0][:, 128:256], Y[:], (identb if nmode['cur'] == 'bf16' else ident)[:], op=ALU.add), reads=[tag + "Y", "identb", "ident"], writes=[kPR[0]])
        K.op("dve", lambda e: e.tensor_copy(Pt[0][:], pb_[:, 0:128]), reads=[kb], writes=[kPt[0]])
        cur = 0
        for l in range(1, 7):
            nxt = 1 - cur
            last = (l == 6)
            n0 = 128 if last else 0
            K.op("pe", lambda e, cur=cur, n0=n0: e.matmul(pa_[:, n0:256], lhsT=mmv(Pt[cur][:]), rhs=mmv(PR[cur][:, n0:256]), start=True, stop=False),
                 reads=[kPt[cur], kPR[cur]], writes=[ka])
            K.op("pe", lambda e, cur=cur: e.matmul(pa_[:, 128:256], lhsT=mmv((identb if nmode['cur'] == 'bf16' else ident)[:]), rhs=mmv(PR[cur][:, 128:256]), start=False, stop=True),
                 reads=["identb", "ident", kPR[cur]], writes=[ka])
            if not last:
                K.op("pe", lambda e, cur=cur: e.matmul(pb_[:, 0:128], lhsT=mmv(PR[cur][:, 0:128]), rhs=mmv(Pt[cur][:]), start=True, stop=True),
                     reads=[kPt[cur], kPR[cur]], writes=[kb])
            K.op("act", lambda e, nxt=nxt, n0=n0: e.copy(PR[nxt][:, n0:256], pa_[:, n0:256]), reads=[ka], writes=[kPR[nxt]])
            if not last:
                K.op("dve", lambda e, nxt=nxt: e.tensor_copy(Pt[nxt][:], pb_[:, 0:128]), reads=[kb], writes=[kPt[nxt]])
            cur = nxt
        if nmode['cur'] == 'bf16':
            return PR[cur][:, 128:256], kPR[cur]
        fin = nm_tiles[tag]["fin"]
        K.op("act", lambda e, cur=cur: e.copy(fin[:], PR[cur][:, 128:256]), reads=[kPR[cur]], writes=[tag + "fin"])
        return fin[:], tag + "fin"

    nm_tiles = {}
    with ExitStack() as pg:
        for tag in ("n0", "n1", "n2", "n3"):
            nm_tiles[tag] = dict(PR=[sb(tag + "PR%d" % i, [128, 256], NDT, stack=pg) for i in range(2)],
                                 Pt=[sb(tag + "Pt%d" % i, [128, 128], NDT, stack=pg) for i in range(2)],
                                 fin=sb(tag + "fin", [128, 128], BF16, stack=pg))
        ab = sb("ab", [128, NT, 16], stack=pg)
        dtb_b = sb("dtb_b", [128, 8], stack=pg)
        nA_b = sb("nA_b", [128, 8], stack=pg)
        gg = sb("gg", [128, NT, 8], stack=pg)
        Gc = sb("Gc", [128, NT, 8], stack=pg)
        nbeta = sb("nbeta", [128, NT, 8], stack=pg)
        beta = sb("beta", [128, NT, 8], stack=pg)
        negeG = sb("negeG", [128, NT, 8], stack=pg)
        eG = sb("eG", [128, NT, 8], stack=pg)
        eTG = sb("eTG", [128, NT, 8], stack=pg)
        eTot = sb("eTot", [128, NT, 8], stack=pg)
        pg_ab = ExitStack()
        abT = sb("abT", [16, T], stack=pg_ab)
        K.dma(abT[:, :], pT_d[16, 0:16, :], reads=[("pT", 16)], writes=["abT"], key="abT")
        K.dma(dtb_b[:, :], dtb_d.partition_broadcast(128), writes=["dtb_b"], key="dtb_b")
        K.dma(nA_b[:, :], alog_d.partition_broadcast(128), writes=["nA_b"], key="nA_b")
        for i in range(NT):
            K.op("pe", lambda e, i=i: e.transpose(ps[i // 32][:, (i % 32) * 16:(i % 32) * 16 + 16], abT[0:16, i * 128:(i + 1) * 128], ident[0:16, 0:16]),
                 reads=["abT", "ident"], writes=["ps%d" % (i // 32)])
        for b0 in range(0, NT, 32):
            nb = min(32, NT - b0)
            K.op("dve", lambda e, b0=b0, nb=nb: e.tensor_copy(ab[:, b0:b0 + nb, :].rearrange("p n c -> p (n c)"), ps[b0 // 32][:, 0:nb * 16]),
                 reads=["ps%d" % (b0 // 32)], writes=["ab"])
        K.op("act", lambda e: e.activation(nA_b[:], nA_b[:], AF.Exp), reads=["nA_b"], writes=["nA_b"])
        K.op("dve", lambda e: e.tensor_scalar(nA_b[:], nA_b[:], -1.0, None, op0=ALU.mult), reads=["nA_b"], writes=["nA_b"])
        K.op("dve", lambda e: e.tensor_tensor(gg[:], ab[:, :, 0:8], dtb_b[:].unsqueeze(1).to_broadcast([128, NT, 8]), op=ALU.add),
             reads=["ab", "dtb_b"], writes=["gg"])
        K.op("act", lambda e: e.activation(gg[:], gg[:], AF.Exp), reads=["gg"], writes=["gg"])
        K.op("act", lambda e: e.activation(gg[:], gg[:], AF.Ln, bias=eps_t[:, 3:4]), reads=["gg", "eps"], writes=["gg"])
        K.op("dve", lambda e: e.tensor_tensor(gg[:], gg[:], nA_b[:].unsqueeze(1).to_broadcast([128, NT, 8]), op=ALU.mult),
             reads=["gg", "nA_b"], writes=["gg"])
        K.op("act", lambda e: e.activation(beta[:], ab[:, :, 8:16], AF.Sigmoid), reads=["ab"], writes=["beta"])
        K.op("dve", lambda e: e.tensor_scalar(nbeta[:], beta[:], -1.0, None, op0=ALU.mult), reads=["beta"], writes=["nbeta"])
        ggf = gg[:].rearrange("p n c -> p (n c)")
        K.op("pe", lambda e: e.matmul(ps[2][:, 0:NT * 8], lhsT=m_le[:], rhs=ggf, start=True, stop=True), reads=["m_le", "gg"], writes=["ps2"])
        K.op("pe", lambda e: e.matmul(ps[3][:, 0:NT * 8], lhsT=m_ge[:], rhs=ggf, start=True, stop=True), reads=["m_ge", "gg"], writes=["ps3"])
        K.op("pe", lambda e: e.matmul(ps[4][:, 0:NT * 8], lhsT=ones[:], rhs=ggf, start=True, stop=True), reads=["ones", "gg"], writes=["ps4"])
        K.op("dve", lambda e: e.tensor_copy(Gc[:, :, 0:4], ps[2][:, 0:NT * 8].rearrange("p (n c) -> p n c", c=8)[:, :, 0:4]), reads=["ps2"], writes=["Gc"])
        K.op("dve", lambda e: e.tensor_copy(Gc[:, :, 4:8], ps[3][:, 0:NT * 8].rearrange("p (n c) -> p n c", c=8)[:, :, 4:8]), reads=["ps3"], writes=["Gc"])
        K.op("act", lambda e: e.activation(eG[:], Gc[:], AF.Exp), reads=["Gc"], writes=["eG"])
        K.op("dve", lambda e: e.tensor_scalar(negeG[:], eG[:], -1.0, None, op0=ALU.mult), reads=["eG"], writes=["negeG"])
        K.op("act", lambda e: e.activation(eTot[:].rearrange("p n c -> p (n c)"), ps[4][:, 0:NT * 8], AF.Exp), reads=["ps4"], writes=["eTot"])
        K.op("dve", lambda e: e.tensor_tensor(eTG[:].rearrange("p n c -> p (n c)"), ps[4][:, 0:NT * 8], Gc[:].rearrange("p n c -> p (n c)"), op=ALU.subtract),
             reads=["ps4", "Gc"], writes=["eTG"])
        K.op("act", lambda e: e.activation(eTG[:], eTG[:], AF.Exp), reads=["eTG"], writes=["eTG"])

        K.barrier()
        pg_ab.close()
        stop_at("gdn_scal")
        qT = sb("qT", [128, T], BF16, stack=pg)
        kT = sb("kT", [128, T], BF16, stack=pg)
        vT = sb("vT", [128, T], BF16, stack=pg)
        zs = sb("zs", [128, T], BF16, stack=pg)
        vtok = sb("vtok", [128, NT, 128], BF16, stack=pg)
        ktok = sb("ktok", [128, NT, 128], BF16, stack=pg)
        obuf = [sb("obuf%d" % d_, [128, NT, 128], BF16, stack=pg) for d_ in range(2)]
        AinvAll = [sb("AinvAll%d" % d_, [128, NT, 128], BF16, stack=pg) for d_ in range(2)]
        MqkAll = [sb("MqkAll%d" % d_, [128, NT, 128], BF16, stack=pg) for d_ in range(2)]
        pin = sb("pin", [128, T], stack=pg)
        cv = sb("cv", [128, T], stack=pg)
        sq = pin
        rn = sb("rn", [128, 512], stack=pg)
        S = [sb("S%d" % d_, [128, 128], stack=pg) for d_ in range(2)]
        Sb = [sb("Sb%d" % d_, [128, 128], BF16, stack=pg) for d_ in range(2)]
        mst = sb("mst", [128, TL], BF16, stack=pg)
        dgl = [sb("dgl%d" % d_, [128, 128], stack=pg) for d_ in range(4)]
        arg = dgl
        DTi = [sb("DTi%d" % d_, [128, 128], stack=pg) for d_ in range(4)]
        DTs = dgl
        Yb = [sb("Yb%d" % d_, [128, 128], NDT, stack=pg) for d_ in range(4)]
        Ytb = [sb("Ytb%d" % d_, [128, 128], NDT, stack=pg) for d_ in range(4)]
        Rb = [sb("Rb%d" % d_, [128, 128], BF16, stack=pg) for d_ in range(2)]
        Xb = [sb("Xb%d" % d_, [128, 128], BF16, stack=pg) for d_ in range(2)]
        Xs = [sb("Xs%d" % d_, [128, 128], BF16, stack=pg) for d_ in range(2)]
        QSe = [sb("QSe%d" % d_, [128, 128], stack=pg) for d_ in range(2)]
        on_ = sb("on_", [128, 128], stack=pg)
        oj = sb("oj", [128, 128], stack=pg)
        onn = sb("onn", [128, 128], stack=pg)
        oss = sb("oss", [128, 4], stack=pg)

        def conv_silu(cidx, dst, dkey, final_silu_to):
            cw0 = PK1["CW"]
            K.op("dve", lambda e: e.tensor_scalar(cv[:], pin[:], pk1[:, cw0 + 2 * 12 + cidx:cw0 + 2 * 12 + cidx + 1], None, op0=ALU.mult),
                 reads=["pin", "pk1"], writes=["cv"])
            for j in (0, 1, 3, 4):
                sh = j - 2
                wcol = pk1[:, cw0 + j * 12 + cidx:cw0 + j * 12 + cidx + 1]
                for (s0, s1) in ((0, TC), (TC, T)):
                    lo = max(s0, s0 - sh); hi = min(s1, s1 - sh)
                    K.op("dve", lambda e, lo=lo, hi=hi, sh=sh, wcol=wcol: e.scalar_tensor_tensor(
                        cv[:, lo:hi], pin[:, lo + sh:hi + sh], wcol, cv[:, lo:hi], op0=ALU.mult, op1=ALU.add),
                        reads=["pin", "pk1", "cv"], writes=["cv"])
            K.op("act", lambda e: e.activation(final_silu_to[:], cv[:], AF.Silu), reads=["cv"], writes=[dkey])

        def l2n(src, skey, dst, dkey, scale):
            K.op("pool", lambda e: e.tensor_tensor(sq[:], src[:], src[:], op=ALU.mult), reads=[skey], writes=["pin"])
            for n in range((T + 511) // 512):
                t0 = n * 512; tn = min(512, T - t0)
                pt = ps[5 + (n % 2)]; pk = "ps%d" % (5 + (n % 2))
                K.op("pe", lambda e, pt=pt, t0=t0, tn=tn: e.matmul(pt[:, 0:tn], lhsT=ones[:], rhs=sq[:, t0:t0 + tn], start=True, stop=True),
                     reads=["ones", "pin"], writes=[pk])
                K.op("act", lambda e, pt=pt, tn=tn: e.activation(rn[:, 0:tn], pt[:, 0:tn], AF.Sqrt, bias=eps_t[:, 1:2]), reads=[pk, "eps"], writes=["rn"])
                K.op("dve", lambda e, tn=tn: e.reciprocal(rn[:, 0:tn], rn[:, 0:tn]), reads=["rn"], writes=["rn"])
                K.op("dve", lambda e, t0=t0, tn=tn: e.scalar_tensor_tensor(dst[:, t0:t0 + tn], src[:, t0:t0 + tn], scale, rn[:, 0:tn], op0=ALU.mult, op1=ALU.mult),
                     reads=[skey, "rn"], writes=[dkey])

        def to_tok(src, skey, dst, dkey):
            for i in range(NT):
                pt = psb[5 + (i % 2)]; pk = "ps%d" % (5 + (i % 2))
                K.op("pe", lambda e, i=i, pt=pt: e.transpose(pt[:, 0:128], src[:, i * 128:(i + 1) * 128], identb[:]), reads=[skey, "identb"], writes=[pk])
                eng = "act" if i % 2 == 0 else "dve"
                if eng == "act":
                    K.op("act", lambda e, i=i, pt=pt: e.copy(dst[:, i, :], pt[:, 0:128]), reads=[pk], writes=[dkey])
                else:
                    K.op("dve", lambda e, i=i, pt=pt: e.tensor_copy(dst[:, i, :], pt[:, 0:128]), reads=[pk], writes=[dkey])

        for h in range(4):
            K.dma(pin[:, :], pT_d[h, :, :], reads=[("pT", h)], writes=["pin"], key="pin")
            conv_silu(h, cv, "cv", cv)
            l2n(cv, "cv", qT, "qT", float(128 ** -0.5))
            K.dma(pin[:, :], pT_d[4 + h, :, :], reads=[("pT", 4 + h)], writes=["pin"], key="pin")
            conv_silu(4 + h, cv, "cv", cv)
            l2n(cv, "cv", kT, "kT", 1.0)
            to_tok(kT, "kT", ktok, "ktok")
            K.dma(pin[:, :], pT_d[8 + h, :, :], reads=[("pT", 8 + h)], writes=["pin"], key="pin")
            conv_silu(8 + h, vT, "vT", vT)
            to_tok(vT, "vT", vtok, "vtok")
            K.dma(pin[:, :], pT_d[12 + h, :, :], reads=[("pT", 12 + h)], writes=["pin"], key="pin")
            K.op("act", lambda e: e.activation(zs[:], pin[:], AF.Silu), reads=["pin"], writes=["zs"])
            stop_at("gdn_prep")
            for d_ in range(2):
                K.op("pool", lambda e, d_=d_: e.memset(S[d_][:], 0.0), writes=["S%d" % d_])
                K.op("pool", lambda e, d_=d_: e.memset(Sb[d_][:], 0.0), writes=["Sb%d" % d_])
            def g_pre(step, d_, sl_):
                i = fwd_order[step] if d_ == 0 else bwd_order[step]
                par = step % 2
                r = d_ * 4 + h
                tag = "n%d" % sl_
                sl = slice(i * 128, (i + 1) * 128)
                want_o = i >= 2
                pA = ps[2 * sl_]; kA = "ps%d" % (2 * sl_)
                dk_ = "%d" % sl_
                K.op("dve", lambda e: e.tensor_scalar(dgl[sl_][:], ident[:], Gc[:, i, r:r + 1], None, op0=ALU.mult),
                     reads=["ident", "Gc"], writes=["dgl" + dk_])
                K.op("pe", lambda e: e.matmul(pA[:, 256:384], lhsT=ones[:], rhs=dgl[sl_][:], start=True, stop=True),
                     reads=["ones", "dgl" + dk_], writes=[kA])
                negm = negm_le if d_ == 0 else negm_ge
                mstrict = m_lt if d_ == 0 else m_gt
                K.op("dve", lambda e: e.scalar_tensor_tensor(
                    arg[sl_][:], pA[:, 256:384], Gc[:, i, r:r + 1], negm[:], op0=ALU.subtract, op1=ALU.add),
                    reads=[kA, "Gc", "negm_le", "negm_ge"], writes=["dgl" + dk_])
                K.op("act", lambda e: e.activation(DTi[sl_][:], arg[sl_][:], AF.Exp), reads=["dgl" + dk_], writes=["DTi" + dk_])
                K.op("pool", lambda e: e.tensor_tensor(DTs[sl_][:], DTi[sl_][:], mstrict[:], op=ALU.mult),
                     reads=["DTi" + dk_, "m_lt", "m_gt"], writes=["dgl" + dk_])
                K.op("pe", lambda e: e.matmul(pA[:, 0:128], lhsT=kT[:, sl], rhs=kT[:, sl], start=True, stop=True),
                     reads=["kT"], writes=[kA])
                K.op("dve", lambda e: e.scalar_tensor_tensor(
                    Yb[sl_][:], pA[:, 0:128], nbeta[:, i, r:r + 1], DTs[sl_][:], op0=ALU.mult, op1=ALU.mult),
                    reads=[kA, "nbeta", "dgl" + dk_], writes=[tag + "Y"])
                if want_o:
                    K.op("pe", lambda e: e.matmul(pA[:, 128:256], lhsT=kT[:, sl], rhs=qT[:, sl], start=True, stop=True),
                         reads=["kT", "qT"], writes=[kA])
                    K.op("dve", lambda e: e.tensor_tensor(MqkAll[d_][:, step, :], pA[:, 128:256], DTi[sl_][:], op=ALU.mult),
                         reads=[kA, "DTi" + dk_], writes=[("Mqk", d_, step)])
                pB = (psb if NMODE == "bf16" else ps)[2 * sl_ + 1]; kB = "ps%d" % (2 * sl_ + 1)
                K.op("pe", lambda e: e.transpose(pB[:, 0:128], Yb[sl_][:], identn[:]), reads=[tag + "Y", "identb", "ident"], writes=[kB])
                K.op("act", lambda e: e.copy(Ytb[sl_][:], pB[:, 0:128]), reads=[kB], writes=[tag + "Yt"])
                AinvT, kAinv = neumann(Yb[sl_], Ytb[sl_], tag, 2 * sl_ + 1, pb=(2 * sl_, 384))
                K.op("pool", lambda e: e.tensor_copy(AinvAll[d_][:, step, :], AinvT), reads=[kAinv], writes=[("gAinv", d_, step)])

            def g_chain(step, d_):
                i = fwd_order[step] if d_ == 0 else bwd_order[step]
                par = step % 2
                r = d_ * 4 + h
                sl = slice(i * 128, (i + 1) * 128)
                want_o = i >= 2
                pC = ps[d_ * 4 + 3]; kC = "ps%d" % (d_ * 4 + 3)
                dk_ = "%d" % d_
                kAinv = ("gAinv", d_, step)
                kMqk = ("Mqk", d_, step)
                K.op("pe", lambda e: e.matmul(pC[:, 0:128], lhsT=kT[:, sl], rhs=Sb[d_][:], start=True, stop=True),
                     reads=["kT", "Sb" + dk_], writes=[kC])
                if want_o:
                    K.op("pe", lambda e: e.matmul(pC[:, 128:256], lhsT=qT[:, sl], rhs=Sb[d_][:], start=True, stop=True),
                         reads=["qT", "Sb" + dk_], writes=[kC])
                K.op("dve", lambda e: e.scalar_tensor_tensor(
                    Rb[d_][:], pC[:, 0:128], negeG[:, i, r:r + 1], vtok[:, i, :], op0=ALU.mult, op1=ALU.add),
                    reads=[kC, "negeG", "vtok"], writes=["Rb" + dk_])
                if want_o:
                    K.op("act", lambda e: e.activation(QSe[d_][:], pC[:, 128:256], AF.Identity, scale=eG[:, i, r:r + 1]),
                         reads=[kC, "eG"], writes=["QSe" + dk_])
                K.op("pe", lambda e: e.matmul(pC[:, 0:128], lhsT=AinvAll[d_][:, step, :], rhs=Rb[d_][:], start=True, stop=True),
                     reads=[kAinv, "Rb" + dk_], writes=[kC])
                K.op("dve", lambda e: e.tensor_scalar(Xb[d_][:], pC[:, 0:128], beta[:, i, r:r + 1], None, op0=ALU.mult),
                     reads=[kC, "beta"], writes=["Xb" + dk_])
                K.op("pool", lambda e: e.tensor_scalar(Xs[d_][:], Xb[d_][:], eTG[:, i, r:r + 1], None, op0=ALU.mult),
                     reads=["Xb" + dk_, "eTG"], writes=["Xs" + dk_])
                if want_o:
                    K.op("pe", lambda e: e.matmul(pC[:, 384:512], lhsT=MqkAll[d_][:, step, :], rhs=Xb[d_][:], start=True, stop=True),
                         reads=[kMqk, "Xb" + dk_], writes=[kC])
                    K.op("dve", lambda e: e.tensor_tensor(obuf[d_][:, i, :], pC[:, 384:512], QSe[d_][:], op=ALU.add),
                         reads=[kC, "QSe" + dk_], writes=["obuf" + dk_])
                K.op("pe", lambda e: e.matmul(pC[:, 256:384], lhsT=ktok[:, i, :], rhs=Xs[d_][:], start=True, stop=True),
                     reads=["ktok", "Xs" + dk_], writes=[kC])
                K.op("dve", lambda e: e.scalar_tensor_tensor(
                    S[d_][:], S[d_][:], eTot[:, i, r:r + 1], pC[:, 256:384], op0=ALU.mult, op1=ALU.add),
                    reads=["S" + dk_, "eTot", kC], writes=["S" + dk_])
                K.op("act", lambda e: e.copy(Sb[d_][:], S[d_][:]), reads=["S" + dk_], writes=["Sb" + dk_])

            inst = [(st_, dd_) for st_ in range(NT) for dd_ in range(2)]
            for g0 in range(0, len(inst), 4):
                grp = inst[g0:g0 + 4]
                for q_ in K.streams(len(grp)):
                    g_pre(grp[q_][0], grp[q_][1], q_)
            for step in range(NT):
                for d_ in K.streams(2):
                    g_chain(step, d_)
            stop_at("gdn_scan")
            for i in range(2, NT):
                K.op("dve", lambda e, i=i: e.tensor_tensor(on_[:], obuf[0][:, i, :], obuf[1][:, i, :], op=ALU.add),
                     reads=["obuf0", "obuf1"], writes=["on_"])
                K.op("act", lambda e: e.activation(oj[:], on_[:], AF.Square), reads=["on_"], writes=["oj"])
                K.op("dve", lambda e: e.reduce_sum(oss[:, 0:1], oj[:], axis=AX.X), reads=["oj"], writes=["oss"])
                K.op("act", lambda e: e.activation(oss[:, 1:2], oss[:, 0:1], AF.Sqrt, bias=eps_t[:, 0:1], scale=1.0 / 128), reads=["oss", "eps"], writes=["oss1"])
                K.op("dve", lambda e: e.reciprocal(oss[:, 2:3], oss[:, 1:2]), reads=["oss1"], writes=["oss2"])
                K.op("dve", lambda e: e.tensor_scalar(onn[:], on_[:], oss[:, 2:3], None, op0=ALU.mult), reads=["on_", "oss2"], writes=["onn"])
                pt = ps[i % 2]; pk = "ps%d" % (i % 2)
                K.op("pe", lambda e, pt=pt: e.transpose(pt[:, 0:128], onn[:], ident[:]), reads=["onn", "ident"], writes=[pk])
                gcol = pk2[:, PK2["GNORM"]:PK2["GNORM"] + 1]
                K.op("dve", lambda e, i=i, pt=pt, gcol=gcol: e.scalar_tensor_tensor(
                    mst[:, (i - 2) * 128:(i - 1) * 128], pt[:, 0:128], gcol, zs[:, i * 128:(i + 1) * 128], op0=ALU.mult, op1=ALU.mult),
                    reads=[pk, "pk2", "zs"], writes=["mst"])
            K.dma(mT_d[h, :, :], mst[:, :], reads=["mst"], writes=[("mT", h)], key="st_mst")

    K.barrier()
    pm_d = nc.dram_tensor("pm_s", [12, 128, T], F32).ap()
    lora_d = nc.dram_tensor("lora_s", [3, 128, T], BF16).ap()
    CW_ = float(np.exp(-0.5))
    NRW = n_rows
    with ExitStack() as pr:
        cidx_i = sb("cidx_i", [128, 15], I32, stack=pr)
        cidx = sb("cidx", [128, 15], stack=pr)
        mum = {nm: sb("mum_" + nm, [128, 15], stack=pr) for nm in ("om", "L", "R", "U", "D", "P", "N")}
        tmpm = sb("tmpm", [128, 15], stack=pr)
        K.op("pool", lambda e: e.iota(cidx_i[:], pattern=[[128, 15]], base=0, channel_multiplier=1), writes=["cidx_i"])
        K.op("dve", lambda e: e.tensor_copy(cidx[:], cidx_i[:]), reads=["cidx_i"], writes=["cidx"])
        mu_ap = pk2[:, PK2["MU"]:PK2["MU"] + 15]
        K.op("dve", lambda e: e.tensor_scalar(mum["om"][:], mu_ap, -1.0, 1.0, op0=ALU.mult, op1=ALU.add), reads=["pk2"], writes=["mum"])

        def band(nm, lo, hi):
            K.op("dve", lambda e: e.tensor_scalar(tmpm[:], cidx[:], float(lo), None, op0=ALU.is_ge), reads=["cidx"], writes=["tmpm"])
            K.op("dve", lambda e: e.scalar_tensor_tensor(tmpm[:], cidx[:], float(hi), tmpm[:], op0=ALU.is_lt, op1=ALU.mult), reads=["cidx", "tmpm"], writes=["tmpm"])
            K.op("dve", lambda e: e.tensor_tensor(mum[nm][:], tmpm[:], mu_ap, op=ALU.mult), reads=["tmpm", "pk2"], writes=["mum"])

        band("L", 0, 480); band("R", 480, 960); band("U", 960, 1440); band("D", 1440, 1920)
        band("P", 0, 960); band("N", 960, 1920)
        pins = [sb("rpin%d" % i, [128, T], stack=pr) for i in range(2)]
        pmx = [sb("pmx%d" % i, [128, T], stack=pr) for i in range(2)]
        lob = sb("lob", [128, T], BF16, stack=pr)
        for j in range(15):
            pin_ = pins[j % 2]; pk_ = "rpin%d" % (j % 2)
            po = pmx[j % 2]; ok_ = "pmx%d" % (j % 2)
            K.dma(pin_[:, :], pT_d[17 + j, :, :], reads=[("pT", 17 + j)], writes=[pk_], key=pk_)
            K.op("dve", lambda e, j=j, pin_=pin_, po=po: e.tensor_scalar(po[:], pin_[:], mum["om"][:, j:j + 1], None, op0=ALU.mult),
                 reads=[pk_, "mum"], writes=[ok_])
            c0, c1 = j * 128, j * 128 + 128

            def has(lo, hi):
                return c0 < hi and c1 > lo

            def acc(dst, src, nm, j=j, pin_=pin_, po=po, pk_=pk_, ok_=ok_, eng="dve"):
                K.op("dve", lambda e: e.scalar_tensor_tensor(dst(po), src(pin_), mum[nm][:, j:j + 1], dst(po), op0=ALU.mult, op1=ALU.add),
                     reads=[pk_, "mum", ok_], writes=[ok_])

            lat = lambda t: t[:, TC:T].rearrange("p (r w) -> p r w", w=64)
            if has(0, 960):
                acc(lambda t: t[:, 1:TC], lambda t: t[:, 0:TC - 1], "P")
            if has(960, 1920):
                acc(lambda t: t[:, 0:TC - 1], lambda t: t[:, 1:TC], "N")
            if has(0, 480):
                acc(lambda t: lat(t)[:, :, 1:64], lambda t: lat(t)[:, :, 0:63], "L")
            if has(480, 960):
                acc(lambda t: lat(t)[:, :, 0:63], lambda t: lat(t)[:, :, 1:64], "R")
            if has(960, 1440) and NRW > 1:
                acc(lambda t: lat(t)[:, 1:NRW, :], lambda t: lat(t)[:, 0:NRW - 1, :], "U")
            if has(1440, 1920) and NRW > 1:
                acc(lambda t: lat(t)[:, 0:NRW - 1, :], lambda t: lat(t)[:, 1:NRW, :], "D")
            if j < 12:
                K.dma(pm_d[j, :, :], po[:, :], reads=[ok_], writes=[("pm", j)], key="st_" + ok_)
            else:
                fn_ = {12: AF.Tanh, 13: AF.Identity, 14: AF.Sigmoid}[j]
                K.op("act", lambda e, po=po, fn_=fn_: e.activation(lob[:], po[:], fn_), reads=[ok_], writes=["lob"])
                K.dma(lora_d[j - 12, :, :], lob[:, :], reads=["lob"], writes=[("lora", j - 12)], key="st_lob")
    K.barrier()
    stop_at("rw_mix")
    if "pm" in dbg:
        d_o = dbgt("pm", [12, 128, T])
        K.dma(d_o[:, :, :], pm_d[:, :, :], reads=[("pm", j) for j in range(12)], writes=["dbgpm"], key="dbg")

    comb_d = nc.dram_tensor("comb_s", [16384, 2 * D], BF16).ap()
    with ExitStack() as pw:
        nm_tiles.clear()
        RW_NDT = BF16 if RW_NM[0] == "bf16" else F32
        nmode['cur'] = RW_NM[0]
        for tag in ("n0", "n1", "n2", "n3"):
            nm_tiles[tag] = dict(PR=[sb(tag + "rPR%d" % i, [128, 256], RW_NDT, stack=pw) for i in range(2)],
                                 Pt=[sb(tag + "rPt%d" % i, [128, 128], RW_NDT, stack=pw) for i in range(2)],
                                 fin=sb(tag + "rfin", [128, 128], BF16, stack=pw))
        BL = min(256, T)
        blocks = [(b0, min(BL, T - b0)) for b0 in range(0, T, BL)]
        wst_ = sb("rw_wst", [128, 3, 512], stack=pw)
        wlb = sb("rw_wlb", [128, 3, 512], BF16, stack=pw)
        for q_, src in enumerate((w2_d, a2_d, g2w_d)):
            K.dma(wst_[:, q_, :], src[:, :], writes=["rw_wst"], key="rw_wst")
        K.op("dve", lambda e: e.tensor_copy(wlb[:], wst_[:]), reads=["rw_wst"], writes=["wlb"])
        bones = sb("bones", [128, 128], stack=pw)
        K.op("pool", lambda e: e.memset(bones[:], 0.0), writes=["bones"])
        K.op("pool", lambda e: e.memset(bones[0:64, 0:64], 1.0), writes=["bones"])
        K.op("pool", lambda e: e.memset(bones[64:128, 64:128], 1.0), writes=["bones"])
        rmask = sb("rmask", [128, BL], stack=pw)
        K.op("pool", lambda e: e.memset(rmask[:], 1.0), writes=["rmask"])
        K.op("pool", lambda e: e.memset(rmask[:].rearrange("p (n t) -> p n t", t=128)[:, :, 0:1], 0.0), writes=["rmask"])
        mask4 = [sb("mask4_%d" % d_, [128, 512], stack=pw) for d_ in range(2)]
        for d_ in range(2):
            ms_, mi_ = (m_lt, m_le) if d_ == 0 else (m_gt, m_ge)
            for q_ in range(4):
                src = ms_ if q_ % 2 == 0 else mi_
                K.op("dve", lambda e, d_=d_, q_=q_, src=src: e.tensor_copy(mask4[d_][:, q_ * 128:(q_ + 1) * 128], src[:]),
                     reads=["m_lt", "m_le", "m_gt", "m_ge"], writes=["mask4"])
        rT = [sb("rT%d" % d_, [128, T], BF16, stack=pw) for d_ in range(2)]
        kpT = [sb("kpT%d" % d_, [128, T], BF16, stack=pw) for d_ in range(2)]
        ktT = [sb("ktT%d" % d_, [128, T], BF16, stack=pw) for d_ in range(2)]
        nbT = [sb("nbT%d" % d_, [128, T], BF16, stack=pw) for d_ in range(2)]
        Lam = [sb("Lam%d" % d_, [128, NT], stack=pw) for d_ in range(2)]
        vtk = sb("rvtok", [128, NT, 128], BF16, stack=pw)
        bonus = sb("bonus", [128, T], BF16, stack=pw)
        gateT = sb("gateT", [128, T], BF16, stack=pw)
        ybuf = [sb("ybuf%d" % d_, [128, NT, 128], BF16, stack=pw) for d_ in range(2)]
        rmst = sb("rmst", [128, TL], BF16, stack=pw)
        bt = {nm: sb("b_" + nm, [128, BL], stack=pw) for nm in
              ("r", "k", "v", "kap", "sig", "cum", "w", "iw", "wp", "a", "t1", "t2", "rk")}
        lbt = sb("b_lora", [128, 3, BL], BF16, stack=pw)
        vb16 = sb("b_vb16", [128, BL], BF16, stack=pw)
        Z = [sb("Z%d" % d_, [128, 128], stack=pw) for d_ in range(2)]
        Zb = [sb("Zb%d" % d_, [128, 128], BF16, stack=pw) for d_ in range(2)]
        AK = [sb("AK%d" % d_, [128, 512], BF16, stack=pw) for d_ in range(2)]
        ANB = [sb("ANB%d" % d_, [128, 512], BF16, stack=pw) for d_ in range(2)]
        YY = [[sb("YY%d%d" % (d_, hh), [128, 128], RW_NDT, stack=pw) for hh in range(2)] for d_ in range(2)]
        YYt = [[sb("YYt%d%d" % (d_, hh), [128, 128], RW_NDT, stack=pw) for hh in range(2)] for d_ in range(2)]
        AinvS = [[sb("Ainv%d%d" % (d_, hh), [128, 128], BF16, stack=pw) for hh in range(2)] for d_ in range(2)]
        ktok_ = [sb("rktok%d" % d_, [128, 128], BF16, stack=pw) for d_ in range(2)]
        nbtok_ = [sb("rnbtok%d" % d_, [128, 128], BF16, stack=pw) for d_ in range(2)]
        P1b = [sb("P1b%d" % d_, [128, 128], BF16, stack=pw) for d_ in range(2)]
        Ub = [sb("Ub%d" % d_, [128, 128], BF16, stack=pw) for d_ in range(2)]
        zt = [sb("zt%d" % d_, [128, 128], stack=pw) for d_ in range(2)]
        yo = sb("yo", [128, 128], stack=pw)
        yc = sb("yc", [128, 128], stack=pw)
        ysq = sb("ysq", [128, 128], stack=pw)
        yst = sb("yst", [128, 8], stack=pw)
        yt2 = sb("yt2", [128, 128], stack=pw)
        cst = [sb("cst%d" % i_, [128, 2, D], stack=pw) for i_ in range(2)]
        cbf = [sb("cbf%d" % i_, [128, 2 * D], BF16, stack=pw) for i_ in range(2)]
        conv_n = [0, 0]

        def conv_load():
            c_ = conv_n[0]
            if c_ >= 128:
                return
            conv_n[0] += 1
            st_ = cst[c_ % 2]; sk_ = "cst%d" % (c_ % 2)
            K.dma(st_[:, 0, :], down_d[c_ * 128:(c_ + 1) * 128, :], writes=[sk_], key=sk_)
            K.dma(st_[:, 1, :], up_d[c_ * 128:(c_ + 1) * 128, :], writes=[sk_], key=sk_)

        def conv_emit():
            c_ = conv_n[1]
            if c_ >= 128:
                return
            conv_n[1] += 1
            conv_load()
            st_ = cst[c_ % 2]; bf_ = cbf[c_ % 2]
            sk_ = "cst%d" % (c_ % 2); bk_ = "cbf%d" % (c_ % 2)
            K.op("act", lambda e, st_=st_, bf_=bf_: e.copy(bf_[:, 0:D], st_[:, 0, :]), reads=[sk_], writes=[bk_ + "a"])
            K.op("pool", lambda e, st_=st_, bf_=bf_: e.tensor_copy(bf_[:, D:2 * D], st_[:, 1, :]), reads=[sk_], writes=[bk_ + "b"])
            K.dma(comb_d[c_ * 128:(c_ + 1) * 128, :], bf_[:, :], reads=[bk_ + "a", bk_ + "b"], writes=["comb"], key="st_cbf")

        conv_load()

        for P in range(4):
            ch = slice(P * 128, (P + 1) * 128)
            for (b0, bn) in blocks:
                bs = slice(b0, b0 + bn)
                ntb = bn // 128
                for nm, jj in (("r", P), ("k", 4 + P), ("v", 8 + P)):
                    K.dma(bt[nm][:, 0:bn], pm_d[jj, :, bs], reads=[("pm", jj)], writes=["b_" + nm], key="b_" + nm)
                K.dma(lbt[:, :, 0:bn], lora_d[:, :, bs].rearrange("q p t -> p q t"), reads=[("lora", 0), ("lora", 1), ("lora", 2)], writes=["b_lora"], key="b_lora")
                K.op("pool", lambda e, bn=bn: e.tensor_copy(vb16[:, 0:bn], bt["v"][:, 0:bn]), reads=["b_v"], writes=["vb16"])
                for ii in range(ntb):
                    gi = b0 // 128 + ii
                    pt = psb[6 + (ii % 2)]; pk = "ps%d" % (6 + (ii % 2))
                    K.op("pe", lambda e, ii=ii, pt=pt: e.transpose(pt[:, 0:128], vb16[:, ii * 128:(ii + 1) * 128], identb[:]), reads=["vb16", "identb"], writes=[pk])
                    K.op("act", lambda e, gi=gi, pt=pt: e.copy(vtk[:, gi, :], pt[:, 0:128]), reads=[pk], writes=["rvtok"])
                kkc = pk2[:, PK2["KK"] + P:PK2["KK"] + P + 1]
                K.op("dve", lambda e, bn=bn, kkc=kkc: e.tensor_scalar(bt["kap"][:, 0:bn], bt["k"][:, 0:bn], kkc, None, op0=ALU.mult), reads=["b_k", "pk2"], writes=["b_kap"])
                K.op("pool", lambda e, bn=bn: e.tensor_tensor(bt["t1"][:, 0:bn], bt["kap"][:, 0:bn], bt["kap"][:, 0:bn], op=ALU.mult), reads=["b_kap"], writes=["b_t1"])
                for n in range((bn + 511) // 512):
                    t0 = n * 512; tn = min(512, bn - t0)
                    K.op("pe", lambda e, t0=t0, tn=tn: e.matmul(ps[0][:, 0:tn], lhsT=bones[:], rhs=bt["t1"][:, t0:t0 + tn], start=True, stop=True), reads=["bones", "b_t1"], writes=["ps0"])
                    K.op("act", lambda e, t0=t0, tn=tn: e.activation(bt["t2"][:, t0:t0 + tn], ps[0][:, 0:tn], AF.Sqrt, bias=eps_t[:, 1:2]), reads=["ps0", "eps"], writes=["b_t2"])
                K.op("dve", lambda e, bn=bn: e.reciprocal(bt["t2"][:, 0:bn], bt["t2"][:, 0:bn]), reads=["b_t2"], writes=["b_t2"])
                K.op("dve", lambda e, bn=bn: e.tensor_tensor(bt["kap"][:, 0:bn], bt["kap"][:, 0:bn], bt["t2"][:, 0:bn], op=ALU.mult), reads=["b_kap", "b_t2"], writes=["b_kap"])
                for n in range((bn + 511) // 512):
                    t0 = n * 512; tn = min(512, bn - t0)
                    K.op("pe", lambda e, t0=t0, tn=tn: e.matmul(ps[1][:, 0:tn], lhsT=wlb[:, 2, ch], rhs=lbt[:, 2, t0:t0 + tn], start=True, stop=True), reads=["wlb", "b_lora"], writes=["ps1"])
                    K.op("act", lambda e, t0=t0, tn=tn: e.copy(gateT[:, b0 + t0:b0 + t0 + tn], ps[1][:, 0:tn]), reads=["ps1"], writes=["gateT"])
                first_rk = True
                for d_ in range(2):
                    ds = slice(d_ * 64, (d_ + 1) * 64)
                    w0c = pk2[:, PK2["W0"] + d_ * 4 + P:PK2["W0"] + d_ * 4 + P + 1]
                    a0c = pk2[:, PK2["A0"] + d_ * 4 + P:PK2["A0"] + d_ * 4 + P + 1]
                    for n in range((bn + 511) // 512):
                        t0 = n * 512; tn = min(512, bn - t0)
                        K.op("pe", lambda e, t0=t0, tn=tn, ds=ds: e.matmul(ps[2][:, 0:tn], lhsT=wlb[ds, 0, ch], rhs=lbt[ds, 0, t0:t0 + tn], start=True, stop=True), reads=["wlb", "b_lora"], writes=["ps2"])
                        K.op("act", lambda e, t0=t0, tn=tn, w0c=w0c: e.activation(bt["sig"][:, t0:t0 + tn], ps[2][:, 0:tn], AF.Sigmoid, bias=w0c), reads=["ps2", "pk2"], writes=["b_sig"])
                        K.op("pe", lambda e, t0=t0, tn=tn, ds=ds: e.matmul(ps[3][:, 0:tn], lhsT=wlb[ds, 1, ch], rhs=lbt[ds, 1, t0:t0 + tn], start=True, stop=True), reads=["wlb", "b_lora"], writes=["ps3"])
                        K.op("act", lambda e, t0=t0, tn=tn, a0c=a0c: e.activation(bt["a"][:, t0:t0 + tn], ps[3][:, 0:tn], AF.Sigmoid, bias=a0c), reads=["ps3", "pk2"], writes=["b_a"])
                    K.op("dve", lambda e, bn=bn: e.tensor_tensor_scan(bt["cum"][:, 0:bn], rmask[:, 0:bn], bt["sig"][:, 0:bn], 0.0, op0=ALU.mult, op1=ALU.add),
                         reads=["rmask", "b_sig"], writes=["b_cum"])
                    c3 = bt["cum"][:, 0:bn].rearrange("p (n t) -> p n t", t=128)
                    tot_b = c3[:, :, 127:128].to_broadcast([128, ntb, 128])
                    K.op("act", lambda e, d_=d_, c3=c3, ntb=ntb: e.activation(Lam[d_][:, b0 // 128:b0 // 128 + ntb], c3[:, :, 127], AF.Exp, scale=-CW_),
                         reads=["b_cum"], writes=["Lam%d" % d_])
                    if d_ == 1:
                        K.op("dve", lambda e, bn=bn, c3=c3, tot_b=tot_b, ntb=ntb: e.tensor_tensor(
                            bt["t1"][:, 0:bn].rearrange("p (n t) -> p n t", t=128), tot_b, c3, op=ALU.subtract), reads=["b_cum"], writes=["b_t1"])
                        K.op("dve", lambda e, bn=bn: e.tensor_tensor(bt["cum"][:, 0:bn], bt["t1"][:, 0:bn], bt["sig"][:, 0:bn], op=ALU.add),
                             reads=["b_t1", "b_sig"], writes=["b_cum"])
                    K.op("act", lambda e, bn=bn: e.activation(bt["w"][:, 0:bn], bt["cum"][:, 0:bn], AF.Exp, scale=-CW_), reads=["b_cum"], writes=["b_w"])
                    K.op("act", lambda e, bn=bn: e.activation(bt["iw"][:, 0:bn], bt["cum"][:, 0:bn], AF.Exp, scale=CW_), reads=["b_cum"], writes=["b_iw"])
                    K.op("pool", lambda e, bn=bn: e.tensor_tensor(bt["t1"][:, 0:bn], bt["cum"][:, 0:bn], bt["sig"][:, 0:bn], op=ALU.subtract), reads=["b_cum", "b_sig"], writes=["b_t1"])
                    K.op("act", lambda e, bn=bn: e.activation(bt["wp"][:, 0:bn], bt["t1"][:, 0:bn], AF.Exp, scale=-CW_), reads=["b_t1"], writes=["b_wp"])
                    K.op("dve", lambda e, d_=d_, bn=bn: e.tensor_tensor(rT[d_][:, bs], bt["r"][:, 0:bn], bt["w"][:, 0:bn], op=ALU.mult), reads=["b_r", "b_w"], writes=["rT%d" % d_])
                    K.op("pool", lambda e, d_=d_, bn=bn: e.tensor_tensor(kpT[d_][:, bs], bt["kap"][:, 0:bn], bt["wp"][:, 0:bn], op=ALU.mult), reads=["b_kap", "b_wp"], writes=["kpT%d" % d_])
                    kac = pk2[:, PK2["KA"] + P:PK2["KA"] + P + 1]
                    K.op("dve", lambda e, bn=bn, kac=kac: e.tensor_scalar(bt["t1"][:, 0:bn], bt["a"][:, 0:bn], -1.0, kac, op0=ALU.add, op1=ALU.mult), reads=["b_a", "pk2"], writes=["b_t1"])
                    K.op("dve", lambda e, bn=bn: e.scalar_tensor_tensor(bt["t1"][:, 0:bn], bt["t1"][:, 0:bn], 1.0, bt["k"][:, 0:bn], op0=ALU.add, op1=ALU.mult), reads=["b_t1", "b_k"], writes=["b_t1"])
                    K.op("dve", lambda e, d_=d_, bn=bn: e.tensor_tensor(ktT[d_][:, bs], bt["t1"][:, 0:bn], bt["iw"][:, 0:bn], op=ALU.mult), reads=["b_t1", "b_iw"], writes=["ktT%d" % d_])
                    if first_rk:
                        K.op("pool", lambda e, bn=bn: e.tensor_tensor(bt["rk"][:, 0:bn], bt["t1"][:, 0:bn], bt["r"][:, 0:bn], op=ALU.mult), reads=["b_t1", "b_r"], writes=["b_rk"])
                        first_rk = False
                    else:
                        K.op("pool", lambda e, bn=bn: e.tensor_tensor(bt["t2"][:, 0:bn], bt["t1"][:, 0:bn], bt["r"][:, 0:bn], op=ALU.mult), reads=["b_t1", "b_r"], writes=["b_t2"])
                        K.op("pool", lambda e, bn=bn: e.tensor_tensor(bt["rk"][:, 0:bn], bt["rk"][:, 0:bn], bt["t2"][:, 0:bn], op=ALU.add), reads=["b_rk", "b_t2"], writes=["b_rk"])
                    K.op("dve", lambda e, bn=bn: e.scalar_tensor_tensor(bt["t2"][:, 0:bn], bt["kap"][:, 0:bn], -1.0, bt["a"][:, 0:bn], op0=ALU.mult, op1=ALU.mult), reads=["b_kap", "b_a"], writes=["b_t2"])
                    K.op("dve", lambda e, d_=d_, bn=bn: e.tensor_tensor(nbT[d_][:, bs], bt["t2"][:, 0:bn], bt["iw"][:, 0:bn], op=ALU.mult), reads=["b_t2", "b_iw"], writes=["nbT%d" % d_])
                rkc = pk2[:, PK2["RK"] + P:PK2["RK"] + P + 1]
                K.op("dve", lambda e, bn=bn, rkc=rkc: e.tensor_scalar(bt["rk"][:, 0:bn], bt["rk"][:, 0:bn], rkc, None, op0=ALU.mult), reads=["b_rk", "pk2"], writes=["b_rk"])
                for n in range((bn + 511) // 512):
                    t0 = n * 512; tn = min(512, bn - t0)
                    K.op("pe", lambda e, t0=t0, tn=tn: e.matmul(ps[4][:, 0:tn], lhsT=bones[:], rhs=bt["rk"][:, t0:t0 + tn], start=True, stop=True), reads=["bones", "b_rk"], writes=["ps4"])
                    K.op("dve", lambda e, t0=t0, tn=tn: e.tensor_tensor(bonus[:, b0 + t0:b0 + t0 + tn], ps[4][:, 0:tn], bt["v"][:, t0:t0 + tn], op=ALU.mult), reads=["ps4", "b_v"], writes=["bonus"])
            stop_at("rw_prep")
            for d_ in range(2):
                K.op("pool", lambda e, d_=d_: e.memset(Z[d_][:], 0.0), writes=["Z%d" % d_])
                K.op("pool", lambda e, d_=d_: e.memset(Zb[d_][:], 0.0), writes=["Zb%d" % d_])
            def r1(step, d_):
                i = fwd_order[step] if d_ == 0 else bwd_order[step]
                sl = slice(i * 128, (i + 1) * 128)
                want_o = i >= 2
                dk_ = "%d" % d_
                b_ = d_ * 4
                for hh in range(2):
                    hs = slice(hh * 64, (hh + 1) * 64)
                    pt = ps[b_ + hh]; pk = "ps%d" % (b_ + hh)
                    for qq, src in enumerate((ktT, nbT)):
                        K.op("pe", lambda e, src=src, pt=pt, qq=qq, hs=hs, d_=d_, sl=sl: e.matmul(pt[:, qq * 256:qq * 256 + 128], lhsT=src[d_][hs, sl], rhs=kpT[d_][hs, sl], start=True, stop=True),
                             reads=["ktT" + dk_, "nbT" + dk_, "kpT" + dk_], writes=[pk])
                        K.op("pe", lambda e, src=src, pt=pt, qq=qq, hs=hs, d_=d_, sl=sl: e.matmul(pt[:, qq * 256 + 128:qq * 256 + 256], lhsT=src[d_][hs, sl], rhs=rT[d_][hs, sl], start=True, stop=True),
                             reads=["ktT" + dk_, "nbT" + dk_, "rT" + dk_], writes=[pk])
                for hh in range(2):
                    K.op("dve", lambda e, d_=d_, hh=hh: e.tensor_tensor(AK[d_][:, hh * 256:(hh + 1) * 256], ps[d_ * 4 + hh][:, 0:256], mask4[d_][:, 0:256], op=ALU.mult),
                         reads=["ps%d" % (b_ + hh), "mask4"], writes=["AK" + dk_])
                    K.op("dve", lambda e, d_=d_, hh=hh: e.tensor_tensor(ANB[d_][:, hh * 256:(hh + 1) * 256], ps[d_ * 4 + hh][:, 256:512], mask4[d_][:, 0:256], op=ALU.mult),
                         reads=["ps%d" % (b_ + hh), "mask4"], writes=["ANB" + dk_])
                pT2 = psb[b_ + 2]; kT2 = "ps%d" % (b_ + 2)
                K.op("pe", lambda e, d_=d_, sl=sl, pT2=pT2: e.transpose(pT2[:, 0:128], ktT[d_][:, sl], identb[:]), reads=["ktT" + dk_, "identb"], writes=[kT2])
                K.op("pe", lambda e, d_=d_, sl=sl, pT2=pT2: e.transpose(pT2[:, 128:256], nbT[d_][:, sl], identb[:]), reads=["nbT" + dk_, "identb"], writes=[kT2])
                K.op("act", lambda e, d_=d_, pT2=pT2: e.copy(ktok_[d_][:], pT2[:, 0:128]), reads=[kT2], writes=["rktok" + dk_])
                K.op("act", lambda e, d_=d_, pT2=pT2: e.copy(nbtok_[d_][:], pT2[:, 128:256]), reads=[kT2], writes=["rnbtok" + dk_])
            def r2(step, d_, hh):
                dk_ = "%d" % d_
                tag = "n%d" % (d_ * 2 + hh)
                bank = d_ * 4 + hh
                kB2 = "ps%d" % bank
                K.op("pool", lambda e: e.tensor_copy(YY[d_][hh][:], ANB[d_][:, hh * 256:hh * 256 + 128]), reads=["ANB" + dk_], writes=[tag + "Y"])
                pB = (psb if RW_NM[0] == "bf16" else ps)[bank]
                idn_ = identb if RW_NM[0] == "bf16" else ident
                K.op("pe", lambda e: e.transpose(pB[:, 0:128], YY[d_][hh][:], idn_[:]), reads=[tag + "Y", "identb", "ident"], writes=[kB2])
                K.op("act", lambda e: e.copy(YYt[d_][hh][:], pB[:, 0:128]), reads=[kB2], writes=[tag + "Yt"])
                Ai, kAi = neumann(YY[d_][hh], YYt[d_][hh], tag, bank, pb=(bank, 384))
                K.op("dve", lambda e: e.tensor_copy(AinvS[d_][hh][:], Ai), reads=[kAi], writes=["Ainv%d%d" % (d_, hh)])

            def r3(step, d_):
                i = fwd_order[step] if d_ == 0 else bwd_order[step]
                sl = slice(i * 128, (i + 1) * 128)
                want_o = i >= 2
                dk_ = "%d" % d_
                b_ = d_ * 4
                pC = ps[b_ + 3]; kC = "ps%d" % (b_ + 3)
                for hh in range(2):
                    K.op("pe", lambda e, d_=d_, hh=hh, i=i, pC=pC: e.matmul(pC[:, hh * 64:(hh + 1) * 64], lhsT=AK[d_][:, hh * 256:hh * 256 + 128], rhs=vtk[:, i, hh * 64:(hh + 1) * 64], start=(hh == 0), stop=False),
                         reads=["AK" + dk_, "rvtok"], writes=[kC])
                K.op("pe", lambda e, d_=d_, sl=sl, pC=pC: e.matmul(pC[:, 0:128], lhsT=kpT[d_][:, sl], rhs=Zb[d_][:], start=False, stop=True), reads=["kpT" + dk_, "Zb" + dk_], writes=[kC])
                K.op("act", lambda e, d_=d_, pC=pC: e.copy(P1b[d_][:], pC[:, 0:128]), reads=[kC], writes=["P1b" + dk_])
                for hh in range(2):
                    K.op("pe", lambda e, d_=d_, hh=hh, pC=pC: e.matmul(pC[:, 128 + hh * 64:128 + (hh + 1) * 64], lhsT=AinvS[d_][hh][:], rhs=P1b[d_][:, hh * 64:(hh + 1) * 64], start=True, stop=True),
                         reads=["Ainv%d%d" % (d_, hh), "P1b" + dk_], writes=[kC])
                K.op("dve", lambda e, d_=d_, pC=pC: e.tensor_copy(Ub[d_][:], pC[:, 128:256]), reads=[kC], writes=["Ub" + dk_])
                if want_o:
                    for hh in range(2):
                        K.op("pe", lambda e, d_=d_, hh=hh, i=i, pC=pC: e.matmul(pC[:, 256 + hh * 64:256 + (hh + 1) * 64], lhsT=AK[d_][:, hh * 256 + 128:hh * 256 + 256], rhs=vtk[:, i, hh * 64:(hh + 1) * 64], start=(hh == 0), stop=False),
                             reads=["AK" + dk_, "rvtok"], writes=[kC])
                    K.op("pe", lambda e, d_=d_, sl=sl, pC=pC: e.matmul(pC[:, 256:384], lhsT=rT[d_][:, sl], rhs=Zb[d_][:], start=False, stop=False), reads=["rT" + dk_, "Zb" + dk_], writes=[kC])
                    for hh in range(2):
                        K.op("pe", lambda e, d_=d_, hh=hh, pC=pC: e.matmul(pC[:, 256 + hh * 64:256 + (hh + 1) * 64], lhsT=ANB[d_][:, hh * 256 + 128:hh * 256 + 256], rhs=Ub[d_][:, hh * 64:(hh + 1) * 64], start=False, stop=(hh == 1)),
                             reads=["ANB" + dk_, "Ub" + dk_], writes=[kC])
                    K.op("act", lambda e, d_=d_, i=i, pC=pC: e.copy(ybuf[d_][:, i, :], pC[:, 256:384]), reads=[kC], writes=["ybuf" + dk_])
                pD = ps[b_ + 2]; kD = "ps%d" % (b_ + 2)
                K.op("pe", lambda e, d_=d_, i=i, pD=pD: e.matmul(pD[:, 256:384], lhsT=ktok_[d_][:], rhs=vtk[:, i, :], start=True, stop=False), reads=["rktok" + dk_, "rvtok"], writes=[kD])
                K.op("pe", lambda e, d_=d_, pD=pD: e.matmul(pD[:, 256:384], lhsT=nbtok_[d_][:], rhs=Ub[d_][:], start=False, stop=True), reads=["rnbtok" + dk_, "Ub" + dk_], writes=[kD])
                K.op("dve", lambda e, d_=d_, pD=pD: e.tensor_tensor(zt[d_][:], pD[:, 256:384], Z[d_][:], op=ALU.add), reads=[kD, "Z" + dk_], writes=["zt" + dk_])
                K.op("dve", lambda e, d_=d_, i=i: e.scalar_tensor_tensor(Z[d_][:], zt[d_][:], Lam[d_][:, i:i + 1], bones[:], op0=ALU.mult, op1=ALU.mult),
                     reads=["zt" + dk_, "Lam" + dk_, "bones"], writes=["Z" + dk_])
                K.op("act", lambda e, d_=d_: e.copy(Zb[d_][:], Z[d_][:]), reads=["Z" + dk_], writes=["Zb" + dk_])
            for step in range(NT):
                conv_emit()
                for d_ in K.streams(2):
                    r1(step, d_)
                for q_ in K.streams(4):
                    r2(step, q_ // 2, q_ % 2)
                for d_ in K.streams(2):
                    r3(step, d_)
            stop_at("rw_scan")
            gwc = pk2[:, PK2["GNW"] + P:PK2["GNW"] + P + 1]
            gbc = pk2[:, PK2["GNB"] + P:PK2["GNB"] + P + 1]
            for i in range(2, NT):
                K.op("dve", lambda e, i=i: e.tensor_tensor(yo[:], ybuf[0][:, i, :], ybuf[1][:, i, :], op=ALU.add), reads=["ybuf0", "ybuf1"], writes=["yo"])
                y3 = yo[:].rearrange("p (h c) -> p h c", c=64)
                K.op("dve", lambda e, y3=y3: e.reduce_sum(yst[:, 0:2], y3, axis=AX.X), reads=["yo"], writes=["yst0"])
                K.op("dve", lambda e: e.tensor_scalar(yst[:, 2:4], yst[:, 0:2], 1.0 / 64, None, op0=ALU.mult), reads=["yst0"], writes=["yst1"])
                K.op("dve", lambda e, y3=y3: e.tensor_tensor(yc[:].rearrange("p (h c) -> p h c", c=64), y3, yst[:, 2:4].unsqueeze(2).to_broadcast([128, 2, 64]), op=ALU.subtract),
                     reads=["yo", "yst1"], writes=["yc"])
                K.op("act", lambda e: e.activation(ysq[:], yc[:], AF.Square), reads=["yc"], writes=["ysq"])
                K.op("dve", lambda e: e.reduce_sum(yst[:, 4:6], ysq[:].rearrange("p (h c) -> p h c", c=64), axis=AX.X), reads=["ysq"], writes=["yst2"])
                K.op("act", lambda e: e.activation(yst[:, 6:8], yst[:, 4:6], AF.Sqrt, bias=eps_t[:, 2:3], scale=1.0 / 64), reads=["yst2", "eps"], writes=["yst3"])
                K.op("dve", lambda e: e.reciprocal(yst[:, 6:8], yst[:, 6:8]), reads=["yst3"], writes=["yst3"])
                K.op("dve", lambda e: e.tensor_tensor(yt2[:].rearrange("p (h c) -> p h c", c=64), yc[:].rearrange("p (h c) -> p h c", c=64),
                                                      yst[:, 6:8].unsqueeze(2).to_broadcast([128, 2, 64]), op=ALU.mult), reads=["yc", "yst3"], writes=["yt2"])
                pt = ps[i % 2]; pk = "ps%d" % (i % 2)
                K.op("pe", lambda e, pt=pt: e.transpose(pt[:, 0:128], yt2[:], ident[:]), reads=["yt2", "ident"], writes=[pk])
                K.op("act", lambda e, pt=pt: e.activation(yc[:], pt[:, 0:128], AF.Identity, bias=gbc, scale=gwc), reads=[pk, "pk2", "yc"], writes=["yc"])
                K.op("dve", lambda e, i=i: e.tensor_tensor(yc[:], yc[:], bonus[:, i * 128:(i + 1) * 128], op=ALU.add), reads=["yc", "bonus"], writes=["yc"])
                K.op("dve", lambda e, i=i: e.tensor_tensor(rmst[:, (i - 2) * 128:(i - 1) * 128], yc[:], gateT[:, i * 128:(i + 1) * 128], op=ALU.mult), reads=["yc", "gateT"], writes=["rmst"])
            K.dma(mT_d[4 + P, :, :], rmst[:, :], reads=["rmst"], writes=[("mT", 4 + P)], key="st_rmst")
        while conv_n[1] < 128:
            conv_emit()
    K.barrier()

    stop_at("mix_done")
    with ExitStack() as pp_:
        mTs = [sb("mTs%d" % i_, [128, 8, 128], BF16, stack=pp_) for i_ in range(2)]
        woutb = sb("woutb", [128, 8, D], BF16, stack=pp_)
        wqb = sb("wqb", [128, 8, 2048], BF16, stack=pp_)
        skT = sb("skT", [128, 16, 128], BF16, stack=pp_)
        with ExitStack() as pset:
            wstg = sb("wstg", [128, 8, 512], stack=pset)
            skst = sb("skst", [128, 16, 128], stack=pset)
            for hf in range(2):
                K.dma(wstg[:, :, :], wout_d[:, hf * 512:(hf + 1) * 512].rearrange("(k p) c -> p k c", p=128), writes=["wstg"], key="wstg")
                K.op("pool", lambda e, hf=hf: e.tensor_copy(woutb[:, :, hf * 512:(hf + 1) * 512], wstg[:]), reads=["wstg"], writes=["woutb"])
            for hf in range(4):
                K.dma(wstg[:, :, :], wq_d[:, hf * 512:(hf + 1) * 512].rearrange("(k p) c -> p k c", p=128), writes=["wstg"], key="wstg")
                K.op("pool", lambda e, hf=hf: e.tensor_copy(wqb[:, :, hf * 512:(hf + 1) * 512], wstg[:]), reads=["wstg"], writes=["wqb"])
            K.dma(skst[:, :, :], sk_d[:, :, :].rearrange("g k d -> k g d"), writes=["skst"], key="skst")
            for g in range(16):
                pt = ps[g % 2]; pk = "ps%d" % (g % 2)
                K.op("pe", lambda e, g=g, pt=pt: e.transpose(pt[:, 0:128], skst[:, g, :], ident[:]), reads=["skst", "ident"], writes=[pk])
                K.op("act", lambda e, g=g, pt=pt: e.copy(skT[:, g, :], pt[:, 0:128]), reads=[pk], writes=["skT"])
            K.barrier()
        gtB = sb("gtB2", [128, 4, D], stack=pp_)
        K.dma(gtB[:].rearrange("p q d -> p (q d)"), gt_d[:, :], reads=["gt_d"], writes=["gtB"], key="gtB2")
        A2row = sb("A2row", [128, D], stack=pp_)
        fgB = sb("fgB", [128, D], stack=pp_)
        K.dma(A2row[:, :], g2row_d.partition_broadcast(128), writes=["A2row"], key="A2row")
        K.dma(fgB[:, :], fng_d.partition_broadcast(128), writes=["fgB"], key="fgB")
        K.op("dve", lambda e: e.scalar_tensor_tensor(A2row[:], gtB[:, 2, :], 1.0, A2row[:], op0=ALU.add, op1=ALU.mult), reads=["gtB", "A2row"], writes=["A2row"])
        iota16i = sb("iota16i", [128, 16], I32, stack=pp_)
        iota16 = sb("iota16", [128, 16], stack=pp_)
        K.op("pool", lambda e: e.iota(iota16i[:], pattern=[[1, 16]], base=0, channel_multiplier=0), writes=["iota16i"])
        K.op("dve", lambda e: e.tensor_copy(iota16[:], iota16i[:]), reads=["iota16i"], writes=["iota16"])

        xt_ = sb("p_xt", [128, D], stack=pp_)
        x1 = sb("p_x1", [128, D], stack=pp_)
        h2 = sb("p_h2", [128, D], stack=pp_)
        yacc = sb("p_y", [128, D], stack=pp_)
        pj = sb("p_junk", [128, D], stack=pp_)
        pss = sb("p_ss", [128, 1], stack=pp_)
        prs = sb("p_rs", [128, 2], stack=pp_)
        pxs = sb("p_xs", [128, D], stack=pp_)
        h2T = sb("p_h2T", [128, 8, 128], BF16, stack=pp_)
        qTs = sb("p_qT", [128, 16, 128], BF16, stack=pp_)
        scs = sb("p_sc", [128, 16, 128], stack=pp_)
        tmp1 = sb("p_tmp1", [128, 16, 128], stack=pp_)
        tv = sb("p_tv", [128, 16, 16], stack=pp_)
        tiu = sb("p_tiu", [128, 16, 16], U32, stack=pp_)
        tif = sb("p_tif", [128, 16, 16], stack=pp_)
        cand = sb("p_cand", [128, 8, 256], stack=pp_)
        tmp2 = sb("p_tmp2", [128, 8, 256], stack=pp_)
        eq = tmp2
        bsv = sb("p_bs", [128, 8, 16], stack=pp_)
        posu = sb("p_posu", [128, 8, 16], U32, stack=pp_)
        pau = sb("p_pau", [128, 8, 16], U32, stack=pp_)
        pbu = sb("p_pbu", [128, 8, 16], U32, stack=pp_)
        paf = sb("p_paf", [128, 8, 16], stack=pp_)
        pbf = sb("p_pbf", [128, 8, 16], stack=pp_)
        i0f = sb("p_i0f", [128, 8, 16], stack=pp_)
        i1f = sb("p_i1f", [128, 8, 16], stack=pp_)
        eidx = sb("p_eidx", [128, 128], U32, stack=pp_)
        gat = sb("p_gate", [128, 8, 16], stack=pp_)
        gsum = sb("p_gsum", [128, 8], stack=pp_)
        apre = sb("p_apre", [128, 128], stack=pp_)
        coef = sb("p_coef", [128, 128], stack=pp_)
        NRB = 8
        rowc = [sb("p_rowc%d" % i, [128, 2 * D], BF16, stack=pp_) for i in range(NRB)]
        pjb = sb("p_junkb", [128, D], BF16, stack=pp_)
        h2b = sb("p_h2b", [128, D], BF16, stack=pp_)
        dgs = [sb("p_dg%d" % i, [128, 128], BF16, stack=pp_) for i in range(4)]
        gflat = sb("p_gflat", [128, 128], stack=pp_)
        NEG = -1.0e30

        for i in range(NTL):
            tsl = slice(i * 128, (i + 1) * 128)
            K.dma(xt_[:, :], x_d[tsl, :], writes=["p_xt"], key="p_xt")
            mTt = mTs[i % 2]; mk_ = "mTs%d" % (i % 2)
            K.dma(mTt[:, :, :], mT_d[:, :, tsl].rearrange("k p t -> p k t"), reads=[("mT", j) for j in range(8)], writes=[mk_], key=mk_)
            for hf in range(2):
                pt = ps[hf]; pk = "ps%d" % hf
                for k in range(8):
                    K.op("pe", lambda e, k=k, hf=hf, pt=pt, mTt=mTt: e.matmul(pt[:, :], lhsT=mTt[:, k, :], rhs=woutb[:, k, hf * 512:(hf + 1) * 512], start=(k == 0), stop=(k == 7)),
                         reads=[mk_, "woutb"], writes=[pk])
                K.op("dve", lambda e, hf=hf, pt=pt: e.tensor_tensor(x1[:, hf * 512:(hf + 1) * 512], pt[:, :], gtB[:, 0, hf * 512:(hf + 1) * 512], op=ALU.mult),
                     reads=[pk, "gtB"], writes=["p_x1"])
            K.op("pool", lambda e: e.tensor_tensor(x1[:], x1[:], xt_[:], op=ALU.add), reads=["p_x1", "p_xt"], writes=["p_x1"])
            K.op("act", lambda e: e.activation(pj[:], x1[:], AF.Square), reads=["p_x1"], writes=["p_junk"])
            K.op("dve", lambda e: e.reduce_sum(pss[:, 0:1], pj[:], axis=AX.X), reads=["p_junk"], writes=["p_ss"])
            K.op("act", lambda e: e.activation(prs[:, 0:1], pss[:, 0:1], AF.Sqrt, bias=eps_t[:, 0:1], scale=1.0 / D), reads=["p_ss", "eps"], writes=["p_rs"])
            K.op("dve", lambda e: e.reciprocal(prs[:, 1:2], prs[:, 0:1]), reads=["p_rs"], writes=["p_rs2"])
            K.op("dve", lambda e: e.tensor_scalar(pxs[:], x1[:], prs[:, 1:2], None, op0=ALU.mult), reads=["p_x1", "p_rs2"], writes=["p_xs"])
            for hf in range(2):
                pt = ps[2 + hf]; pk = "ps%d" % (2 + hf)
                for kk in range(4):
                    k = hf * 4 + kk
                    K.op("pe", lambda e, k=k, kk=kk, pt=pt: e.transpose(pt[:, kk * 128:(kk + 1) * 128], pxs[:, k * 128:(k + 1) * 128], ident[:]), reads=["p_xs", "ident"], writes=[pk])
                for kk in range(4):
                    k = hf * 4 + kk
                    K.op("act", lambda e, k=k, kk=kk, pt=pt: e.activation(h2T[:, k, :], pt[:, kk * 128:(kk + 1) * 128], AF.Identity, bias=B2[:, k:k + 1], scale=A2[:, k:k + 1]),
                         reads=[pk, "mods"], writes=["p_h2T"])
            K.op("dve", lambda e: e.tensor_tensor(h2[:], pxs[:], A2row[:], op=ALU.mult), reads=["p_xs", "A2row"], writes=["p_h2"])
            K.op("pool", lambda e: e.tensor_tensor(h2[:], h2[:], gtB[:, 1, :], op=ALU.add), reads=["p_h2", "gtB"], writes=["p_h2"])
            for g in range(16):
                pt = ps[4 + (g % 2)]; pk = "ps%d" % (4 + (g % 2))
                for k in range(8):
                    K.op("pe", lambda e, g=g, k=k, pt=pt: e.matmul(pt[:, 0:128], lhsT=wqb[:, k, g * 128:(g + 1) * 128], rhs=h2T[:, k, :], start=(k == 0), stop=(k == 7)),
                         reads=["wqb", "p_h2T"], writes=[pk])
                K.op("act", lambda e, g=g, pt=pt: e.copy(qTs[:, g, :], pt[:, 0:128]), reads=[pk], writes=[("p_qT", g)])
            for g in range(16):
                pt = ps[6 + (g // 4) % 2]; pk = "ps%d" % (6 + (g // 4) % 2)
                K.op("pe", lambda e, g=g, pt=pt: e.matmul(pt[:, (g % 4) * 128:(g % 4 + 1) * 128], lhsT=qTs[:, g, :], rhs=skT[:, g, :], start=True, stop=True),
                     reads=[("p_qT", g), "skT"], writes=[pk])
                if g % 4 == 3:
                    K.op("dve", lambda e, g=g, pt=pt: e.tensor_copy(scs[:, g - 3:g + 1, :].rearrange("p g k -> p (g k)"), pt[:, :]), reads=[pk], writes=[("p_sc", g // 4)])
            for g in range(16):
                K.op("dve", lambda e, g=g: e.max(tv[:, g, 0:8], scs[:, g, :]), reads=[("p_sc", g // 4)], writes=[("tv", g)])
            for g in range(16):
                K.op("dve", lambda e, g=g: e.max_index(tiu[:, g, 0:8], tv[:, g, 0:8], scs[:, g, :]), reads=[("p_sc", g // 4), ("tv", g)], writes=[("tiu", g)])
            for g in range(16):
                K.op("dve", lambda e, g=g: e.match_replace(tmp1[:, g, :], tv[:, g, 0:8], scs[:, g, :], NEG), reads=[("p_sc", g // 4), ("tv", g)], writes=[("tmp1", g)])
            for g in range(16):
                K.op("dve", lambda e, g=g: e.max(tv[:, g, 8:16], tmp1[:, g, :]), reads=[("tmp1", g)], writes=[("tv2", g)])
            for g in range(16):
                K.op("dve", lambda e, g=g: e.max_index(tiu[:, g, 8:16], tv[:, g, 8:16], tmp1[:, g, :]), reads=[("tmp1", g), ("tv2", g)], writes=[("tiu2", g)])
            allg = [("tv", g) for g in range(16)] + [("tv2", g) for g in range(16)]
            alli = [("tiu", g) for g in range(16)] + [("tiu2", g) for g in range(16)]
            K.op("dve", lambda e: e.tensor_copy(tif[:], tiu[:]), reads=alli, writes=["p_tif"])
            tvv = tv[:].rearrange("p (h q) a -> p h q a", q=2)
            tfv = tif[:].rearrange("p (h q) a -> p h q a", q=2)
            c4 = cand[:].rearrange("p h (a b) -> p h a b", b=16)
            K.op("dve", lambda e: e.tensor_tensor(c4, tvv[:, :, 0, :].unsqueeze(3).to_broadcast([128, 8, 16, 16]),
                                                  tvv[:, :, 1, :].unsqueeze(2).to_broadcast([128, 8, 16, 16]), op=ALU.add), reads=allg, writes=["p_cand"])
            for hh in range(8):
                K.op("dve", lambda e, hh=hh: e.max(bsv[:, hh, 0:8], cand[:, hh, :]), reads=["p_cand"], writes=[("bs", hh)])
            for hh in range(8):
                K.op("dve", lambda e, hh=hh: e.max_index(posu[:, hh, 0:8], bsv[:, hh, 0:8], cand[:, hh, :]), reads=["p_cand", ("bs", hh)], writes=[("pos", hh)])
            for hh in range(8):
                K.op("dve", lambda e, hh=hh: e.match_replace(tmp2[:, hh, :], bsv[:, hh, 0:8], cand[:, hh, :], NEG), reads=["p_cand", ("bs", hh), "p_eq"], writes=[("tmp2", hh)])
            for hh in range(8):
                K.op("dve", lambda e, hh=hh: e.max(bsv[:, hh, 8:16], tmp2[:, hh, :]), reads=[("tmp2", hh)], writes=[("bs2", hh)])
            for hh in range(8):
                K.op("dve", lambda e, hh=hh: e.max_index(posu[:, hh, 8:16], bsv[:, hh, 8:16], tmp2[:, hh, :]), reads=[("tmp2", hh), ("bs2", hh)], writes=[("pos2", hh)])
            allb = [("bs", hh) for hh in range(8)] + [("bs2", hh) for hh in range(8)]
            allp = [("pos", hh) for hh in range(8)] + [("pos2", hh) for hh in range(8)]
            K.op("dve", lambda e: e.tensor_single_scalar(pau[:], posu[:], 4, op=ALU.logical_shift_right), reads=allp, writes=["p_pau"])
            K.op("dve", lambda e: e.tensor_single_scalar(pbu[:], posu[:], 15, op=ALU.bitwise_and), reads=allp, writes=["p_pbu"])
            K.op("dve", lambda e: e.tensor_copy(paf[:], pau[:]), reads=["p_pau"], writes=["p_paf"])
            K.op("dve", lambda e: e.tensor_copy(pbf[:], pbu[:]), reads=["p_pbu"], writes=["p_pbf"])
            e4 = eq[:].rearrange("p h (k a) -> p h k a", a=16)
            io4 = iota16[:].unsqueeze(1).unsqueeze(1).to_broadcast([128, 8, 16, 16])
            for (pf, q_, dst, nm) in ((paf, 0, i0f, "i0f"), (pbf, 1, i1f, "i1f")):
                K.op("dve", lambda e, pf=pf: e.tensor_tensor(e4, pf[:].unsqueeze(3).to_broadcast([128, 8, 16, 16]), io4, op=ALU.is_equal),
                     reads=["p_paf", "p_pbf", "iota16"], writes=["p_eq"] + [("tmp2", hh_) for hh_ in range(8)])
                K.op("dve", lambda e, q_=q_: e.tensor_tensor(e4, e4, tfv[:, :, q_, :].unsqueeze(2).to_broadcast([128, 8, 16, 16]), op=ALU.mult),
                     reads=["p_eq", "p_tif"], writes=["p_eq"])
                K.op("dve", lambda e, dst=dst: e.reduce_sum(dst[:], e4, axis=AX.X), reads=["p_eq"], writes=["p_" + nm])
            K.op("dve", lambda e: e.scalar_tensor_tensor(i0f[:], i0f[:], 128.0, i1f[:], op0=ALU.mult, op1=ALU.add), reads=["p_i0f", "p_i1f"], writes=["p_i0f"])
            K.op("dve", lambda e: e.tensor_copy(eidx[:], i0f[:].rearrange("p h k -> p (h k)")), reads=["p_i0f"], writes=["p_eidx"])
            K.op("dve", lambda e: e.tensor_tensor(gat[:], bsv[:], bsv[:, :, 0:1].to_broadcast([128, 8, 16]), op=ALU.subtract), reads=allb, writes=["p_gate"])
            K.op("act", lambda e: e.activation(gat[:], gat[:], AF.Exp), reads=["p_gate"], writes=["p_gate"])
            K.op("dve", lambda e: e.reduce_sum(gsum[:], gat[:], axis=AX.X), reads=["p_gate"], writes=["p_gsum"])
            K.op("dve", lambda e: e.reciprocal(gsum[:], gsum[:]), reads=["p_gsum"], writes=["p_gsum"])
            K.op("dve", lambda e: e.tensor_tensor(gat[:], gat[:], gsum[:].unsqueeze(2).to_broadcast([128, 8, 16]), op=ALU.mult), reads=["p_gate", "p_gsum"], writes=["p_gate"])
            K.op("act", lambda e: e.copy(h2b[:], h2[:]), reads=["p_h2"], writes=["p_h2b"])
            K.op("dve", lambda e: e.tensor_copy(gflat[:], gat[:].rearrange("p h k -> p (h k)")), reads=["p_gate"], writes=["p_gflat"])
            GRP = 4
            for g0 in range(0, 128, GRP):
                for kslot in range(g0, g0 + GRP):
                    rb = rowc[kslot % NRB]; rk_ = "p_rowc%d" % (kslot % NRB)
                    K.gather(rb[:, :], comb_d[:, :], eidx[:, kslot:kslot + 1], reads=["p_eidx", "comb"], writes=[rk_], key=rk_)
                    K.op("dve", lambda e, rb=rb, kslot=kslot: e.scalar_tensor_tensor(pjb[:], rb[:, 0:D], 1.0, h2b[:], op0=ALU.mult, op1=ALU.mult, accum_out=apre[:, kslot:kslot + 1]),
                         reads=[rk_, "p_h2b"], writes=["p_junkb", ("apre", g0 // GRP)])
                K.op("dve", lambda e, g0=g0: e.tensor_copy(coef[:, g0:g0 + GRP], apre[:, g0:g0 + GRP]), reads=[("apre", g0 // GRP)], writes=[("cf0", g0 // GRP)])
                K.op("act", lambda e, g0=g0: e.activation(coef[:, g0:g0 + GRP], coef[:, g0:g0 + GRP], AF.Gelu), reads=[("cf0", g0 // GRP)], writes=[("cf1", g0 // GRP)])
                K.op("dve", lambda e, g0=g0: e.tensor_tensor(coef[:, g0:g0 + GRP], coef[:, g0:g0 + GRP], gflat[:, g0:g0 + GRP], op=ALU.mult),
                     reads=[("cf1", g0 // GRP), "p_gflat"], writes=[("cf2", g0 // GRP)])
                for kslot in range(g0, g0 + GRP):
                    rb = rowc[kslot % NRB]; rk_ = "p_rowc%d" % (kslot % NRB)
                    dg = dgs[kslot % 4]; dk__ = "p_dg%d" % (kslot % 4)
                    K.op("act", lambda e, dg=dg, kslot=kslot: e.activation(dg[:], identb[:], AF.Identity, scale=coef[:, kslot:kslot + 1]),
                         reads=["identb", ("cf2", g0 // GRP)], writes=[dk__])
                    for hf in range(2):
                        K.op("pe", lambda e, dg=dg, rb=rb, hf=hf, kslot=kslot: e.matmul(ps[hf][:, :], lhsT=dg[:], rhs=rb[:, D + hf * 512:D + (hf + 1) * 512],
                                                                                       start=(kslot == 0), stop=(kslot == 127)),
                             reads=[dk__, rk_], writes=["ps%d" % hf])
            for hf in range(2):
                K.op("act", lambda e, hf=hf: e.copy(yacc[:, hf * 512:(hf + 1) * 512], ps[hf][:, :]), reads=["ps%d" % hf], writes=["p_y"])
            K.op("dve", lambda e: e.tensor_tensor(yacc[:], yacc[:], gtB[:, 3, :], op=ALU.mult), reads=["p_y", "gtB"], writes=["p_y"])
            K.op("pool", lambda e: e.tensor_tensor(yacc[:], yacc[:], x1[:], op=ALU.add), reads=["p_y", "p_x1"], writes=["p_y"])
            K.op("act", lambda e: e.activation(pj[:], yacc[:], AF.Square), reads=["p_y"], writes=["p_junk"])
            K.op("dve", lambda e: e.reduce_sum(pss[:, 0:1], pj[:], axis=AX.X), reads=["p_junk"], writes=["p_ss"])
            K.op("act", lambda e: e.activation(prs[:, 0:1], pss[:, 0:1], AF.Sqrt, bias=eps_t[:, 0:1], scale=1.0 / D), reads=["p_ss", "eps"], writes=["p_rs"])
            K.op("dve", lambda e: e.reciprocal(prs[:, 1:2], prs[:, 0:1]), reads=["p_rs"], writes=["p_rs2"])
            K.op("dve", lambda e: e.scalar_tensor_tensor(pxs[:], yacc[:], prs[:, 1:2], fgB[:], op0=ALU.mult, op1=ALU.mult), reads=["p_y", "p_rs2", "fgB"], writes=["p_xs"])
            K.dma(out_d[tsl, :], pxs[:, :], reads=["p_xs"], writes=["outdone"], key="st_out")
            if i == 0:
                stop_at("peer_t0")
    K.barrier()
    if "mT" in dbg:
        d_o = dbgt("mT", [8, 128, TL], BF16)
        K.dma(d_o[:, :, :], mT_d[:, :, :], reads=[("mT", j) for j in range(8)], writes=["dbgmT"], key="dbg")

    K.finish([k for k in K.st.keys() if (isinstance(k, str) and k.startswith("dbg")) or k == "outdone"])
    return dbg_out


def _inputs_for_core(inp, b, n_rows):
    TL = 64 * n_rows
    f = lambda a: np.ascontiguousarray(np.asarray(a, dtype=np.float32))
    m = {
        "x": f(inp["x"][b, :TL]),
        "c": f(inp["c"][b:b + 1]),
        "ctx": f(inp["ctx"][b]),
        "c_ctx": f(inp["c_ctx"][None, :]),
        "ada_w": f(inp["ada_w"][0]),
        "ada_b": f(inp["ada_b"][0].reshape(48, 128)),
        "ada_b_row": f(inp["ada_b"][0].reshape(1, 6144)),
        "norm1_g": f(inp["norm1_g"][0].reshape(8, 128)),
        "w_in": f(inp["w_in"][0]),
        "gdn_conv_w": f(inp["gdn_conv_w"][0].reshape(60, 128)),
        "gdn_a_log": f(inp["gdn_a_log"][0].reshape(1, 8)),
        "gdn_dt_bias": f(inp["gdn_dt_bias"][0].reshape(1, 8)),
        "gdn_norm_w": f(inp["gdn_norm_w"][0].reshape(1, 128)),
        "rwkv_mu": f(inp["rwkv_mu"][0].reshape(15, 128)),
        "rwkv_w0": f(inp["rwkv_w0"][0].reshape(8, 128)),
        "rwkv_w2": f(inp["rwkv_w2"][0].reshape(128, 512)),
        "rwkv_a0": f(inp["rwkv_a0"][0].reshape(8, 128)),
        "rwkv_a2": f(inp["rwkv_a2"][0].reshape(128, 512)),
        "rwkv_g2": f(inp["rwkv_g2"][0]),
        "rwkv_k_k": f(inp["rwkv_k_k"][0].reshape(4, 128)),
        "rwkv_k_a": f(inp["rwkv_k_a"][0].reshape(4, 128)),
        "rwkv_r_k": f(inp["rwkv_r_k"][0].reshape(4, 128)),
        "rwkv_gn_w": f(inp["rwkv_gn_w"][0].reshape(4, 128)),
        "rwkv_gn_b": f(inp["rwkv_gn_b"][0].reshape(4, 128)),
        "w_out": f(inp["w_out"][0]),
        "norm2_g": f(inp["norm2_g"][0].reshape(8, 128)),
        "peer_w_query": f(inp["peer_w_query"][0]),
        "peer_sub_keys": f(inp["peer_sub_keys"][0].reshape(16, 128, 128)),
        "peer_down": f(inp["peer_down"][0]),
        "peer_up": f(inp["peer_up"][0]),
        "final_norm_g": f(inp["final_norm_g"][None, :]),
        "norm2_g_row": f(inp["norm2_g"][0].reshape(1, 1024)),
    }
    return m


def run(inp, n_rows=64, cores=None, dbg=(), stop=None):
    nb = inp["x"].shape[0]
    cores = list(range(nb)) if cores is None else cores
    nc = bass.Bass("TRN2", target_bir_lowering=False)
    build(nc, n_rows=n_rows, dbg=dbg, stop=stop)
    in_maps = [_inputs_for_core(inp, b, n_rows) for b in cores]
    res = run_bass_kernel_spmd(nc, in_maps, core_ids=list(range(len(cores))))
    return res.results


def kernel(**inputs):
    res = run(inputs, n_rows=64)
    return np.stack([np.asarray(r["out"], dtype=np.float32) for r in res], axis=0)
```

```python
from contextlib import ExitStack
import numpy as np
import concourse.bass as bass
import concourse.mybir as mybir
from concourse.bass_utils import run_bass_kernel_spmd

F32 = mybir.dt.float32
BF16 = mybir.dt.bfloat16
I32 = mybir.dt.int32
U32 = mybir.dt.uint32
AF = mybir.ActivationFunctionType
ALU = mybir.AluOpType
AX = mybir.AxisListType

D = 1024
TC = 256
IN_COLS = 3984
GDN_COLS = 2064
NORM_EPS = 1e-6
L2_EPS = 1e-6
GN_EPS = 64e-5


class Ctx:
    def __init__(self, nc):
        self.nc = nc
        self.es = ExitStack()
        self.eng = dict(pe=nc.tensor, act=nc.scalar, dve=nc.vector, pool=nc.gpsimd, sp=nc.sync)
        self.csem = {}
        self.cnt = {}
        for e in ("pe", "act", "dve", "pool"):
            self.csem[e] = self.es.enter_context(nc.semaphore("cs_" + e))
            self.cnt[e] = 0
        self.dsem = {}
        self.seen = {e: {} for e in self.eng}
        self.st = {}
        self.ninst = 0
        self._rec = None

    def _sem(self, sk):
        if sk[0] == "c":
            return self.csem[sk[1]]
        return self.dsem[sk[1]][0]

    def _deps(self, reads, writes, e=None):
        need = {}

        def add(m):
            if m is None:
                return
            sk, v = m
            if need.get(sk, 0) < v:
                need[sk] = v

        for r in reads:
            s = self.st.get(r)
            if s is not None:
                add(s[0])
                if isinstance(r, str) and r.startswith("ps") and r[2:].isdigit():
                    for sk, v in s[1].items():
                        if sk != ("c", e):
                            add((sk, v))
        for w in writes:
            s = self.st.get(w)
            if s is not None:
                add(s[0])
                for sk, v in s[1].items():
                    add((sk, v))
        return need

    def _wait(self, e, need):
        eng = self.eng[e]
        seen = self.seen[e]
        for sk, v in need.items():
            if e == "pe" and sk == ("c", "pe"):
                continue
            if sk[0] == "d":
                v = max(v, self.dsem[sk[1]][1])
            if seen.get(sk, 0) >= v:
                continue
            eng.wait_ge(self._sem(sk), v)
            seen[sk] = v

    def _mark(self, mark, reads, writes):
        for w in writes:
            self.st[w] = [mark, {}]
        for r in reads:
            s = self.st.get(r)
            if s is None:
                s = self.st[r] = [None, {}]
            sk, v = mark
            if s[1].get(sk, 0) < v:
                s[1][sk] = v

    def streams(self, n):
        lists = []
        for d in range(n):
            self._rec = []
            yield d
            lists.append(self._rec)
            self._rec = None
        idx = [0] * n
        left = sum(len(l) for l in lists)
        while left:
            for d in range(n):
                if idx[d] < len(lists[d]):
                    kind, args, kw = lists[d][idx[d]]
                    idx[d] += 1
                    left -= 1
                    getattr(self, kind)(*args, **kw)

    def op(self, e, fn, reads=(), writes=()):
        if self._rec is not None:
            self._rec.append(("op", (e, fn, tuple(reads), tuple(writes)), {}))
            return
        need = self._deps(reads, writes, e)
        self._wait(e, need)
        ins = fn(self.eng[e])
        ins.then_inc(self.csem[e], 1)
        self.cnt[e] += 1
        self.ninst += 1
        self._mark((("c", e), self.cnt[e]), reads, writes)

    def dma(self, out, in_, reads=(), writes=(), key=None, q="sp", **kw):
        if self._rec is not None:
            self._rec.append(("dma", (out, in_, tuple(reads), tuple(writes), key, q), kw))
            return
        if key not in self.dsem:
            self.dsem[key] = [self.es.enter_context(self.nc.semaphore("ds_%d" % len(self.dsem))), 0]
        need = self._deps(reads, writes)
        self._wait(q, need)
        d = self.dsem[key]
        self.eng[q].dma_start(out=out, in_=in_, **kw).then_inc(d[0], 16)
        d[1] += 16
        self.ninst += 1
        self._mark((("d", key), d[1]), reads, writes)

    def gather(self, out, in_, idx_ap, reads=(), writes=(), key=None):
        if key not in self.dsem:
            self.dsem[key] = [self.es.enter_context(self.nc.semaphore("ds_%d" % len(self.dsem))), 0]
        need = self._deps(reads, writes)
        self._wait("pool", need)
        d = self.dsem[key]
        self.nc.gpsimd.indirect_dma_start(
            out=out, out_offset=None, in_=in_,
            in_offset=bass.IndirectOffsetOnAxis(ap=idx_ap, axis=0)).then_inc(d[0], 16)
        d[1] += 16
        self.ninst += 1
        self._mark((("d", key), d[1]), reads, writes)

    def barrier(self):
        need = {("c", e): v for e, v in self.cnt.items() if v > 0}
        for k, d in self.dsem.items():
            if d[1] > 0:
                need[("d", k)] = d[1]
        for e in self.eng:
            self._wait(e, need)

    def finish(self, keys):
        need = self._deps(keys, ())
        self._wait("sp", need)


NM_MODE = ["f32"]
RW_NM = ["bf16"]


class _Stop(Exception):
    pass


def build(nc, n_rows=64, dbg=(), stop=None):
    K = Ctx(nc)
    try:
        return _build(nc, K, n_rows, dbg, stop)
    except _Stop:
        K.barrier()
        return None


def _build(nc, K, n_rows, dbg, stop):
    def stop_at(name):
        if stop == name:
            raise _Stop()

    TL = 64 * n_rows
    T = TC + TL
    NT = T // 128
    NTL = TL // 128
    es = K.es

    def din(name, shape, dt=F32):
        return nc.dram_tensor(name, list(shape), dt, kind="ExternalInput").ap()

    x_d = din("x", [TL, D])
    c_d = din("c", [1, D])
    ctx_d = din("ctx", [TC, D])
    cctx_d = din("c_ctx", [1, D])
    adaw_d = din("ada_w", [D, 6144])
    adab_d = din("ada_b", [48, 128])
    adabr_d = din("ada_b_row", [1, 6144])
    g1_d = din("norm1_g", [8, 128])
    win_d = din("w_in", [D, IN_COLS])
    convw_d = din("gdn_conv_w", [60, 128])
    alog_d = din("gdn_a_log", [1, 8])
    dtb_d = din("gdn_dt_bias", [1, 8])
    gnorm_d = din("gdn_norm_w", [1, 128])
    mu_d = din("rwkv_mu", [15, 128])
    w0_d = din("rwkv_w0", [8, 128])
    w2_d = din("rwkv_w2", [128, 512])
    a0_d = din("rwkv_a0", [8, 128])
    a2_d = din("rwkv_a2", [128, 512])
    g2w_d = din("rwkv_g2", [128, 512])
    kk_d = din("rwkv_k_k", [4, 128])
    ka_d = din("rwkv_k_a", [4, 128])
    rk_d = din("rwkv_r_k", [4, 128])
    gnw_d = din("rwkv_gn_w", [4, 128])
    gnb_d = din("rwkv_gn_b", [4, 128])
    wout_d = din("w_out", [D, D])
    g2_d = din("norm2_g", [8, 128])
    wq_d = din("peer_w_query", [D, 2048])
    sk_d = din("peer_sub_keys", [16, 128, 128])
    down_d = din("peer_down", [16384, D])
    up_d = din("peer_up", [16384, D])
    fng_d = din("final_norm_g", [1, D])
    g2row_d = din("norm2_g_row", [1, D])
    out_d = nc.dram_tensor("out", [TL, D], F32, kind="ExternalOutput").ap()

    dbg_out = {}

    def dbgt(name, shape, dt=F32):
        dbg_out[name] = nc.dram_tensor("dbg_" + name, list(shape), dt, kind="ExternalOutput").ap()
        return dbg_out[name]

    pT_d = nc.dram_tensor("pT_s", [32, 128, T], F32).ap()

    def sb(name, shape, dt=F32, stack=es):
        return stack.enter_context(nc.sbuf_tensor(name, list(shape), dt))

    ps = [es.enter_context(nc.psum_tensor("ps%d" % i, [128, 512], F32)) for i in range(8)]

    dI = sb("dI", [128, 128], I32)
    ident = sb("ident", [128, 128])
    identb = sb("identb", [128, 128], BF16)
    ones = sb("ones", [128, 128])
    onesb = sb("onesb", [128, 128], BF16)
    m_lt = sb("m_lt", [128, 128])
    m_le = sb("m_le", [128, 128])
    m_gt = sb("m_gt", [128, 128])
    m_ge = sb("m_ge", [128, 128])
    e0 = sb("e0", [2, 128])
    K.op("pool", lambda e: e.iota(dI[:], pattern=[[1, 128]], base=0, channel_multiplier=-1), writes=["dI"])
    for t, op_, nm in ((ident, ALU.is_equal, "ident"), (m_lt, ALU.is_gt, "m_lt"), (m_le, ALU.is_ge, "m_le"),
                       (m_gt, ALU.is_lt, "m_gt"), (m_ge, ALU.is_le, "m_ge")):
        K.op("dve", lambda e, t=t, op_=op_: e.tensor_scalar(t[:], dI[:], 0.0, None, op0=op_), reads=["dI"], writes=[nm])
    K.op("dve", lambda e: e.tensor_copy(identb[:], ident[:]), reads=["ident"], writes=["identb"])
    K.op("pool", lambda e: e.memset(ones[:], 1.0), writes=["ones"])
    K.op("pool", lambda e: e.memset(onesb[:], 1.0), writes=["onesb"])
    e0i = sb("e0i", [2, 128], I32)
    K.op("pool", lambda e: e.iota(e0i[:], pattern=[[0, 128]], base=1, channel_multiplier=-1), writes=["e0i"])
    K.op("dve", lambda e: e.tensor_copy(e0[:], e0i[:]), reads=["e0i"], writes=["e0"])

    PK1 = dict(ADAB=0, G1=48, G2=56, CW=64)
    PK2 = dict(MU=0, W0=15, A0=23, KK=31, KA=35, RK=39, GNW=43, GNB=47, GNORM=51)
    pk1s = sb("pk1s", [128, 128])
    pk2s = sb("pk2s", [128, 128])
    pk1 = sb("pk1", [128, 128])
    pk2 = sb("pk2", [128, 128])
    K.op("pool", lambda e: e.memset(pk1s[:], 0.0), writes=["pk1s"])
    K.op("pool", lambda e: e.memset(pk2s[:], 0.0), writes=["pk2s"])
    for src, off, n in ((adab_d, 0, 48), (g1_d, 48, 8), (g2_d, 56, 8), (convw_d, 64, 60)):
        K.dma(pk1s[off:off + n, :], src[:, :], writes=["pk1s"], key="pk1s")
    for src, off, n in ((mu_d, 0, 15), (w0_d, 15, 8), (a0_d, 23, 8), (kk_d, 31, 4), (ka_d, 35, 4),
                        (rk_d, 39, 4), (gnw_d, 43, 4), (gnb_d, 47, 4), (gnorm_d, 51, 1)):
        K.dma(pk2s[off:off + n, :], src[:, :], writes=["pk2s"], key="pk2s")
    K.op("pe", lambda e: e.transpose(ps[0][:, 0:128], pk1s[:], ident[:]), reads=["pk1s", "ident"], writes=["ps0"])
    K.op("pe", lambda e: e.transpose(ps[0][:, 128:256], pk2s[:], ident[:]), reads=["pk2s", "ident"], writes=["ps0"])
    K.op("dve", lambda e: e.tensor_copy(pk1[:], ps[0][:, 0:128]), reads=["ps0"], writes=["pk1"])
    K.op("dve", lambda e: e.tensor_copy(pk2[:], ps[0][:, 128:256]), reads=["ps0"], writes=["pk2"])

    modT = sb("modT", [128, 48, 2])
    gt_d = nc.dram_tensor("gt_s", [128, 4 * D], F32).ap()
    A1 = sb("A1", [128, 8]); B1 = sb("B1", [128, 8])
    A1c = sb("A1c", [128, 8]); B1c = sb("B1c", [128, 8])
    A2 = sb("A2", [128, 8]); B2 = sb("B2", [128, 8])
    with ExitStack() as pa:
        cc = sb("cc", [2, D], stack=pa)
        scT = sb("scT", [128, 8, 2], stack=pa)
        aw = [sb("aw%d" % i, [128, 8, 1024], stack=pa) for i in range(2)]
        gtrow = sb("gtrow", [2, 4, D], stack=pa)
        gtB = sb("gtB", [128, 4, D], stack=pa)
        K.dma(cc[0:1, :], c_d[:, :], writes=["cc"], key="cc")
        K.dma(cc[1:2, :], cctx_d[:, :], writes=["cc"], key="cc")
        K.op("act", lambda e: e.activation(cc[:], cc[:], AF.Silu), reads=["cc"], writes=["cc"])
        for k in range(8):
            K.op("pe", lambda e, k=k: e.transpose(ps[1][:, 2 * k:2 * k + 2], cc[0:2, k * 128:(k + 1) * 128], ident[0:2, 0:2]),
                 reads=["cc", "ident"], writes=["ps1"])
        K.op("dve", lambda e: e.tensor_copy(scT[:].rearrange("p k c -> p (k c)"), ps[1][:, 0:16]), reads=["ps1"], writes=["scT"])
        K.op("pool", lambda e: e.memset(gtrow[:], 0.0), writes=["gtrow"])
        for q_ in range(4):
            K.dma(gtrow[0:1, q_, :], adabr_d[:, 2048 + q_ * 1024:3072 + q_ * 1024], writes=["gtrow"], key="gtrow")
        for g in range(6):
            a = aw[g % 2]
            an = "aw%d" % (g % 2)
            K.dma(a[:], adaw_d[:, g * 1024:(g + 1) * 1024].rearrange("(k p) c -> p k c", p=128), writes=[an], key=an)
            for j in range(8):
                col = (g * 8 + j) * 2
                for k in range(8):
                    K.op("pe", lambda e, a=a, j=j, k=k, col=col: e.matmul(
                        ps[2][:, col:col + 2], lhsT=a[:, k, j * 128:(j + 1) * 128], rhs=scT[:, k, :],
                        start=(k == 0), stop=(k == 7)), reads=[an, "scT"], writes=["ps2"])
            if g >= 2:
                q = g - 2
                for half in range(2):
                    for k in range(8):
                        K.op("pe", lambda e, a=a, k=k, half=half: e.matmul(
                            ps[3][0:2, :], lhsT=scT[:, k, :], rhs=a[:, k, half * 512:(half + 1) * 512],
                            start=(k == 0), stop=(k == 7)), reads=[an, "scT"], writes=["ps3"])
                    K.op("dve", lambda e, q=q, half=half: e.tensor_tensor(
                        gtrow[:, q, half * 512:(half + 1) * 512], ps[3][0:2, :], gtrow[:, q, half * 512:(half + 1) * 512], op=ALU.add),
                        reads=["ps3", "gtrow"], writes=["gtrow"])
                    K.op("pe", lambda e, q=q, half=half: e.matmul(
                        ps[4][:, :], lhsT=e0[:, :], rhs=gtrow[:, q, half * 512:(half + 1) * 512], start=True, stop=True),
                        reads=["e0", "gtrow"], writes=["ps4"])
                    K.op("act", lambda e, q=q, half=half: e.copy(gtB[:, q, half * 512:(half + 1) * 512], ps[4][:, :]),
                         reads=["ps4"], writes=["gtB"])
        K.op("dve", lambda e: e.tensor_tensor(
            modT[:], ps[2][:, 0:96].rearrange("p (j c) -> p j c", c=2),
            pk1[:, 0:48].unsqueeze(2).to_broadcast([128, 48, 2]), op=ALU.add), reads=["ps2", "pk1"], writes=["modT"])
        for (A, B, gname, sc0, sh0, col, nm) in ((A1, B1, "G1", 8, 0, 0, "1"), (A1c, B1c, "G1", 8, 0, 1, "1c"),
                                                 (A2, B2, "G2", 32, 24, 0, "2")):
            g0 = PK1[gname]
            K.op("dve", lambda e, A=A, g0=g0, sc0=sc0, col=col: e.scalar_tensor_tensor(
                A[:], modT[:, sc0:sc0 + 8, col], 1.0, pk1[:, g0:g0 + 8], op0=ALU.add, op1=ALU.mult),
                reads=["modT", "pk1"], writes=["A" + nm])
            K.op("dve", lambda e, B=B, sh0=sh0, col=col: e.tensor_copy(B[:], modT[:, sh0:sh0 + 8, col]),
                 reads=["modT"], writes=["B" + nm])

        K.dma(gt_d[:, :], gtB[:].rearrange("p q d -> p (q d)"), reads=["gtB"], writes=["gt_d"], key="st_gt")
        K.barrier()
    if "mod" in dbg:
        d_ = dbgt("mod", [128, 96])
        K.dma(d_[:, :], modT[:].rearrange("p j c -> p (j c)"), reads=["modT"], writes=["dbgmod"], key="dbg")

    def norm_tile(xt, xkey, A, B, hT, hkey, col0, pp, stage):
        junk, ss, rs, xs = stage
        K.op("act", lambda e: e.activation(junk[:], xt[:], AF.Square), reads=[xkey], writes=["n_junk"])
        K.op("dve", lambda e: e.reduce_sum(ss[:, 0:1], junk[:], axis=AX.X), reads=["n_junk"], writes=["n_ss"])
        K.op("act", lambda e: e.activation(rs[:, 0:1], ss[:, 0:1], AF.Sqrt, bias=eps_t[:, 0:1], scale=1.0 / D),
             reads=["n_ss", "eps"], writes=["n_rs"])
        K.op("dve", lambda e: e.reciprocal(rs[:, 1:2], rs[:, 0:1]), reads=["n_rs"], writes=["n_rs2"])
        K.op("dve", lambda e: e.tensor_scalar(xs[:], xt[:], rs[:, 1:2], None, op0=ALU.mult),
             reads=[xkey, "n_rs2"], writes=["n_xs"])
        for half in range(2):
            pt = ps[pp + half]
            pk = "ps%d" % (pp + half)
            for kk in range(4):
                k = half * 4 + kk
                K.op("pe", lambda e, k=k, kk=kk, pt=pt: e.transpose(pt[:, kk * 128:(kk + 1) * 128], xs[:, k * 128:(k + 1) * 128], ident[:]),
                     reads=["n_xs", "ident"], writes=[pk])
            for kk in range(4):
                k = half * 4 + kk
                eng = "act" if kk % 2 == 0 else "dve"
                if eng == "act":
                    K.op("act", lambda e, k=k, kk=kk, pt=pt: e.activation(
                        hT[:, k, col0:col0 + 128], pt[:, kk * 128:(kk + 1) * 128], AF.Identity,
                        bias=B[:, k:k + 1], scale=A[:, k:k + 1]), reads=[pk, "mods"], writes=[hkey])
                else:
                    K.op("dve", lambda e, k=k, kk=kk, pt=pt: e.tensor_scalar(
                        hT[:, k, col0:col0 + 128], pt[:, kk * 128:(kk + 1) * 128], A[:, k:k + 1], B[:, k:k + 1],
                        op0=ALU.mult, op1=ALU.add), reads=[pk, "mods"], writes=[hkey])

    eps_t = sb("eps_t", [128, 4])
    K.op("pool", lambda e: e.memset(eps_t[:, 0:1], NORM_EPS), writes=["eps"])
    K.op("pool", lambda e: e.memset(eps_t[:, 1:2], L2_EPS), writes=["eps"])
    K.op("pool", lambda e: e.memset(eps_t[:, 2:3], GN_EPS), writes=["eps"])
    K.op("pool", lambda e: e.memset(eps_t[:, 3:4], 1.0), writes=["eps"])
    K.op("dve", lambda e: e.tensor_copy(A1[:, 0:1], A1[:, 0:1]), reads=["A1", "B1", "A1c", "B1c", "A2", "B2"], writes=["mods"])

    with ExitStack() as pb:
        hT = sb("hT", [128, 8, T], BF16, stack=pb)
        junk = sb("junk", [128, D], stack=pb)
        ss = sb("ss", [128, 1], stack=pb)
        rs = sb("rs", [128, 2], stack=pb)
        xs = sb("xs", [128, D], stack=pb)
        xts = [sb("xt%d" % i, [128, D], stack=pb) for i in range(3)]
        for i in range(NT):
            xt = xts[i % 3]
            xk = "xt%d" % (i % 3)
            src = ctx_d[i * 128:(i + 1) * 128, :] if i < 2 else x_d[(i - 2) * 128:(i - 1) * 128, :]
            K.dma(xt[:], src, writes=[xk], key=xk)
            A, B = (A1c, B1c) if i < 2 else (A1, B1)
            norm_tile(xt, xk, A, B, hT, "hT", i * 128, 0, (junk, ss, rs, xs))
        if "hT" in dbg:
            d_ = dbgt("ss", [128, 1])
            K.dma(d_[:, :], ss[:, :], reads=["n_ss"], writes=["dbgss"], key="dbg")
            d_ = dbgt("rs", [128, 2])
            K.dma(d_[:, :], rs[:, :], reads=["n_rs", "n_rs2"], writes=["dbgrs"], key="dbg")
            d_ = dbgt("hT", [128, 8 * T], BF16)
            K.dma(d_[:, :], hT[:].rearrange("p k t -> p (k t)"), reads=["hT"], writes=["dbghT"], key="dbg")
        wst = [sb("wst%d" % i, [128, 8, 128], stack=pb) for i in range(2)]
        wbf = [sb("wbf%d" % i, [128, 8, 128], BF16, stack=pb) for i in range(2)]
        pcs = [sb("pc%d" % i, [128, T], stack=pb) for i in range(2)]
        nblk = (T + 511) // 512
        chunks = [(j, j * 128, 128) for j in range(16)] + [(16, 2048, 16)] + \
                 [(17 + j, GDN_COLS + j * 128, 128) for j in range(15)]
        ev = 0
        for ci, (dst, c0, ncol) in enumerate(chunks):
            w_s = wst[ci % 2]; w_b = wbf[ci % 2]; pc = pcs[ci % 2]
            ws_k = "wst%d" % (ci % 2); wb_k = "wbf%d" % (ci % 2); pc_k = "pc%d" % (ci % 2)
            K.dma(w_s[:, :, 0:ncol], win_d[:, c0:c0 + ncol].rearrange("(k p) c -> p k c", p=128), writes=[ws_k], key=ws_k)
            K.op("pool", lambda e, w_s=w_s, w_b=w_b, ncol=ncol: e.tensor_copy(w_b[:, :, 0:ncol], w_s[:, :, 0:ncol]),
                 reads=[ws_k], writes=[wb_k])
            for n in range(nblk):
                t0 = n * 512
                tn = min(512, T - t0)
                pt = ps[2 + (n % 4)]
                pk = "ps%d" % (2 + (n % 4))
                for k in range(8):
                    K.op("pe", lambda e, k=k, pt=pt, w_b=w_b, ncol=ncol, t0=t0, tn=tn: e.matmul(
                        pt[0:ncol, 0:tn], lhsT=w_b[:, k, 0:ncol], rhs=hT[:, k, t0:t0 + tn], start=(k == 0), stop=(k == 7)),
                        reads=[wb_k, "hT"], writes=[pk])
                eng = "act" if ev % 2 == 0 else "dve"
                ev += 1
                if eng == "act":
                    K.op("act", lambda e, pt=pt, pc=pc, ncol=ncol, t0=t0, tn=tn: e.copy(pc[0:ncol, t0:t0 + tn], pt[0:ncol, 0:tn]),
                         reads=[pk], writes=[pc_k])
                else:
                    K.op("dve", lambda e, pt=pt, pc=pc, ncol=ncol, t0=t0, tn=tn: e.tensor_copy(pc[0:ncol, t0:t0 + tn], pt[0:ncol, 0:tn]),
                         reads=[pk], writes=[pc_k])
            K.dma(pT_d[dst, 0:ncol, :], pc[0:ncol, :], reads=[pc_k], writes=[("pT", dst)], key="st_" + pc_k)

    K.barrier()
    if "pT" in dbg:
        d_ = dbgt("pT", [32, 128, T])
        K.dma(d_[:, :, :], pT_d[:, :, :], reads=[("pT", i) for i in range(32)], writes=["dbgpT"], key="dbg")


    mT_d = nc.dram_tensor("mT_s", [8, 128, TL], BF16).ap()
    psb = [p.bitcast(BF16) for p in ps]
    negm_le = sb("negm_le", [128, 128])
    negm_ge = sb("negm_ge", [128, 128])
    K.op("dve", lambda e: e.tensor_scalar(negm_le[:], m_le[:], 30000.0, -30000.0, op0=ALU.mult, op1=ALU.add), reads=["m_le"], writes=["negm_le"])
    K.op("dve", lambda e: e.tensor_scalar(negm_ge[:], m_ge[:], 30000.0, -30000.0, op0=ALU.mult, op1=ALU.add), reads=["m_ge"], writes=["negm_ge"])
    fwd_order = list(range(NT))
    bwd_order = [1, 0] + list(range(NT - 1, 1, -1))

    NMODE = NM_MODE[0]
    NDT = BF16 if NMODE == "bf16" else F32

    def mmv(ap):
        return ap

    identn = identb if NMODE == "bf16" else ident

    nmode = {'cur': NMODE}

    def neumann(Y, Yt, tag, pbank, pb=None):
        PR = nm_tiles[tag]["PR"]; Pt = nm_tiles[tag]["Pt"]
        kPR = [tag + "PR0", tag + "PR1"]; kPt = [tag + "Pt0", tag + "Pt1"]
        pa_ = ps[pbank]
        ka = "ps%d" % pbank
        if pb is None:
            pb_, kb = ps[pbank + 1], "ps%d" % (pbank + 1)
        else:
            pb_, kb = ps[pb[0]][:, pb[1]:pb[1] + 128], "ps%d" % pb[0]
        K.op("pe", lambda e: e.matmul(pa_[:, 0:128], lhsT=mmv(Yt[:]), rhs=mmv(Y[:]), start=True, stop=True), reads=[tag + "Y", tag + "Yt"], writes=[ka])
        K.op("pe", lambda e: e.matmul(pb_[:, 0:128], lhsT=mmv(Y[:]), rhs=mmv(Yt[:]), start=True, stop=True), reads=[tag + "Y", tag + "Yt"], writes=[kb])
        K.op("act", lambda e: e.copy(PR[0][:, 0:128], pa_[:, 0:128]), reads=[ka], writes=[kPR[0]])
        K.op("dve", lambda e: e.tensor_tensor(PR[0][:, 128:256], Y[:], (identb if nmode['cur'] == 'bf16' else ident)[:], op=ALU.add), reads=[tag + "Y", "identb", "ident"], writes=[kPR[0]])
        K.op("dve", lambda e: e.tensor_copy(Pt[0][:], pb_[:, 0:128]), reads=[kb], writes=[kPt[0]])
        cur = 0
        for l in range(1, 7):
            nxt = 1 - cur
            last = (l == 6)
            n0 = 128 if last else 0
            K.op("pe", lambda e, cur=cur, n0=n0: e.matmul(pa_[:, n0:256], lhsT=mmv(Pt[cur][:]), rhs=mmv(PR[cur][:, n0:256]), start=True, stop=False),
                 reads=[kPt[cur], kPR[cur]], writes=[ka])
            K.op("pe", lambda e, cur=cur: e.matmul(pa_[:, 128:256], lhsT=mmv((identb if nmode['cur'] == 'bf16' else ident)[:]), rhs=mmv(PR[cur][:, 128:256]), start=False, stop=True),
                 reads=["identb", "ident", kPR[cur]], writes=[ka])
            if not last:
                K.op("pe", lambda e, cur=cur: e.matmul(pb_[:, 0:128], lhsT=mmv(PR[cur][:, 0:128]), rhs=mmv(Pt[cur][:]), start=True, stop=True),
                     reads=[kPt[cur], kPR[cur]], writes=[kb])
            K.op("act", lambda e, nxt=nxt, n0=n0: e.copy(PR[nxt][:, n0:256], pa_[:, n0:256]), reads=[ka], writes=[kPR[nxt]])
            if not last:
                K.op("dve", lambda e, nxt=nxt: e.tensor_copy(Pt[nxt][:], pb_[:, 0:128]), reads=[kb], writes=[kPt[nxt]])
            cur = nxt
        if nmode['cur'] == 'bf16':
            return PR[cur][:, 128:256], kPR[cur]
        fin = nm_tiles[tag]["fin"]
        K.op("act", lambda e, cur=cur: e.copy(fin[:], PR[cur][:, 128:256]), reads=[kPR[cur]], writes=[tag + "fin"])
        return fin[:], tag + "fin"

    nm_tiles = {}
    with ExitStack() as pg:
        for tag in ("n0", "n1", "n2", "n3"):
            nm_tiles[tag] = dict(PR=[sb(tag + "PR%d" % i, [128, 256], NDT, stack=pg) for i in range(2)],
                                 Pt=[sb(tag + "Pt%d" % i, [128, 128], NDT, stack=pg) for i in range(2)],
                                 fin=sb(tag + "fin", [128, 128], BF16, stack=pg))
        ab = sb("ab", [128, NT, 16], stack=pg)
        dtb_b = sb("dtb_b", [128, 8], stack=pg)
        nA_b = sb("nA_b", [128, 8], stack=pg)
        gg = sb("gg", [128, NT, 8], stack=pg)
        Gc = sb("Gc", [128, NT, 8], stack=pg)
        nbeta = sb("nbeta", [128, NT, 8], stack=pg)
        beta = sb("beta", [128, NT, 8], stack=pg)
        negeG = sb("negeG", [128, NT, 8], stack=pg)
        eG = sb("eG", [128, NT, 8], stack=pg)
        eTG = sb("eTG", [128, NT, 8], stack=pg)
        eTot = sb("eTot", [128, NT, 8], stack=pg)
        pg_ab = ExitStack()
        abT = sb("abT", [16, T], stack=pg_ab)
        K.dma(abT[:, :], pT_d[16, 0:16, :], reads=[("pT", 16)], writes=["abT"], key="abT")
        K.dma(dtb_b[:, :], dtb_d.partition_broadcast(128), writes=["dtb_b"], key="dtb_b")
        K.dma(nA_b[:, :], alog_d.partition_broadcast(128), writes=["nA_b"], key="nA_b")
        for i in range(NT):
            K.op("pe", lambda e, i=i: e.transpose(ps[i // 32][:, (i % 32) * 16:(i % 32) * 16 + 16], abT[0:16, i * 128:(i + 1) * 128], ident[0:16, 0:16]),
                 reads=["abT", "ident"], writes=["ps%d" % (i // 32)])
        for b0 in range(0, NT, 32):
            nb = min(32, NT - b0)
            K.op("dve", lambda e, b0=b0, nb=nb: e.tensor_copy(ab[:, b0:b0 + nb, :].rearrange("p n c -> p (n c)"), ps[b0 // 32][:, 0:nb * 16]),
                 reads=["ps%d" % (b0 // 32)], writes=["ab"])
        K.op("act", lambda e: e.activation(nA_b[:], nA_b[:], AF.Exp), reads=["nA_b"], writes=["nA_b"])
        K.op("dve", lambda e: e.tensor_scalar(nA_b[:], nA_b[:], -1.0, None, op0=ALU.mult), reads=["nA_b"], writes=["nA_b"])
        K.op("dve", lambda e: e.tensor_tensor(gg[:], ab[:, :, 0:8], dtb_b[:].unsqueeze(1).to_broadcast([128, NT, 8]), op=ALU.add),
             reads=["ab", "dtb_b"], writes=["gg"])
        K.op("act", lambda e: e.activation(gg[:], gg[:], AF.Exp), reads=["gg"], writes=["gg"])
        K.op("act", lambda e: e.activation(gg[:], gg[:], AF.Ln, bias=eps_t[:, 3:4]), reads=["gg", "eps"], writes=["gg"])
        K.op("dve", lambda e: e.tensor_tensor(gg[:], gg[:], nA_b[:].unsqueeze(1).to_broadcast([128, NT, 8]), op=ALU.mult),
             reads=["gg", "nA_b"], writes=["gg"])
        K.op("act", lambda e: e.activation(beta[:], ab[:, :, 8:16], AF.Sigmoid), reads=["ab"], writes=["beta"])
        K.op("dve", lambda e: e.tensor_scalar(nbeta[:], beta[:], -1.0, None, op0=ALU.mult), reads=["beta"], writes=["nbeta"])
        ggf = gg[:].rearrange("p n c -> p (n c)")
        K.op("pe", lambda e: e.matmul(ps[2][:, 0:NT * 8], lhsT=m_le[:], rhs=ggf, start=True, stop=True), reads=["m_le", "gg"], writes=["ps2"])
        K.op("pe", lambda e: e.matmul(ps[3][:, 0:NT * 8], lhsT=m_ge[:], rhs=ggf, start=True, stop=True), reads=["m_ge", "gg"], writes=["ps3"])
        K.op("pe", lambda e: e.matmul(ps[4][:, 0:NT * 8], lhsT=ones[:], rhs=ggf, start=True, stop=True), reads=["ones", "gg"], writes=["ps4"])
        K.op("dve", lambda e: e.tensor_copy(Gc[:, :, 0:4], ps[2][:, 0:NT * 8].rearrange("p (n c) -> p n c", c=8)[:, :, 0:4]), reads=["ps2"], writes=["Gc"])
        K.op("dve", lambda e: e.tensor_copy(Gc[:, :, 4:8], ps[3][:, 0:NT * 8].rearrange("p (n c) -> p n c", c=8)[:, :, 4:8]), reads=["ps3"], writes=["Gc"])
        K.op("act", lambda e: e.activation(eG[:], Gc[:], AF.Exp), reads=["Gc"], writes=["eG"])
        K.op("dve", lambda e: e.tensor_scalar(negeG[:], eG[:], -1.0, None, op0=ALU.mult), reads=["eG"], writes=["negeG"])
        K.op("act", lambda e: e.activation(eTot[:].rearrange("p n c -> p (n c)"), ps[4][:, 0:NT * 8], AF.Exp), reads=["ps4"], writes=["eTot"])
        K.op("dve", lambda e: e.tensor_tensor(eTG[:].rearrange("p n c -> p (n c)"), ps[4][:, 0:NT * 8], Gc[:].rearrange("p n c -> p (n c)"), op=ALU.subtract),
             reads=["ps4", "Gc"], writes=["eTG"])
        K.op("act", lambda e: e.activation(eTG[:], eTG[:], AF.Exp), reads=["eTG"], writes=["eTG"])

        K.barrier()
        pg_ab.close()
        stop_at("gdn_scal")
        qT = sb("qT", [128, T], BF16, stack=pg)
        kT = sb("kT", [128, T], BF16, stack=pg)
        vT = sb("vT", [128, T], BF16, stack=pg)
        zs = sb("zs", [128, T], BF16, stack=pg)
        vtok = sb("vtok", [128, NT, 128], BF16, stack=pg)
        ktok = sb("ktok", [128, NT, 128], BF16, stack=pg)
        obuf = [sb("obuf%d" % d_, [128, NT, 128], BF16, stack=pg) for d_ in range(2)]
        AinvAll = [sb("AinvAll%d" % d_, [128, NT, 128], BF16, stack=pg) for d_ in range(2)]
        MqkAll = [sb("MqkAll%d" % d_, [128, NT, 128], BF16, stack=pg) for d_ in range(2)]
        pin = sb("pin", [128, T], stack=pg)
        cv = sb("cv", [128, T], stack=pg)
        sq = pin
        rn = sb("rn", [128, 512], stack=pg)
        S = [sb("S%d" % d_, [128, 128], stack=pg) for d_ in range(2)]
        Sb = [sb("Sb%d" % d_, [128, 128], BF16, stack=pg) for d_ in range(2)]
        mst = sb("mst", [128, TL], BF16, stack=pg)
        dgl = [sb("dgl%d" % d_, [128, 128], stack=pg) for d_ in range(4)]
        arg = dgl
        DTi = [sb("DTi%d" % d_, [128, 128], stack=pg) for d_ in range(4)]
        DTs = dgl
        Yb = [sb("Yb%d" % d_, [128, 128], NDT, stack=pg) for d_ in range(4)]
        Ytb = [sb("Ytb%d" % d_, [128, 128], NDT, stack=pg) for d_ in range(4)]
        Rb = [sb("Rb%d" % d_, [128, 128], BF16, stack=pg) for d_ in range(2)]
        Xb = [sb("Xb%d" % d_, [128, 128], BF16, stack=pg) for d_ in range(2)]
        Xs = [sb("Xs%d" % d_, [128, 128], BF16, stack=pg) for d_ in range(2)]
        QSe = [sb("QSe%d" % d_, [128, 128], stack=pg) for d_ in range(2)]
        on_ = sb("on_", [128, 128], stack=pg)
        oj = sb("oj", [128, 128], stack=pg)
        onn = sb("onn", [128, 128], stack=pg)
        oss = sb("oss", [128, 4], stack=pg)

        def conv_silu(cidx, dst, dkey, final_silu_to):
            cw0 = PK1["CW"]
            K.op("dve", lambda e: e.tensor_scalar(cv[:], pin[:], pk1[:, cw0 + 2 * 12 + cidx:cw0 + 2 * 12 + cidx + 1], None, op0=ALU.mult),
                 reads=["pin", "pk1"], writes=["cv"])
            for j in (0, 1, 3, 4):
                sh = j - 2
                wcol = pk1[:, cw0 + j * 12 + cidx:cw0 + j * 12 + cidx + 1]
                for (s0, s1) in ((0, TC), (TC, T)):
                    lo = max(s0, s0 - sh); hi = min(s1, s1 - sh)
                    K.op("dve", lambda e, lo=lo, hi=hi, sh=sh, wcol=wcol: e.scalar_tensor_tensor(
                        cv[:, lo:hi], pin[:, lo + sh:hi + sh], wcol, cv[:, lo:hi], op0=ALU.mult, op1=ALU.add),
                        reads=["pin", "pk1", "cv"], writes=["cv"])
            K.op("act", lambda e: e.activation(final_silu_to[:], cv[:], AF.Silu), reads=["cv"], writes=[dkey])

        def l2n(src, skey, dst, dkey, scale):
            K.op("pool", lambda e: e.tensor_tensor(sq[:], src[:], src[:], op=ALU.mult), reads=[skey], writes=["pin"])
            for n in range((T + 511) // 512):
                t0 = n * 512; tn = min(512, T - t0)
                pt = ps[5 + (n % 2)]; pk = "ps%d" % (5 + (n % 2))
                K.op("pe", lambda e, pt=pt, t0=t0, tn=tn: e.matmul(pt[:, 0:tn], lhsT=ones[:], rhs=sq[:, t0:t0 + tn], start=True, stop=True),
                     reads=["ones", "pin"], writes=[pk])
                K.op("act", lambda e, pt=pt, tn=tn: e.activation(rn[:, 0:tn], pt[:, 0:tn], AF.Sqrt, bias=eps_t[:, 1:2]), reads=[pk, "eps"], writes=["rn"])
                K.op("dve", lambda e, tn=tn: e.reciprocal(rn[:, 0:tn], rn[:, 0:tn]), reads=["rn"], writes=["rn"])
                K.op("dve", lambda e, t0=t0, tn=tn: e.scalar_tensor_tensor(dst[:, t0:t0 + tn], src[:, t0:t0 + tn], scale, rn[:, 0:tn], op0=ALU.mult, op1=ALU.mult),
                     reads=[skey, "rn"], writes=[dkey])

        def to_tok(src, skey, dst, dkey):
            for i in range(NT):
                pt = psb[5 + (i % 2)]; pk = "ps%d" % (5 + (i % 2))
                K.op("pe", lambda e, i=i, pt=pt: e.transpose(pt[:, 0:128], src[:, i * 128:(i + 1) * 128], identb[:]), reads=[skey, "identb"], writes=[pk])
                eng = "act" if i % 2 == 0 else "dve"
                if eng == "act":
                    K.op("act", lambda e, i=i, pt=pt: e.copy(dst[:, i, :], pt[:, 0:128]), reads=[pk], writes=[dkey])
                else:
                    K.op("dve", lambda e, i=i, pt=pt: e.tensor_copy(dst[:, i, :], pt[:, 0:128]), reads=[pk], writes=[dkey])

        for h in range(4):
            K.dma(pin[:, :], pT_d[h, :, :], reads=[("pT", h)], writes=["pin"], key="pin")
            conv_silu(h, cv, "cv", cv)
            l2n(cv, "cv", qT, "qT", float(128 ** -0.5))
            K.dma(pin[:, :], pT_d[4 + h, :, :], reads=[("pT", 4 + h)], writes=["pin"], key="pin")
            conv_silu(4 + h, cv, "cv", cv)
            l2n(cv, "cv", kT, "kT", 1.0)
            to_tok(kT, "kT", ktok, "ktok")
            K.dma(pin[:, :], pT_d[8 + h, :, :], reads=[("pT", 8 + h)], writes=["pin"], key="pin")
            conv_silu(8 + h, vT, "vT", vT)
            to_tok(vT, "vT", vtok, "vtok")
            K.dma(pin[:, :], pT_d[12 + h, :, :], reads=[("pT", 12 + h)], writes=["pin"], key="pin")
            K.op("act", lambda e: e.activation(zs[:], pin[:], AF.Silu), reads=["pin"], writes=["zs"])
            stop_at("gdn_prep")
            for d_ in range(2):
                K.op("pool", lambda e, d_=d_: e.memset(S[d_][:], 0.0), writes=["S%d" % d_])
                K.op("pool", lambda e, d_=d_: e.memset(Sb[d_][:], 0.0), writes=["Sb%d" % d_])
            def g_pre(step, d_, sl_):
                i = fwd_order[step] if d_ == 0 else bwd_order[step]
                par = step % 2
                r = d_ * 4 + h
                tag = "n%d" % sl_
                sl = slice(i * 128, (i + 1) * 128)
                want_o = i >= 2
                pA = ps[2 * sl_]; kA = "ps%d" % (2 * sl_)
                dk_ = "%d" % sl_
                K.op("dve", lambda e: e.tensor_scalar(dgl[sl_][:], ident[:], Gc[:, i, r:r + 1], None, op0=ALU.mult),
                     reads=["ident", "Gc"], writes=["dgl" + dk_])
                K.op("pe", lambda e: e.matmul(pA[:, 256:384], lhsT=ones[:], rhs=dgl[sl_][:], start=True, stop=True),
                     reads=["ones", "dgl" + dk_], writes=[kA])
                negm = negm_le if d_ == 0 else negm_ge
                mstrict = m_lt if d_ == 0 else m_gt
                K.op("dve", lambda e: e.scalar_tensor_tensor(
                    arg[sl_][:], pA[:, 256:384], Gc[:, i, r:r + 1], negm[:], op0=ALU.subtract, op1=ALU.add),
                    reads=[kA, "Gc", "negm_le", "negm_ge"], writes=["dgl" + dk_])
                K.op("act", lambda e: e.activation(DTi[sl_][:], arg[sl_][:], AF.Exp), reads=["dgl" + dk_], writes=["DTi" + dk_])
                K.op("pool", lambda e: e.tensor_tensor(DTs[sl_][:], DTi[sl_][:], mstrict[:], op=ALU.mult),
                     reads=["DTi" + dk_, "m_lt", "m_gt"], writes=["dgl" + dk_])
                K.op("pe", lambda e: e.matmul(pA[:, 0:128], lhsT=kT[:, sl], rhs=kT[:, sl], start=True, stop=True),
                     reads=["kT"], writes=[kA])
                K.op("dve", lambda e: e.scalar_tensor_tensor(
                    Yb[sl_][:], pA[:, 0:128], nbeta[:, i, r:r + 1], DTs[sl_][:], op0=ALU.mult, op1=ALU.mult),
                    reads=[kA, "nbeta", "dgl" + dk_], writes=[tag + "Y"])
                if want_o:
                    K.op("pe", lambda e: e.matmul(pA[:, 128:256], lhsT=kT[:, sl], rhs=qT[:, sl], start=True, stop=True),
                         reads=["kT", "qT"], writes=[kA])
                    K.op("dve", lambda e: e.tensor_tensor(MqkAll[d_][:, step, :], pA[:, 128:256], DTi[sl_][:], op=ALU.mult),
                         reads=[kA, "DTi" + dk_], writes=[("Mqk", d_, step)])
                pB = (psb if NMODE == "bf16" else ps)[2 * sl_ + 1]; kB = "ps%d" % (2 * sl_ + 1)
                K.op("pe", lambda e: e.transpose(pB[:, 0:128], Yb[sl_][:], identn[:]), reads=[tag + "Y", "identb", "ident"], writes=[kB])
                K.op("act", lambda e: e.copy(Ytb[sl_][:], pB[:, 0:128]), reads=[kB], writes=[tag + "Yt"])
                AinvT, kAinv = neumann(Yb[sl_], Ytb[sl_], tag, 2 * sl_ + 1, pb=(2 * sl_, 384))
                K.op("pool", lambda e: e.tensor_copy(AinvAll[d_][:, step, :], AinvT), reads=[kAinv], writes=[("gAinv", d_, step)])

            def g_chain(step, d_):
                i = fwd_order[step] if d_ == 0 else bwd_order[step]
                par = step % 2
                r = d_ * 4 + h
                sl = slice(i * 128, (i + 1) * 128)
                want_o = i >= 2
                pC = ps[d_ * 4 + 3]; kC = "ps%d" % (d_ * 4 + 3)
                dk_ = "%d" % d_
                kAinv = ("gAinv", d_, step)
                kMqk = ("Mqk", d_, step)
                K.op("pe", lambda e: e.matmul(pC[:, 0:128], lhsT=kT[:, sl], rhs=Sb[d_][:], start=True, stop=True),
                     reads=["kT", "Sb" + dk_], writes=[kC])
                if want_o:
                    K.op("pe", lambda e: e.matmul(pC[:, 128:256], lhsT=qT[:, sl], rhs=Sb[d_][:], start=True, stop=True),
                         reads=["qT", "Sb" + dk_], writes=[kC])
                K.op("dve", lambda e: e.scalar_tensor_tensor(
                    Rb[d_][:], pC[:, 0:128], negeG[:, i, r:r + 1], vtok[:, i, :], op0=ALU.mult, op1=ALU.add),
                    reads=[kC, "negeG", "vtok"], writes=["Rb" + dk_])
                if want_o:
                    K.op("act", lambda e: e.activation(QSe[d_][:], pC[:, 128:256], AF.Identity, scale=eG[:, i, r:r + 1]),
                         reads=[kC, "eG"], writes=["QSe" + dk_])
                K.op("pe", lambda e: e.matmul(pC[:, 0:128], lhsT=AinvAll[d_][:, step, :], rhs=Rb[d_][:], start=True, stop=True),
                     reads=[kAinv, "Rb" + dk_], writes=[kC])
                K.op("dve", lambda e: e.tensor_scalar(Xb[d_][:], pC[:, 0:128], beta[:, i, r:r + 1], None, op0=ALU.mult),
                     reads=[kC, "beta"], writes=["Xb" + dk_])
                K.op("pool", lambda e: e.tensor_scalar(Xs[d_][:], Xb[d_][:], eTG[:, i, r:r + 1], None, op0=ALU.mult),
                     reads=["Xb" + dk_, "eTG"], writes=["Xs" + dk_])
                if want_o:
                    K.op("pe", lambda e: e.matmul(pC[:, 384:512], lhsT=MqkAll[d_][:, step, :], rhs=Xb[d_][:], start=True, stop=True),
                         reads=[kMqk, "Xb" + dk_], writes=[kC])
                    K.op("dve", lambda e: e.tensor_tensor(obuf[d_][:, i, :], pC[:, 384:512], QSe[d_][:], op=ALU.add),
                         reads=[kC, "QSe" + dk_], writes=["obuf" + dk_])
                K.op("pe", lambda e: e.matmul(pC[:, 256:384], lhsT=ktok[:, i, :], rhs=Xs[d_][:], start=True, stop=True),
                     reads=["ktok", "Xs" + dk_], writes=[kC])
                K.op("dve", lambda e: e.scalar_tensor_tensor(
                    S[d_][:], S[d_][:], eTot[:, i, r:r + 1], pC[:, 256:384], op0=ALU.mult, op1=ALU.add),
                    reads=["S" + dk_, "eTot", kC], writes=["S" + dk_])
                K.op("act", lambda e: e.copy(Sb[d_][:], S[d_][:]), reads=["S" + dk_], writes=["Sb" + dk_])

            inst = [(st_, dd_) for st_ in range(NT) for dd_ in range(2)]
            for g0 in range(0, len(inst), 4):
                grp = inst[g0:g0 + 4]
                for q_ in K.streams(len(grp)):
                    g_pre(grp[q_][0], grp[q_][1], q_)
            for step in range(NT):
                for d_ in K.streams(2):
                    g_chain(step, d_)
            stop_at("gdn_scan")
            for i in range(2, NT):
                K.op("dve", lambda e, i=i: e.tensor_tensor(on_[:], obuf[0][:, i, :], obuf[1][:, i, :], op=ALU.add),
                     reads=["obuf0", "obuf1"], writes=["on_"])
                K.op("act", lambda e: e.activation(oj[:], on_[:], AF.Square), reads=["on_"], writes=["oj"])
                K.op("dve", lambda e: e.reduce_sum(oss[:, 0:1], oj[:], axis=AX.X), reads=["oj"], writes=["oss"])
                K.op("act", lambda e: e.activation(oss[:, 1:2], oss[:, 0:1], AF.Sqrt, bias=eps_t[:, 0:1], scale=1.0 / 128), reads=["oss", "eps"], writes=["oss1"])
                K.op("dve", lambda e: e.reciprocal(oss[:, 2:3], oss[:, 1:2]), reads=["oss1"], writes=["oss2"])
                K.op("dve", lambda e: e.tensor_scalar(onn[:], on_[:], oss[:, 2:3], None, op0=ALU.mult), reads=["on_", "oss2"], writes=["onn"])
                pt = ps[i % 2]; pk = "ps%d" % (i % 2)
                K.op("pe", lambda e, pt=pt: e.transpose(pt[:, 0:128], onn[:], ident[:]), reads=["onn", "ident"], writes=[pk])
                gcol = pk2[:, PK2["GNORM"]:PK2["GNORM"] + 1]
                K.op("dve", lambda e, i=i, pt=pt, gcol=gcol: e.scalar_tensor_tensor(
                    mst[:, (i - 2) * 128:(i - 1) * 128], pt[:, 0:128], gcol, zs[:, i * 128:(i + 1) * 128], op0=ALU.mult, op1=ALU.mult),
                    reads=[pk, "pk2", "zs"], writes=["mst"])
            K.dma(mT_d[h, :, :], mst[:, :], reads=["mst"], writes=[("mT", h)], key="st_mst")

    K.barrier()
    pm_d = nc.dram_tensor("pm_s", [12, 128, T], F32).ap()
    lora_d = nc.dram_tensor("lora_s", [3, 128, T], BF16).ap()
    CW_ = float(np.exp(-0.5))
    NRW = n_rows
    with ExitStack() as pr:
        cidx_i = sb("cidx_i", [128, 15], I32, stack=pr)
        cidx = sb("cidx", [128, 15], stack=pr)
        mum = {nm: sb("mum_" + nm, [128, 15], stack=pr) for nm in ("om", "L", "R", "U", "D", "P", "N")}
        tmpm = sb("tmpm", [128, 15], stack=pr)
        K.op("pool", lambda e: e.iota(cidx_i[:], pattern=[[128, 15]], base=0, channel_multiplier=1), writes=["cidx_i"])
        K.op("dve", lambda e: e.tensor_copy(cidx[:], cidx_i[:]), reads=["cidx_i"], writes=["cidx"])
        mu_ap = pk2[:, PK2["MU"]:PK2["MU"] + 15]
        K.op("dve", lambda e: e.tensor_scalar(mum["om"][:], mu_ap, -1.0, 1.0, op0=ALU.mult, op1=ALU.add), reads=["pk2"], writes=["mum"])

        def band(nm, lo, hi):
            K.op("dve", lambda e: e.tensor_scalar(tmpm[:], cidx[:], float(lo), None, op0=ALU.is_ge), reads=["cidx"], writes=["tmpm"])
            K.op("dve", lambda e: e.scalar_tensor_tensor(tmpm[:], cidx[:], float(hi), tmpm[:], op0=ALU.is_lt, op1=ALU.mult), reads=["cidx", "tmpm"], writes=["tmpm"])
            K.op("dve", lambda e: e.tensor_tensor(mum[nm][:], tmpm[:], mu_ap, op=ALU.mult), reads=["tmpm", "pk2"], writes=["mum"])

        band("L", 0, 480); band("R", 480, 960); band("U", 960, 1440); band("D", 1440, 1920)
        band("P", 0, 960); band("N", 960, 1920)
        pins = [sb("rpin%d" % i, [128, T], stack=pr) for i in range(2)]
        pmx = [sb("pmx%d" % i, [128, T], stack=pr) for i in range(2)]
        lob = sb("lob", [128, T], BF16, stack=pr)
        for j in range(15):
            pin_ = pins[j % 2]; pk_ = "rpin%d" % (j % 2)
            po = pmx[j % 2]; ok_ = "pmx%d" % (j % 2)
            K.dma(pin_[:, :], pT_d[17 + j, :, :], reads=[("pT", 17 + j)], writes=[pk_], key=pk_)
            K.op("dve", lambda e, j=j, pin_=pin_, po=po: e.tensor_scalar(po[:], pin_[:], mum["om"][:, j:j + 1], None, op0=ALU.mult),
                 reads=[pk_, "mum"], writes=[ok_])
            c0, c1 = j * 128, j * 128 + 128

            def has(lo, hi):
                return c0 < hi and c1 > lo

            def acc(dst, src, nm, j=j, pin_=pin_, po=po, pk_=pk_, ok_=ok_, eng="dve"):
                K.op("dve", lambda e: e.scalar_tensor_tensor(dst(po), src(pin_), mum[nm][:, j:j + 1], dst(po), op0=ALU.mult, op1=ALU.add),
                     reads=[pk_, "mum", ok_], writes=[ok_])

            lat = lambda t: t[:, TC:T].rearrange("p (r w) -> p r w", w=64)
            if has(0, 960):
                acc(lambda t: t[:, 1:TC], lambda t: t[:, 0:TC - 1], "P")
            if has(960, 1920):
                acc(lambda t: t[:, 0:TC - 1], lambda t: t[:, 1:TC], "N")
            if has(0, 480):
                acc(lambda t: lat(t)[:, :, 1:64], lambda t: lat(t)[:, :, 0:63], "L")
            if has(480, 960):
                acc(lambda t: lat(t)[:, :, 0:63], lambda t: lat(t)[:, :, 1:64], "R")
            if has(960, 1440) and NRW > 1:
                acc(lambda t: lat(t)[:, 1:NRW, :], lambda t: lat(t)[:, 0:NRW - 1, :], "U")
            if has(1440, 1920) and NRW > 1:
                acc(lambda t: lat(t)[:, 0:NRW - 1, :], lambda t: lat(t)[:, 1:NRW, :], "D")
            if j < 12:
                K.dma(pm_d[j, :, :], po[:, :], reads=[ok_], writes=[("pm", j)], key="st_" + ok_)
            else:
                fn_ = {12: AF.Tanh, 13: AF.Identity, 14: AF.Sigmoid}[j]
                K.op("act", lambda e, po=po, fn_=fn_: e.activation(lob[:], po[:], fn_), reads=[ok_], writes=["lob"])
                K.dma(lora_d[j - 12, :, :], lob[:, :], reads=["lob"], writes=[("lora", j - 12)], key="st_lob")
    K.barrier()
    stop_at("rw_mix")
    if "pm" in dbg:
        d_o = dbgt("pm", [12, 128, T])
        K.dma(d_o[:, :, :], pm_d[:, :, :], reads=[("pm", j) for j in range(12)], writes=["dbgpm"], key="dbg")

    comb_d = nc.dram_tensor("comb_s", [16384, 2 * D], BF16).ap()
    with ExitStack() as pw:
        nm_tiles.clear()
        RW_NDT = BF16 if RW_NM[0] == "bf16" else F32
        nmode['cur'] = RW_NM[0]
        for tag in ("n0", "n1", "n2", "n3"):
            nm_tiles[tag] = dict(PR=[sb(tag + "rPR%d" % i, [128, 256], RW_NDT, stack=pw) for i in range(2)],
                                 Pt=[sb(tag + "rPt%d" % i, [128, 128], RW_NDT, stack=pw) for i in range(2)],
                                 fin=sb(tag + "rfin", [128, 128], BF16, stack=pw))
        BL = min(256, T)
        blocks = [(b0, min(BL, T - b0)) for b0 in range(0, T, BL)]
        wst_ = sb("rw_wst", [128, 3, 512], stack=pw)
        wlb = sb("rw_wlb", [128, 3, 512], BF16, stack=pw)
        for q_, src in enumerate((w2_d, a2_d, g2w_d)):
            K.dma(wst_[:, q_, :], src[:, :], writes=["rw_wst"], key="rw_wst")
        K.op("dve", lambda e: e.tensor_copy(wlb[:], wst_[:]), reads=["rw_wst"], writes=["wlb"])
        bones = sb("bones", [128, 128], stack=pw)
        K.op("pool", lambda e: e.memset(bones[:], 0.0), writes=["bones"])
        K.op("pool", lambda e: e.memset(bones[0:64, 0:64], 1.0), writes=["bones"])
        K.op("pool", lambda e: e.memset(bones[64:128, 64:128], 1.0), writes=["bones"])
        rmask = sb("rmask", [128, BL], stack=pw)
        K.op("pool", lambda e: e.memset(rmask[:], 1.0), writes=["rmask"])
        K.op("pool", lambda e: e.memset(rmask[:].rearrange("p (n t) -> p n t", t=128)[:, :, 0:1], 0.0), writes=["rmask"])
        mask4 = [sb("mask4_%d" % d_, [128, 512], stack=pw) for d_ in range(2)]
        for d_ in range(2):
            ms_, mi_ = (m_lt, m_le) if d_ == 0 else (m_gt, m_ge)
            for q_ in range(4):
                src = ms_ if q_ % 2 == 0 else mi_
                K.op("dve", lambda e, d_=d_, q_=q_, src=src: e.tensor_copy(mask4[d_][:, q_ * 128:(q_ + 1) * 128], src[:]),
                     reads=["m_lt", "m_le", "m_gt", "m_ge"], writes=["mask4"])
        rT = [sb("rT%d" % d_, [128, T], BF16, stack=pw) for d_ in range(2)]
        kpT = [sb("kpT%d" % d_, [128, T], BF16, stack=pw) for d_ in range(2)]
        ktT = [sb("ktT%d" % d_, [128, T], BF16, stack=pw) for d_ in range(2)]
        nbT = [sb("nbT%d" % d_, [128, T], BF16, stack=pw) for d_ in range(2)]
        Lam = [sb("Lam%d" % d_, [128, NT], stack=pw) for d_ in range(2)]
        vtk = sb("rvtok", [128, NT, 128], BF16, stack=pw)
        bonus = sb("bonus", [128, T], BF16, stack=pw)
        gateT = sb("gateT", [128, T], BF16, stack=pw)
        ybuf = [sb("ybuf%d" % d_, [128, NT, 128], BF16, stack=pw) for d_ in range(2)]
        rmst = sb("rmst", [128, TL], BF16, stack=pw)
        bt = {nm: sb("b_" + nm, [128, BL], stack=pw) for nm in
              ("r", "k", "v", "kap", "sig", "cum", "w", "iw", "wp", "a", "t1", "t2", "rk")}
        lbt = sb("b_lora", [128, 3, BL], BF16, stack=pw)
        vb16 = sb("b_vb16", [128, BL], BF16, stack=pw)
        Z = [sb("Z%d" % d_, [128, 128], stack=pw) for d_ in range(2)]
        Zb = [sb("Zb%d" % d_, [128, 128], BF16, stack=pw) for d_ in range(2)]
        AK = [[sb("AK%d_%d" % (d_, z_), [128, 512], BF16, stack=pw) for z_ in range(2)] for d_ in range(2)]
        ANB = [[sb("ANB%d_%d" % (d_, z_), [128, 512], BF16, stack=pw) for z_ in range(2)] for d_ in range(2)]
        YY = [[sb("YY%d%d" % (d_, hh), [128, 128], RW_NDT, stack=pw) for hh in range(2)] for d_ in range(2)]
        YYt = [[sb("YYt%d%d" % (d_, hh), [128, 128], RW_NDT, stack=pw) for hh in range(2)] for d_ in range(2)]
        AinvS = [[[sb("Ainv%d%d_%d" % (d_, hh, z_), [128, 128], BF16, stack=pw) for z_ in range(2)] for hh in range(2)] for d_ in range(2)]
        ktok_ = [[sb("rktok%d_%d" % (d_, z_), [128, 128], BF16, stack=pw) for z_ in range(2)] for d_ in range(2)]
        nbtok_ = [[sb("rnbtok%d_%d" % (d_, z_), [128, 128], BF16, stack=pw) for z_ in range(2)] for d_ in range(2)]
        P1b = [sb("P1b%d" % d_, [128, 128], BF16, stack=pw) for d_ in range(2)]
        Ub = [sb("Ub%d" % d_, [128, 128], BF16, stack=pw) for d_ in range(2)]
        zt = [sb("zt%d" % d_, [128, 128], stack=pw) for d_ in range(2)]
        yo = sb("yo", [128, 128], stack=pw)
        yc = sb("yc", [128, 128], stack=pw)
        ysq = sb("ysq", [128, 128], stack=pw)
        yst = sb("yst", [128, 8], stack=pw)
        yt2 = sb("yt2", [128, 128], stack=pw)
        cst = [sb("cst%d" % i_, [128, 2, D], stack=pw) for i_ in range(2)]
        cbf = [sb("cbf%d" % i_, [128, 2 * D], BF16, stack=pw) for i_ in range(2)]
        conv_n = [0, 0]

        def conv_load():
            c_ = conv_n[0]
            if c_ >= 128:
                return
            conv_n[0] += 1
            st_ = cst[c_ % 2]; sk_ = "cst%d" % (c_ % 2)
            K.dma(st_[:, 0, :], down_d[c_ * 128:(c_ + 1) * 128, :], writes=[sk_], key=sk_)
            K.dma(st_[:, 1, :], up_d[c_ * 128:(c_ + 1) * 128, :], writes=[sk_], key=sk_)

        def conv_emit():
            c_ = conv_n[1]
            if c_ >= 128:
                return
            conv_n[1] += 1
            conv_load()
            st_ = cst[c_ % 2]; bf_ = cbf[c_ % 2]
            sk_ = "cst%d" % (c_ % 2); bk_ = "cbf%d" % (c_ % 2)
            K.op("act", lambda e, st_=st_, bf_=bf_: e.copy(bf_[:, 0:D], st_[:, 0, :]), reads=[sk_], writes=[bk_ + "a"])
            K.op("pool", lambda e, st_=st_, bf_=bf_: e.tensor_copy(bf_[:, D:2 * D], st_[:, 1, :]), reads=[sk_], writes=[bk_ + "b"])
            K.dma(comb_d[c_ * 128:(c_ + 1) * 128, :], bf_[:, :], reads=[bk_ + "a", bk_ + "b"], writes=["comb"], key="st_cbf")

        conv_load()

        for P in range(4):
            ch = slice(P * 128, (P + 1) * 128)
            for (b0, bn) in blocks:
                bs = slice(b0, b0 + bn)
                ntb = bn // 128
                for nm, jj in (("r", P), ("k", 4 + P), ("v", 8 + P)):
                    K.dma(bt[nm][:, 0:bn], pm_d[jj, :, bs], reads=[("pm", jj)], writes=["b_" + nm], key="b_" + nm)
                K.dma(lbt[:, :, 0:bn], lora_d[:, :, bs].rearrange("q p t -> p q t"), reads=[("lora", 0), ("lora", 1), ("lora", 2)], writes=["b_lora"], key="b_lora")
                K.op("pool", lambda e, bn=bn: e.tensor_copy(vb16[:, 0:bn], bt["v"][:, 0:bn]), reads=["b_v"], writes=["vb16"])
                for ii in range(ntb):
                    gi = b0 // 128 + ii
                    pt = psb[6 + (ii % 2)]; pk = "ps%d" % (6 + (ii % 2))
                    K.op("pe", lambda e, ii=ii, pt=pt: e.transpose(pt[:, 0:128], vb16[:, ii * 128:(ii + 1) * 128], identb[:]), reads=["vb16", "identb"], writes=[pk])
                    K.op("act", lambda e, gi=gi, pt=pt: e.copy(vtk[:, gi, :], pt[:, 0:128]), reads=[pk], writes=["rvtok"])
                kkc = pk2[:, PK2["KK"] + P:PK2["KK"] + P + 1]
                K.op("dve", lambda e, bn=bn, kkc=kkc: e.tensor_scalar(bt["kap"][:, 0:bn], bt["k"][:, 0:bn], kkc, None, op0=ALU.mult), reads=["b_k", "pk2"], writes=["b_kap"])
                K.op("pool", lambda e, bn=bn: e.tensor_tensor(bt["t1"][:, 0:bn], bt["kap"][:, 0:bn], bt["kap"][:, 0:bn], op=ALU.mult), reads=["b_kap"], writes=["b_t1"])
                for n in range((bn + 511) // 512):
                    t0 = n * 512; tn = min(512, bn - t0)
                    K.op("pe", lambda e, t0=t0, tn=tn: e.matmul(ps[0][:, 0:tn], lhsT=bones[:], rhs=bt["t1"][:, t0:t0 + tn], start=True, stop=True), reads=["bones", "b_t1"], writes=["ps0"])
                    K.op("act", lambda e, t0=t0, tn=tn: e.activation(bt["t2"][:, t0:t0 + tn], ps[0][:, 0:tn], AF.Sqrt, bias=eps_t[:, 1:2]), reads=["ps0", "eps"], writes=["b_t2"])
                K.op("dve", lambda e, bn=bn: e.reciprocal(bt["t2"][:, 0:bn], bt["t2"][:, 0:bn]), reads=["b_t2"], writes=["b_t2"])
                K.op("dve", lambda e, bn=bn: e.tensor_tensor(bt["kap"][:, 0:bn], bt["kap"][:, 0:bn], bt["t2"][:, 0:bn], op=ALU.mult), reads=["b_kap", "b_t2"], writes=["b_kap"])
                for n in range((bn + 511) // 512):
                    t0 = n * 512; tn = min(512, bn - t0)
                    K.op("pe", lambda e, t0=t0, tn=tn: e.matmul(ps[1][:, 0:tn], lhsT=wlb[:, 2, ch], rhs=lbt[:, 2, t0:t0 + tn], start=True, stop=True), reads=["wlb", "b_lora"], writes=["ps1"])
                    K.op("act", lambda e, t0=t0, tn=tn: e.copy(gateT[:, b0 + t0:b0 + t0 + tn], ps[1][:, 0:tn]), reads=["ps1"], writes=["gateT"])
                first_rk = True
                for d_ in range(2):
                    ds = slice(d_ * 64, (d_ + 1) * 64)
                    w0c = pk2[:, PK2["W0"] + d_ * 4 + P:PK2["W0"] + d_ * 4 + P + 1]
                    a0c = pk2[:, PK2["A0"] + d_ * 4 + P:PK2["A0"] + d_ * 4 + P + 1]
                    for n in range((bn + 511) // 512):
                        t0 = n * 512; tn = min(512, bn - t0)
                        K.op("pe", lambda e, t0=t0, tn=tn, ds=ds: e.matmul(ps[2][:, 0:tn], lhsT=wlb[ds, 0, ch], rhs=lbt[ds, 0, t0:t0 + tn], start=True, stop=True), reads=["wlb", "b_lora"], writes=["ps2"])
                        K.op("act", lambda e, t0=t0, tn=tn, w0c=w0c: e.activation(bt["sig"][:, t0:t0 + tn], ps[2][:, 0:tn], AF.Sigmoid, bias=w0c), reads=["ps2", "pk2"], writes=["b_sig"])
                        K.op("pe", lambda e, t0=t0, tn=tn, ds=ds: e.matmul(ps[3][:, 0:tn], lhsT=wlb[ds, 1, ch], rhs=lbt[ds, 1, t0:t0 + tn], start=True, stop=True), reads=["wlb", "b_lora"], writes=["ps3"])
                        K.op("act", lambda e, t0=t0, tn=tn, a0c=a0c: e.activation(bt["a"][:, t0:t0 + tn], ps[3][:, 0:tn], AF.Sigmoid, bias=a0c), reads=["ps3", "pk2"], writes=["b_a"])
                    K.op("dve", lambda e, bn=bn: e.tensor_tensor_scan(bt["cum"][:, 0:bn], rmask[:, 0:bn], bt["sig"][:, 0:bn], 0.0, op0=ALU.mult, op1=ALU.add),
                         reads=["rmask", "b_sig"], writes=["b_cum"])
                    c3 = bt["cum"][:, 0:bn].rearrange("p (n t) -> p n t", t=128)
                    tot_b = c3[:, :, 127:128].to_broadcast([128, ntb, 128])
                    K.op("act", lambda e, d_=d_, c3=c3, ntb=ntb: e.activation(Lam[d_][:, b0 // 128:b0 // 128 + ntb], c3[:, :, 127], AF.Exp, scale=-CW_),
                         reads=["b_cum"], writes=["Lam%d" % d_])
                    if d_ == 1:
                        K.op("dve", lambda e, bn=bn, c3=c3, tot_b=tot_b, ntb=ntb: e.tensor_tensor(
                            bt["t1"][:, 0:bn].rearrange("p (n t) -> p n t", t=128), tot_b, c3, op=ALU.subtract), reads=["b_cum"], writes=["b_t1"])
                        K.op("dve", lambda e, bn=bn: e.tensor_tensor(bt["cum"][:, 0:bn], bt["t1"][:, 0:bn], bt["sig"][:, 0:bn], op=ALU.add),
                             reads=["b_t1", "b_sig"], writes=["b_cum"])
                    K.op("act", lambda e, bn=bn: e.activation(bt["w"][:, 0:bn], bt["cum"][:, 0:bn], AF.Exp, scale=-CW_), reads=["b_cum"], writes=["b_w"])
                    K.op("act", lambda e, bn=bn: e.activation(bt["iw"][:, 0:bn], bt["cum"][:, 0:bn], AF.Exp, scale=CW_), reads=["b_cum"], writes=["b_iw"])
                    K.op("pool", lambda e, bn=bn: e.tensor_tensor(bt["t1"][:, 0:bn], bt["cum"][:, 0:bn], bt["sig"][:, 0:bn], op=ALU.subtract), reads=["b_cum", "b_sig"], writes=["b_t1"])
                    K.op("act", lambda e, bn=bn: e.activation(bt["wp"][:, 0:bn], bt["t1"][:, 0:bn], AF.Exp, scale=-CW_), reads=["b_t1"], writes=["b_wp"])
                    K.op("dve", lambda e, d_=d_, bn=bn: e.tensor_tensor(rT[d_][:, bs], bt["r"][:, 0:bn], bt["w"][:, 0:bn], op=ALU.mult), reads=["b_r", "b_w"], writes=["rT%d" % d_])
                    K.op("pool", lambda e, d_=d_, bn=bn: e.tensor_tensor(kpT[d_][:, bs], bt["kap"][:, 0:bn], bt["wp"][:, 0:bn], op=ALU.mult), reads=["b_kap", "b_wp"], writes=["kpT%d" % d_])
                    kac = pk2[:, PK2["KA"] + P:PK2["KA"] + P + 1]
                    K.op("dve", lambda e, bn=bn, kac=kac: e.tensor_scalar(bt["t1"][:, 0:bn], bt["a"][:, 0:bn], -1.0, kac, op0=ALU.add, op1=ALU.mult), reads=["b_a", "pk2"], writes=["b_t1"])
                    K.op("dve", lambda e, bn=bn: e.scalar_tensor_tensor(bt["t1"][:, 0:bn], bt["t1"][:, 0:bn], 1.0, bt["k"][:, 0:bn], op0=ALU.add, op1=ALU.mult), reads=["b_t1", "b_k"], writes=["b_t1"])
                    K.op("dve", lambda e, d_=d_, bn=bn: e.tensor_tensor(ktT[d_][:, bs], bt["t1"][:, 0:bn], bt["iw"][:, 0:bn], op=ALU.mult), reads=["b_t1", "b_iw"], writes=["ktT%d" % d_])
                    if first_rk:
                        K.op("pool", lambda e, bn=bn: e.tensor_tensor(bt["rk"][:, 0:bn], bt["t1"][:, 0:bn], bt["r"][:, 0:bn], op=ALU.mult), reads=["b_t1", "b_r"], writes=["b_rk"])
                        first_rk = False
                    else:
                        K.op("pool", lambda e, bn=bn: e.tensor_tensor(bt["t2"][:, 0:bn], bt["t1"][:, 0:bn], bt["r"][:, 0:bn], op=ALU.mult), reads=["b_t1", "b_r"], writes=["b_t2"])
                        K.op("pool", lambda e, bn=bn: e.tensor_tensor(bt["rk"][:, 0:bn], bt["rk"][:, 0:bn], bt["t2"][:, 0:bn], op=ALU.add), reads=["b_rk", "b_t2"], writes=["b_rk"])
                    K.op("dve", lambda e, bn=bn: e.scalar_tensor_tensor(bt["t2"][:, 0:bn], bt["kap"][:, 0:bn], -1.0, bt["a"][:, 0:bn], op0=ALU.mult, op1=ALU.mult), reads=["b_kap", "b_a"], writes=["b_t2"])
                    K.op("dve", lambda e, d_=d_, bn=bn: e.tensor_tensor(nbT[d_][:, bs], bt["t2"][:, 0:bn], bt["iw"][:, 0:bn], op=ALU.mult), reads=["b_t2", "b_iw"], writes=["nbT%d" % d_])
                rkc = pk2[:, PK2["RK"] + P:PK2["RK"] + P + 1]
                K.op("dve", lambda e, bn=bn, rkc=rkc: e.tensor_scalar(bt["rk"][:, 0:bn], bt["rk"][:, 0:bn], rkc, None, op0=ALU.mult), reads=["b_rk", "pk2"], writes=["b_rk"])
                for n in range((bn + 511) // 512):
                    t0 = n * 512; tn = min(512, bn - t0)
                    K.op("pe", lambda e, t0=t0, tn=tn: e.matmul(ps[4][:, 0:tn], lhsT=bones[:], rhs=bt["rk"][:, t0:t0 + tn], start=True, stop=True), reads=["bones", "b_rk"], writes=["ps4"])
                    K.op("dve", lambda e, t0=t0, tn=tn: e.tensor_tensor(bonus[:, b0 + t0:b0 + t0 + tn], ps[4][:, 0:tn], bt["v"][:, t0:t0 + tn], op=ALU.mult), reads=["ps4", "b_v"], writes=["bonus"])
            stop_at("rw_prep")
            for d_ in range(2):
                K.op("pool", lambda e, d_=d_: e.memset(Z[d_][:], 0.0), writes=["Z%d" % d_])
                K.op("pool", lambda e, d_=d_: e.memset(Zb[d_][:], 0.0), writes=["Zb%d" % d_])
            def r1(step, d_):
                i = fwd_order[step] if d_ == 0 else bwd_order[step]
                sl = slice(i * 128, (i + 1) * 128)
                want_o = i >= 2
                dk_ = "%d" % d_
                pz = step % 2
                pk_ = "%d_%d" % (d_, pz)
                b_ = d_ * 4
                for hh in range(2):
                    hs = slice(hh * 64, (hh + 1) * 64)
                    pt = ps[b_ + hh]; pk = "ps%d" % (b_ + hh)
                    for qq, src in enumerate((ktT, nbT)):
                        K.op("pe", lambda e, src=src, pt=pt, qq=qq, hs=hs, d_=d_, sl=sl: e.matmul(pt[:, qq * 256:qq * 256 + 128], lhsT=src[d_][hs, sl], rhs=kpT[d_][hs, sl], start=True, stop=True),
                             reads=["ktT" + dk_, "nbT" + dk_, "kpT" + dk_], writes=[pk])
                        K.op("pe", lambda e, src=src, pt=pt, qq=qq, hs=hs, d_=d_, sl=sl: e.matmul(pt[:, qq * 256 + 128:qq * 256 + 256], lhsT=src[d_][hs, sl], rhs=rT[d_][hs, sl], start=True, stop=True),
                             reads=["ktT" + dk_, "nbT" + dk_, "rT" + dk_], writes=[pk])
                for hh in range(2):
                    K.op("dve", lambda e, pz=pz, d_=d_, hh=hh: e.tensor_tensor(AK[d_][pz][:, hh * 256:(hh + 1) * 256], ps[d_ * 4 + hh][:, 0:256], mask4[d_][:, 0:256], op=ALU.mult),
                         reads=["ps%d" % (b_ + hh), "mask4"], writes=["AK" + pk_])
                    K.op("dve", lambda e, pz=pz, d_=d_, hh=hh: e.tensor_tensor(ANB[d_][pz][:, hh * 256:(hh + 1) * 256], ps[d_ * 4 + hh][:, 256:512], mask4[d_][:, 0:256], op=ALU.mult),
                         reads=["ps%d" % (b_ + hh), "mask4"], writes=["ANB" + pk_])
                pT2 = psb[b_ + 2]; kT2 = "ps%d" % (b_ + 2)
                K.op("pe", lambda e, pz=pz, d_=d_, sl=sl, pT2=pT2: e.transpose(pT2[:, 0:128], ktT[d_][:, sl], identb[:]), reads=["ktT" + dk_, "identb"], writes=[kT2])
                K.op("pe", lambda e, pz=pz, d_=d_, sl=sl, pT2=pT2: e.transpose(pT2[:, 128:256], nbT[d_][:, sl], identb[:]), reads=["nbT" + dk_, "identb"], writes=[kT2])
                K.op("act", lambda e, pz=pz, d_=d_, pT2=pT2: e.copy(ktok_[d_][pz][:], pT2[:, 0:128]), reads=[kT2], writes=["rktok" + pk_])
                K.op("act", lambda e, pz=pz, d_=d_, pT2=pT2: e.copy(nbtok_[d_][pz][:], pT2[:, 128:256]), reads=[kT2], writes=["rnbtok" + pk_])
            def r2(step, d_, hh):
                dk_ = "%d" % d_
                pz = step % 2
                pk_ = "%d_%d" % (d_, pz)
                tag = "n%d" % (d_ * 2 + hh)
                bank = d_ * 4 + hh
                kB2 = "ps%d" % bank
                K.op("pool", lambda e: e.tensor_copy(YY[d_][hh][:], ANB[d_][pz][:, hh * 256:hh * 256 + 128]), reads=["ANB" + pk_], writes=[tag + "Y"])
                pB = (psb if RW_NM[0] == "bf16" else ps)[bank]
                idn_ = identb if RW_NM[0] == "bf16" else ident
                K.op("pe", lambda e: e.transpose(pB[:, 0:128], YY[d_][hh][:], idn_[:]), reads=[tag + "Y", "identb", "ident"], writes=[kB2])
                K.op("act", lambda e: e.copy(YYt[d_][hh][:], pB[:, 0:128]), reads=[kB2], writes=[tag + "Yt"])
                Ai, kAi = neumann(YY[d_][hh], YYt[d_][hh], tag, bank, pb=(bank, 384))
                K.op("dve", lambda e: e.tensor_copy(AinvS[d_][hh][pz][:], Ai), reads=[kAi], writes=["Ainv%d%d_%d" % (d_, hh, pz)])

            def r3(step, d_):
                i = fwd_order[step] if d_ == 0 else bwd_order[step]
                sl = slice(i * 128, (i + 1) * 128)
                want_o = i >= 2
                dk_ = "%d" % d_
                pz = step % 2
                pk_ = "%d_%d" % (d_, pz)
                b_ = d_ * 4
                pC = ps[b_ + 3]; kC = "ps%d" % (b_ + 3)
                for hh in range(2):
                    K.op("pe", lambda e, pz=pz, d_=d_, hh=hh, i=i, pC=pC: e.matmul(pC[:, hh * 64:(hh + 1) * 64], lhsT=AK[d_][pz][:, hh * 256:hh * 256 + 128], rhs=vtk[:, i, hh * 64:(hh + 1) * 64], start=(hh == 0), stop=False),
                         reads=["AK" + pk_, "rvtok"], writes=[kC])
                K.op("pe", lambda e, pz=pz, d_=d_, sl=sl, pC=pC: e.matmul(pC[:, 0:128], lhsT=kpT[d_][:, sl], rhs=Zb[d_][:], start=False, stop=True), reads=["kpT" + dk_, "Zb" + dk_], writes=[kC])
                K.op("act", lambda e, pz=pz, d_=d_, pC=pC: e.copy(P1b[d_][:], pC[:, 0:128]), reads=[kC], writes=["P1b" + dk_])
                for hh in range(2):
                    K.op("pe", lambda e, pz=pz, d_=d_, hh=hh, pC=pC: e.matmul(pC[:, 128 + hh * 64:128 + (hh + 1) * 64], lhsT=AinvS[d_][hh][pz][:], rhs=P1b[d_][:, hh * 64:(hh + 1) * 64], start=True, stop=True),
                         reads=["Ainv%d%d_%d" % (d_, hh, pz), "P1b" + dk_], writes=[kC])
                K.op("dve", lambda e, pz=pz, d_=d_, pC=pC: e.tensor_copy(Ub[d_][:], pC[:, 128:256]), reads=[kC], writes=["Ub" + dk_])
                if want_o:
                    for hh in range(2):
                        K.op("pe", lambda e, pz=pz, d_=d_, hh=hh, i=i, pC=pC: e.matmul(pC[:, 256 + hh * 64:256 + (hh + 1) * 64], lhsT=AK[d_][pz][:, hh * 256 + 128:hh * 256 + 256], rhs=vtk[:, i, hh * 64:(hh + 1) * 64], start=(hh == 0), stop=False),
                             reads=["AK" + pk_, "rvtok"], writes=[kC])
                    K.op("pe", lambda e, pz=pz, d_=d_, sl=sl, pC=pC: e.matmul(pC[:, 256:384], lhsT=rT[d_][:, sl], rhs=Zb[d_][:], start=False, stop=False), reads=["rT" + dk_, "Zb" + dk_], writes=[kC])
                    for hh in range(2):
                        K.op("pe", lambda e, pz=pz, d_=d_, hh=hh, pC=pC: e.matmul(pC[:, 256 + hh * 64:256 + (hh + 1) * 64], lhsT=ANB[d_][pz][:, hh * 256 + 128:hh * 256 + 256], rhs=Ub[d_][:, hh * 64:(hh + 1) * 64], start=False, stop=(hh == 1)),
                             reads=["ANB" + pk_, "Ub" + dk_], writes=[kC])
                    K.op("act", lambda e, pz=pz, d_=d_, i=i, pC=pC: e.copy(ybuf[d_][:, i, :], pC[:, 256:384]), reads=[kC], writes=["ybuf" + dk_])
                pD = ps[b_ + 3]; kD = "ps%d" % (b_ + 3)
                K.op("pe", lambda e, pz=pz, d_=d_, i=i, pD=pD: e.matmul(pD[:, 384:512], lhsT=ktok_[d_][pz][:], rhs=vtk[:, i, :], start=True, stop=False), reads=["rktok" + pk_, "rvtok"], writes=[kD])
                K.op("pe", lambda e, pz=pz, d_=d_, pD=pD: e.matmul(pD[:, 384:512], lhsT=nbtok_[d_][pz][:], rhs=Ub[d_][:], start=False, stop=True), reads=["rnbtok" + pk_, "Ub" + dk_], writes=[kD])
                K.op("dve", lambda e, pz=pz, d_=d_, pD=pD: e.tensor_tensor(zt[d_][:], pD[:, 384:512], Z[d_][:], op=ALU.add), reads=[kD, "Z" + dk_], writes=["zt" + dk_])
                K.op("dve", lambda e, pz=pz, d_=d_, i=i: e.scalar_tensor_tensor(Z[d_][:], zt[d_][:], Lam[d_][:, i:i + 1], bones[:], op0=ALU.mult, op1=ALU.mult),
                     reads=["zt" + dk_, "Lam" + dk_, "bones"], writes=["Z" + dk_])
                K.op("act", lambda e, pz=pz, d_=d_: e.copy(Zb[d_][:], Z[d_][:]), reads=["Z" + dk_], writes=["Zb" + dk_])
            for d_ in K.streams(2):
                r1(0, d_)
            for q_ in K.streams(4):
                r2(0, q_ // 2, q_ % 2)
            for step in range(NT):
                conv_emit()
                if step + 1 < NT:
                    for d_ in K.streams(2):
                        r1(step + 1, d_)
                    for q_ in K.streams(6):
                        if q_ < 2:
                            r3(step, q_)
                        else:
                            r2(step + 1, (q_ - 2) // 2, (q_ - 2) % 2)
                else:
                    for d_ in K.streams(2):
                        r3(step, d_)
            stop_at("rw_scan")
            gwc = pk2[:, PK2["GNW"] + P:PK2["GNW"] + P + 1]
            gbc = pk2[:, PK2["GNB"] + P:PK2["GNB"] + P + 1]
            for i in range(2, NT):
                K.op("dve", lambda e, i=i: e.tensor_tensor(yo[:], ybuf[0][:, i, :], ybuf[1][:, i, :], op=ALU.add), reads=["ybuf0", "ybuf1"], writes=["yo"])
                y3 = yo[:].rearrange("p (h c) -> p h c", c=64)
                K.op("dve", lambda e, y3=y3: e.reduce_sum(yst[:, 0:2], y3, axis=AX.X), reads=["yo"], writes=["yst0"])
                K.op("dve", lambda e: e.tensor_scalar(yst[:, 2:4], yst[:, 0:2], 1.0 / 64, None, op0=ALU.mult), reads=["yst0"], writes=["yst1"])
                K.op("dve", lambda e, y3=y3: e.tensor_tensor(yc[:].rearrange("p (h c) -> p h c", c=64), y3, yst[:, 2:4].unsqueeze(2).to_broadcast([128, 2, 64]), op=ALU.subtract),
                     reads=["yo", "yst1"], writes=["yc"])
                K.op("act", lambda e: e.activation(ysq[:], yc[:], AF.Square), reads=["yc"], writes=["ysq"])
                K.op("dve", lambda e: e.reduce_sum(yst[:, 4:6], ysq[:].rearrange("p (h c) -> p h c", c=64), axis=AX.X), reads=["ysq"], writes=["yst2"])
                K.op("act", lambda e: e.activation(yst[:, 6:8], yst[:, 4:6], AF.Sqrt, bias=eps_t[:, 2:3], scale=1.0 / 64), reads=["yst2", "eps"], writes=["yst3"])
                K.op("dve", lambda e: e.reciprocal(yst[:, 6:8], yst[:, 6:8]), reads=["yst3"], writes=["yst3"])
                K.op("dve", lambda e: e.tensor_tensor(yt2[:].rearrange("p (h c) -> p h c", c=64), yc[:].rearrange("p (h c) -> p h c", c=64),
                                                      yst[:, 6:8].unsqueeze(2).to_broadcast([128, 2, 64]), op=ALU.mult), reads=["yc", "yst3"], writes=["yt2"])
                pt = ps[i % 2]; pk = "ps%d" % (i % 2)
                K.op("pe", lambda e, pt=pt: e.transpose(pt[:, 0:128], yt2[:], ident[:]), reads=["yt2", "ident"], writes=[pk])
                K.op("act", lambda e, pt=pt: e.activation(yc[:], pt[:, 0:128], AF.Identity, bias=gbc, scale=gwc), reads=[pk, "pk2", "yc"], writes=["yc"])
                K.op("dve", lambda e, i=i: e.tensor_tensor(yc[:], yc[:], bonus[:, i * 128:(i + 1) * 128], op=ALU.add), reads=["yc", "bonus"], writes=["yc"])
                K.op("dve", lambda e, i=i: e.tensor_tensor(rmst[:, (i - 2) * 128:(i - 1) * 128], yc[:], gateT[:, i * 128:(i + 1) * 128], op=ALU.mult), reads=["yc", "gateT"], writes=["rmst"])
            K.dma(mT_d[4 + P, :, :], rmst[:, :], reads=["rmst"], writes=[("mT", 4 + P)], key="st_rmst")
        while conv_n[1] < 128:
            conv_emit()
    K.barrier()

    stop_at("mix_done")
    with ExitStack() as pp_:
        mTs = [sb("mTs%d" % i_, [128, 8, 128], BF16, stack=pp_) for i_ in range(2)]
        woutb = sb("woutb", [128, 8, D], BF16, stack=pp_)
        wqb = sb("wqb", [128, 8, 2048], BF16, stack=pp_)
        skT = sb("skT", [128, 16, 128], BF16, stack=pp_)
        with ExitStack() as pset:
            wstg = sb("wstg", [128, 8, 512], stack=pset)
            skst = sb("skst", [128, 16, 128], stack=pset)
            for hf in range(2):
                K.dma(wstg[:, :, :], wout_d[:, hf * 512:(hf + 1) * 512].rearrange("(k p) c -> p k c", p=128), writes=["wstg"], key="wstg")
                K.op("pool", lambda e, hf=hf: e.tensor_copy(woutb[:, :, hf * 512:(hf + 1) * 512], wstg[:]), reads=["wstg"], writes=["woutb"])
            for hf in range(4):
                K.dma(wstg[:, :, :], wq_d[:, hf * 512:(hf + 1) * 512].rearrange("(k p) c -> p k c", p=128), writes=["wstg"], key="wstg")
                K.op("pool", lambda e, hf=hf: e.tensor_copy(wqb[:, :, hf * 512:(hf + 1) * 512], wstg[:]), reads=["wstg"], writes=["wqb"])
            K.dma(skst[:, :, :], sk_d[:, :, :].rearrange("g k d -> k g d"), writes=["skst"], key="skst")
            for g in range(16):
                pt = ps[g % 2]; pk = "ps%d" % (g % 2)
                K.op("pe", lambda e, g=g, pt=pt: e.transpose(pt[:, 0:128], skst[:, g, :], ident[:]), reads=["skst", "ident"], writes=[pk])
                K.op("act", lambda e, g=g, pt=pt: e.copy(skT[:, g, :], pt[:, 0:128]), reads=[pk], writes=["skT"])
            K.barrier()
        gtB = sb("gtB2", [128, 4, D], stack=pp_)
        K.dma(gtB[:].rearrange("p q d -> p (q d)"), gt_d[:, :], reads=["gt_d"], writes=["gtB"], key="gtB2")
        A2row = sb("A2row", [128, D], stack=pp_)
        fgB = sb("fgB", [128, D], stack=pp_)
        K.dma(A2row[:, :], g2row_d.partition_broadcast(128), writes=["A2row"], key="A2row")
        K.dma(fgB[:, :], fng_d.partition_broadcast(128), writes=["fgB"], key="fgB")
        K.op("dve", lambda e: e.scalar_tensor_tensor(A2row[:], gtB[:, 2, :], 1.0, A2row[:], op0=ALU.add, op1=ALU.mult), reads=["gtB", "A2row"], writes=["A2row"])
        iota16i = sb("iota16i", [128, 16], I32, stack=pp_)
        iota16 = sb("iota16", [128, 16], stack=pp_)
        K.op("pool", lambda e: e.iota(iota16i[:], pattern=[[1, 16]], base=0, channel_multiplier=0), writes=["iota16i"])
        K.op("dve", lambda e: e.tensor_copy(iota16[:], iota16i[:]), reads=["iota16i"], writes=["iota16"])

        xt_ = sb("p_xt", [128, D], stack=pp_)
        x1 = sb("p_x1", [128, D], stack=pp_)
        h2 = sb("p_h2", [128, D], stack=pp_)
        yacc = sb("p_y", [128, D], stack=pp_)
        pj = sb("p_junk", [128, D], stack=pp_)
        pss = sb("p_ss", [128, 1], stack=pp_)
        prs = sb("p_rs", [128, 2], stack=pp_)
        pxs = sb("p_xs", [128, D], stack=pp_)
        h2T = sb("p_h2T", [128, 8, 128], BF16, stack=pp_)
        qTs = sb("p_qT", [128, 16, 128], BF16, stack=pp_)
        scs = sb("p_sc", [128, 16, 128], stack=pp_)
        tmp1 = sb("p_tmp1", [128, 16, 128], stack=pp_)
        tv = sb("p_tv", [128, 16, 16], stack=pp_)
        tiu = sb("p_tiu", [128, 16, 16], U32, stack=pp_)
        tif = sb("p_tif", [128, 16, 16], stack=pp_)
        cand = sb("p_cand", [128, 8, 256], stack=pp_)
        tmp2 = sb("p_tmp2", [128, 8, 256], stack=pp_)
        eq = tmp2
        bsv = sb("p_bs", [128, 8, 16], stack=pp_)
        posu = sb("p_posu", [128, 8, 16], U32, stack=pp_)
        pau = sb("p_pau", [128, 8, 16], U32, stack=pp_)
        pbu = sb("p_pbu", [128, 8, 16], U32, stack=pp_)
        paf = sb("p_paf", [128, 8, 16], stack=pp_)
        pbf = sb("p_pbf", [128, 8, 16], stack=pp_)
        i0f = sb("p_i0f", [128, 8, 16], stack=pp_)
        i1f = sb("p_i1f", [128, 8, 16], stack=pp_)
        eidx = sb("p_eidx", [128, 128], U32, stack=pp_)
        gat = sb("p_gate", [128, 8, 16], stack=pp_)
        gsum = sb("p_gsum", [128, 8], stack=pp_)
        apre = sb("p_apre", [128, 128], stack=pp_)
        coef = sb("p_coef", [128, 128], stack=pp_)
        NRB = 8
        rowc = [sb("p_rowc%d" % i, [128, 2 * D], BF16, stack=pp_) for i in range(NRB)]
        pjb = sb("p_junkb", [128, D], BF16, stack=pp_)
        h2b = sb("p_h2b", [128, D], BF16, stack=pp_)
        dgs = [sb("p_dg%d" % i, [128, 128], BF16, stack=pp_) for i in range(4)]
        gflat = sb("p_gflat", [128, 128], stack=pp_)
        NEG = -1.0e30

        for i in range(NTL):
            tsl = slice(i * 128, (i + 1) * 128)
            K.dma(xt_[:, :], x_d[tsl, :], writes=["p_xt"], key="p_xt")
            mTt = mTs[i % 2]; mk_ = "mTs%d" % (i % 2)
            K.dma(mTt[:, :, :], mT_d[:, :, tsl].rearrange("k p t -> p k t"), reads=[("mT", j) for j in range(8)], writes=[mk_], key=mk_)
            for hf in range(2):
                pt = ps[hf]; pk = "ps%d" % hf
                for k in range(8):
                    K.op("pe", lambda e, k=k, hf=hf, pt=pt, mTt=mTt: e.matmul(pt[:, :], lhsT=mTt[:, k, :], rhs=woutb[:, k, hf * 512:(hf + 1) * 512], start=(k == 0), stop=(k == 7)),
                         reads=[mk_, "woutb"], writes=[pk])
                K.op("dve", lambda e, hf=hf, pt=pt: e.tensor_tensor(x1[:, hf * 512:(hf + 1) * 512], pt[:, :], gtB[:, 0, hf * 512:(hf + 1) * 512], op=ALU.mult),
                     reads=[pk, "gtB"], writes=["p_x1"])
            K.op("pool", lambda e: e.tensor_tensor(x1[:], x1[:], xt_[:], op=ALU.add), reads=["p_x1", "p_xt"], writes=["p_x1"])
            K.op("act", lambda e: e.activation(pj[:], x1[:], AF.Square), reads=["p_x1"], writes=["p_junk"])
            K.op("dve", lambda e: e.reduce_sum(pss[:, 0:1], pj[:], axis=AX.X), reads=["p_junk"], writes=["p_ss"])
            K.op("act", lambda e: e.activation(prs[:, 0:1], pss[:, 0:1], AF.Sqrt, bias=eps_t[:, 0:1], scale=1.0 / D), reads=["p_ss", "eps"], writes=["p_rs"])
            K.op("dve", lambda e: e.reciprocal(prs[:, 1:2], prs[:, 0:1]), reads=["p_rs"], writes=["p_rs2"])
            K.op("dve", lambda e: e.tensor_scalar(pxs[:], x1[:], prs[:, 1:2], None, op0=ALU.mult), reads=["p_x1", "p_rs2"], writes=["p_xs"])
            for hf in range(2):
                pt = ps[2 + hf]; pk = "ps%d" % (2 + hf)
                for kk in range(4):
                    k = hf * 4 + kk
                    K.op("pe", lambda e, k=k, kk=kk, pt=pt: e.transpose(pt[:, kk * 128:(kk + 1) * 128], pxs[:, k * 128:(k + 1) * 128], ident[:]), reads=["p_xs", "ident"], writes=[pk])
                for kk in range(4):
                    k = hf * 4 + kk
                    K.op("act", lambda e, k=k, kk=kk, pt=pt: e.activation(h2T[:, k, :], pt[:, kk * 128:(kk + 1) * 128], AF.Identity, bias=B2[:, k:k + 1], scale=A2[:, k:k + 1]),
                         reads=[pk, "mods"], writes=["p_h2T"])
            K.op("dve", lambda e: e.tensor_tensor(h2[:], pxs[:], A2row[:], op=ALU.mult), reads=["p_xs", "A2row"], writes=["p_h2"])
            K.op("pool", lambda e: e.tensor_tensor(h2[:], h2[:], gtB[:, 1, :], op=ALU.add), reads=["p_h2", "gtB"], writes=["p_h2"])
            for g in range(16):
                pt = ps[4 + (g % 2)]; pk = "ps%d" % (4 + (g % 2))
                for k in range(8):
                    K.op("pe", lambda e, g=g, k=k, pt=pt: e.matmul(pt[:, 0:128], lhsT=wqb[:, k, g * 128:(g + 1) * 128], rhs=h2T[:, k, :], start=(k == 0), stop=(k == 7)),
                         reads=["wqb", "p_h2T"], writes=[pk])
                K.op("act", lambda e, g=g, pt=pt: e.copy(qTs[:, g, :], pt[:, 0:128]), reads=[pk], writes=[("p_qT", g)])
            for g in range(16):
                pt = ps[6 + (g // 4) % 2]; pk = "ps%d" % (6 + (g // 4) % 2)
                K.op("pe", lambda e, g=g, pt=pt: e.matmul(pt[:, (g % 4) * 128:(g % 4 + 1) * 128], lhsT=qTs[:, g, :], rhs=skT[:, g, :], start=True, stop=True),
                     reads=[("p_qT", g), "skT"], writes=[pk])
                if g % 4 == 3:
                    K.op("dve", lambda e, g=g, pt=pt: e.tensor_copy(scs[:, g - 3:g + 1, :].rearrange("p g k -> p (g k)"), pt[:, :]), reads=[pk], writes=[("p_sc", g // 4)])
            for g in range(16):
                K.op("dve", lambda e, g=g: e.max(tv[:, g, 0:8], scs[:, g, :]), reads=[("p_sc", g // 4)], writes=[("tv", g)])
            for g in range(16):
                K.op("dve", lambda e, g=g: e.max_index(tiu[:, g, 0:8], tv[:, g, 0:8], scs[:, g, :]), reads=[("p_sc", g // 4), ("tv", g)], writes=[("tiu", g)])
            for g in range(16):
                K.op("dve", lambda e, g=g: e.match_replace(tmp1[:, g, :], tv[:, g, 0:8], scs[:, g, :], NEG), reads=[("p_sc", g // 4), ("tv", g)], writes=[("tmp1", g)])
            for g in range(16):
                K.op("dve", lambda e, g=g: e.max(tv[:, g, 8:16], tmp1[:, g, :]), reads=[("tmp1", g)], writes=[("tv2", g)])
            for g in range(16):
                K.op("dve", lambda e, g=g: e.max_index(tiu[:, g, 8:16], tv[:, g, 8:16], tmp1[:, g, :]), reads=[("tmp1", g), ("tv2", g)], writes=[("tiu2", g)])
            allg = [("tv", g) for g in range(16)] + [("tv2", g) for g in range(16)]
            alli = [("tiu", g) for g in range(16)] + [("tiu2", g) for g in range(16)]
            K.op("dve", lambda e: e.tensor_copy(tif[:], tiu[:]), reads=alli, writes=["p_tif"])
            tvv = tv[:].rearrange("p (h q) a -> p h q a", q=2)
            tfv = tif[:].rearrange("p (h q) a -> p h q a", q=2)
            c4 = cand[:].rearrange("p h (a b) -> p h a b", b=16)
            K.op("dve", lambda e: e.tensor_tensor(c4, tvv[:, :, 0, :].unsqueeze(3).to_broadcast([128, 8, 16, 16]),
                                                  tvv[:, :, 1, :].unsqueeze(2).to_broadcast([128, 8, 16, 16]), op=ALU.add), reads=allg, writes=["p_cand"])
            for hh in range(8):
                K.op("dve", lambda e, hh=hh: e.max(bsv[:, hh, 0:8], cand[:, hh, :]), reads=["p_cand"], writes=[("bs", hh)])
            for hh in range(8):
                K.op("dve", lambda e, hh=hh: e.max_index(posu[:, hh, 0:8], bsv[:, hh, 0:8], cand[:, hh, :]), reads=["p_cand", ("bs", hh)], writes=[("pos", hh)])
            for hh in range(8):
                K.op("dve", lambda e, hh=hh: e.match_replace(tmp2[:, hh, :], bsv[:, hh, 0:8], cand[:, hh, :], NEG), reads=["p_cand", ("bs", hh), "p_eq"], writes=[("tmp2", hh)])
            for hh in range(8):
                K.op("dve", lambda e, hh=hh: e.max(bsv[:, hh, 8:16], tmp2[:, hh, :]), reads=[("tmp2", hh)], writes=[("bs2", hh)])
            for hh in range(8):
                K.op("dve", lambda e, hh=hh: e.max_index(posu[:, hh, 8:16], bsv[:, hh, 8:16], tmp2[:, hh, :]), reads=[("tmp2", hh), ("bs2", hh)], writes=[("pos2", hh)])
            allb = [("bs", hh) for hh in range(8)] + [("bs2", hh) for hh in range(8)]
            allp = [("pos", hh) for hh in range(8)] + [("pos2", hh) for hh in range(8)]
            K.op("dve", lambda e: e.tensor_single_scalar(pau[:], posu[:], 4, op=ALU.logical_shift_right), reads=allp, writes=["p_pau"])
            K.op("dve", lambda e: e.tensor_single_scalar(pbu[:], posu[:], 15, op=ALU.bitwise_and), reads=allp, writes=["p_pbu"])
            K.op("dve", lambda e: e.tensor_copy(paf[:], pau[:]), reads=["p_pau"], writes=["p_paf"])
            K.op("dve", lambda e: e.tensor_copy(pbf[:], pbu[:]), reads=["p_pbu"], writes=["p_pbf"])
            e4 = eq[:].rearrange("p h (k a) -> p h k a", a=16)
            io4 = iota16[:].unsqueeze(1).unsqueeze(1).to_broadcast([128, 8, 16, 16])
            for (pf, q_, dst, nm) in ((paf, 0, i0f, "i0f"), (pbf, 1, i1f, "i1f")):
                K.op("dve", lambda e, pf=pf: e.tensor_tensor(e4, pf[:].unsqueeze(3).to_broadcast([128, 8, 16, 16]), io4, op=ALU.is_equal),
                     reads=["p_paf", "p_pbf", "iota16"], writes=["p_eq"] + [("tmp2", hh_) for hh_ in range(8)])
                K.op("dve", lambda e, q_=q_: e.tensor_tensor(e4, e4, tfv[:, :, q_, :].unsqueeze(2).to_broadcast([128, 8, 16, 16]), op=ALU.mult),
                     reads=["p_eq", "p_tif"], writes=["p_eq"])
                K.op("dve", lambda e, dst=dst: e.reduce_sum(dst[:], e4, axis=AX.X), reads=["p_eq"], writes=["p_" + nm])
            K.op("dve", lambda e: e.scalar_tensor_tensor(i0f[:], i0f[:], 128.0, i1f[:], op0=ALU.mult, op1=ALU.add), reads=["p_i0f", "p_i1f"], writes=["p_i0f"])
            K.op("dve", lambda e: e.tensor_copy(eidx[:], i0f[:].rearrange("p h k -> p (h k)")), reads=["p_i0f"], writes=["p_eidx"])
            K.op("dve", lambda e: e.tensor_tensor(gat[:], bsv[:], bsv[:, :, 0:1].to_broadcast([128, 8, 16]), op=ALU.subtract), reads=allb, writes=["p_gate"])
            K.op("act", lambda e: e.activation(gat[:], gat[:], AF.Exp), reads=["p_gate"], writes=["p_gate"])
            K.op("dve", lambda e: e.reduce_sum(gsum[:], gat[:], axis=AX.X), reads=["p_gate"], writes=["p_gsum"])
            K.op("dve", lambda e: e.reciprocal(gsum[:], gsum[:]), reads=["p_gsum"], writes=["p_gsum"])
            K.op("dve", lambda e: e.tensor_tensor(gat[:], gat[:], gsum[:].unsqueeze(2).to_broadcast([128, 8, 16]), op=ALU.mult), reads=["p_gate", "p_gsum"], writes=["p_gate"])
            K.op("act", lambda e: e.copy(h2b[:], h2[:]), reads=["p_h2"], writes=["p_h2b"])
            K.op("dve", lambda e: e.tensor_copy(gflat[:], gat[:].rearrange("p h k -> p (h k)")), reads=["p_gate"], writes=["p_gflat"])
            GRP = 4
            for g0 in range(0, 128, GRP):
                for kslot in range(g0, g0 + GRP):
                    rb = rowc[kslot % NRB]; rk_ = "p_rowc%d" % (kslot % NRB)
                    K.gather(rb[:, :], comb_d[:, :], eidx[:, kslot:kslot + 1], reads=["p_eidx", "comb"], writes=[rk_], key=rk_)
                    K.op("dve", lambda e, rb=rb, kslot=kslot: e.scalar_tensor_tensor(pjb[:], rb[:, 0:D], 1.0, h2b[:], op0=ALU.mult, op1=ALU.mult, accum_out=apre[:, kslot:kslot + 1]),
                         reads=[rk_, "p_h2b"], writes=["p_junkb", ("apre", g0 // GRP)])
                K.op("dve", lambda e, g0=g0: e.tensor_copy(coef[:, g0:g0 + GRP], apre[:, g0:g0 + GRP]), reads=[("apre", g0 // GRP)], writes=[("cf0", g0 // GRP)])
                K.op("act", lambda e, g0=g0: e.activation(coef[:, g0:g0 + GRP], coef[:, g0:g0 + GRP], AF.Gelu), reads=[("cf0", g0 // GRP)], writes=[("cf1", g0 // GRP)])
                K.op("dve", lambda e, g0=g0: e.tensor_tensor(coef[:, g0:g0 + GRP], coef[:, g0:g0 + GRP], gflat[:, g0:g0 + GRP], op=ALU.mult),
                     reads=[("cf1", g0 // GRP), "p_gflat"], writes=[("cf2", g0 // GRP)])
                for kslot in range(g0, g0 + GRP):
                    rb = rowc[kslot % NRB]; rk_ = "p_rowc%d" % (kslot % NRB)
                    dg = dgs[kslot % 4]; dk__ = "p_dg%d" % (kslot % 4)
                    K.op("act", lambda e, dg=dg, kslot=kslot: e.activation(dg[:], identb[:], AF.Identity, scale=coef[:, kslot:kslot + 1]),
                         reads=["identb", ("cf2", g0 // GRP)], writes=[dk__])
                    for hf in range(2):
                        K.op("pe", lambda e, dg=dg, rb=rb, hf=hf, kslot=kslot: e.matmul(ps[hf][:, :], lhsT=dg[:], rhs=rb[:, D + hf * 512:D + (hf + 1) * 512],
                                                                                       start=(kslot == 0), stop=(kslot == 127)),
                             reads=[dk__, rk_], writes=["ps%d" % hf])
            for hf in range(2):
                K.op("act", lambda e, hf=hf: e.copy(yacc[:, hf * 512:(hf + 1) * 512], ps[hf][:, :]), reads=["ps%d" % hf], writes=["p_y"])
            K.op("dve", lambda e: e.tensor_tensor(yacc[:], yacc[:], gtB[:, 3, :], op=ALU.mult), reads=["p_y", "gtB"], writes=["p_y"])
            K.op("pool", lambda e: e.tensor_tensor(yacc[:], yacc[:], x1[:], op=ALU.add), reads=["p_y", "p_x1"], writes=["p_y"])
            K.op("act", lambda e: e.activation(pj[:], yacc[:], AF.Square), reads=["p_y"], writes=["p_junk"])
            K.op("dve", lambda e: e.reduce_sum(pss[:, 0:1], pj[:], axis=AX.X), reads=["p_junk"], writes=["p_ss"])
            K.op("act", lambda e: e.activation(prs[:, 0:1], pss[:, 0:1], AF.Sqrt, bias=eps_t[:, 0:1], scale=1.0 / D), reads=["p_ss", "eps"], writes=["p_rs"])
            K.op("dve", lambda e: e.reciprocal(prs[:, 1:2], prs[:, 0:1]), reads=["p_rs"], writes=["p_rs2"])
            K.op("dve", lambda e: e.scalar_tensor_tensor(pxs[:], yacc[:], prs[:, 1:2], fgB[:], op0=ALU.mult, op1=ALU.mult), reads=["p_y", "p_rs2", "fgB"], writes=["p_xs"])
            K.dma(out_d[tsl, :], pxs[:, :], reads=["p_xs"], writes=["outdone"], key="st_out")
            if i == 0:
                stop_at("peer_t0")
    K.barrier()
    if "mT" in dbg:
        d_o = dbgt("mT", [8, 128, TL], BF16)
        K.dma(d_o[:, :, :], mT_d[:, :, :], reads=[("mT", j) for j in range(8)], writes=["dbgmT"], key="dbg")

    K.finish([k for k in K.st.keys() if (isinstance(k, str) and k.startswith("dbg")) or k == "outdone"])
    return dbg_out


def _inputs_for_core(inp, b, n_rows):
    TL = 64 * n_rows
    f = lambda a: np.ascontiguousarray(np.asarray(a, dtype=np.float32))
    m = {
        "x": f(inp["x"][b, :TL]),
        "c": f(inp["c"][b:b + 1]),
        "ctx": f(inp["ctx"][b]),
        "c_ctx": f(inp["c_ctx"][None, :]),
        "ada_w": f(inp["ada_w"][0]),
        "ada_b": f(inp["ada_b"][0].reshape(48, 128)),
        "ada_b_row": f(inp["ada_b"][0].reshape(1, 6144)),
        "norm1_g": f(inp["norm1_g"][0].reshape(8, 128)),
        "w_in": f(inp["w_in"][0]),
        "gdn_conv_w": f(inp["gdn_conv_w"][0].reshape(60, 128)),
        "gdn_a_log": f(inp["gdn_a_log"][0].reshape(1, 8)),
        "gdn_dt_bias": f(inp["gdn_dt_bias"][0].reshape(1, 8)),
        "gdn_norm_w": f(inp["gdn_norm_w"][0].reshape(1, 128)),
        "rwkv_mu": f(inp["rwkv_mu"][0].reshape(15, 128)),
        "rwkv_w0": f(inp["rwkv_w0"][0].reshape(8, 128)),
        "rwkv_w2": f(inp["rwkv_w2"][0].reshape(128, 512)),
        "rwkv_a0": f(inp["rwkv_a0"][0].reshape(8, 128)),
        "rwkv_a2": f(inp["rwkv_a2"][0].reshape(128, 512)),
        "rwkv_g2": f(inp["rwkv_g2"][0]),
        "rwkv_k_k": f(inp["rwkv_k_k"][0].reshape(4, 128)),
        "rwkv_k_a": f(inp["rwkv_k_a"][0].reshape(4, 128)),
        "rwkv_r_k": f(inp["rwkv_r_k"][0].reshape(4, 128)),
        "rwkv_gn_w": f(inp["rwkv_gn_w"][0].reshape(4, 128)),
        "rwkv_gn_b": f(inp["rwkv_gn_b"][0].reshape(4, 128)),
        "w_out": f(inp["w_out"][0]),
        "norm2_g": f(inp["norm2_g"][0].reshape(8, 128)),
        "peer_w_query": f(inp["peer_w_query"][0]),
        "peer_sub_keys": f(inp["peer_sub_keys"][0].reshape(16, 128, 128)),
        "peer_down": f(inp["peer_down"][0]),
        "peer_up": f(inp["peer_up"][0]),
        "final_norm_g": f(inp["final_norm_g"][None, :]),
        "norm2_g_row": f(inp["norm2_g"][0].reshape(1, 1024)),
    }
    return m


def run(inp, n_rows=64, cores=None, dbg=(), stop=None):
    nb = inp["x"].shape[0]
    cores = list(range(nb)) if cores is None else cores
    nc = bass.Bass("TRN2", target_bir_lowering=False)
    build(nc, n_rows=n_rows, dbg=dbg, stop=stop)
    in_maps = [_inputs_for_core(inp, b, n_rows) for b in cores]
    res = run_bass_kernel_spmd(nc, in_maps, core_ids=list(range(len(cores))))
    return res.results


def kernel(**inputs):
    res = run(inputs, n_rows=64)
    return np.stack([np.asarray(r["out"], dtype=np.float32) for r in res], axis=0)
```

```python
from contextlib import ExitStack
import numpy as np
import concourse.bass as bass
import concourse.mybir as mybir
from concourse.bass_utils import run_bass_kernel_spmd

F32 = mybir.dt.float32
BF16 = mybir.dt.bfloat16
I32 = mybir.dt.int32
U32 = mybir.dt.uint32
AF = mybir.ActivationFunctionType
ALU = mybir.AluOpType
AX = mybir.AxisListType

D = 1024
TC = 256
IN_COLS = 3984
GDN_COLS = 2064
NORM_EPS = 1e-6
L2_EPS = 1e-6
GN_EPS = 64e-5


class Ctx:
    def __init__(self, nc):
        self.nc = nc
        self.es = ExitStack()
        self.eng = dict(pe=nc.tensor, act=nc.scalar, dve=nc.vector, pool=nc.gpsimd, sp=nc.sync)
        self.csem = {}
        self.cnt = {}
        for e in ("pe", "act", "dve", "pool"):
            self.csem[e] = self.es.enter_context(nc.semaphore("cs_" + e))
            self.cnt[e] = 0
        self.dsem = {}
        self.seen = {e: {} for e in self.eng}
        self.st = {}
        self.ninst = 0
        self._rec = None

    def _sem(self, sk):
        if sk[0] == "c":
            return self.csem[sk[1]]
        return self.dsem[sk[1]][0]

    def _deps(self, reads, writes, e=None):
        need = {}

        def add(m):
            if m is None:
                return
            sk, v = m
            if need.get(sk, 0) < v:
                need[sk] = v

        for r in reads:
            s = self.st.get(r)
            if s is not None:
                add(s[0])
                if isinstance(r, str) and r.startswith("ps") and r[2:].isdigit():
                    for sk, v in s[1].items():
                        if sk != ("c", e):
                            add((sk, v))
        for w in writes:
            s = self.st.get(w)
            if s is not None:
                add(s[0])
                for sk, v in s[1].items():
                    add((sk, v))
        return need

    def _wait(self, e, need):
        eng = self.eng[e]
        seen = self.seen[e]
        for sk, v in need.items():
            if e == "pe" and sk == ("c", "pe"):
                continue
            if sk[0] == "d":
                v = max(v, self.dsem[sk[1]][1])
            if seen.get(sk, 0) >= v:
                continue
            eng.wait_ge(self._sem(sk), v)
            seen[sk] = v

    def _mark(self, mark, reads, writes):
        for w in writes:
            self.st[w] = [mark, {}]
        for r in reads:
            s = self.st.get(r)
            if s is None:
                s = self.st[r] = [None, {}]
            sk, v = mark
            if s[1].get(sk, 0) < v:
                s[1][sk] = v

    def streams(self, n):
        lists = []
        for d in range(n):
            self._rec = []
            yield d
            lists.append(self._rec)
            self._rec = None
        idx = [0] * n
        left = sum(len(l) for l in lists)
        while left:
            for d in range(n):
                if idx[d] < len(lists[d]):
                    kind, args, kw = lists[d][idx[d]]
                    idx[d] += 1
                    left -= 1
                    getattr(self, kind)(*args, **kw)

    def op(self, e, fn, reads=(), writes=()):
        if self._rec is not None:
            self._rec.append(("op", (e, fn, tuple(reads), tuple(writes)), {}))
            return
        need = self._deps(reads, writes, e)
        self._wait(e, need)
        ins = fn(self.eng[e])
        ins.then_inc(self.csem[e], 1)
        self.cnt[e] += 1
        self.ninst += 1
        self._mark((("c", e), self.cnt[e]), reads, writes)

    def dma(self, out, in_, reads=(), writes=(), key=None, q="sp", **kw):
        if self._rec is not None:
            self._rec.append(("dma", (out, in_, tuple(reads), tuple(writes), key, q), kw))
            return
        if key not in self.dsem:
            self.dsem[key] = [self.es.enter_context(self.nc.semaphore("ds_%d" % len(self.dsem))), 0]
        need = self._deps(reads, writes)
        self._wait(q, need)
        d = self.dsem[key]
        self.eng[q].dma_start(out=out, in_=in_, **kw).then_inc(d[0], 16)
        d[1] += 16
        self.ninst += 1
        self._mark((("d", key), d[1]), reads, writes)

    def gather(self, out, in_, idx_ap, reads=(), writes=(), key=None):
        if key not in self.dsem:
            self.dsem[key] = [self.es.enter_context(self.nc.semaphore("ds_%d" % len(self.dsem))), 0]
        need = self._deps(reads, writes)
        self._wait("pool", need)
        d = self.dsem[key]
        self.nc.gpsimd.indirect_dma_start(
            out=out, out_offset=None, in_=in_,
            in_offset=bass.IndirectOffsetOnAxis(ap=idx_ap, axis=0)).then_inc(d[0], 16)
        d[1] += 16
        self.ninst += 1
        self._mark((("d", key), d[1]), reads, writes)

    def barrier(self):
        need = {("c", e): v for e, v in self.cnt.items() if v > 0}
        for k, d in self.dsem.items():
            if d[1] > 0:
                need[("d", k)] = d[1]
        for e in self.eng:
            self._wait(e, need)

    def finish(self, keys):
        need = self._deps(keys, ())
        self._wait("sp", need)


NM_MODE = ["f32"]
RW_NM = ["bf16"]


class _Stop(Exception):
    pass


def build(nc, n_rows=64, dbg=(), stop=None):
    K = Ctx(nc)
    try:
        return _build(nc, K, n_rows, dbg, stop)
    except _Stop:
        K.barrier()
        return None


def _build(nc, K, n_rows, dbg, stop):
    def stop_at(name):
        if stop == name:
            raise _Stop()

    TL = 64 * n_rows
    T = TC + TL
    NT = T // 128
    NTL = TL // 128
    es = K.es

    def din(name, shape, dt=F32):
        return nc.dram_tensor(name, list(shape), dt, kind="ExternalInput").ap()

    x_d = din("x", [TL, D])
    c_d = din("c", [1, D])
    ctx_d = din("ctx", [TC, D])
    cctx_d = din("c_ctx", [1, D])
    adaw_d = din("ada_w", [D, 6144])
    adab_d = din("ada_b", [48, 128])
    adabr_d = din("ada_b_row", [1, 6144])
    g1_d = din("norm1_g", [8, 128])
    win_d = din("w_in", [D, IN_COLS])
    convw_d = din("gdn_conv_w", [60, 128])
    alog_d = din("gdn_a_log", [1, 8])
    dtb_d = din("gdn_dt_bias", [1, 8])
    gnorm_d = din("gdn_norm_w", [1, 128])
    mu_d = din("rwkv_mu", [15, 128])
    w0_d = din("rwkv_w0", [8, 128])
    w2_d = din("rwkv_w2", [128, 512])
    a0_d = din("rwkv_a0", [8, 128])
    a2_d = din("rwkv_a2", [128, 512])
    g2w_d = din("rwkv_g2", [128, 512])
    kk_d = din("rwkv_k_k", [4, 128])
    ka_d = din("rwkv_k_a", [4, 128])
    rk_d = din("rwkv_r_k", [4, 128])
    gnw_d = din("rwkv_gn_w", [4, 128])
    gnb_d = din("rwkv_gn_b", [4, 128])
    wout_d = din("w_out", [D, D])
    g2_d = din("norm2_g", [8, 128])
    wq_d = din("peer_w_query", [D, 2048])
    sk_d = din("peer_sub_keys", [16, 128, 128])
    down_d = din("peer_down", [16384, D])
    up_d = din("peer_up", [16384, D])
    fng_d = din("final_norm_g", [1, D])
    g2row_d = din("norm2_g_row", [1, D])
    out_d = nc.dram_tensor("out", [TL, D], F32, kind="ExternalOutput").ap()

    dbg_out = {}

    def dbgt(name, shape, dt=F32):
        dbg_out[name] = nc.dram_tensor("dbg_" + name, list(shape), dt, kind="ExternalOutput").ap()
        return dbg_out[name]

    pT_d = nc.dram_tensor("pT_s", [32, 128, T], F32).ap()

    def sb(name, shape, dt=F32, stack=es):
        return stack.enter_context(nc.sbuf_tensor(name, list(shape), dt))

    ps = [es.enter_context(nc.psum_tensor("ps%d" % i, [128, 512], F32)) for i in range(8)]

    dI = sb("dI", [128, 128], I32)
    ident = sb("ident", [128, 128])
    identb = sb("identb", [128, 128], BF16)
    ones = sb("ones", [128, 128])
    onesb = sb("onesb", [128, 128], BF16)
    m_lt = sb("m_lt", [128, 128])
    m_le = sb("m_le", [128, 128])
    m_gt = sb("m_gt", [128, 128])
    m_ge = sb("m_ge", [128, 128])
    e0 = sb("e0", [2, 128])
    K.op("pool", lambda e: e.iota(dI[:], pattern=[[1, 128]], base=0, channel_multiplier=-1), writes=["dI"])
    for t, op_, nm in ((ident, ALU.is_equal, "ident"), (m_lt, ALU.is_gt, "m_lt"), (m_le, ALU.is_ge, "m_le"),
                       (m_gt, ALU.is_lt, "m_gt"), (m_ge, ALU.is_le, "m_ge")):
        K.op("dve", lambda e, t=t, op_=op_: e.tensor_scalar(t[:], dI[:], 0.0, None, op0=op_), reads=["dI"], writes=[nm])
    K.op("dve", lambda e: e.tensor_copy(identb[:], ident[:]), reads=["ident"], writes=["identb"])
    K.op("pool", lambda e: e.memset(ones[:], 1.0), writes=["ones"])
    K.op("pool", lambda e: e.memset(onesb[:], 1.0), writes=["onesb"])
    e0i = sb("e0i", [2, 128], I32)
    K.op("pool", lambda e: e.iota(e0i[:], pattern=[[0, 128]], base=1, channel_multiplier=-1), writes=["e0i"])
    K.op("dve", lambda e: e.tensor_copy(e0[:], e0i[:]), reads=["e0i"], writes=["e0"])

    PK1 = dict(ADAB=0, G1=48, G2=56, CW=64)
    PK2 = dict(MU=0, W0=15, A0=23, KK=31, KA=35, RK=39, GNW=43, GNB=47, GNORM=51)
    pk1s = sb("pk1s", [128, 128])
    pk2s = sb("pk2s", [128, 128])
    pk1 = sb("pk1", [128, 128])
    pk2 = sb("pk2", [128, 128])
    K.op("pool", lambda e: e.memset(pk1s[:], 0.0), writes=["pk1s"])
    K.op("pool", lambda e: e.memset(pk2s[:], 0.0), writes=["pk2s"])
    for src, off, n in ((adab_d, 0, 48), (g1_d, 48, 8), (g2_d, 56, 8), (convw_d, 64, 60)):
        K.dma(pk1s[off:off + n, :], src[:, :], writes=["pk1s"], key="pk1s")
    for src, off, n in ((mu_d, 0, 15), (w0_d, 15, 8), (a0_d, 23, 8), (kk_d, 31, 4), (ka_d, 35, 4),
                        (rk_d, 39, 4), (gnw_d, 43, 4), (gnb_d, 47, 4), (gnorm_d, 51, 1)):
        K.dma(pk2s[off:off + n, :], src[:, :], writes=["pk2s"], key="pk2s")
    K.op("pe", lambda e: e.transpose(ps[0][:, 0:128], pk1s[:], ident[:]), reads=["pk1s", "ident"], writes=["ps0"])
    K.op("pe", lambda e: e.transpose(ps[0][:, 128:256], pk2s[:], ident[:]), reads=["pk2s", "ident"], writes=["ps0"])
    K.op("dve", lambda e: e.tensor_copy(pk1[:], ps[0][:, 0:128]), reads=["ps0"], writes=["pk1"])
    K.op("dve", lambda e: e.tensor_copy(pk2[:], ps[0][:, 128:256]), reads=["ps0"], writes=["pk2"])

    modT = sb("modT", [128, 48, 2])
    gt_d = nc.dram_tensor("gt_s", [128, 4 * D], F32).ap()
    A1 = sb("A1", [128, 8]); B1 = sb("B1", [128, 8])
    A1c = sb("A1c", [128, 8]); B1c = sb("B1c", [128, 8])
    A2 = sb("A2", [128, 8]); B2 = sb("B2", [128, 8])
    with ExitStack() as pa:
        cc = sb("cc", [2, D], stack=pa)
        scT = sb("scT", [128, 8, 2], stack=pa)
        aw = [sb("aw%d" % i, [128, 8, 1024], stack=pa) for i in range(2)]
        gtrow = sb("gtrow", [2, 4, D], stack=pa)
        gtB = sb("gtB", [128, 4, D], stack=pa)
        K.dma(cc[0:1, :], c_d[:, :], writes=["cc"], key="cc")
        K.dma(cc[1:2, :], cctx_d[:, :], writes=["cc"], key="cc")
        K.op("act", lambda e: e.activation(cc[:], cc[:], AF.Silu), reads=["cc"], writes=["cc"])
        for k in range(8):
            K.op("pe", lambda e, k=k: e.transpose(ps[1][:, 2 * k:2 * k + 2], cc[0:2, k * 128:(k + 1) * 128], ident[0:2, 0:2]),
                 reads=["cc", "ident"], writes=["ps1"])
        K.op("dve", lambda e: e.tensor_copy(scT[:].rearrange("p k c -> p (k c)"), ps[1][:, 0:16]), reads=["ps1"], writes=["scT"])
        K.op("pool", lambda e: e.memset(gtrow[:], 0.0), writes=["gtrow"])
        for q_ in range(4):
            K.dma(gtrow[0:1, q_, :], adabr_d[:, 2048 + q_ * 1024:3072 + q_ * 1024], writes=["gtrow"], key="gtrow")
        for g in range(6):
            a = aw[g % 2]
            an = "aw%d" % (g % 2)
            K.dma(a[:], adaw_d[:, g * 1024:(g + 1) * 1024].rearrange("(k p) c -> p k c", p=128), writes=[an], key=an)
            for j in range(8):
                col = (g * 8 + j) * 2
                for k in range(8):
                    K.op("pe", lambda e, a=a, j=j, k=k, col=col: e.matmul(
                        ps[2][:, col:col + 2], lhsT=a[:, k, j * 128:(j + 1) * 128], rhs=scT[:, k, :],
                        start=(k == 0), stop=(k == 7)), reads=[an, "scT"], writes=["ps2"])
            if g >= 2:
                q = g - 2
                for half in range(2):
                    for k in range(8):
                        K.op("pe", lambda e, a=a, k=k, half=half: e.matmul(
                            ps[3][0:2, :], lhsT=scT[:, k, :], rhs=a[:, k, half * 512:(half + 1) * 512],
                            start=(k == 0), stop=(k == 7)), reads=[an, "scT"], writes=["ps3"])
                    K.op("dve", lambda e, q=q, half=half: e.tensor_tensor(
                        gtrow[:, q, half * 512:(half + 1) * 512], ps[3][0:2, :], gtrow[:, q, half * 512:(half + 1) * 512], op=ALU.add),
                        reads=["ps3", "gtrow"], writes=["gtrow"])
                    K.op("pe", lambda e, q=q, half=half: e.matmul(
                        ps[4][:, :], lhsT=e0[:, :], rhs=gtrow[:, q, half * 512:(half + 1) * 512], start=True, stop=True),
                        reads=["e0", "gtrow"], writes=["ps4"])
                    K.op("act", lambda e, q=q, half=half: e.copy(gtB[:, q, half * 512:(half + 1) * 512], ps[4][:, :]),
                         reads=["ps4"], writes=["gtB"])
        K.op("dve", lambda e: e.tensor_tensor(
            modT[:], ps[2][:, 0:96].rearrange("p (j c) -> p j c", c=2),
            pk1[:, 0:48].unsqueeze(2).to_broadcast([128, 48, 2]), op=ALU.add), reads=["ps2", "pk1"], writes=["modT"])
        for (A, B, gname, sc0, sh0, col, nm) in ((A1, B1, "G1", 8, 0, 0, "1"), (A1c, B1c, "G1", 8, 0, 1, "1c"),
                                                 (A2, B2, "G2", 32, 24, 0, "2")):
            g0 = PK1[gname]
            K.op("dve", lambda e, A=A, g0=g0, sc0=sc0, col=col: e.scalar_tensor_tensor(
                A[:], modT[:, sc0:sc0 + 8, col], 1.0, pk1[:, g0:g0 + 8], op0=ALU.add, op1=ALU.mult),
                reads=["modT", "pk1"], writes=["A" + nm])
            K.op("dve", lambda e, B=B, sh0=sh0, col=col: e.tensor_copy(B[:], modT[:, sh0:sh0 + 8, col]),
                 reads=["modT"], writes=["B" + nm])

        K.dma(gt_d[:, :], gtB[:].rearrange("p q d -> p (q d)"), reads=["gtB"], writes=["gt_d"], key="st_gt")
        K.barrier()
    if "mod" in dbg:
        d_ = dbgt("mod", [128, 96])
        K.dma(d_[:, :], modT[:].rearrange("p j c -> p (j c)"), reads=["modT"], writes=["dbgmod"], key="dbg")

    def norm_tile(xt, xkey, A, B, hT, hkey, col0, pp, stage):
        junk, ss, rs, xs = stage
        K.op("act", lambda e: e.activation(junk[:], xt[:], AF.Square), reads=[xkey], writes=["n_junk"])
        K.op("dve", lambda e: e.reduce_sum(ss[:, 0:1], junk[:], axis=AX.X), reads=["n_junk"], writes=["n_ss"])
        K.op("act", lambda e: e.activation(rs[:, 0:1], ss[:, 0:1], AF.Sqrt, bias=eps_t[:, 0:1], scale=1.0 / D),
             reads=["n_ss", "eps"], writes=["n_rs"])
        K.op("dve", lambda e: e.reciprocal(rs[:, 1:2], rs[:, 0:1]), reads=["n_rs"], writes=["n_rs2"])
        K.op("dve", lambda e: e.tensor_scalar(xs[:], xt[:], rs[:, 1:2], None, op0=ALU.mult),
             reads=[xkey, "n_rs2"], writes=["n_xs"])
        for half in range(2):
            pt = ps[pp + half]
            pk = "ps%d" % (pp + half)
            for kk in range(4):
                k = half * 4 + kk
                K.op("pe", lambda e, k=k, kk=kk, pt=pt: e.transpose(pt[:, kk * 128:(kk + 1) * 128], xs[:, k * 128:(k + 1) * 128], ident[:]),
                     reads=["n_xs", "ident"], writes=[pk])
            for kk in range(4):
                k = half * 4 + kk
                eng = "act" if kk % 2 == 0 else "dve"
                if eng == "act":
                    K.op("act", lambda e, k=k, kk=kk, pt=pt: e.activation(
                        hT[:, k, col0:col0 + 128], pt[:, kk * 128:(kk + 1) * 128], AF.Identity,
                        bias=B[:, k:k + 1], scale=A[:, k:k + 1]), reads=[pk, "mods"], writes=[hkey])
                else:
                    K.op("dve", lambda e, k=k, kk=kk, pt=pt: e.tensor_scalar(
                        hT[:, k, col0:col0 + 128], pt[:, kk * 128:(kk + 1) * 128], A[:, k:k + 1], B[:, k:k + 1],
                        op0=ALU.mult, op1=ALU.add), reads=[pk, "mods"], writes=[hkey])

    eps_t = sb("eps_t", [128, 4])
    K.op("pool", lambda e: e.memset(eps_t[:, 0:1], NORM_EPS), writes=["eps"])
    K.op("pool", lambda e: e.memset(eps_t[:, 1:2], L2_EPS), writes=["eps"])
    K.op("pool", lambda e: e.memset(eps_t[:, 2:3], GN_EPS), writes=["eps"])
    K.op("pool", lambda e: e.memset(eps_t[:, 3:4], 1.0), writes=["eps"])
    K.op("dve", lambda e: e.tensor_copy(A1[:, 0:1], A1[:, 0:1]), reads=["A1", "B1", "A1c", "B1c", "A2", "B2"], writes=["mods"])

    with ExitStack() as pb:
        hT = sb("hT", [128, 8, T], BF16, stack=pb)
        junk = sb("junk", [128, D], stack=pb)
        ss = sb("ss", [128, 1], stack=pb)
        rs = sb("rs", [128, 2], stack=pb)
        xs = sb("xs", [128, D], stack=pb)
        xts = [sb("xt%d" % i, [128, D], stack=pb) for i in range(3)]
        for i in range(NT):
            xt = xts[i % 3]
            xk = "xt%d" % (i % 3)
            src = ctx_d[i * 128:(i + 1) * 128, :] if i < 2 else x_d[(i - 2) * 128:(i - 1) * 128, :]
            K.dma(xt[:], src, writes=[xk], key=xk)
            A, B = (A1c, B1c) if i < 2 else (A1, B1)
            norm_tile(xt, xk, A, B, hT, "hT", i * 128, 0, (junk, ss, rs, xs))
        if "hT" in dbg:
            d_ = dbgt("ss", [128, 1])
            K.dma(d_[:, :], ss[:, :], reads=["n_ss"], writes=["dbgss"], key="dbg")
            d_ = dbgt("rs", [128, 2])
            K.dma(d_[:, :], rs[:, :], reads=["n_rs", "n_rs2"], writes=["dbgrs"], key="dbg")
            d_ = dbgt("hT", [128, 8 * T], BF16)
            K.dma(d_[:, :], hT[:].rearrange("p k t -> p (k t)"), reads=["hT"], writes=["dbghT"], key="dbg")
        wst = [sb("wst%d" % i, [128, 8, 128], stack=pb) for i in range(2)]
        wbf = [sb("wbf%d" % i, [128, 8, 128], BF16, stack=pb) for i in range(2)]
        pcs = [sb("pc%d" % i, [128, T], stack=pb) for i in range(2)]
        nblk = (T + 511) // 512
        chunks = [(j, j * 128, 128) for j in range(16)] + [(16, 2048, 16)] + \
                 [(17 + j, GDN_COLS + j * 128, 128) for j in range(15)]
        ev = 0
        for ci, (dst, c0, ncol) in enumerate(chunks):
            w_s = wst[ci % 2]; w_b = wbf[ci % 2]; pc = pcs[ci % 2]
            ws_k = "wst%d" % (ci % 2); wb_k = "wbf%d" % (ci % 2); pc_k = "pc%d" % (ci % 2)
            K.dma(w_s[:, :, 0:ncol], win_d[:, c0:c0 + ncol].rearrange("(k p) c -> p k c", p=128), writes=[ws_k], key=ws_k)
            K.op("pool", lambda e, w_s=w_s, w_b=w_b, ncol=ncol: e.tensor_copy(w_b[:, :, 0:ncol], w_s[:, :, 0:ncol]),
                 reads=[ws_k], writes=[wb_k])
            for n in range(nblk):
                t0 = n * 512
                tn = min(512, T - t0)
                pt = ps[2 + (n % 4)]
                pk = "ps%d" % (2 + (n % 4))
                for k in range(8):
                    K.op("pe", lambda e, k=k, pt=pt, w_b=w_b, ncol=ncol, t0=t0, tn=tn: e.matmul(
                        pt[0:ncol, 0:tn], lhsT=w_b[:, k, 0:ncol], rhs=hT[:, k, t0:t0 + tn], start=(k == 0), stop=(k == 7)),
                        reads=[wb_k, "hT"], writes=[pk])
                eng = "act" if ev % 2 == 0 else "dve"
                ev += 1
                if eng == "act":
                    K.op("act", lambda e, pt=pt, pc=pc, ncol=ncol, t0=t0, tn=tn: e.copy(pc[0:ncol, t0:t0 + tn], pt[0:ncol, 0:tn]),
                         reads=[pk], writes=[pc_k])
                else:
                    K.op("dve", lambda e, pt=pt, pc=pc, ncol=ncol, t0=t0, tn=tn: e.tensor_copy(pc[0:ncol, t0:t0 + tn], pt[0:ncol, 0:tn]),
                         reads=[pk], writes=[pc_k])
            K.dma(pT_d[dst, 0:ncol, :], pc[0:ncol, :], reads=[pc_k], writes=[("pT", dst)], key="st_" + pc_k)

    K.barrier()
    if "pT" in dbg:
        d_ = dbgt("pT", [32, 128, T])
        K.dma(d_[:, :, :], pT_d[:, :, :], reads=[("pT", i) for i in range(32)], writes=["dbgpT"], key="dbg")


    mT_d = nc.dram_tensor("mT_s", [8, 128, TL], BF16).ap()
    psb = [p.bitcast(BF16) for p in ps]
    negm_le = sb("negm_le", [128, 128])
    negm_ge = sb("negm_ge", [128, 128])
    K.op("dve", lambda e: e.tensor_scalar(negm_le[:], m_le[:], 30000.0, -30000.0, op0=ALU.mult, op1=ALU.add), reads=["m_le"], writes=["negm_le"])
    K.op("dve", lambda e: e.tensor_scalar(negm_ge[:], m_ge[:], 30000.0, -30000.0, op0=ALU.mult, op1=ALU.add), reads=["m_ge"], writes=["negm_ge"])
    fwd_order = list(range(NT))
    bwd_order = [1, 0] + list(range(NT - 1, 1, -1))

    NMODE = NM_MODE[0]
    NDT = BF16 if NMODE == "bf16" else F32

    def mmv(ap):
        return ap

    identn = identb if NMODE == "bf16" else ident

    nmode = {'cur': NMODE}

    def neumann(Y, Yt, tag, pbank, pb=None):
        PR = nm_tiles[tag]["PR"]; Pt = nm_tiles[tag]["Pt"]
        kPR = [tag + "PR0", tag + "PR1"]; kPt = [tag + "Pt0", tag + "Pt1"]
        pa_ = ps[pbank]
        ka = "ps%d" % pbank
        if pb is None:
            pb_, kb = ps[pbank + 1], "ps%d" % (pbank + 1)
        else:
            pb_, kb = ps[pb[0]][:, pb[1]:pb[1] + 128], "ps%d" % pb[0]
        K.op("pe", lambda e: e.matmul(pa_[:, 0:128], lhsT=mmv(Yt[:]), rhs=mmv(Y[:]), start=True, stop=True), reads=[tag + "Y", tag + "Yt"], writes=[ka])
        K.op("pe", lambda e: e.matmul(pb_[:, 0:128], lhsT=mmv(Y[:]), rhs=mmv(Yt[:]), start=True, stop=True), reads=[tag + "Y", tag + "Yt"], writes=[kb])
        K.op("act", lambda e: e.copy(PR[0][:, 0:128], pa_[:, 0:128]), reads=[ka], writes=[kPR[0]])
        K.op("dve", lambda e: e.tensor_tensor(PR[0][:, 128:256], Y[:], (identb if nmode['cur'] == 'bf16' else ident)[:], op=ALU.add), reads=[tag + "Y", "identb", "ident"], writes=[kPR[0]])
        K.op("dve", lambda e: e.tensor_copy(Pt[0][:], pb_[:, 0:128]), reads=[kb], writes=[kPt[0]])
        cur = 0
        for l in range(1, 7):
            nxt = 1 - cur
            last = (l == 6)
            n0 = 128 if last else 0
            K.op("pe", lambda e, cur=cur, n0=n0: e.matmul(pa_[:, n0:256], lhsT=mmv(Pt[cur][:]), rhs=mmv(PR[cur][:, n0:256]), start=True, stop=False),
                 reads=[kPt[cur], kPR[cur]], writes=[ka])
            K.op("pe", lambda e, cur=cur: e.matmul(pa_[:, 128:256], lhsT=mmv((identb if nmode['cur'] == 'bf16' else ident)[:]), rhs=mmv(PR[cur][:, 128:256]), start=False, stop=True),
                 reads=["identb", "ident", kPR[cur]], writes=[ka])
            if not last:
                K.op("pe", lambda e, cur=cur: e.matmul(pb_[:, 0:128], lhsT=mmv(PR[cur][:, 0:128]), rhs=mmv(Pt[cur][:]), start=True, stop=True),
                     reads=[kPt[cur], kPR[cur]], writes=[kb])
            K.op("act", lambda e, nxt=nxt, n0=n0: e.copy(PR[nxt][:, n0:256], pa_[:, n0:256]), reads=[ka], writes=[kPR[nxt]])
            if not last:
                K.op("dve", lambda e, nxt=nxt: e.tensor_copy(Pt[nxt][:], pb_[:, 0:128]), reads=[kb], writes=[kPt[nxt]])
            cur = nxt
        if nmode['cur'] == 'bf16':
            return PR[cur][:, 128:256], kPR[cur]
        fin = nm_tiles[tag]["fin"]
        K.op("act", lambda e, cur=cur: e.copy(fin[:], PR[cur][:, 128:256]), reads=[kPR[cur]], writes=[tag + "fin"])
        return fin[:], tag + "fin"

    nm_tiles = {}
    with ExitStack() as pg:
        for tag in ("n0", "n1", "n2", "n3"):
            nm_tiles[tag] = dict(PR=[sb(tag + "PR%d" % i, [128, 256], NDT, stack=pg) for i in range(2)],
                                 Pt=[sb(tag + "Pt%d" % i, [128, 128], NDT, stack=pg) for i in range(2)],
                                 fin=sb(tag + "fin", [128, 128], BF16, stack=pg))
        ab = sb("ab", [128, NT, 16], stack=pg)
        dtb_b = sb("dtb_b", [128, 8], stack=pg)
        nA_b = sb("nA_b", [128, 8], stack=pg)
        gg = sb("gg", [128, NT, 8], stack=pg)
        Gc = sb("Gc", [128, NT, 8], stack=pg)
        nbeta = sb("nbeta", [128, NT, 8], stack=pg)
        beta = sb("beta", [128, NT, 8], stack=pg)
        negeG = sb("negeG", [128, NT, 8], stack=pg)
        eG = sb("eG", [128, NT, 8], stack=pg)
        eTG = sb("eTG", [128, NT, 8], stack=pg)
        eTot = sb("eTot", [128, NT, 8], stack=pg)
        pg_ab = ExitStack()
        abT = sb("abT", [16, T], stack=pg_ab)
        K.dma(abT[:, :], pT_d[16, 0:16, :], reads=[("pT", 16)], writes=["abT"], key="abT")
        K.dma(dtb_b[:, :], dtb_d.partition_broadcast(128), writes=["dtb_b"], key="dtb_b")
        K.dma(nA_b[:, :], alog_d.partition_broadcast(128), writes=["nA_b"], key="nA_b")
        for i in range(NT):
            K.op("pe", lambda e, i=i: e.transpose(ps[i // 32][:, (i % 32) * 16:(i % 32) * 16 + 16], abT[0:16, i * 128:(i + 1) * 128], ident[0:16, 0:16]),
                 reads=["abT", "ident"], writes=["ps%d" % (i // 32)])
        for b0 in range(0, NT, 32):
            nb = min(32, NT - b0)
            K.op("dve", lambda e, b0=b0, nb=nb: e.tensor_copy(ab[:, b0:b0 + nb, :].rearrange("p n c -> p (n c)"), ps[b0 // 32][:, 0:nb * 16]),
                 reads=["ps%d" % (b0 // 32)], writes=["ab"])
        K.op("act", lambda e: e.activation(nA_b[:], nA_b[:], AF.Exp), reads=["nA_b"], writes=["nA_b"])
        K.op("dve", lambda e: e.tensor_scalar(nA_b[:], nA_b[:], -1.0, None, op0=ALU.mult), reads=["nA_b"], writes=["nA_b"])
        K.op("dve", lambda e: e.tensor_tensor(gg[:], ab[:, :, 0:8], dtb_b[:].unsqueeze(1).to_broadcast([128, NT, 8]), op=ALU.add),
             reads=["ab", "dtb_b"], writes=["gg"])
        K.op("act", lambda e: e.activation(gg[:], gg[:], AF.Exp), reads=["gg"], writes=["gg"])
        K.op("act", lambda e: e.activation(gg[:], gg[:], AF.Ln, bias=eps_t[:, 3:4]), reads=["gg", "eps"], writes=["gg"])
        K.op("dve", lambda e: e.tensor_tensor(gg[:], gg[:], nA_b[:].unsqueeze(1).to_broadcast([128, NT, 8]), op=ALU.mult),
             reads=["gg", "nA_b"], writes=["gg"])
        K.op("act", lambda e: e.activation(beta[:], ab[:, :, 8:16], AF.Sigmoid), reads=["ab"], writes=["beta"])
        K.op("dve", lambda e: e.tensor_scalar(nbeta[:], beta[:], -1.0, None, op0=ALU.mult), reads=["beta"], writes=["nbeta"])
        ggf = gg[:].rearrange("p n c -> p (n c)")
        K.op("pe", lambda e: e.matmul(ps[2][:, 0:NT * 8], lhsT=m_le[:], rhs=ggf, start=True, stop=True), reads=["m_le", "gg"], writes=["ps2"])
        K.op("pe", lambda e: e.matmul(ps[3][:, 0:NT * 8], lhsT=m_ge[:], rhs=ggf, start=True, stop=True), reads=["m_ge", "gg"], writes=["ps3"])
        K.op("pe", lambda e: e.matmul(ps[4][:, 0:NT * 8], lhsT=ones[:], rhs=ggf, start=True, stop=True), reads=["ones", "gg"], writes=["ps4"])
        K.op("dve", lambda e: e.tensor_copy(Gc[:, :, 0:4], ps[2][:, 0:NT * 8].rearrange("p (n c) -> p n c", c=8)[:, :, 0:4]), reads=["ps2"], writes=["Gc"])
        K.op("dve", lambda e: e.tensor_copy(Gc[:, :, 4:8], ps[3][:, 0:NT * 8].rearrange("p (n c) -> p n c", c=8)[:, :, 4:8]), reads=["ps3"], writes=["Gc"])
        K.op("act", lambda e: e.activation(eG[:], Gc[:], AF.Exp), reads=["Gc"], writes=["eG"])
        K.op("dve", lambda e: e.tensor_scalar(negeG[:], eG[:], -1.0, None, op0=ALU.mult), reads=["eG"], writes=["negeG"])
        K.op("act", lambda e: e.activation(eTot[:].rearrange("p n c -> p (n c)"), ps[4][:, 0:NT * 8], AF.Exp), reads=["ps4"], writes=["eTot"])
        K.op("dve", lambda e: e.tensor_tensor(eTG[:].rearrange("p n c -> p (n c)"), ps[4][:, 0:NT * 8], Gc[:].rearrange("p n c -> p (n c)"), op=ALU.subtract),
             reads=["ps4", "Gc"], writes=["eTG"])
        K.op("act", lambda e: e.activation(eTG[:], eTG[:], AF.Exp), reads=["eTG"], writes=["eTG"])

        K.barrier()
        pg_ab.close()
        stop_at("gdn_scal")
        qT = sb("qT", [128, T], BF16, stack=pg)
        kT = sb("kT", [128, T], BF16, stack=pg)
        vT = sb("vT", [128, T], BF16, stack=pg)
        zs = sb("zs", [128, T], BF16, stack=pg)
        vtok = sb("vtok", [128, NT, 128], BF16, stack=pg)
        ktok = sb("ktok", [128, NT, 128], BF16, stack=pg)
        obuf = [sb("obuf%d" % d_, [128, NT, 128], BF16, stack=pg) for d_ in range(2)]
        AinvAll = [sb("AinvAll%d" % d_, [128, NT, 128], BF16, stack=pg) for d_ in range(2)]
        MqkAll = [sb("MqkAll%d" % d_, [128, NT, 128], BF16, stack=pg) for d_ in range(2)]
        pin = sb("pin", [128, T], stack=pg)
        cv = sb("cv", [128, T], stack=pg)
        sq = pin
        rn = sb("rn", [128, 512], stack=pg)
        S = [sb("S%d" % d_, [128, 128], stack=pg) for d_ in range(2)]
        Sb = [sb("Sb%d" % d_, [128, 128], BF16, stack=pg) for d_ in range(2)]
        mst = sb("mst", [128, TL], BF16, stack=pg)
        dgl = [sb("dgl%d" % d_, [128, 128], stack=pg) for d_ in range(4)]
        arg = dgl
        DTi = [sb("DTi%d" % d_, [128, 128], stack=pg) for d_ in range(4)]
        DTs = dgl
        Yb = [sb("Yb%d" % d_, [128, 128], NDT, stack=pg) for d_ in range(4)]
        Ytb = [sb("Ytb%d" % d_, [128, 128], NDT, stack=pg) for d_ in range(4)]
        Rb = [sb("Rb%d" % d_, [128, 128], BF16, stack=pg) for d_ in range(2)]
        Xb = [sb("Xb%d" % d_, [128, 128], BF16, stack=pg) for d_ in range(2)]
        Xs = [sb("Xs%d" % d_, [128, 128], BF16, stack=pg) for d_ in range(2)]
        QSe = [sb("QSe%d" % d_, [128, 128], stack=pg) for d_ in range(2)]
        on_ = sb("on_", [128, 128], stack=pg)
        oj = sb("oj", [128, 128], stack=pg)
        onn = sb("onn", [128, 128], stack=pg)
        oss = sb("oss", [128, 4], stack=pg)

        def conv_silu(cidx, dst, dkey, final_silu_to):
            cw0 = PK1["CW"]
            K.op("dve", lambda e: e.tensor_scalar(cv[:], pin[:], pk1[:, cw0 + 2 * 12 + cidx:cw0 + 2 * 12 + cidx + 1], None, op0=ALU.mult),
                 reads=["pin", "pk1"], writes=["cv"])
            for j in (0, 1, 3, 4):
                sh = j - 2
                wcol = pk1[:, cw0 + j * 12 + cidx:cw0 + j * 12 + cidx + 1]
                for (s0, s1) in ((0, TC), (TC, T)):
                    lo = max(s0, s0 - sh); hi = min(s1, s1 - sh)
                    K.op("dve", lambda e, lo=lo, hi=hi, sh=sh, wcol=wcol: e.scalar_tensor_tensor(
                        cv[:, lo:hi], pin[:, lo + sh:hi + sh], wcol, cv[:, lo:hi], op0=ALU.mult, op1=ALU.add),
                        reads=["pin", "pk1", "cv"], writes=["cv"])
            K.op("act", lambda e: e.activation(final_silu_to[:], cv[:], AF.Silu), reads=["cv"], writes=[dkey])

        def l2n(src, skey, dst, dkey, scale):
            K.op("pool", lambda e: e.tensor_tensor(sq[:], src[:], src[:], op=ALU.mult), reads=[skey], writes=["pin"])
            for n in range((T + 511) // 512):
                t0 = n * 512; tn = min(512, T - t0)
                pt = ps[5 + (n % 2)]; pk = "ps%d" % (5 + (n % 2))
                K.op("pe", lambda e, pt=pt, t0=t0, tn=tn: e.matmul(pt[:, 0:tn], lhsT=ones[:], rhs=sq[:, t0:t0 + tn], start=True, stop=True),
                     reads=["ones", "pin"], writes=[pk])
                K.op("act", lambda e, pt=pt, tn=tn: e.activation(rn[:, 0:tn], pt[:, 0:tn], AF.Sqrt, bias=eps_t[:, 1:2]), reads=[pk, "eps"], writes=["rn"])
                K.op("dve", lambda e, tn=tn: e.reciprocal(rn[:, 0:tn], rn[:, 0:tn]), reads=["rn"], writes=["rn"])
                K.op("dve", lambda e, t0=t0, tn=tn: e.scalar_tensor_tensor(dst[:, t0:t0 + tn], src[:, t0:t0 + tn], scale, rn[:, 0:tn], op0=ALU.mult, op1=ALU.mult),
                     reads=[skey, "rn"], writes=[dkey])

        def to_tok(src, skey, dst, dkey):
            for i in range(NT):
                pt = psb[5 + (i % 2)]; pk = "ps%d" % (5 + (i % 2))
                K.op("pe", lambda e, i=i, pt=pt: e.transpose(pt[:, 0:128], src[:, i * 128:(i + 1) * 128], identb[:]), reads=[skey, "identb"], writes=[pk])
                eng = "act" if i % 2 == 0 else "dve"
                if eng == "act":
                    K.op("act", lambda e, i=i, pt=pt: e.copy(dst[:, i, :], pt[:, 0:128]), reads=[pk], writes=[dkey])
                else:
                    K.op("dve", lambda e, i=i, pt=pt: e.tensor_copy(dst[:, i, :], pt[:, 0:128]), reads=[pk], writes=[dkey])

        for h in range(4):
            K.dma(pin[:, :], pT_d[h, :, :], reads=[("pT", h)], writes=["pin"], key="pin")
            conv_silu(h, cv, "cv", cv)
            l2n(cv, "cv", qT, "qT", float(128 ** -0.5))
            K.dma(pin[:, :], pT_d[4 + h, :, :], reads=[("pT", 4 + h)], writes=["pin"], key="pin")
            conv_silu(4 + h, cv, "cv", cv)
            l2n(cv, "cv", kT, "kT", 1.0)
            to_tok(kT, "kT", ktok, "ktok")
            K.dma(pin[:, :], pT_d[8 + h, :, :], reads=[("pT", 8 + h)], writes=["pin"], key="pin")
            conv_silu(8 + h, vT, "vT", vT)
            to_tok(vT, "vT", vtok, "vtok")
            K.dma(pin[:, :], pT_d[12 + h, :, :], reads=[("pT", 12 + h)], writes=["pin"], key="pin")
            K.op("act", lambda e: e.activation(zs[:], pin[:], AF.Silu), reads=["pin"], writes=["zs"])
            stop_at("gdn_prep")
            for d_ in range(2):
                K.op("pool", lambda e, d_=d_: e.memset(S[d_][:], 0.0), writes=["S%d" % d_])
                K.op("pool", lambda e, d_=d_: e.memset(Sb[d_][:], 0.0), writes=["Sb%d" % d_])
            def g_pre(step, d_, sl_):
                i = fwd_order[step] if d_ == 0 else bwd_order[step]
                par = step % 2
                r = d_ * 4 + h
                tag = "n%d" % sl_
                sl = slice(i * 128, (i + 1) * 128)
                want_o = i >= 2
                pA = ps[2 * sl_]; kA = "ps%d" % (2 * sl_)
                dk_ = "%d" % sl_
                K.op("dve", lambda e: e.tensor_scalar(dgl[sl_][:], ident[:], Gc[:, i, r:r + 1], None, op0=ALU.mult),
                     reads=["ident", "Gc"], writes=["dgl" + dk_])
                K.op("pe", lambda e: e.matmul(pA[:, 256:384], lhsT=ones[:], rhs=dgl[sl_][:], start=True, stop=True),
                     reads=["ones", "dgl" + dk_], writes=[kA])
                negm = negm_le if d_ == 0 else negm_ge
                mstrict = m_lt if d_ == 0 else m_gt
                K.op("dve", lambda e: e.scalar_tensor_tensor(
                    arg[sl_][:], pA[:, 256:384], Gc[:, i, r:r + 1], negm[:], op0=ALU.subtract, op1=ALU.add),
                    reads=[kA, "Gc", "negm_le", "negm_ge"], writes=["dgl" + dk_])
                K.op("act", lambda e: e.activation(DTi[sl_][:], arg[sl_][:], AF.Exp), reads=["dgl" + dk_], writes=["DTi" + dk_])
                K.op("pool", lambda e: e.tensor_tensor(DTs[sl_][:], DTi[sl_][:], mstrict[:], op=ALU.mult),
                     reads=["DTi" + dk_, "m_lt", "m_gt"], writes=["dgl" + dk_])
                K.op("pe", lambda e: e.matmul(pA[:, 0:128], lhsT=kT[:, sl], rhs=kT[:, sl], start=True, stop=True),
                     reads=["kT"], writes=[kA])
                K.op("dve", lambda e: e.scalar_tensor_tensor(
                    Yb[sl_][:], pA[:, 0:128], nbeta[:, i, r:r + 1], DTs[sl_][:], op0=ALU.mult, op1=ALU.mult),
                    reads=[kA, "nbeta", "dgl" + dk_], writes=[tag + "Y"])
                if want_o:
                    K.op("pe", lambda e: e.matmul(pA[:, 128:256], lhsT=kT[:, sl], rhs=qT[:, sl], start=True, stop=True),
                         reads=["kT", "qT"], writes=[kA])
                    K.op("dve", lambda e: e.tensor_tensor(MqkAll[d_][:, step, :], pA[:, 128:256], DTi[sl_][:], op=ALU.mult),
                         reads=[kA, "DTi" + dk_], writes=[("Mqk", d_, step)])
                pB = (psb if NMODE == "bf16" else ps)[2 * sl_ + 1]; kB = "ps%d" % (2 * sl_ + 1)
                K.op("pe", lambda e: e.transpose(pB[:, 0:128], Yb[sl_][:], identn[:]), reads=[tag + "Y", "identb", "ident"], writes=[kB])
                K.op("act", lambda e: e.copy(Ytb[sl_][:], pB[:, 0:128]), reads=[kB], writes=[tag + "Yt"])
                AinvT, kAinv = neumann(Yb[sl_], Ytb[sl_], tag, 2 * sl_ + 1, pb=(2 * sl_, 384))
                K.op("pool", lambda e: e.tensor_copy(AinvAll[d_][:, step, :], AinvT), reads=[kAinv], writes=[("gAinv", d_, step)])

            def g_chain(step, d_):
                i = fwd_order[step] if d_ == 0 else bwd_order[step]
                par = step % 2
                r = d_ * 4 + h
                sl = slice(i * 128, (i + 1) * 128)
                want_o = i >= 2
                pC = ps[6 + d_]; kC = "ps%d" % (6 + d_)
                dk_ = "%d" % d_
                kAinv = ("gAinv", d_, step)
                kMqk = ("Mqk", d_, step)
                K.op("pe", lambda e: e.matmul(pC[:, 0:128], lhsT=kT[:, sl], rhs=Sb[d_][:], start=True, stop=True),
                     reads=["kT", "Sb" + dk_], writes=[kC])
                if want_o:
                    K.op("pe", lambda e: e.matmul(pC[:, 128:256], lhsT=qT[:, sl], rhs=Sb[d_][:], start=True, stop=True),
                         reads=["qT", "Sb" + dk_], writes=[kC])
                K.op("dve", lambda e: e.scalar_tensor_tensor(
                    Rb[d_][:], pC[:, 0:128], negeG[:, i, r:r + 1], vtok[:, i, :], op0=ALU.mult, op1=ALU.add),
                    reads=[kC, "negeG", "vtok"], writes=["Rb" + dk_])
                if want_o:
                    K.op("act", lambda e: e.activation(QSe[d_][:], pC[:, 128:256], AF.Identity, scale=eG[:, i, r:r + 1]),
                         reads=[kC, "eG"], writes=["QSe" + dk_])
                K.op("pe", lambda e: e.matmul(pC[:, 0:128], lhsT=AinvAll[d_][:, step, :], rhs=Rb[d_][:], start=True, stop=True),
                     reads=[kAinv, "Rb" + dk_], writes=[kC])
                K.op("dve", lambda e: e.tensor_scalar(Xb[d_][:], pC[:, 0:128], beta[:, i, r:r + 1], None, op0=ALU.mult),
                     reads=[kC, "beta"], writes=["Xb" + dk_])
                K.op("pool", lambda e: e.tensor_scalar(Xs[d_][:], Xb[d_][:], eTG[:, i, r:r + 1], None, op0=ALU.mult),
                     reads=["Xb" + dk_, "eTG"], writes=["Xs" + dk_])
                if want_o:
                    K.op("pe", lambda e: e.matmul(pC[:, 384:512], lhsT=MqkAll[d_][:, step, :], rhs=Xb[d_][:], start=True, stop=True),
                         reads=[kMqk, "Xb" + dk_], writes=[kC])
                    K.op("dve", lambda e: e.tensor_tensor(obuf[d_][:, i, :], pC[:, 384:512], QSe[d_][:], op=ALU.add),
                         reads=[kC, "QSe" + dk_], writes=["obuf" + dk_])
                K.op("pe", lambda e: e.matmul(pC[:, 256:384], lhsT=ktok[:, i, :], rhs=Xs[d_][:], start=True, stop=True),
                     reads=["ktok", "Xs" + dk_], writes=[kC])
                K.op("dve", lambda e: e.scalar_tensor_tensor(
                    S[d_][:], S[d_][:], eTot[:, i, r:r + 1], pC[:, 256:384], op0=ALU.mult, op1=ALU.add),
                    reads=["S" + dk_, "eTot", kC], writes=["S" + dk_])
                K.op("act", lambda e: e.copy(Sb[d_][:], S[d_][:]), reads=["S" + dk_], writes=["Sb" + dk_])

            inst = [(st_, dd_) for st_ in range(NT) for dd_ in range(2)]
            groups = [inst[g0:g0 + 3] for g0 in range(0, len(inst), 3)]
            gi = 0
            pre_done = 0
            step = 0
            while step < NT:
                ready = []
                while step + len(ready) < NT and 2 * (step + len(ready)) + 2 <= pre_done:
                    ready.append(step + len(ready))
                grp = []
                if gi < len(groups):
                    grp = groups[gi]
                    gi += 1
                ns_ = len(grp) + (2 if ready else 0)
                for q_ in K.streams(ns_):
                    if q_ < len(grp):
                        g_pre(grp[q_][0], grp[q_][1], q_)
                    else:
                        for s_ in ready:
                            g_chain(s_, q_ - len(grp))
                pre_done += len(grp)
                step += len(ready)
            stop_at("gdn_scan")
            for i in range(2, NT):
                K.op("dve", lambda e, i=i: e.tensor_tensor(on_[:], obuf[0][:, i, :], obuf[1][:, i, :], op=ALU.add),
                     reads=["obuf0", "obuf1"], writes=["on_"])
                K.op("act", lambda e: e.activation(oj[:], on_[:], AF.Square), reads=["on_"], writes=["oj"])
                K.op("dve", lambda e: e.reduce_sum(oss[:, 0:1], oj[:], axis=AX.X), reads=["oj"], writes=["oss"])
                K.op("act", lambda e: e.activation(oss[:, 1:2], oss[:, 0:1], AF.Sqrt, bias=eps_t[:, 0:1], scale=1.0 / 128), reads=["oss", "eps"], writes=["oss1"])
                K.op("dve", lambda e: e.reciprocal(oss[:, 2:3], oss[:, 1:2]), reads=["oss1"], writes=["oss2"])
                K.op("dve", lambda e: e.tensor_scalar(onn[:], on_[:], oss[:, 2:3], None, op0=ALU.mult), reads=["on_", "oss2"], writes=["onn"])
                pt = ps[i % 2]; pk = "ps%d" % (i % 2)
                K.op("pe", lambda e, pt=pt: e.transpose(pt[:, 0:128], onn[:], ident[:]), reads=["onn", "ident"], writes=[pk])
                gcol = pk2[:, PK2["GNORM"]:PK2["GNORM"] + 1]
                K.op("dve", lambda e, i=i, pt=pt, gcol=gcol: e.scalar_tensor_tensor(
                    mst[:, (i - 2) * 128:(i - 1) * 128], pt[:, 0:128], gcol, zs[:, i * 128:(i + 1) * 128], op0=ALU.mult, op1=ALU.mult),
                    reads=[pk, "pk2", "zs"], writes=["mst"])
            K.dma(mT_d[h, :, :], mst[:, :], reads=["mst"], writes=[("mT", h)], key="st_mst")

    K.barrier()
    pm_d = nc.dram_tensor("pm_s", [12, 128, T], F32).ap()
    lora_d = nc.dram_tensor("lora_s", [3, 128, T], BF16).ap()
    CW_ = float(np.exp(-0.5))
    NRW = n_rows
    with ExitStack() as pr:
        cidx_i = sb("cidx_i", [128, 15], I32, stack=pr)
        cidx = sb("cidx", [128, 15], stack=pr)
        mum = {nm: sb("mum_" + nm, [128, 15], stack=pr) for nm in ("om", "L", "R", "U", "D", "P", "N")}
        tmpm = sb("tmpm", [128, 15], stack=pr)
        K.op("pool", lambda e: e.iota(cidx_i[:], pattern=[[128, 15]], base=0, channel_multiplier=1), writes=["cidx_i"])
        K.op("dve", lambda e: e.tensor_copy(cidx[:], cidx_i[:]), reads=["cidx_i"], writes=["cidx"])
        mu_ap = pk2[:, PK2["MU"]:PK2["MU"] + 15]
        K.op("dve", lambda e: e.tensor_scalar(mum["om"][:], mu_ap, -1.0, 1.0, op0=ALU.mult, op1=ALU.add), reads=["pk2"], writes=["mum"])

        def band(nm, lo, hi):
            K.op("dve", lambda e: e.tensor_scalar(tmpm[:], cidx[:], float(lo), None, op0=ALU.is_ge), reads=["cidx"], writes=["tmpm"])
            K.op("dve", lambda e: e.scalar_tensor_tensor(tmpm[:], cidx[:], float(hi), tmpm[:], op0=ALU.is_lt, op1=ALU.mult), reads=["cidx", "tmpm"], writes=["tmpm"])
            K.op("dve", lambda e: e.tensor_tensor(mum[nm][:], tmpm[:], mu_ap, op=ALU.mult), reads=["tmpm", "pk2"], writes=["mum"])

        band("L", 0, 480); band("R", 480, 960); band("U", 960, 1440); band("D", 1440, 1920)
        band("P", 0, 960); band("N", 960, 1920)
        pins = [sb("rpin%d" % i, [128, T], stack=pr) for i in range(2)]
        pmx = [sb("pmx%d" % i, [128, T], stack=pr) for i in range(2)]
        lob = sb("lob", [128, T], BF16, stack=pr)
        for j in range(15):
            pin_ = pins[j % 2]; pk_ = "rpin%d" % (j % 2)
            po = pmx[j % 2]; ok_ = "pmx%d" % (j % 2)
            K.dma(pin_[:, :], pT_d[17 + j, :, :], reads=[("pT", 17 + j)], writes=[pk_], key=pk_)
            K.op("dve", lambda e, j=j, pin_=pin_, po=po: e.tensor_scalar(po[:], pin_[:], mum["om"][:, j:j + 1], None, op0=ALU.mult),
                 reads=[pk_, "mum"], writes=[ok_])
            c0, c1 = j * 128, j * 128 + 128

            def has(lo, hi):
                return c0 < hi and c1 > lo

            def acc(dst, src, nm, j=j, pin_=pin_, po=po, pk_=pk_, ok_=ok_, eng="dve"):
                K.op("dve", lambda e: e.scalar_tensor_tensor(dst(po), src(pin_), mum[nm][:, j:j + 1], dst(po), op0=ALU.mult, op1=ALU.add),
                     reads=[pk_, "mum", ok_], writes=[ok_])

            lat = lambda t: t[:, TC:T].rearrange("p (r w) -> p r w", w=64)
            if has(0, 960):
                acc(lambda t: t[:, 1:TC], lambda t: t[:, 0:TC - 1], "P")
            if has(960, 1920):
                acc(lambda t: t[:, 0:TC - 1], lambda t: t[:, 1:TC], "N")
            if has(0, 480):
                acc(lambda t: lat(t)[:, :, 1:64], lambda t: lat(t)[:, :, 0:63], "L")
            if has(480, 960):
                acc(lambda t: lat(t)[:, :, 0:63], lambda t: lat(t)[:, :, 1:64], "R")
            if has(960, 1440) and NRW > 1:
                acc(lambda t: lat(t)[:, 1:NRW, :], lambda t: lat(t)[:, 0:NRW - 1, :], "U")
            if has(1440, 1920) and NRW > 1:
                acc(lambda t: lat(t)[:, 0:NRW - 1, :], lambda t: lat(t)[:, 1:NRW, :], "D")
            if j < 12:
                K.dma(pm_d[j, :, :], po[:, :], reads=[ok_], writes=[("pm", j)], key="st_" + ok_)
            else:
                fn_ = {12: AF.Tanh, 13: AF.Identity, 14: AF.Sigmoid}[j]
                K.op("act", lambda e, po=po, fn_=fn_: e.activation(lob[:], po[:], fn_), reads=[ok_], writes=["lob"])
                K.dma(lora_d[j - 12, :, :], lob[:, :], reads=["lob"], writes=[("lora", j - 12)], key="st_lob")
    K.barrier()
    stop_at("rw_mix")
    if "pm" in dbg:
        d_o = dbgt("pm", [12, 128, T])
        K.dma(d_o[:, :, :], pm_d[:, :, :], reads=[("pm", j) for j in range(12)], writes=["dbgpm"], key="dbg")

    comb_d = nc.dram_tensor("comb_s", [16384, 2 * D], BF16).ap()
    with ExitStack() as pw:
        nm_tiles.clear()
        RW_NDT = BF16 if RW_NM[0] == "bf16" else F32
        nmode['cur'] = RW_NM[0]
        for tag in ("n0", "n1", "n2", "n3"):
            nm_tiles[tag] = dict(PR=[sb(tag + "rPR%d" % i, [128, 256], RW_NDT, stack=pw) for i in range(2)],
                                 Pt=[sb(tag + "rPt%d" % i, [128, 128], RW_NDT, stack=pw) for i in range(2)],
                                 fin=sb(tag + "rfin", [128, 128], BF16, stack=pw))
        BL = min(256, T)
        blocks = [(b0, min(BL, T - b0)) for b0 in range(0, T, BL)]
        wst_ = sb("rw_wst", [128, 3, 512], stack=pw)
        wlb = sb("rw_wlb", [128, 3, 512], BF16, stack=pw)
        for q_, src in enumerate((w2_d, a2_d, g2w_d)):
            K.dma(wst_[:, q_, :], src[:, :], writes=["rw_wst"], key="rw_wst")
        K.op("dve", lambda e: e.tensor_copy(wlb[:], wst_[:]), reads=["rw_wst"], writes=["wlb"])
        bones = sb("bones", [128, 128], stack=pw)
        K.op("pool", lambda e: e.memset(bones[:], 0.0), writes=["bones"])
        K.op("pool", lambda e: e.memset(bones[0:64, 0:64], 1.0), writes=["bones"])
        K.op("pool", lambda e: e.memset(bones[64:128, 64:128], 1.0), writes=["bones"])
        rmask = sb("rmask", [128, BL], stack=pw)
        K.op("pool", lambda e: e.memset(rmask[:], 1.0), writes=["rmask"])
        K.op("pool", lambda e: e.memset(rmask[:].rearrange("p (n t) -> p n t", t=128)[:, :, 0:1], 0.0), writes=["rmask"])
        mask4 = [sb("mask4_%d" % d_, [128, 512], stack=pw) for d_ in range(2)]
        for d_ in range(2):
            ms_, mi_ = (m_lt, m_le) if d_ == 0 else (m_gt, m_ge)
            for q_ in range(4):
                src = ms_ if q_ % 2 == 0 else mi_
                K.op("dve", lambda e, d_=d_, q_=q_, src=src: e.tensor_copy(mask4[d_][:, q_ * 128:(q_ + 1) * 128], src[:]),
                     reads=["m_lt", "m_le", "m_gt", "m_ge"], writes=["mask4"])
        rT = [sb("rT%d" % d_, [128, T], BF16, stack=pw) for d_ in range(2)]
        kpT = [sb("kpT%d" % d_, [128, T], BF16, stack=pw) for d_ in range(2)]
        ktT = [sb("ktT%d" % d_, [128, T], BF16, stack=pw) for d_ in range(2)]
        nbT = [sb("nbT%d" % d_, [128, T], BF16, stack=pw) for d_ in range(2)]
        Lam = [sb("Lam%d" % d_, [128, NT], stack=pw) for d_ in range(2)]
        vtk = sb("rvtok", [128, NT, 128], BF16, stack=pw)
        bonus = sb("bonus", [128, T], BF16, stack=pw)
        gateT = sb("gateT", [128, T], BF16, stack=pw)
        ybuf = [sb("ybuf%d" % d_, [128, NT, 128], BF16, stack=pw) for d_ in range(2)]
        rmst = sb("rmst", [128, TL], BF16, stack=pw)
        bt = {nm: sb("b_" + nm, [128, BL], stack=pw) for nm in
              ("r", "k", "v", "kap", "sig", "cum", "w", "iw", "wp", "a", "t1", "t2", "rk")}
        lbt = sb("b_lora", [128, 3, BL], BF16, stack=pw)
        vb16 = sb("b_vb16", [128, BL], BF16, stack=pw)
        Z = [sb("Z%d" % d_, [128, 128], stack=pw) for d_ in range(2)]
        Zb = [sb("Zb%d" % d_, [128, 128], BF16, stack=pw) for d_ in range(2)]
        AK = [[sb("AK%d_%d" % (d_, z_), [128, 512], BF16, stack=pw) for z_ in range(2)] for d_ in range(2)]
        ANB = [[sb("ANB%d_%d" % (d_, z_), [128, 512], BF16, stack=pw) for z_ in range(2)] for d_ in range(2)]
        YY = [[sb("YY%d%d" % (d_, hh), [128, 128], RW_NDT, stack=pw) for hh in range(2)] for d_ in range(2)]
        YYt = [[sb("YYt%d%d" % (d_, hh), [128, 128], RW_NDT, stack=pw) for hh in range(2)] for d_ in range(2)]
        AinvS = [[[sb("Ainv%d%d_%d" % (d_, hh, z_), [128, 128], BF16, stack=pw) for z_ in range(2)] for hh in range(2)] for d_ in range(2)]
        ktok_ = [[sb("rktok%d_%d" % (d_, z_), [128, 128], BF16, stack=pw) for z_ in range(2)] for d_ in range(2)]
        nbtok_ = [[sb("rnbtok%d_%d" % (d_, z_), [128, 128], BF16, stack=pw) for z_ in range(2)] for d_ in range(2)]
        P1b = [sb("P1b%d" % d_, [128, 128], BF16, stack=pw) for d_ in range(2)]
        Ub = [sb("Ub%d" % d_, [128, 128], BF16, stack=pw) for d_ in range(2)]
        zt = [sb("zt%d" % d_, [128, 128], stack=pw) for d_ in range(2)]
        yo = sb("yo", [128, 128], stack=pw)
        yc = sb("yc", [128, 128], stack=pw)
        ysq = sb("ysq", [128, 128], stack=pw)
        yst = sb("yst", [128, 8], stack=pw)
        yt2 = sb("yt2", [128, 128], stack=pw)
        cst = [sb("cst%d" % i_, [128, 2, D], stack=pw) for i_ in range(2)]
        cbf = [sb("cbf%d" % i_, [128, 2 * D], BF16, stack=pw) for i_ in range(2)]
        conv_n = [0, 0]

        def conv_load():
            c_ = conv_n[0]
            if c_ >= 128:
                return
            conv_n[0] += 1
            st_ = cst[c_ % 2]; sk_ = "cst%d" % (c_ % 2)
            K.dma(st_[:, 0, :], down_d[c_ * 128:(c_ + 1) * 128, :], writes=[sk_], key=sk_)
            K.dma(st_[:, 1, :], up_d[c_ * 128:(c_ + 1) * 128, :], writes=[sk_], key=sk_)

        def conv_emit():
            c_ = conv_n[1]
            if c_ >= 128:
                return
            conv_n[1] += 1
            conv_load()
            st_ = cst[c_ % 2]; bf_ = cbf[c_ % 2]
            sk_ = "cst%d" % (c_ % 2); bk_ = "cbf%d" % (c_ % 2)
            K.op("act", lambda e, st_=st_, bf_=bf_: e.copy(bf_[:, 0:D], st_[:, 0, :]), reads=[sk_], writes=[bk_ + "a"])
            K.op("pool", lambda e, st_=st_, bf_=bf_: e.tensor_copy(bf_[:, D:2 * D], st_[:, 1, :]), reads=[sk_], writes=[bk_ + "b"])
            K.dma(comb_d[c_ * 128:(c_ + 1) * 128, :], bf_[:, :], reads=[bk_ + "a", bk_ + "b"], writes=["comb"], key="st_cbf")

        conv_load()

        for P in range(4):
            ch = slice(P * 128, (P + 1) * 128)
            for (b0, bn) in blocks:
                bs = slice(b0, b0 + bn)
                ntb = bn // 128
                for nm, jj in (("r", P), ("k", 4 + P), ("v", 8 + P)):
                    K.dma(bt[nm][:, 0:bn], pm_d[jj, :, bs], reads=[("pm", jj)], writes=["b_" + nm], key="b_" + nm)
                K.dma(lbt[:, :, 0:bn], lora_d[:, :, bs].rearrange("q p t -> p q t"), reads=[("lora", 0), ("lora", 1), ("lora", 2)], writes=["b_lora"], key="b_lora")
                K.op("pool", lambda e, bn=bn: e.tensor_copy(vb16[:, 0:bn], bt["v"][:, 0:bn]), reads=["b_v"], writes=["vb16"])
                for ii in range(ntb):
                    gi = b0 // 128 + ii
                    pt = psb[6 + (ii % 2)]; pk = "ps%d" % (6 + (ii % 2))
                    K.op("pe", lambda e, ii=ii, pt=pt: e.transpose(pt[:, 0:128], vb16[:, ii * 128:(ii + 1) * 128], identb[:]), reads=["vb16", "identb"], writes=[pk])
                    K.op("act", lambda e, gi=gi, pt=pt: e.copy(vtk[:, gi, :], pt[:, 0:128]), reads=[pk], writes=["rvtok"])
                kkc = pk2[:, PK2["KK"] + P:PK2["KK"] + P + 1]
                K.op("dve", lambda e, bn=bn, kkc=kkc: e.tensor_scalar(bt["kap"][:, 0:bn], bt["k"][:, 0:bn], kkc, None, op0=ALU.mult), reads=["b_k", "pk2"], writes=["b_kap"])
                K.op("pool", lambda e, bn=bn: e.tensor_tensor(bt["t1"][:, 0:bn], bt["kap"][:, 0:bn], bt["kap"][:, 0:bn], op=ALU.mult), reads=["b_kap"], writes=["b_t1"])
                for n in range((bn + 511) // 512):
                    t0 = n * 512; tn = min(512, bn - t0)
                    K.op("pe", lambda e, t0=t0, tn=tn: e.matmul(ps[0][:, 0:tn], lhsT=bones[:], rhs=bt["t1"][:, t0:t0 + tn], start=True, stop=True), reads=["bones", "b_t1"], writes=["ps0"])
                    K.op("act", lambda e, t0=t0, tn=tn: e.activation(bt["t2"][:, t0:t0 + tn], ps[0][:, 0:tn], AF.Sqrt, bias=eps_t[:, 1:2]), reads=["ps0", "eps"], writes=["b_t2"])
                K.op("dve", lambda e, bn=bn: e.reciprocal(bt["t2"][:, 0:bn], bt["t2"][:, 0:bn]), reads=["b_t2"], writes=["b_t2"])
                K.op("dve", lambda e, bn=bn: e.tensor_tensor(bt["kap"][:, 0:bn], bt["kap"][:, 0:bn], bt["t2"][:, 0:bn], op=ALU.mult), reads=["b_kap", "b_t2"], writes=["b_kap"])
                for n in range((bn + 511) // 512):
                    t0 = n * 512; tn = min(512, bn - t0)
                    K.op("pe", lambda e, t0=t0, tn=tn: e.matmul(ps[1][:, 0:tn], lhsT=wlb[:, 2, ch], rhs=lbt[:, 2, t0:t0 + tn], start=True, stop=True), reads=["wlb", "b_lora"], writes=["ps1"])
                    K.op("act", lambda e, t0=t0, tn=tn: e.copy(gateT[:, b0 + t0:b0 + t0 + tn], ps[1][:, 0:tn]), reads=["ps1"], writes=["gateT"])
                first_rk = True
                for d_ in range(2):
                    ds = slice(d_ * 64, (d_ + 1) * 64)
                    w0c = pk2[:, PK2["W0"] + d_ * 4 + P:PK2["W0"] + d_ * 4 + P + 1]
                    a0c = pk2[:, PK2["A0"] + d_ * 4 + P:PK2["A0"] + d_ * 4 + P + 1]
                    for n in range((bn + 511) // 512):
                        t0 = n * 512; tn = min(512, bn - t0)
                        K.op("pe", lambda e, t0=t0, tn=tn, ds=ds: e.matmul(ps[2][:, 0:tn], lhsT=wlb[ds, 0, ch], rhs=lbt[ds, 0, t0:t0 + tn], start=True, stop=True), reads=["wlb", "b_lora"], writes=["ps2"])
                        K.op("act", lambda e, t0=t0, tn=tn, w0c=w0c: e.activation(bt["sig"][:, t0:t0 + tn], ps[2][:, 0:tn], AF.Sigmoid, bias=w0c), reads=["ps2", "pk2"], writes=["b_sig"])
                        K.op("pe", lambda e, t0=t0, tn=tn, ds=ds: e.matmul(ps[3][:, 0:tn], lhsT=wlb[ds, 1, ch], rhs=lbt[ds, 1, t0:t0 + tn], start=True, stop=True), reads=["wlb", "b_lora"], writes=["ps3"])
                        K.op("act", lambda e, t0=t0, tn=tn, a0c=a0c: e.activation(bt["a"][:, t0:t0 + tn], ps[3][:, 0:tn], AF.Sigmoid, bias=a0c), reads=["ps3", "pk2"], writes=["b_a"])
                    K.op("dve", lambda e, bn=bn: e.tensor_tensor_scan(bt["cum"][:, 0:bn], rmask[:, 0:bn], bt["sig"][:, 0:bn], 0.0, op0=ALU.mult, op1=ALU.add),
                         reads=["rmask", "b_sig"], writes=["b_cum"])
                    c3 = bt["cum"][:, 0:bn].rearrange("p (n t) -> p n t", t=128)
                    tot_b = c3[:, :, 127:128].to_broadcast([128, ntb, 128])
                    K.op("act", lambda e, d_=d_, c3=c3, ntb=ntb: e.activation(Lam[d_][:, b0 // 128:b0 // 128 + ntb], c3[:, :, 127], AF.Exp, scale=-CW_),
                         reads=["b_cum"], writes=["Lam%d" % d_])
                    if d_ == 1:
                        K.op("dve", lambda e, bn=bn, c3=c3, tot_b=tot_b, ntb=ntb: e.tensor_tensor(
                            bt["t1"][:, 0:bn].rearrange("p (n t) -> p n t", t=128), tot_b, c3, op=ALU.subtract), reads=["b_cum"], writes=["b_t1"])
                        K.op("dve", lambda e, bn=bn: e.tensor_tensor(bt["cum"][:, 0:bn], bt["t1"][:, 0:bn], bt["sig"][:, 0:bn], op=ALU.add),
                             reads=["b_t1", "b_sig"], writes=["b_cum"])
                    K.op("act", lambda e, bn=bn: e.activation(bt["w"][:, 0:bn], bt["cum"][:, 0:bn], AF.Exp, scale=-CW_), reads=["b_cum"], writes=["b_w"])
                    K.op("act", lambda e, bn=bn: e.activation(bt["iw"][:, 0:bn], bt["cum"][:, 0:bn], AF.Exp, scale=CW_), reads=["b_cum"], writes=["b_iw"])
                    K.op("pool", lambda e, bn=bn: e.tensor_tensor(bt["t1"][:, 0:bn], bt["cum"][:, 0:bn], bt["sig"][:, 0:bn], op=ALU.subtract), reads=["b_cum", "b_sig"], writes=["b_t1"])
                    K.op("act", lambda e, bn=bn: e.activation(bt["wp"][:, 0:bn], bt["t1"][:, 0:bn], AF.Exp, scale=-CW_), reads=["b_t1"], writes=["b_wp"])
                    K.op("dve", lambda e, d_=d_, bn=bn: e.tensor_tensor(rT[d_][:, bs], bt["r"][:, 0:bn], bt["w"][:, 0:bn], op=ALU.mult), reads=["b_r", "b_w"], writes=["rT%d" % d_])
                    K.op("pool", lambda e, d_=d_, bn=bn: e.tensor_tensor(kpT[d_][:, bs], bt["kap"][:, 0:bn], bt["wp"][:, 0:bn], op=ALU.mult), reads=["b_kap", "b_wp"], writes=["kpT%d" % d_])
                    kac = pk2[:, PK2["KA"] + P:PK2["KA"] + P + 1]
                    K.op("dve", lambda e, bn=bn, kac=kac: e.tensor_scalar(bt["t1"][:, 0:bn], bt["a"][:, 0:bn], -1.0, kac, op0=ALU.add, op1=ALU.mult), reads=["b_a", "pk2"], writes=["b_t1"])
                    K.op("dve", lambda e, bn=bn: e.scalar_tensor_tensor(bt["t1"][:, 0:bn], bt["t1"][:, 0:bn], 1.0, bt["k"][:, 0:bn], op0=ALU.add, op1=ALU.mult), reads=["b_t1", "b_k"], writes=["b_t1"])
                    K.op("dve", lambda e, d_=d_, bn=bn: e.tensor_tensor(ktT[d_][:, bs], bt["t1"][:, 0:bn], bt["iw"][:, 0:bn], op=ALU.mult), reads=["b_t1", "b_iw"], writes=["ktT%d" % d_])
                    if first_rk:
                        K.op("pool", lambda e, bn=bn: e.tensor_tensor(bt["rk"][:, 0:bn], bt["t1"][:, 0:bn], bt["r"][:, 0:bn], op=ALU.mult), reads=["b_t1", "b_r"], writes=["b_rk"])
                        first_rk = False
                    else:
                        K.op("pool", lambda e, bn=bn: e.tensor_tensor(bt["t2"][:, 0:bn], bt["t1"][:, 0:bn], bt["r"][:, 0:bn], op=ALU.mult), reads=["b_t1", "b_r"], writes=["b_t2"])
                        K.op("pool", lambda e, bn=bn: e.tensor_tensor(bt["rk"][:, 0:bn], bt["rk"][:, 0:bn], bt["t2"][:, 0:bn], op=ALU.add), reads=["b_rk", "b_t2"], writes=["b_rk"])
                    K.op("dve", lambda e, bn=bn: e.scalar_tensor_tensor(bt["t2"][:, 0:bn], bt["kap"][:, 0:bn], -1.0, bt["a"][:, 0:bn], op0=ALU.mult, op1=ALU.mult), reads=["b_kap", "b_a"], writes=["b_t2"])
                    K.op("dve", lambda e, d_=d_, bn=bn: e.tensor_tensor(nbT[d_][:, bs], bt["t2"][:, 0:bn], bt["iw"][:, 0:bn], op=ALU.mult), reads=["b_t2", "b_iw"], writes=["nbT%d" % d_])
                rkc = pk2[:, PK2["RK"] + P:PK2["RK"] + P + 1]
                K.op("dve", lambda e, bn=bn, rkc=rkc: e.tensor_scalar(bt["rk"][:, 0:bn], bt["rk"][:, 0:bn], rkc, None, op0=ALU.mult), reads=["b_rk", "pk2"], writes=["b_rk"])
                for n in range((bn + 511) // 512):
                    t0 = n * 512; tn = min(512, bn - t0)
                    K.op("pe", lambda e, t0=t0, tn=tn: e.matmul(ps[4][:, 0:tn], lhsT=bones[:], rhs=bt["rk"][:, t0:t0 + tn], start=True, stop=True), reads=["bones", "b_rk"], writes=["ps4"])
                    K.op("dve", lambda e, t0=t0, tn=tn: e.tensor_tensor(bonus[:, b0 + t0:b0 + t0 + tn], ps[4][:, 0:tn], bt["v"][:, t0:t0 + tn], op=ALU.mult), reads=["ps4", "b_v"], writes=["bonus"])
            stop_at("rw_prep")
            for d_ in range(2):
                K.op("pool", lambda e, d_=d_: e.memset(Z[d_][:], 0.0), writes=["Z%d" % d_])
                K.op("pool", lambda e, d_=d_: e.memset(Zb[d_][:], 0.0), writes=["Zb%d" % d_])
            def r1(step, d_):
                i = fwd_order[step] if d_ == 0 else bwd_order[step]
                sl = slice(i * 128, (i + 1) * 128)
                want_o = i >= 2
                dk_ = "%d" % d_
                pz = step % 2
                pk_ = "%d_%d" % (d_, pz)
                b_ = d_ * 4
                for hh in range(2):
                    hs = slice(hh * 64, (hh + 1) * 64)
                    pt = ps[b_ + hh]; pk = "ps%d" % (b_ + hh)
                    for qq, src in enumerate((ktT, nbT)):
                        K.op("pe", lambda e, src=src, pt=pt, qq=qq, hs=hs, d_=d_, sl=sl: e.matmul(pt[:, qq * 256:qq * 256 + 128], lhsT=src[d_][hs, sl], rhs=kpT[d_][hs, sl], start=True, stop=True),
                             reads=["ktT" + dk_, "nbT" + dk_, "kpT" + dk_], writes=[pk])
                        K.op("pe", lambda e, src=src, pt=pt, qq=qq, hs=hs, d_=d_, sl=sl: e.matmul(pt[:, qq * 256 + 128:qq * 256 + 256], lhsT=src[d_][hs, sl], rhs=rT[d_][hs, sl], start=True, stop=True),
                             reads=["ktT" + dk_, "nbT" + dk_, "rT" + dk_], writes=[pk])
                for hh in range(2):
                    K.op("dve", lambda e, pz=pz, d_=d_, hh=hh: e.tensor_tensor(AK[d_][pz][:, hh * 256:(hh + 1) * 256], ps[d_ * 4 + hh][:, 0:256], mask4[d_][:, 0:256], op=ALU.mult),
                         reads=["ps%d" % (b_ + hh), "mask4"], writes=["AK" + pk_])
                    K.op("dve", lambda e, pz=pz, d_=d_, hh=hh: e.tensor_tensor(ANB[d_][pz][:, hh * 256:(hh + 1) * 256], ps[d_ * 4 + hh][:, 256:512], mask4[d_][:, 0:256], op=ALU.mult),
                         reads=["ps%d" % (b_ + hh), "mask4"], writes=["ANB" + pk_])
                pT2 = psb[b_ + 2]; kT2 = "ps%d" % (b_ + 2)
                K.op("pe", lambda e, pz=pz, d_=d_, sl=sl, pT2=pT2: e.transpose(pT2[:, 0:128], ktT[d_][:, sl], identb[:]), reads=["ktT" + dk_, "identb"], writes=[kT2])
                K.op("pe", lambda e, pz=pz, d_=d_, sl=sl, pT2=pT2: e.transpose(pT2[:, 128:256], nbT[d_][:, sl], identb[:]), reads=["nbT" + dk_, "identb"], writes=[kT2])
                K.op("act", lambda e, pz=pz, d_=d_, pT2=pT2: e.copy(ktok_[d_][pz][:], pT2[:, 0:128]), reads=[kT2], writes=["rktok" + pk_])
                K.op("act", lambda e, pz=pz, d_=d_, pT2=pT2: e.copy(nbtok_[d_][pz][:], pT2[:, 128:256]), reads=[kT2], writes=["rnbtok" + pk_])
            def r2(step, d_, hh):
                dk_ = "%d" % d_
                pz = step % 2
                pk_ = "%d_%d" % (d_, pz)
                tag = "n%d" % (d_ * 2 + hh)
                bank = d_ * 4 + hh
                kB2 = "ps%d" % bank
                K.op("pool", lambda e: e.tensor_copy(YY[d_][hh][:], ANB[d_][pz][:, hh * 256:hh * 256 + 128]), reads=["ANB" + pk_], writes=[tag + "Y"])
                pB = (psb if RW_NM[0] == "bf16" else ps)[bank]
                idn_ = identb if RW_NM[0] == "bf16" else ident
                K.op("pe", lambda e: e.transpose(pB[:, 0:128], YY[d_][hh][:], idn_[:]), reads=[tag + "Y", "identb", "ident"], writes=[kB2])
                K.op("act", lambda e: e.copy(YYt[d_][hh][:], pB[:, 0:128]), reads=[kB2], writes=[tag + "Yt"])
                Ai, kAi = neumann(YY[d_][hh], YYt[d_][hh], tag, bank, pb=(bank, 384))
                K.op("dve", lambda e: e.tensor_copy(AinvS[d_][hh][pz][:], Ai), reads=[kAi], writes=["Ainv%d%d_%d" % (d_, hh, pz)])

            def r3(step, d_):
                i = fwd_order[step] if d_ == 0 else bwd_order[step]
                sl = slice(i * 128, (i + 1) * 128)
                want_o = i >= 2
                dk_ = "%d" % d_
                pz = step % 2
                pk_ = "%d_%d" % (d_, pz)
                b_ = d_ * 4
                pC = ps[b_ + 3]; kC = "ps%d" % (b_ + 3)
                for hh in range(2):
                    K.op("pe", lambda e, pz=pz, d_=d_, hh=hh, i=i, pC=pC: e.matmul(pC[:, hh * 64:(hh + 1) * 64], lhsT=AK[d_][pz][:, hh * 256:hh * 256 + 128], rhs=vtk[:, i, hh * 64:(hh + 1) * 64], start=(hh == 0), stop=False),
                         reads=["AK" + pk_, "rvtok"], writes=[kC])
                K.op("pe", lambda e, pz=pz, d_=d_, sl=sl, pC=pC: e.matmul(pC[:, 0:128], lhsT=kpT[d_][:, sl], rhs=Zb[d_][:], start=False, stop=True), reads=["kpT" + dk_, "Zb" + dk_], writes=[kC])
                K.op("act", lambda e, pz=pz, d_=d_, pC=pC: e.copy(P1b[d_][:], pC[:, 0:128]), reads=[kC], writes=["P1b" + dk_])
                for hh in range(2):
                    K.op("pe", lambda e, pz=pz, d_=d_, hh=hh, pC=pC: e.matmul(pC[:, 128 + hh * 64:128 + (hh + 1) * 64], lhsT=AinvS[d_][hh][pz][:], rhs=P1b[d_][:, hh * 64:(hh + 1) * 64], start=True, stop=True),
                         reads=["Ainv%d%d_%d" % (d_, hh, pz), "P1b" + dk_], writes=[kC])
                K.op("dve", lambda e, pz=pz, d_=d_, pC=pC: e.tensor_copy(Ub[d_][:], pC[:, 128:256]), reads=[kC], writes=["Ub" + dk_])
                if want_o:
                    for hh in range(2):
                        K.op("pe", lambda e, pz=pz, d_=d_, hh=hh, i=i, pC=pC: e.matmul(pC[:, 256 + hh * 64:256 + (hh + 1) * 64], lhsT=AK[d_][pz][:, hh * 256 + 128:hh * 256 + 256], rhs=vtk[:, i, hh * 64:(hh + 1) * 64], start=(hh == 0), stop=False),
                             reads=["AK" + pk_, "rvtok"], writes=[kC])
                    K.op("pe", lambda e, pz=pz, d_=d_, sl=sl, pC=pC: e.matmul(pC[:, 256:384], lhsT=rT[d_][:, sl], rhs=Zb[d_][:], start=False, stop=False), reads=["rT" + dk_, "Zb" + dk_], writes=[kC])
                    for hh in range(2):
                        K.op("pe", lambda e, pz=pz, d_=d_, hh=hh, pC=pC: e.matmul(pC[:, 256 + hh * 64:256 + (hh + 1) * 64], lhsT=ANB[d_][pz][:, hh * 256 + 128:hh * 256 + 256], rhs=Ub[d_][:, hh * 64:(hh + 1) * 64], start=False, stop=(hh == 1)),
                             reads=["ANB" + pk_, "Ub" + dk_], writes=[kC])
                    K.op("act", lambda e, pz=pz, d_=d_, i=i, pC=pC: e.copy(ybuf[d_][:, i, :], pC[:, 256:384]), reads=[kC], writes=["ybuf" + dk_])
                pD = ps[b_ + 3]; kD = "ps%d" % (b_ + 3)
                K.op("pe", lambda e, pz=pz, d_=d_, i=i, pD=pD: e.matmul(pD[:, 384:512], lhsT=ktok_[d_][pz][:], rhs=vtk[:, i, :], start=True, stop=False), reads=["rktok" + pk_, "rvtok"], writes=[kD])
                K.op("pe", lambda e, pz=pz, d_=d_, pD=pD: e.matmul(pD[:, 384:512], lhsT=nbtok_[d_][pz][:], rhs=Ub[d_][:], start=False, stop=True), reads=["rnbtok" + pk_, "Ub" + dk_], writes=[kD])
                K.op("dve", lambda e, pz=pz, d_=d_, pD=pD: e.tensor_tensor(zt[d_][:], pD[:, 384:512], Z[d_][:], op=ALU.add), reads=[kD, "Z" + dk_], writes=["zt" + dk_])
                K.op("dve", lambda e, pz=pz, d_=d_, i=i: e.scalar_tensor_tensor(Z[d_][:], zt[d_][:], Lam[d_][:, i:i + 1], bones[:], op0=ALU.mult, op1=ALU.mult),
                     reads=["zt" + dk_, "Lam" + dk_, "bones"], writes=["Z" + dk_])
                K.op("act", lambda e, pz=pz, d_=d_: e.copy(Zb[d_][:], Z[d_][:]), reads=["Z" + dk_], writes=["Zb" + dk_])
            for d_ in K.streams(2):
                r1(0, d_)
            for q_ in K.streams(4):
                r2(0, q_ // 2, q_ % 2)
            for step in range(NT):
                conv_emit()
                if step + 1 < NT:
                    for d_ in K.streams(2):
                        r1(step + 1, d_)
                    for q_ in K.streams(6):
                        if q_ < 2:
                            r3(step, q_)
                        else:
                            r2(step + 1, (q_ - 2) // 2, (q_ - 2) % 2)
                else:
                    for d_ in K.streams(2):
                        r3(step, d_)
            stop_at("rw_scan")
            gwc = pk2[:, PK2["GNW"] + P:PK2["GNW"] + P + 1]
            gbc = pk2[:, PK2["GNB"] + P:PK2["GNB"] + P + 1]
            for i in range(2, NT):
                K.op("dve", lambda e, i=i: e.tensor_tensor(yo[:], ybuf[0][:, i, :], ybuf[1][:, i, :], op=ALU.add), reads=["ybuf0", "ybuf1"], writes=["yo"])
                y3 = yo[:].rearrange("p (h c) -> p h c", c=64)
                K.op("dve", lambda e, y3=y3: e.reduce_sum(yst[:, 0:2], y3, axis=AX.X), reads=["yo"], writes=["yst0"])
                K.op("dve", lambda e: e.tensor_scalar(yst[:, 2:4], yst[:, 0:2], 1.0 / 64, None, op0=ALU.mult), reads=["yst0"], writes=["yst1"])
                K.op("dve", lambda e, y3=y3: e.tensor_tensor(yc[:].rearrange("p (h c) -> p h c", c=64), y3, yst[:, 2:4].unsqueeze(2).to_broadcast([128, 2, 64]), op=ALU.subtract),
                     reads=["yo", "yst1"], writes=["yc"])
                K.op("act", lambda e: e.activation(ysq[:], yc[:], AF.Square), reads=["yc"], writes=["ysq"])
                K.op("dve", lambda e: e.reduce_sum(yst[:, 4:6], ysq[:].rearrange("p (h c) -> p h c", c=64), axis=AX.X), reads=["ysq"], writes=["yst2"])
                K.op("act", lambda e: e.activation(yst[:, 6:8], yst[:, 4:6], AF.Sqrt, bias=eps_t[:, 2:3], scale=1.0 / 64), reads=["yst2", "eps"], writes=["yst3"])
                K.op("dve", lambda e: e.reciprocal(yst[:, 6:8], yst[:, 6:8]), reads=["yst3"], writes=["yst3"])
                K.op("dve", lambda e: e.tensor_tensor(yt2[:].rearrange("p (h c) -> p h c", c=64), yc[:].rearrange("p (h c) -> p h c", c=64),
                                                      yst[:, 6:8].unsqueeze(2).to_broadcast([128, 2, 64]), op=ALU.mult), reads=["yc", "yst3"], writes=["yt2"])
                pt = ps[i % 2]; pk = "ps%d" % (i % 2)
                K.op("pe", lambda e, pt=pt: e.transpose(pt[:, 0:128], yt2[:], ident[:]), reads=["yt2", "ident"], writes=[pk])
                K.op("act", lambda e, pt=pt: e.activation(yc[:], pt[:, 0:128], AF.Identity, bias=gbc, scale=gwc), reads=[pk, "pk2", "yc"], writes=["yc"])
                K.op("dve", lambda e, i=i: e.tensor_tensor(yc[:], yc[:], bonus[:, i * 128:(i + 1) * 128], op=ALU.add), reads=["yc", "bonus"], writes=["yc"])
                K.op("dve", lambda e, i=i: e.tensor_tensor(rmst[:, (i - 2) * 128:(i - 1) * 128], yc[:], gateT[:, i * 128:(i + 1) * 128], op=ALU.mult), reads=["yc", "gateT"], writes=["rmst"])
            K.dma(mT_d[4 + P, :, :], rmst[:, :], reads=["rmst"], writes=[("mT", 4 + P)], key="st_rmst")
        while conv_n[1] < 128:
            conv_emit()
    K.barrier()

    stop_at("mix_done")
    with ExitStack() as pp_:
        mTs = [sb("mTs%d" % i_, [128, 8, 128], BF16, stack=pp_) for i_ in range(2)]
        woutb = sb("woutb", [128, 8, D], BF16, stack=pp_)
        wqb = sb("wqb", [128, 8, 2048], BF16, stack=pp_)
        skT = sb("skT", [128, 16, 128], BF16, stack=pp_)
        with ExitStack() as pset:
            wstg = sb("wstg", [128, 8, 512], stack=pset)
            skst = sb("skst", [128, 16, 128], stack=pset)
            for hf in range(2):
                K.dma(wstg[:, :, :], wout_d[:, hf * 512:(hf + 1) * 512].rearrange("(k p) c -> p k c", p=128), writes=["wstg"], key="wstg")
                K.op("pool", lambda e, hf=hf: e.tensor_copy(woutb[:, :, hf * 512:(hf + 1) * 512], wstg[:]), reads=["wstg"], writes=["woutb"])
            for hf in range(4):
                K.dma(wstg[:, :, :], wq_d[:, hf * 512:(hf + 1) * 512].rearrange("(k p) c -> p k c", p=128), writes=["wstg"], key="wstg")
                K.op("pool", lambda e, hf=hf: e.tensor_copy(wqb[:, :, hf * 512:(hf + 1) * 512], wstg[:]), reads=["wstg"], writes=["wqb"])
            K.dma(skst[:, :, :], sk_d[:, :, :].rearrange("g k d -> k g d"), writes=["skst"], key="skst")
            for g in range(16):
                pt = ps[g % 2]; pk = "ps%d" % (g % 2)
                K.op("pe", lambda e, g=g, pt=pt: e.transpose(pt[:, 0:128], skst[:, g, :], ident[:]), reads=["skst", "ident"], writes=[pk])
                K.op("act", lambda e, g=g, pt=pt: e.copy(skT[:, g, :], pt[:, 0:128]), reads=[pk], writes=["skT"])
            K.barrier()
        gtB = sb("gtB2", [128, 4, D], stack=pp_)
        K.dma(gtB[:].rearrange("p q d -> p (q d)"), gt_d[:, :], reads=["gt_d"], writes=["gtB"], key="gtB2")
        A2row = sb("A2row", [128, D], stack=pp_)
        fgB = sb("fgB", [128, D], stack=pp_)
        K.dma(A2row[:, :], g2row_d.partition_broadcast(128), writes=["A2row"], key="A2row")
        K.dma(fgB[:, :], fng_d.partition_broadcast(128), writes=["fgB"], key="fgB")
        K.op("dve", lambda e: e.scalar_tensor_tensor(A2row[:], gtB[:, 2, :], 1.0, A2row[:], op0=ALU.add, op1=ALU.mult), reads=["gtB", "A2row"], writes=["A2row"])
        iota16i = sb("iota16i", [128, 16], I32, stack=pp_)
        iota16 = sb("iota16", [128, 16], stack=pp_)
        K.op("pool", lambda e: e.iota(iota16i[:], pattern=[[1, 16]], base=0, channel_multiplier=0), writes=["iota16i"])
        K.op("dve", lambda e: e.tensor_copy(iota16[:], iota16i[:]), reads=["iota16i"], writes=["iota16"])

        xt_ = sb("p_xt", [128, D], stack=pp_)
        x1 = sb("p_x1", [128, D], stack=pp_)
        h2 = sb("p_h2", [128, D], stack=pp_)
        yacc = sb("p_y", [128, D], stack=pp_)
        pj = sb("p_junk", [128, D], stack=pp_)
        pss = sb("p_ss", [128, 1], stack=pp_)
        prs = sb("p_rs", [128, 2], stack=pp_)
        pxs = sb("p_xs", [128, D], stack=pp_)
        h2T = sb("p_h2T", [128, 8, 128], BF16, stack=pp_)
        qTs = sb("p_qT", [128, 16, 128], BF16, stack=pp_)
        scs = sb("p_sc", [128, 16, 128], stack=pp_)
        tmp1 = sb("p_tmp1", [128, 16, 128], stack=pp_)
        tv = sb("p_tv", [128, 16, 16], stack=pp_)
        tiu = sb("p_tiu", [128, 16, 16], U32, stack=pp_)
        tif = sb("p_tif", [128, 16, 16], stack=pp_)
        cand = sb("p_cand", [128, 8, 256], stack=pp_)
        tmp2 = sb("p_tmp2", [128, 8, 256], stack=pp_)
        eq = tmp2
        bsv = sb("p_bs", [128, 8, 16], stack=pp_)
        posu = sb("p_posu", [128, 8, 16], U32, stack=pp_)
        pau = sb("p_pau", [128, 8, 16], U32, stack=pp_)
        pbu = sb("p_pbu", [128, 8, 16], U32, stack=pp_)
        paf = sb("p_paf", [128, 8, 16], stack=pp_)
        pbf = sb("p_pbf", [128, 8, 16], stack=pp_)
        i0f = sb("p_i0f", [128, 8, 16], stack=pp_)
        i1f = sb("p_i1f", [128, 8, 16], stack=pp_)
        eidx = sb("p_eidx", [128, 128], U32, stack=pp_)
        gat = sb("p_gate", [128, 8, 16], stack=pp_)
        gsum = sb("p_gsum", [128, 8], stack=pp_)
        apre = sb("p_apre", [128, 128], stack=pp_)
        coef = sb("p_coef", [128, 128], stack=pp_)
        NRB = 8
        rowc = [sb("p_rowc%d" % i, [128, 2 * D], BF16, stack=pp_) for i in range(NRB)]
        pjb = sb("p_junkb", [128, D], BF16, stack=pp_)
        h2b = sb("p_h2b", [128, D], BF16, stack=pp_)
        dgs = [sb("p_dg%d" % i, [128, 128], BF16, stack=pp_) for i in range(4)]
        gflat = sb("p_gflat", [128, 128], stack=pp_)
        NEG = -1.0e30

        for i in range(NTL):
            tsl = slice(i * 128, (i + 1) * 128)
            K.dma(xt_[:, :], x_d[tsl, :], writes=["p_xt"], key="p_xt")
            mTt = mTs[i % 2]; mk_ = "mTs%d" % (i % 2)
            K.dma(mTt[:, :, :], mT_d[:, :, tsl].rearrange("k p t -> p k t"), reads=[("mT", j) for j in range(8)], writes=[mk_], key=mk_)
            for hf in range(2):
                pt = ps[hf]; pk = "ps%d" % hf
                for k in range(8):
                    K.op("pe", lambda e, k=k, hf=hf, pt=pt, mTt=mTt: e.matmul(pt[:, :], lhsT=mTt[:, k, :], rhs=woutb[:, k, hf * 512:(hf + 1) * 512], start=(k == 0), stop=(k == 7)),
                         reads=[mk_, "woutb"], writes=[pk])
                K.op("dve", lambda e, hf=hf, pt=pt: e.tensor_tensor(x1[:, hf * 512:(hf + 1) * 512], pt[:, :], gtB[:, 0, hf * 512:(hf + 1) * 512], op=ALU.mult),
                     reads=[pk, "gtB"], writes=["p_x1"])
            K.op("pool", lambda e: e.tensor_tensor(x1[:], x1[:], xt_[:], op=ALU.add), reads=["p_x1", "p_xt"], writes=["p_x1"])
            K.op("act", lambda e: e.activation(pj[:], x1[:], AF.Square), reads=["p_x1"], writes=["p_junk"])
            K.op("dve", lambda e: e.reduce_sum(pss[:, 0:1], pj[:], axis=AX.X), reads=["p_junk"], writes=["p_ss"])
            K.op("act", lambda e: e.activation(prs[:, 0:1], pss[:, 0:1], AF.Sqrt, bias=eps_t[:, 0:1], scale=1.0 / D), reads=["p_ss", "eps"], writes=["p_rs"])
            K.op("dve", lambda e: e.reciprocal(prs[:, 1:2], prs[:, 0:1]), reads=["p_rs"], writes=["p_rs2"])
            K.op("dve", lambda e: e.tensor_scalar(pxs[:], x1[:], prs[:, 1:2], None, op0=ALU.mult), reads=["p_x1", "p_rs2"], writes=["p_xs"])
            for hf in range(2):
                pt = ps[2 + hf]; pk = "ps%d" % (2 + hf)
                for kk in range(4):
                    k = hf * 4 + kk
                    K.op("pe", lambda e, k=k, kk=kk, pt=pt: e.transpose(pt[:, kk * 128:(kk + 1) * 128], pxs[:, k * 128:(k + 1) * 128], ident[:]), reads=["p_xs", "ident"], writes=[pk])
                for kk in range(4):
                    k = hf * 4 + kk
                    K.op("act", lambda e, k=k, kk=kk, pt=pt: e.activation(h2T[:, k, :], pt[:, kk * 128:(kk + 1) * 128], AF.Identity, bias=B2[:, k:k + 1], scale=A2[:, k:k + 1]),
                         reads=[pk, "mods"], writes=["p_h2T"])
            K.op("dve", lambda e: e.tensor_tensor(h2[:], pxs[:], A2row[:], op=ALU.mult), reads=["p_xs", "A2row"], writes=["p_h2"])
            K.op("pool", lambda e: e.tensor_tensor(h2[:], h2[:], gtB[:, 1, :], op=ALU.add), reads=["p_h2", "gtB"], writes=["p_h2"])
            for g in range(16):
                pt = ps[4 + (g % 2)]; pk = "ps%d" % (4 + (g % 2))
                for k in range(8):
                    K.op("pe", lambda e, g=g, k=k, pt=pt: e.matmul(pt[:, 0:128], lhsT=wqb[:, k, g * 128:(g + 1) * 128], rhs=h2T[:, k, :], start=(k == 0), stop=(k == 7)),
                         reads=["wqb", "p_h2T"], writes=[pk])
                K.op("act", lambda e, g=g, pt=pt: e.copy(qTs[:, g, :], pt[:, 0:128]), reads=[pk], writes=[("p_qT", g)])
            for g in range(16):
                pt = ps[6 + (g // 4) % 2]; pk = "ps%d" % (6 + (g // 4) % 2)
                K.op("pe", lambda e, g=g, pt=pt: e.matmul(pt[:, (g % 4) * 128:(g % 4 + 1) * 128], lhsT=qTs[:, g, :], rhs=skT[:, g, :], start=True, stop=True),
                     reads=[("p_qT", g), "skT"], writes=[pk])
                if g % 4 == 3:
                    K.op("dve", lambda e, g=g, pt=pt: e.tensor_copy(scs[:, g - 3:g + 1, :].rearrange("p g k -> p (g k)"), pt[:, :]), reads=[pk], writes=[("p_sc", g // 4)])
            for g in range(16):
                K.op("dve", lambda e, g=g: e.max(tv[:, g, 0:8], scs[:, g, :]), reads=[("p_sc", g // 4)], writes=[("tv", g)])
            for g in range(16):
                K.op("dve", lambda e, g=g: e.max_index(tiu[:, g, 0:8], tv[:, g, 0:8], scs[:, g, :]), reads=[("p_sc", g // 4), ("tv", g)], writes=[("tiu", g)])
            for g in range(16):
                K.op("dve", lambda e, g=g: e.match_replace(tmp1[:, g, :], tv[:, g, 0:8], scs[:, g, :], NEG), reads=[("p_sc", g // 4), ("tv", g)], writes=[("tmp1", g)])
            for g in range(16):
                K.op("dve", lambda e, g=g: e.max(tv[:, g, 8:16], tmp1[:, g, :]), reads=[("tmp1", g)], writes=[("tv2", g)])
            for g in range(16):
                K.op("dve", lambda e, g=g: e.max_index(tiu[:, g, 8:16], tv[:, g, 8:16], tmp1[:, g, :]), reads=[("tmp1", g), ("tv2", g)], writes=[("tiu2", g)])
            allg = [("tv", g) for g in range(16)] + [("tv2", g) for g in range(16)]
            alli = [("tiu", g) for g in range(16)] + [("tiu2", g) for g in range(16)]
            K.op("dve", lambda e: e.tensor_copy(tif[:], tiu[:]), reads=alli, writes=["p_tif"])
            tvv = tv[:].rearrange("p (h q) a -> p h q a", q=2)
            tfv = tif[:].rearrange("p (h q) a -> p h q a", q=2)
            c4 = cand[:].rearrange("p h (a b) -> p h a b", b=16)
            K.op("dve", lambda e: e.tensor_tensor(c4, tvv[:, :, 0, :].unsqueeze(3).to_broadcast([128, 8, 16, 16]),
                                                  tvv[:, :, 1, :].unsqueeze(2).to_broadcast([128, 8, 16, 16]), op=ALU.add), reads=allg, writes=["p_cand"])
            for hh in range(8):
                K.op("dve", lambda e, hh=hh: e.max(bsv[:, hh, 0:8], cand[:, hh, :]), reads=["p_cand"], writes=[("bs", hh)])
            for hh in range(8):
                K.op("dve", lambda e, hh=hh: e.max_index(posu[:, hh, 0:8], bsv[:, hh, 0:8], cand[:, hh, :]), reads=["p_cand", ("bs", hh)], writes=[("pos", hh)])
            for hh in range(8):
                K.op("dve", lambda e, hh=hh: e.match_replace(tmp2[:, hh, :], bsv[:, hh, 0:8], cand[:, hh, :], NEG), reads=["p_cand", ("bs", hh), "p_eq"], writes=[("tmp2", hh)])
            for hh in range(8):
                K.op("dve", lambda e, hh=hh: e.max(bsv[:, hh, 8:16], tmp2[:, hh, :]), reads=[("tmp2", hh)], writes=[("bs2", hh)])
            for hh in range(8):
                K.op("dve", lambda e, hh=hh: e.max_index(posu[:, hh, 8:16], bsv[:, hh, 8:16], tmp2[:, hh, :]), reads=[("tmp2", hh), ("bs2", hh)], writes=[("pos2", hh)])
            allb = [("bs", hh) for hh in range(8)] + [("bs2", hh) for hh in range(8)]
            allp = [("pos", hh) for hh in range(8)] + [("pos2", hh) for hh in range(8)]
            K.op("dve", lambda e: e.tensor_single_scalar(pau[:], posu[:], 4, op=ALU.logical_shift_right), reads=allp, writes=["p_pau"])
            K.op("dve", lambda e: e.tensor_single_scalar(pbu[:], posu[:], 15, op=ALU.bitwise_and), reads=allp, writes=["p_pbu"])
            K.op("dve", lambda e: e.tensor_copy(paf[:], pau[:]), reads=["p_pau"], writes=["p_paf"])
            K.op("dve", lambda e: e.tensor_copy(pbf[:], pbu[:]), reads=["p_pbu"], writes=["p_pbf"])
            e4 = eq[:].rearrange("p h (k a) -> p h k a", a=16)
            io4 = iota16[:].unsqueeze(1).unsqueeze(1).to_broadcast([128, 8, 16, 16])
            for (pf, q_, dst, nm) in ((paf, 0, i0f, "i0f"), (pbf, 1, i1f, "i1f")):
                K.op("dve", lambda e, pf=pf: e.tensor_tensor(e4, pf[:].unsqueeze(3).to_broadcast([128, 8, 16, 16]), io4, op=ALU.is_equal),
                     reads=["p_paf", "p_pbf", "iota16"], writes=["p_eq"] + [("tmp2", hh_) for hh_ in range(8)])
                K.op("dve", lambda e, q_=q_: e.tensor_tensor(e4, e4, tfv[:, :, q_, :].unsqueeze(2).to_broadcast([128, 8, 16, 16]), op=ALU.mult),
                     reads=["p_eq", "p_tif"], writes=["p_eq"])
                K.op("dve", lambda e, dst=dst: e.reduce_sum(dst[:], e4, axis=AX.X), reads=["p_eq"], writes=["p_" + nm])
            K.op("dve", lambda e: e.scalar_tensor_tensor(i0f[:], i0f[:], 128.0, i1f[:], op0=ALU.mult, op1=ALU.add), reads=["p_i0f", "p_i1f"], writes=["p_i0f"])
            K.op("dve", lambda e: e.tensor_copy(eidx[:], i0f[:].rearrange("p h k -> p (h k)")), reads=["p_i0f"], writes=["p_eidx"])
            K.op("dve", lambda e: e.tensor_tensor(gat[:], bsv[:], bsv[:, :, 0:1].to_broadcast([128, 8, 16]), op=ALU.subtract), reads=allb, writes=["p_gate"])
            K.op("act", lambda e: e.activation(gat[:], gat[:], AF.Exp), reads=["p_gate"], writes=["p_gate"])
            K.op("dve", lambda e: e.reduce_sum(gsum[:], gat[:], axis=AX.X), reads=["p_gate"], writes=["p_gsum"])
            K.op("dve", lambda e: e.reciprocal(gsum[:], gsum[:]), reads=["p_gsum"], writes=["p_gsum"])
            K.op("dve", lambda e: e.tensor_tensor(gat[:], gat[:], gsum[:].unsqueeze(2).to_broadcast([128, 8, 16]), op=ALU.mult), reads=["p_gate", "p_gsum"], writes=["p_gate"])
            K.op("act", lambda e: e.copy(h2b[:], h2[:]), reads=["p_h2"], writes=["p_h2b"])
            K.op("dve", lambda e: e.tensor_copy(gflat[:], gat[:].rearrange("p h k -> p (h k)")), reads=["p_gate"], writes=["p_gflat"])
            GRP = 4
            for g0 in range(0, 128, GRP):
                for kslot in range(g0, g0 + GRP):
                    rb = rowc[kslot % NRB]; rk_ = "p_rowc%d" % (kslot % NRB)
                    K.gather(rb[:, :], comb_d[:, :], eidx[:, kslot:kslot + 1], reads=["p_eidx", "comb"], writes=[rk_], key=rk_)
                    K.op("dve", lambda e, rb=rb, kslot=kslot: e.scalar_tensor_tensor(pjb[:], rb[:, 0:D], 1.0, h2b[:], op0=ALU.mult, op1=ALU.mult, accum_out=apre[:, kslot:kslot + 1]),
                         reads=[rk_, "p_h2b"], writes=["p_junkb", ("apre", g0 // GRP)])
                K.op("dve", lambda e, g0=g0: e.tensor_copy(coef[:, g0:g0 + GRP], apre[:, g0:g0 + GRP]), reads=[("apre", g0 // GRP)], writes=[("cf0", g0 // GRP)])
                K.op("act", lambda e, g0=g0: e.activation(coef[:, g0:g0 + GRP], coef[:, g0:g0 + GRP], AF.Gelu), reads=[("cf0", g0 // GRP)], writes=[("cf1", g0 // GRP)])
                K.op("dve", lambda e, g0=g0: e.tensor_tensor(coef[:, g0:g0 + GRP], coef[:, g0:g0 + GRP], gflat[:, g0:g0 + GRP], op=ALU.mult),
                     reads=[("cf1", g0 // GRP), "p_gflat"], writes=[("cf2", g0 // GRP)])
                for kslot in range(g0, g0 + GRP):
                    rb = rowc[kslot % NRB]; rk_ = "p_rowc%d" % (kslot % NRB)
                    dg = dgs[kslot % 4]; dk__ = "p_dg%d" % (kslot % 4)
                    K.op("act", lambda e, dg=dg, kslot=kslot: e.activation(dg[:], identb[:], AF.Identity, scale=coef[:, kslot:kslot + 1]),
                         reads=["identb", ("cf2", g0 // GRP)], writes=[dk__])
                    for hf in range(2):
                        K.op("pe", lambda e, dg=dg, rb=rb, hf=hf, kslot=kslot: e.matmul(ps[hf][:, :], lhsT=dg[:], rhs=rb[:, D + hf * 512:D + (hf + 1) * 512],
                                                                                       start=(kslot == 0), stop=(kslot == 127)),
                             reads=[dk__, rk_], writes=["ps%d" % hf])
            for hf in range(2):
                K.op("act", lambda e, hf=hf: e.copy(yacc[:, hf * 512:(hf + 1) * 512], ps[hf][:, :]), reads=["ps%d" % hf], writes=["p_y"])
            K.op("dve", lambda e: e.tensor_tensor(yacc[:], yacc[:], gtB[:, 3, :], op=ALU.mult), reads=["p_y", "gtB"], writes=["p_y"])
            K.op("pool", lambda e: e.tensor_tensor(yacc[:], yacc[:], x1[:], op=ALU.add), reads=["p_y", "p_x1"], writes=["p_y"])
            K.op("act", lambda e: e.activation(pj[:], yacc[:], AF.Square), reads=["p_y"], writes=["p_junk"])
            K.op("dve", lambda e: e.reduce_sum(pss[:, 0:1], pj[:], axis=AX.X), reads=["p_junk"], writes=["p_ss"])
            K.op("act", lambda e: e.activation(prs[:, 0:1], pss[:, 0:1], AF.Sqrt, bias=eps_t[:, 0:1], scale=1.0 / D), reads=["p_ss", "eps"], writes=["p_rs"])
            K.op("dve", lambda e: e.reciprocal(prs[:, 1:2], prs[:, 0:1]), reads=["p_rs"], writes=["p_rs2"])
            K.op("dve", lambda e: e.scalar_tensor_tensor(pxs[:], yacc[:], prs[:, 1:2], fgB[:], op0=ALU.mult, op1=ALU.mult), reads=["p_y", "p_rs2", "fgB"], writes=["p_xs"])
            K.dma(out_d[tsl, :], pxs[:, :], reads=["p_xs"], writes=["outdone"], key="st_out")
            if i == 0:
                stop_at("peer_t0")
    K.barrier()
    if "mT" in dbg:
        d_o = dbgt("mT", [8, 128, TL], BF16)
        K.dma(d_o[:, :, :], mT_d[:, :, :], reads=[("mT", j) for j in range(8)], writes=["dbgmT"], key="dbg")

    K.finish([k for k in K.st.keys() if (isinstance(k, str) and k.startswith("dbg")) or k == "outdone"])
    return dbg_out


def _inputs_for_core(inp, b, n_rows):
    TL = 64 * n_rows
    f = lambda a: np.ascontiguousarray(np.asarray(a, dtype=np.float32))
    m = {
        "x": f(inp["x"][b, :TL]),
        "c": f(inp["c"][b:b + 1]),
        "ctx": f(inp["ctx"][b]),
        "c_ctx": f(inp["c_ctx"][None, :]),
        "ada_w": f(inp["ada_w"][0]),
        "ada_b": f(inp["ada_b"][0].reshape(48, 128)),
        "ada_b_row": f(inp["ada_b"][0].reshape(1, 6144)),
        "norm1_g": f(inp["norm1_g"][0].reshape(8, 128)),
        "w_in": f(inp["w_in"][0]),
        "gdn_conv_w": f(inp["gdn_conv_w"][0].reshape(60, 128)),
        "gdn_a_log": f(inp["gdn_a_log"][0].reshape(1, 8)),
        "gdn_dt_bias": f(inp["gdn_dt_bias"][0].reshape(1, 8)),
        "gdn_norm_w": f(inp["gdn_norm_w"][0].reshape(1, 128)),
        "rwkv_mu": f(inp["rwkv_mu"][0].reshape(15, 128)),
        "rwkv_w0": f(inp["rwkv_w0"][0].reshape(8, 128)),
        "rwkv_w2": f(inp["rwkv_w2"][0].reshape(128, 512)),
        "rwkv_a0": f(inp["rwkv_a0"][0].reshape(8, 128)),
        "rwkv_a2": f(inp["rwkv_a2"][0].reshape(128, 512)),
        "rwkv_g2": f(inp["rwkv_g2"][0]),
        "rwkv_k_k": f(inp["rwkv_k_k"][0].reshape(4, 128)),
        "rwkv_k_a": f(inp["rwkv_k_a"][0].reshape(4, 128)),
        "rwkv_r_k": f(inp["rwkv_r_k"][0].reshape(4, 128)),
        "rwkv_gn_w": f(inp["rwkv_gn_w"][0].reshape(4, 128)),
        "rwkv_gn_b": f(inp["rwkv_gn_b"][0].reshape(4, 128)),
        "w_out": f(inp["w_out"][0]),
        "norm2_g": f(inp["norm2_g"][0].reshape(8, 128)),
        "peer_w_query": f(inp["peer_w_query"][0]),
        "peer_sub_keys": f(inp["peer_sub_keys"][0].reshape(16, 128, 128)),
        "peer_down": f(inp["peer_down"][0]),
        "peer_up": f(inp["peer_up"][0]),
        "final_norm_g": f(inp["final_norm_g"][None, :]),
        "norm2_g_row": f(inp["norm2_g"][0].reshape(1, 1024)),
    }
    return m


def run(inp, n_rows=64, cores=None, dbg=(), stop=None):
    nb = inp["x"].shape[0]
    cores = list(range(nb)) if cores is None else cores
    nc = bass.Bass("TRN2", target_bir_lowering=False)
    build(nc, n_rows=n_rows, dbg=dbg, stop=stop)
    in_maps = [_inputs_for_core(inp, b, n_rows) for b in cores]
    res = run_bass_kernel_spmd(nc, in_maps, core_ids=list(range(len(cores))))
    return res.results


def kernel(**inputs):
    res = run(inputs, n_rows=64)
    return np.stack([np.asarray(r["out"], dtype=np.float32) for r in res], axis=0)
```

```python
from contextlib import ExitStack
import numpy as np
import concourse.bass as bass
import concourse.mybir as mybir
from concourse.bass_utils import run_bass_kernel_spmd

F32 = mybir.dt.float32
BF16 = mybir.dt.bfloat16
I32 = mybir.dt.int32
U32 = mybir.dt.uint32
AF = mybir.ActivationFunctionType
ALU = mybir.AluOpType
AX = mybir.AxisListType

D = 1024
TC = 256
IN_COLS = 3984
GDN_COLS = 2064
NORM_EPS = 1e-6
L2_EPS = 1e-6
GN_EPS = 64e-5


class Ctx:
    def __init__(self, nc):
        self.nc = nc
        self.es = ExitStack()
        self.eng = dict(pe=nc.tensor, act=nc.scalar, dve=nc.vector, pool=nc.gpsimd, sp=nc.sync)
        self.csem = {}
        self.cnt = {}
        for e in ("pe", "act", "dve", "pool"):
            self.csem[e] = self.es.enter_context(nc.semaphore("cs_" + e))
            self.cnt[e] = 0
        self.dsem = {}
        self.seen = {e: {} for e in self.eng}
        self.st = {}
        self.ninst = 0
        self._rec = None

    def _sem(self, sk):
        if sk[0] == "c":
            return self.csem[sk[1]]
        return self.dsem[sk[1]][0]

    def _deps(self, reads, writes, e=None):
        need = {}

        def add(m):
            if m is None:
                return
            sk, v = m
            if need.get(sk, 0) < v:
                need[sk] = v

        for r in reads:
            s = self.st.get(r)
            if s is not None:
                add(s[0])
                if isinstance(r, str) and r.startswith("ps") and r[2:].isdigit():
                    for sk, v in s[1].items():
                        if sk != ("c", e):
                            add((sk, v))
        for w in writes:
            s = self.st.get(w)
            if s is not None:
                add(s[0])
                for sk, v in s[1].items():
                    add((sk, v))
        return need

    def _wait(self, e, need):
        eng = self.eng[e]
        seen = self.seen[e]
        for sk, v in need.items():
            if e == "pe" and sk == ("c", "pe"):
                continue
            if sk[0] == "d":
                v = max(v, self.dsem[sk[1]][1])
            if seen.get(sk, 0) >= v:
                continue
            eng.wait_ge(self._sem(sk), v)
            seen[sk] = v

    def _mark(self, mark, reads, writes):
        for w in writes:
            self.st[w] = [mark, {}]
        for r in reads:
            s = self.st.get(r)
            if s is None:
                s = self.st[r] = [None, {}]
            sk, v = mark
            if s[1].get(sk, 0) < v:
                s[1][sk] = v

    def streams(self, n):
        lists = []
        for d in range(n):
            self._rec = []
            yield d
            lists.append(self._rec)
            self._rec = None
        idx = [0] * n
        left = sum(len(l) for l in lists)
        while left:
            for d in range(n):
                if idx[d] < len(lists[d]):
                    kind, args, kw = lists[d][idx[d]]
                    idx[d] += 1
                    left -= 1
                    getattr(self, kind)(*args, **kw)

    def op(self, e, fn, reads=(), writes=()):
        if self._rec is not None:
            self._rec.append(("op", (e, fn, tuple(reads), tuple(writes)), {}))
            return
        need = self._deps(reads, writes, e)
        self._wait(e, need)
        ins = fn(self.eng[e])
        ins.then_inc(self.csem[e], 1)
        self.cnt[e] += 1
        self.ninst += 1
        self._mark((("c", e), self.cnt[e]), reads, writes)

    def dma(self, out, in_, reads=(), writes=(), key=None, q="sp", **kw):
        if self._rec is not None:
            self._rec.append(("dma", (out, in_, tuple(reads), tuple(writes), key, q), kw))
            return
        if key not in self.dsem:
            self.dsem[key] = [self.es.enter_context(self.nc.semaphore("ds_%d" % len(self.dsem))), 0]
        need = self._deps(reads, writes)
        self._wait(q, need)
        d = self.dsem[key]
        self.eng[q].dma_start(out=out, in_=in_, **kw).then_inc(d[0], 16)
        d[1] += 16
        self.ninst += 1
        self._mark((("d", key), d[1]), reads, writes)

    def gather(self, out, in_, idx_ap, reads=(), writes=(), key=None):
        if key not in self.dsem:
            self.dsem[key] = [self.es.enter_context(self.nc.semaphore("ds_%d" % len(self.dsem))), 0]
        need = self._deps(reads, writes)
        self._wait("pool", need)
        d = self.dsem[key]
        self.nc.gpsimd.indirect_dma_start(
            out=out, out_offset=None, in_=in_,
            in_offset=bass.IndirectOffsetOnAxis(ap=idx_ap, axis=0)).then_inc(d[0], 16)
        d[1] += 16
        self.ninst += 1
        self._mark((("d", key), d[1]), reads, writes)

    def barrier(self):
        need = {("c", e): v for e, v in self.cnt.items() if v > 0}
        for k, d in self.dsem.items():
            if d[1] > 0:
                need[("d", k)] = d[1]
        for e in self.eng:
            self._wait(e, need)

    def finish(self, keys):
        need = self._deps(keys, ())
        self._wait("sp", need)


NM_MODE = ["f32"]
RW_NM = ["bf16"]


class _Stop(Exception):
    pass


def build(nc, n_rows=64, dbg=(), stop=None):
    K = Ctx(nc)
    try:
        return _build(nc, K, n_rows, dbg, stop)
    except _Stop:
        K.barrier()
        return None


def _build(nc, K, n_rows, dbg, stop):
    def stop_at(name):
        if stop == name:
            raise _Stop()

    TL = 64 * n_rows
    T = TC + TL
    NT = T // 128
    NTL = TL // 128
    es = K.es

    def din(name, shape, dt=F32):
        return nc.dram_tensor(name, list(shape), dt, kind="ExternalInput").ap()

    x_d = din("x", [TL, D])
    c_d = din("c", [1, D])
    ctx_d = din("ctx", [TC, D])
    cctx_d = din("c_ctx", [1, D])
    adaw_d = din("ada_w", [D, 6144])
    adab_d = din("ada_b", [48, 128])
    adabr_d = din("ada_b_row", [1, 6144])
    g1_d = din("norm1_g", [8, 128])
    win_d = din("w_in", [D, IN_COLS])
    convw_d = din("gdn_conv_w", [60, 128])
    alog_d = din("gdn_a_log", [1, 8])
    dtb_d = din("gdn_dt_bias", [1, 8])
    gnorm_d = din("gdn_norm_w", [1, 128])
    mu_d = din("rwkv_mu", [15, 128])
    w0_d = din("rwkv_w0", [8, 128])
    w2_d = din("rwkv_w2", [128, 512])
    a0_d = din("rwkv_a0", [8, 128])
    a2_d = din("rwkv_a2", [128, 512])
    g2w_d = din("rwkv_g2", [128, 512])
    kk_d = din("rwkv_k_k", [4, 128])
    ka_d = din("rwkv_k_a", [4, 128])
    rk_d = din("rwkv_r_k", [4, 128])
    gnw_d = din("rwkv_gn_w", [4, 128])
    gnb_d = din("rwkv_gn_b", [4, 128])
    wout_d = din("w_out", [D, D])
    g2_d = din("norm2_g", [8, 128])
    wq_d = din("peer_w_query", [D, 2048])
    sk_d = din("peer_sub_keys", [16, 128, 128])
    down_d = din("peer_down", [16384, D])
    up_d = din("peer_up", [16384, D])
    fng_d = din("final_norm_g", [1, D])
    g2row_d = din("norm2_g_row", [1, D])
    out_d = nc.dram_tensor("out", [TL, D], F32, kind="ExternalOutput").ap()

    dbg_out = {}

    def dbgt(name, shape, dt=F32):
        dbg_out[name] = nc.dram_tensor("dbg_" + name, list(shape), dt, kind="ExternalOutput").ap()
        return dbg_out[name]

    pT_d = nc.dram_tensor("pT_s", [32, 128, T], F32).ap()

    def sb(name, shape, dt=F32, stack=es):
        return stack.enter_context(nc.sbuf_tensor(name, list(shape), dt))

    ps = [es.enter_context(nc.psum_tensor("ps%d" % i, [128, 512], F32)) for i in range(8)]

    dI = sb("dI", [128, 128], I32)
    ident = sb("ident", [128, 128])
    identb = sb("identb", [128, 128], BF16)
    ones = sb("ones", [128, 128])
    onesb = sb("onesb", [128, 128], BF16)
    m_lt = sb("m_lt", [128, 128])
    m_le = sb("m_le", [128, 128])
    m_gt = sb("m_gt", [128, 128])
    m_ge = sb("m_ge", [128, 128])
    e0 = sb("e0", [2, 128])
    K.op("pool", lambda e: e.iota(dI[:], pattern=[[1, 128]], base=0, channel_multiplier=-1), writes=["dI"])
    for t, op_, nm in ((ident, ALU.is_equal, "ident"), (m_lt, ALU.is_gt, "m_lt"), (m_le, ALU.is_ge, "m_le"),
                       (m_gt, ALU.is_lt, "m_gt"), (m_ge, ALU.is_le, "m_ge")):
        K.op("dve", lambda e, t=t, op_=op_: e.tensor_scalar(t[:], dI[:], 0.0, None, op0=op_), reads=["dI"], writes=[nm])
    K.op("dve", lambda e: e.tensor_copy(identb[:], ident[:]), reads=["ident"], writes=["identb"])
    K.op("pool", lambda e: e.memset(ones[:], 1.0), writes=["ones"])
    K.op("pool", lambda e: e.memset(onesb[:], 1.0), writes=["onesb"])
    e0i = sb("e0i", [2, 128], I32)
    K.op("pool", lambda e: e.iota(e0i[:], pattern=[[0, 128]], base=1, channel_multiplier=-1), writes=["e0i"])
    K.op("dve", lambda e: e.tensor_copy(e0[:], e0i[:]), reads=["e0i"], writes=["e0"])

    PK1 = dict(ADAB=0, G1=48, G2=56, CW=64)
    PK2 = dict(MU=0, W0=15, A0=23, KK=31, KA=35, RK=39, GNW=43, GNB=47, GNORM=51)
    pk1s = sb("pk1s", [128, 128])
    pk2s = sb("pk2s", [128, 128])
    pk1 = sb("pk1", [128, 128])
    pk2 = sb("pk2", [128, 128])
    K.op("pool", lambda e: e.memset(pk1s[:], 0.0), writes=["pk1s"])
    K.op("pool", lambda e: e.memset(pk2s[:], 0.0), writes=["pk2s"])
    for src, off, n in ((adab_d, 0, 48), (g1_d, 48, 8), (g2_d, 56, 8), (convw_d, 64, 60)):
        K.dma(pk1s[off:off + n, :], src[:, :], writes=["pk1s"], key="pk1s")
    for src, off, n in ((mu_d, 0, 15), (w0_d, 15, 8), (a0_d, 23, 8), (kk_d, 31, 4), (ka_d, 35, 4),
                        (rk_d, 39, 4), (gnw_d, 43, 4), (gnb_d, 47, 4), (gnorm_d, 51, 1)):
        K.dma(pk2s[off:off + n, :], src[:, :], writes=["pk2s"], key="pk2s")
    K.op("pe", lambda e: e.transpose(ps[0][:, 0:128], pk1s[:], ident[:]), reads=["pk1s", "ident"], writes=["ps0"])
    K.op("pe", lambda e: e.transpose(ps[0][:, 128:256], pk2s[:], ident[:]), reads=["pk2s", "ident"], writes=["ps0"])
    K.op("dve", lambda e: e.tensor_copy(pk1[:], ps[0][:, 0:128]), reads=["ps0"], writes=["pk1"])
    K.op("dve", lambda e: e.tensor_copy(pk2[:], ps[0][:, 128:256]), reads=["ps0"], writes=["pk2"])

    modT = sb("modT", [128, 48, 2])
    gt_d = nc.dram_tensor("gt_s", [128, 4 * D], F32).ap()
    A1 = sb("A1", [128, 8]); B1 = sb("B1", [128, 8])
    A1c = sb("A1c", [128, 8]); B1c = sb("B1c", [128, 8])
    A2 = sb("A2", [128, 8]); B2 = sb("B2", [128, 8])
    with ExitStack() as pa:
        cc = sb("cc", [2, D], stack=pa)
        scT = sb("scT", [128, 8, 2], stack=pa)
        aw = [sb("aw%d" % i, [128, 8, 1024], stack=pa) for i in range(2)]
        gtrow = sb("gtrow", [2, 4, D], stack=pa)
        gtB = sb("gtB", [128, 4, D], stack=pa)
        K.dma(cc[0:1, :], c_d[:, :], writes=["cc"], key="cc")
        K.dma(cc[1:2, :], cctx_d[:, :], writes=["cc"], key="cc")
        K.op("act", lambda e: e.activation(cc[:], cc[:], AF.Silu), reads=["cc"], writes=["cc"])
        for k in range(8):
            K.op("pe", lambda e, k=k: e.transpose(ps[1][:, 2 * k:2 * k + 2], cc[0:2, k * 128:(k + 1) * 128], ident[0:2, 0:2]),
                 reads=["cc", "ident"], writes=["ps1"])
        K.op("dve", lambda e: e.tensor_copy(scT[:].rearrange("p k c -> p (k c)"), ps[1][:, 0:16]), reads=["ps1"], writes=["scT"])
        K.op("pool", lambda e: e.memset(gtrow[:], 0.0), writes=["gtrow"])
        for q_ in range(4):
            K.dma(gtrow[0:1, q_, :], adabr_d[:, 2048 + q_ * 1024:3072 + q_ * 1024], writes=["gtrow"], key="gtrow")
        for g in range(6):
            a = aw[g % 2]
            an = "aw%d" % (g % 2)
            K.dma(a[:], adaw_d[:, g * 1024:(g + 1) * 1024].rearrange("(k p) c -> p k c", p=128), writes=[an], key=an)
            for j in range(8):
                col = (g * 8 + j) * 2
                for k in range(8):
                    K.op("pe", lambda e, a=a, j=j, k=k, col=col: e.matmul(
                        ps[2][:, col:col + 2], lhsT=a[:, k, j * 128:(j + 1) * 128], rhs=scT[:, k, :],
                        start=(k == 0), stop=(k == 7)), reads=[an, "scT"], writes=["ps2"])
            if g >= 2:
                q = g - 2
                for half in range(2):
                    for k in range(8):
                        K.op("pe", lambda e, a=a, k=k, half=half: e.matmul(
                            ps[3][0:2, :], lhsT=scT[:, k, :], rhs=a[:, k, half * 512:(half + 1) * 512],
                            start=(k == 0), stop=(k == 7)), reads=[an, "scT"], writes=["ps3"])
                    K.op("dve", lambda e, q=q, half=half: e.tensor_tensor(
                        gtrow[:, q, half * 512:(half + 1) * 512], ps[3][0:2, :], gtrow[:, q, half * 512:(half + 1) * 512], op=ALU.add),
                        reads=["ps3", "gtrow"], writes=["gtrow"])
                    K.op("pe", lambda e, q=q, half=half: e.matmul(
                        ps[4][:, :], lhsT=e0[:, :], rhs=gtrow[:, q, half * 512:(half + 1) * 512], start=True, stop=True),
                        reads=["e0", "gtrow"], writes=["ps4"])
                    K.op("act", lambda e, q=q, half=half: e.copy(gtB[:, q, half * 512:(half + 1) * 512], ps[4][:, :]),
                         reads=["ps4"], writes=["gtB"])
        K.op("dve", lambda e: e.tensor_tensor(
            modT[:], ps[2][:, 0:96].rearrange("p (j c) -> p j c", c=2),
            pk1[:, 0:48].unsqueeze(2).to_broadcast([128, 48, 2]), op=ALU.add), reads=["ps2", "pk1"], writes=["modT"])
        for (A, B, gname, sc0, sh0, col, nm) in ((A1, B1, "G1", 8, 0, 0, "1"), (A1c, B1c, "G1", 8, 0, 1, "1c"),
                                                 (A2, B2, "G2", 32, 24, 0, "2")):
            g0 = PK1[gname]
            K.op("dve", lambda e, A=A, g0=g0, sc0=sc0, col=col: e.scalar_tensor_tensor(
                A[:], modT[:, sc0:sc0 + 8, col], 1.0, pk1[:, g0:g0 + 8], op0=ALU.add, op1=ALU.mult),
                reads=["modT", "pk1"], writes=["A" + nm])
            K.op("dve", lambda e, B=B, sh0=sh0, col=col: e.tensor_copy(B[:], modT[:, sh0:sh0 + 8, col]),
                 reads=["modT"], writes=["B" + nm])

        K.dma(gt_d[:, :], gtB[:].rearrange("p q d -> p (q d)"), reads=["gtB"], writes=["gt_d"], key="st_gt")
        K.barrier()
    if "mod" in dbg:
        d_ = dbgt("mod", [128, 96])
        K.dma(d_[:, :], modT[:].rearrange("p j c -> p (j c)"), reads=["modT"], writes=["dbgmod"], key="dbg")

    def norm_tile(xt, xkey, A, B, hT, hkey, col0, pp, stage):
        junk, ss, rs, xs = stage
        K.op("act", lambda e: e.activation(junk[:], xt[:], AF.Square), reads=[xkey], writes=["n_junk"])
        K.op("dve", lambda e: e.reduce_sum(ss[:, 0:1], junk[:], axis=AX.X), reads=["n_junk"], writes=["n_ss"])
        K.op("act", lambda e: e.activation(rs[:, 0:1], ss[:, 0:1], AF.Sqrt, bias=eps_t[:, 0:1], scale=1.0 / D),
             reads=["n_ss", "eps"], writes=["n_rs"])
        K.op("dve", lambda e: e.reciprocal(rs[:, 1:2], rs[:, 0:1]), reads=["n_rs"], writes=["n_rs2"])
        K.op("dve", lambda e: e.tensor_scalar(xs[:], xt[:], rs[:, 1:2], None, op0=ALU.mult),
             reads=[xkey, "n_rs2"], writes=["n_xs"])
        for half in range(2):
            pt = ps[pp + half]
            pk = "ps%d" % (pp + half)
            for kk in range(4):
                k = half * 4 + kk
                K.op("pe", lambda e, k=k, kk=kk, pt=pt: e.transpose(pt[:, kk * 128:(kk + 1) * 128], xs[:, k * 128:(k + 1) * 128], ident[:]),
                     reads=["n_xs", "ident"], writes=[pk])
            for kk in range(4):
                k = half * 4 + kk
                eng = "act" if kk % 2 == 0 else "dve"
                if eng == "act":
                    K.op("act", lambda e, k=k, kk=kk, pt=pt: e.activation(
                        hT[:, k, col0:col0 + 128], pt[:, kk * 128:(kk + 1) * 128], AF.Identity,
                        bias=B[:, k:k + 1], scale=A[:, k:k + 1]), reads=[pk, "mods"], writes=[hkey])
                else:
                    K.op("dve", lambda e, k=k, kk=kk, pt=pt: e.tensor_scalar(
                        hT[:, k, col0:col0 + 128], pt[:, kk * 128:(kk + 1) * 128], A[:, k:k + 1], B[:, k:k + 1],
                        op0=ALU.mult, op1=ALU.add), reads=[pk, "mods"], writes=[hkey])

    eps_t = sb("eps_t", [128, 4])
    K.op("pool", lambda e: e.memset(eps_t[:, 0:1], NORM_EPS), writes=["eps"])
    K.op("pool", lambda e: e.memset(eps_t[:, 1:2], L2_EPS), writes=["eps"])
    K.op("pool", lambda e: e.memset(eps_t[:, 2:3], GN_EPS), writes=["eps"])
    K.op("pool", lambda e: e.memset(eps_t[:, 3:4], 1.0), writes=["eps"])
    K.op("dve", lambda e: e.tensor_copy(A1[:, 0:1], A1[:, 0:1]), reads=["A1", "B1", "A1c", "B1c", "A2", "B2"], writes=["mods"])

    with ExitStack() as pb:
        hT = sb("hT", [128, 8, T], BF16, stack=pb)
        junk = sb("junk", [128, D], stack=pb)
        ss = sb("ss", [128, 1], stack=pb)
        rs = sb("rs", [128, 2], stack=pb)
        xs = sb("xs", [128, D], stack=pb)
        xts = [sb("xt%d" % i, [128, D], stack=pb) for i in range(3)]
        for i in range(NT):
            xt = xts[i % 3]
            xk = "xt%d" % (i % 3)
            src = ctx_d[i * 128:(i + 1) * 128, :] if i < 2 else x_d[(i - 2) * 128:(i - 1) * 128, :]
            K.dma(xt[:], src, writes=[xk], key=xk)
            A, B = (A1c, B1c) if i < 2 else (A1, B1)
            norm_tile(xt, xk, A, B, hT, "hT", i * 128, 0, (junk, ss, rs, xs))
        if "hT" in dbg:
            d_ = dbgt("ss", [128, 1])
            K.dma(d_[:, :], ss[:, :], reads=["n_ss"], writes=["dbgss"], key="dbg")
            d_ = dbgt("rs", [128, 2])
            K.dma(d_[:, :], rs[:, :], reads=["n_rs", "n_rs2"], writes=["dbgrs"], key="dbg")
            d_ = dbgt("hT", [128, 8 * T], BF16)
            K.dma(d_[:, :], hT[:].rearrange("p k t -> p (k t)"), reads=["hT"], writes=["dbghT"], key="dbg")
        wst = [sb("wst%d" % i, [128, 8, 128], stack=pb) for i in range(2)]
        wbf = [sb("wbf%d" % i, [128, 8, 128], BF16, stack=pb) for i in range(2)]
        pcs = [sb("pc%d" % i, [128, T], stack=pb) for i in range(2)]
        nblk = (T + 511) // 512
        chunks = [(j, j * 128, 128) for j in range(16)] + [(16, 2048, 16)] + \
                 [(17 + j, GDN_COLS + j * 128, 128) for j in range(15)]
        ev = 0
        for ci, (dst, c0, ncol) in enumerate(chunks):
            w_s = wst[ci % 2]; w_b = wbf[ci % 2]; pc = pcs[ci % 2]
            ws_k = "wst%d" % (ci % 2); wb_k = "wbf%d" % (ci % 2); pc_k = "pc%d" % (ci % 2)
            K.dma(w_s[:, :, 0:ncol], win_d[:, c0:c0 + ncol].rearrange("(k p) c -> p k c", p=128), writes=[ws_k], key=ws_k)
            K.op("pool", lambda e, w_s=w_s, w_b=w_b, ncol=ncol: e.tensor_copy(w_b[:, :, 0:ncol], w_s[:, :, 0:ncol]),
                 reads=[ws_k], writes=[wb_k])
            for n in range(nblk):
                t0 = n * 512
                tn = min(512, T - t0)
                pt = ps[2 + (n % 4)]
                pk = "ps%d" % (2 + (n % 4))
                for k in range(8):
                    K.op("pe", lambda e, k=k, pt=pt, w_b=w_b, ncol=ncol, t0=t0, tn=tn: e.matmul(
                        pt[0:ncol, 0:tn], lhsT=w_b[:, k, 0:ncol], rhs=hT[:, k, t0:t0 + tn], start=(k == 0), stop=(k == 7)),
                        reads=[wb_k, "hT"], writes=[pk])
                eng = "act" if ev % 2 == 0 else "dve"
                ev += 1
                if eng == "act":
                    K.op("act", lambda e, pt=pt, pc=pc, ncol=ncol, t0=t0, tn=tn: e.copy(pc[0:ncol, t0:t0 + tn], pt[0:ncol, 0:tn]),
                         reads=[pk], writes=[pc_k])
                else:
                    K.op("dve", lambda e, pt=pt, pc=pc, ncol=ncol, t0=t0, tn=tn: e.tensor_copy(pc[0:ncol, t0:t0 + tn], pt[0:ncol, 0:tn]),
                         reads=[pk], writes=[pc_k])
            K.dma(pT_d[dst, 0:ncol, :], pc[0:ncol, :], reads=[pc_k], writes=[("pT", dst)], key="st_" + pc_k)

    K.barrier()
    if "pT" in dbg:
        d_ = dbgt("pT", [32, 128, T])
        K.dma(d_[:, :, :], pT_d[:, :, :], reads=[("pT", i) for i in range(32)], writes=["dbgpT"], key="dbg")


    mT_d = nc.dram_tensor("mT_s", [8, 128, TL], BF16).ap()
    psb = [p.bitcast(BF16) for p in ps]
    negm_le = sb("negm_le", [128, 128])
    negm_ge = sb("negm_ge", [128, 128])
    K.op("dve", lambda e: e.tensor_scalar(negm_le[:], m_le[:], 30000.0, -30000.0, op0=ALU.mult, op1=ALU.add), reads=["m_le"], writes=["negm_le"])
    K.op("dve", lambda e: e.tensor_scalar(negm_ge[:], m_ge[:], 30000.0, -30000.0, op0=ALU.mult, op1=ALU.add), reads=["m_ge"], writes=["negm_ge"])
    fwd_order = list(range(NT))
    bwd_order = [1, 0] + list(range(NT - 1, 1, -1))

    NMODE = NM_MODE[0]
    NDT = BF16 if NMODE == "bf16" else F32

    def mmv(ap):
        return ap

    identn = identb if NMODE == "bf16" else ident

    nmode = {'cur': NMODE}

    def neumann(Y, Yt, tag, pbank, pb=None):
        PR = nm_tiles[tag]["PR"]; Pt = nm_tiles[tag]["Pt"]
        kPR = [tag + "PR0", tag + "PR1"]; kPt = [tag + "Pt0", tag + "Pt1"]
        pa_ = ps[pbank]
        ka = "ps%d" % pbank
        if pb is None:
            pb_, kb = ps[pbank + 1], "ps%d" % (pbank + 1)
        else:
            pb_, kb = ps[pb[0]][:, pb[1]:pb[1] + 128], "ps%d" % pb[0]
        K.op("pe", lambda e: e.matmul(pa_[:, 0:128], lhsT=mmv(Yt[:]), rhs=mmv(Y[:]), start=True, stop=True), reads=[tag + "Y", tag + "Yt"], writes=[ka])
        K.op("pe", lambda e: e.matmul(pb_[:, 0:128], lhsT=mmv(Y[:]), rhs=mmv(Yt[:]), start=True, stop=True), reads=[tag + "Y", tag + "Yt"], writes=[kb])
        K.op("act", lambda e: e.copy(PR[0][:, 0:128], pa_[:, 0:128]), reads=[ka], writes=[kPR[0]])
        K.op("dve", lambda e: e.tensor_tensor(PR[0][:, 128:256], Y[:], (identb if nmode['cur'] == 'bf16' else ident)[:], op=ALU.add), reads=[tag + "Y", "identb", "ident"], writes=[kPR[0]])
        K.op("dve", lambda e: e.tensor_copy(Pt[0][:], pb_[:, 0:128]), reads=[kb], writes=[kPt[0]])
        cur = 0
        for l in range(1, 7):
            nxt = 1 - cur
            last = (l == 6)
            n0 = 128 if last else 0
            K.op("pe", lambda e, cur=cur, n0=n0: e.matmul(pa_[:, n0:256], lhsT=mmv(Pt[cur][:]), rhs=mmv(PR[cur][:, n0:256]), start=True, stop=False),
                 reads=[kPt[cur], kPR[cur]], writes=[ka])
            K.op("pe", lambda e, cur=cur: e.matmul(pa_[:, 128:256], lhsT=mmv((identb if nmode['cur'] == 'bf16' else ident)[:]), rhs=mmv(PR[cur][:, 128:256]), start=False, stop=True),
                 reads=["identb", "ident", kPR[cur]], writes=[ka])
            if not last:
                K.op("pe", lambda e, cur=cur: e.matmul(pb_[:, 0:128], lhsT=mmv(PR[cur][:, 0:128]), rhs=mmv(Pt[cur][:]), start=True, stop=True),
                     reads=[kPt[cur], kPR[cur]], writes=[kb])
            K.op("act", lambda e, nxt=nxt, n0=n0: e.copy(PR[nxt][:, n0:256], pa_[:, n0:256]), reads=[ka], writes=[kPR[nxt]])
            if not last:
                K.op("dve", lambda e, nxt=nxt: e.tensor_copy(Pt[nxt][:], pb_[:, 0:128]), reads=[kb], writes=[kPt[nxt]])
            cur = nxt
        if nmode['cur'] == 'bf16':
            return PR[cur][:, 128:256], kPR[cur]
        fin = nm_tiles[tag]["fin"]
        K.op("act", lambda e, cur=cur: e.copy(fin[:], PR[cur][:, 128:256]), reads=[kPR[cur]], writes=[tag + "fin"])
        return fin[:], tag + "fin"

    nm_tiles = {}
    with ExitStack() as pg:
        for tag in ("n0", "n1", "n2", "n3"):
            nm_tiles[tag] = dict(PR=[sb(tag + "PR%d" % i, [128, 256], NDT, stack=pg) for i in range(2)],
                                 Pt=[sb(tag + "Pt%d" % i, [128, 128], NDT, stack=pg) for i in range(2)],
                                 fin=sb(tag + "fin", [128, 128], BF16, stack=pg))
        ab = sb("ab", [128, NT, 16], stack=pg)
        dtb_b = sb("dtb_b", [128, 8], stack=pg)
        nA_b = sb("nA_b", [128, 8], stack=pg)
        gg = sb("gg", [128, NT, 8], stack=pg)
        Gc = sb("Gc", [128, NT, 8], stack=pg)
        nbeta = sb("nbeta", [128, NT, 8], stack=pg)
        beta = sb("beta", [128, NT, 8], stack=pg)
        negeG = sb("negeG", [128, NT, 8], stack=pg)
        eG = sb("eG", [128, NT, 8], stack=pg)
        eTG = sb("eTG", [128, NT, 8], stack=pg)
        eTot = sb("eTot", [128, NT, 8], stack=pg)
        pg_ab = ExitStack()
        abT = sb("abT", [16, T], stack=pg_ab)
        K.dma(abT[:, :], pT_d[16, 0:16, :], reads=[("pT", 16)], writes=["abT"], key="abT")
        K.dma(dtb_b[:, :], dtb_d.partition_broadcast(128), writes=["dtb_b"], key="dtb_b")
        K.dma(nA_b[:, :], alog_d.partition_broadcast(128), writes=["nA_b"], key="nA_b")
        for i in range(NT):
            K.op("pe", lambda e, i=i: e.transpose(ps[i // 32][:, (i % 32) * 16:(i % 32) * 16 + 16], abT[0:16, i * 128:(i + 1) * 128], ident[0:16, 0:16]),
                 reads=["abT", "ident"], writes=["ps%d" % (i // 32)])
        for b0 in range(0, NT, 32):
            nb = min(32, NT - b0)
            K.op("dve", lambda e, b0=b0, nb=nb: e.tensor_copy(ab[:, b0:b0 + nb, :].rearrange("p n c -> p (n c)"), ps[b0 // 32][:, 0:nb * 16]),
                 reads=["ps%d" % (b0 // 32)], writes=["ab"])
        K.op("act", lambda e: e.activation(nA_b[:], nA_b[:], AF.Exp), reads=["nA_b"], writes=["nA_b"])
        K.op("dve", lambda e: e.tensor_scalar(nA_b[:], nA_b[:], -1.0, None, op0=ALU.mult), reads=["nA_b"], writes=["nA_b"])
        K.op("dve", lambda e: e.tensor_tensor(gg[:], ab[:, :, 0:8], dtb_b[:].unsqueeze(1).to_broadcast([128, NT, 8]), op=ALU.add),
             reads=["ab", "dtb_b"], writes=["gg"])
        K.op("act", lambda e: e.activation(gg[:], gg[:], AF.Exp), reads=["gg"], writes=["gg"])
        K.op("act", lambda e: e.activation(gg[:], gg[:], AF.Ln, bias=eps_t[:, 3:4]), reads=["gg", "eps"], writes=["gg"])
        K.op("dve", lambda e: e.tensor_tensor(gg[:], gg[:], nA_b[:].unsqueeze(1).to_broadcast([128, NT, 8]), op=ALU.mult),
             reads=["gg", "nA_b"], writes=["gg"])
        K.op("act", lambda e: e.activation(beta[:], ab[:, :, 8:16], AF.Sigmoid), reads=["ab"], writes=["beta"])
        K.op("dve", lambda e: e.tensor_scalar(nbeta[:], beta[:], -1.0, None, op0=ALU.mult), reads=["beta"], writes=["nbeta"])
        ggf = gg[:].rearrange("p n c -> p (n c)")
        K.op("pe", lambda e: e.matmul(ps[2][:, 0:NT * 8], lhsT=m_le[:], rhs=ggf, start=True, stop=True), reads=["m_le", "gg"], writes=["ps2"])
        K.op("pe", lambda e: e.matmul(ps[3][:, 0:NT * 8], lhsT=m_ge[:], rhs=ggf, start=True, stop=True), reads=["m_ge", "gg"], writes=["ps3"])
        K.op("pe", lambda e: e.matmul(ps[4][:, 0:NT * 8], lhsT=ones[:], rhs=ggf, start=True, stop=True), reads=["ones", "gg"], writes=["ps4"])
        K.op("dve", lambda e: e.tensor_copy(Gc[:, :, 0:4], ps[2][:, 0:NT * 8].rearrange("p (n c) -> p n c", c=8)[:, :, 0:4]), reads=["ps2"], writes=["Gc"])
        K.op("dve", lambda e: e.tensor_copy(Gc[:, :, 4:8], ps[3][:, 0:NT * 8].rearrange("p (n c) -> p n c", c=8)[:, :, 4:8]), reads=["ps3"], writes=["Gc"])
        K.op("act", lambda e: e.activation(eG[:], Gc[:], AF.Exp), reads=["Gc"], writes=["eG"])
        K.op("dve", lambda e: e.tensor_scalar(negeG[:], eG[:], -1.0, None, op0=ALU.mult), reads=["eG"], writes=["negeG"])
        K.op("act", lambda e: e.activation(eTot[:].rearrange("p n c -> p (n c)"), ps[4][:, 0:NT * 8], AF.Exp), reads=["ps4"], writes=["eTot"])
        K.op("dve", lambda e: e.tensor_tensor(eTG[:].rearrange("p n c -> p (n c)"), ps[4][:, 0:NT * 8], Gc[:].rearrange("p n c -> p (n c)"), op=ALU.subtract),
             reads=["ps4", "Gc"], writes=["eTG"])
        K.op("act", lambda e: e.activation(eTG[:], eTG[:], AF.Exp), reads=["eTG"], writes=["eTG"])

        K.barrier()
        pg_ab.close()
        stop_at("gdn_scal")
        qT = sb("qT", [128, T], BF16, stack=pg)
        kT = sb("kT", [128, T], BF16, stack=pg)
        vT = sb("vT", [128, T], BF16, stack=pg)
        zs = sb("zs", [128, T], BF16, stack=pg)
        vtok = sb("vtok", [128, NT, 128], BF16, stack=pg)
        ktok = sb("ktok", [128, NT, 128], BF16, stack=pg)
        obuf = [sb("obuf%d" % d_, [128, NT, 128], BF16, stack=pg) for d_ in range(2)]
        AinvAll = [sb("AinvAll%d" % d_, [128, NT, 128], BF16, stack=pg) for d_ in range(2)]
        MqkAll = [sb("MqkAll%d" % d_, [128, NT, 128], BF16, stack=pg) for d_ in range(2)]
        pin = sb("pin", [128, T], stack=pg)
        cv = sb("cv", [128, T], stack=pg)
        sq = pin
        rn = sb("rn", [128, 512], stack=pg)
        S = [sb("S%d" % d_, [128, 128], stack=pg) for d_ in range(2)]
        Sb = [sb("Sb%d" % d_, [128, 128], BF16, stack=pg) for d_ in range(2)]
        mst = sb("mst", [128, TL], BF16, stack=pg)
        dgl = [sb("dgl%d" % d_, [128, 128], stack=pg) for d_ in range(4)]
        arg = dgl
        DTi = [sb("DTi%d" % d_, [128, 128], stack=pg) for d_ in range(4)]
        DTs = dgl
        Yb = [sb("Yb%d" % d_, [128, 128], NDT, stack=pg) for d_ in range(4)]
        Ytb = [sb("Ytb%d" % d_, [128, 128], NDT, stack=pg) for d_ in range(4)]
        Rb = [sb("Rb%d" % d_, [128, 128], BF16, stack=pg) for d_ in range(2)]
        Xb = [sb("Xb%d" % d_, [128, 128], BF16, stack=pg) for d_ in range(2)]
        Xs = [sb("Xs%d" % d_, [128, 128], BF16, stack=pg) for d_ in range(2)]
        QSe = [sb("QSe%d" % d_, [128, 128], stack=pg) for d_ in range(2)]
        on_ = sb("on_", [128, 128], stack=pg)
        oj = sb("oj", [128, 128], stack=pg)
        onn = sb("onn", [128, 128], stack=pg)
        oss = sb("oss", [128, 4], stack=pg)

        def conv_silu(cidx, dst, dkey, final_silu_to):
            cw0 = PK1["CW"]
            K.op("dve", lambda e: e.tensor_scalar(cv[:], pin[:], pk1[:, cw0 + 2 * 12 + cidx:cw0 + 2 * 12 + cidx + 1], None, op0=ALU.mult),
                 reads=["pin", "pk1"], writes=["cv"])
            for j in (0, 1, 3, 4):
                sh = j - 2
                wcol = pk1[:, cw0 + j * 12 + cidx:cw0 + j * 12 + cidx + 1]
                for (s0, s1) in ((0, TC), (TC, T)):
                    lo = max(s0, s0 - sh); hi = min(s1, s1 - sh)
                    K.op("dve", lambda e, lo=lo, hi=hi, sh=sh, wcol=wcol: e.scalar_tensor_tensor(
                        cv[:, lo:hi], pin[:, lo + sh:hi + sh], wcol, cv[:, lo:hi], op0=ALU.mult, op1=ALU.add),
                        reads=["pin", "pk1", "cv"], writes=["cv"])
            K.op("act", lambda e: e.activation(final_silu_to[:], cv[:], AF.Silu), reads=["cv"], writes=[dkey])

        def l2n(src, skey, dst, dkey, scale):
            K.op("pool", lambda e: e.tensor_tensor(sq[:], src[:], src[:], op=ALU.mult), reads=[skey], writes=["pin"])
            for n in range((T + 511) // 512):
                t0 = n * 512; tn = min(512, T - t0)
                pt = ps[5 + (n % 2)]; pk = "ps%d" % (5 + (n % 2))
                K.op("pe", lambda e, pt=pt, t0=t0, tn=tn: e.matmul(pt[:, 0:tn], lhsT=ones[:], rhs=sq[:, t0:t0 + tn], start=True, stop=True),
                     reads=["ones", "pin"], writes=[pk])
                K.op("act", lambda e, pt=pt, tn=tn: e.activation(rn[:, 0:tn], pt[:, 0:tn], AF.Sqrt, bias=eps_t[:, 1:2]), reads=[pk, "eps"], writes=["rn"])
                K.op("dve", lambda e, tn=tn: e.reciprocal(rn[:, 0:tn], rn[:, 0:tn]), reads=["rn"], writes=["rn"])
                K.op("dve", lambda e, t0=t0, tn=tn: e.scalar_tensor_tensor(dst[:, t0:t0 + tn], src[:, t0:t0 + tn], scale, rn[:, 0:tn], op0=ALU.mult, op1=ALU.mult),
                     reads=[skey, "rn"], writes=[dkey])

        def to_tok(src, skey, dst, dkey):
            for i in range(NT):
                pt = psb[5 + (i % 2)]; pk = "ps%d" % (5 + (i % 2))
                K.op("pe", lambda e, i=i, pt=pt: e.transpose(pt[:, 0:128], src[:, i * 128:(i + 1) * 128], identb[:]), reads=[skey, "identb"], writes=[pk])
                eng = "act" if i % 2 == 0 else "dve"
                if eng == "act":
                    K.op("act", lambda e, i=i, pt=pt: e.copy(dst[:, i, :], pt[:, 0:128]), reads=[pk], writes=[dkey])
                else:
                    K.op("dve", lambda e, i=i, pt=pt: e.tensor_copy(dst[:, i, :], pt[:, 0:128]), reads=[pk], writes=[dkey])

        for h in range(4):
            K.dma(pin[:, :], pT_d[h, :, :], reads=[("pT", h)], writes=["pin"], key="pin")
            conv_silu(h, cv, "cv", cv)
            l2n(cv, "cv", qT, "qT", float(128 ** -0.5))
            K.dma(pin[:, :], pT_d[4 + h, :, :], reads=[("pT", 4 + h)], writes=["pin"], key="pin")
            conv_silu(4 + h, cv, "cv", cv)
            l2n(cv, "cv", kT, "kT", 1.0)
            to_tok(kT, "kT", ktok, "ktok")
            K.dma(pin[:, :], pT_d[8 + h, :, :], reads=[("pT", 8 + h)], writes=["pin"], key="pin")
            conv_silu(8 + h, vT, "vT", vT)
            to_tok(vT, "vT", vtok, "vtok")
            K.dma(pin[:, :], pT_d[12 + h, :, :], reads=[("pT", 12 + h)], writes=["pin"], key="pin")
            K.op("act", lambda e: e.activation(zs[:], pin[:], AF.Silu), reads=["pin"], writes=["zs"])
            stop_at("gdn_prep")
            for d_ in range(2):
                K.op("pool", lambda e, d_=d_: e.memset(S[d_][:], 0.0), writes=["S%d" % d_])
                K.op("pool", lambda e, d_=d_: e.memset(Sb[d_][:], 0.0), writes=["Sb%d" % d_])
            def g_pre(step, d_, sl_):
                i = fwd_order[step] if d_ == 0 else bwd_order[step]
                par = step % 2
                r = d_ * 4 + h
                tag = "n%d" % sl_
                sl = slice(i * 128, (i + 1) * 128)
                want_o = i >= 2
                pA = ps[2 * sl_]; kA = "ps%d" % (2 * sl_)
                dk_ = "%d" % sl_
                K.op("dve", lambda e: e.tensor_scalar(dgl[sl_][:], ident[:], Gc[:, i, r:r + 1], None, op0=ALU.mult),
                     reads=["ident", "Gc"], writes=["dgl" + dk_])
                K.op("pe", lambda e: e.matmul(pA[:, 256:384], lhsT=ones[:], rhs=dgl[sl_][:], start=True, stop=True),
                     reads=["ones", "dgl" + dk_], writes=[kA])
                negm = negm_le if d_ == 0 else negm_ge
                mstrict = m_lt if d_ == 0 else m_gt
                K.op("dve", lambda e: e.scalar_tensor_tensor(
                    arg[sl_][:], pA[:, 256:384], Gc[:, i, r:r + 1], negm[:], op0=ALU.subtract, op1=ALU.add),
                    reads=[kA, "Gc", "negm_le", "negm_ge"], writes=["dgl" + dk_])
                K.op("act", lambda e: e.activation(DTi[sl_][:], arg[sl_][:], AF.Exp), reads=["dgl" + dk_], writes=["DTi" + dk_])
                K.op("pool", lambda e: e.tensor_tensor(DTs[sl_][:], DTi[sl_][:], mstrict[:], op=ALU.mult),
                     reads=["DTi" + dk_, "m_lt", "m_gt"], writes=["dgl" + dk_])
                K.op("pe", lambda e: e.matmul(pA[:, 0:128], lhsT=kT[:, sl], rhs=kT[:, sl], start=True, stop=True),
                     reads=["kT"], writes=[kA])
                K.op("dve", lambda e: e.scalar_tensor_tensor(
                    Yb[sl_][:], pA[:, 0:128], nbeta[:, i, r:r + 1], DTs[sl_][:], op0=ALU.mult, op1=ALU.mult),
                    reads=[kA, "nbeta", "dgl" + dk_], writes=[tag + "Y"])
                if want_o:
                    K.op("pe", lambda e: e.matmul(pA[:, 128:256], lhsT=kT[:, sl], rhs=qT[:, sl], start=True, stop=True),
                         reads=["kT", "qT"], writes=[kA])
                    K.op("dve", lambda e: e.tensor_tensor(MqkAll[d_][:, step, :], pA[:, 128:256], DTi[sl_][:], op=ALU.mult),
                         reads=[kA, "DTi" + dk_], writes=[("Mqk", d_, step)])
                pB = (psb if NMODE == "bf16" else ps)[2 * sl_ + 1]; kB = "ps%d" % (2 * sl_ + 1)
                K.op("pe", lambda e: e.transpose(pB[:, 0:128], Yb[sl_][:], identn[:]), reads=[tag + "Y", "identb", "ident"], writes=[kB])
                K.op("act", lambda e: e.copy(Ytb[sl_][:], pB[:, 0:128]), reads=[kB], writes=[tag + "Yt"])
                AinvT, kAinv = neumann(Yb[sl_], Ytb[sl_], tag, 2 * sl_ + 1, pb=(2 * sl_, 384))
                K.op("pool", lambda e: e.tensor_copy(AinvAll[d_][:, step, :], AinvT), reads=[kAinv], writes=[("gAinv", d_, step)])

            def g_chain(step, d_):
                i = fwd_order[step] if d_ == 0 else bwd_order[step]
                par = step % 2
                r = d_ * 4 + h
                sl = slice(i * 128, (i + 1) * 128)
                want_o = i >= 2
                pC = ps[6 + d_]; kC = "ps%d" % (6 + d_)
                dk_ = "%d" % d_
                kAinv = ("gAinv", d_, step)
                kMqk = ("Mqk", d_, step)
                K.op("pe", lambda e: e.matmul(pC[:, 0:128], lhsT=kT[:, sl], rhs=Sb[d_][:], start=True, stop=True),
                     reads=["kT", "Sb" + dk_], writes=[kC])
                if want_o:
                    K.op("pe", lambda e: e.matmul(pC[:, 128:256], lhsT=qT[:, sl], rhs=Sb[d_][:], start=True, stop=True),
                         reads=["qT", "Sb" + dk_], writes=[kC])
                K.op("dve", lambda e: e.scalar_tensor_tensor(
                    Rb[d_][:], pC[:, 0:128], negeG[:, i, r:r + 1], vtok[:, i, :], op0=ALU.mult, op1=ALU.add),
                    reads=[kC, "negeG", "vtok"], writes=["Rb" + dk_])
                if want_o:
                    K.op("act", lambda e: e.activation(QSe[d_][:], pC[:, 128:256], AF.Identity, scale=eG[:, i, r:r + 1]),
                         reads=[kC, "eG"], writes=["QSe" + dk_])
                K.op("pe", lambda e: e.matmul(pC[:, 0:128], lhsT=AinvAll[d_][:, step, :], rhs=Rb[d_][:], start=True, stop=True),
                     reads=[kAinv, "Rb" + dk_], writes=[kC])
                K.op("dve", lambda e: e.tensor_scalar(Xb[d_][:], pC[:, 0:128], beta[:, i, r:r + 1], None, op0=ALU.mult),
                     reads=[kC, "beta"], writes=["Xb" + dk_])
                K.op("pool", lambda e: e.tensor_scalar(Xs[d_][:], Xb[d_][:], eTG[:, i, r:r + 1], None, op0=ALU.mult),
                     reads=["Xb" + dk_, "eTG"], writes=["Xs" + dk_])
                if want_o:
                    K.op("pe", lambda e: e.matmul(pC[:, 384:512], lhsT=MqkAll[d_][:, step, :], rhs=Xb[d_][:], start=True, stop=True),
                         reads=[kMqk, "Xb" + dk_], writes=[kC])
                    K.op("dve", lambda e: e.tensor_tensor(obuf[d_][:, i, :], pC[:, 384:512], QSe[d_][:], op=ALU.add),
                         reads=[kC, "QSe" + dk_], writes=["obuf" + dk_])
                K.op("pe", lambda e: e.matmul(pC[:, 256:384], lhsT=ktok[:, i, :], rhs=Xs[d_][:], start=True, stop=True),
                     reads=["ktok", "Xs" + dk_], writes=[kC])
                K.op("dve", lambda e: e.scalar_tensor_tensor(
                    S[d_][:], S[d_][:], eTot[:, i, r:r + 1], pC[:, 256:384], op0=ALU.mult, op1=ALU.add),
                    reads=["S" + dk_, "eTot", kC], writes=["S" + dk_])
                K.op("act", lambda e: e.copy(Sb[d_][:], S[d_][:]), reads=["S" + dk_], writes=["Sb" + dk_])

            inst = [(st_, dd_) for st_ in range(NT) for dd_ in range(2)]
            groups = [inst[g0:g0 + 3] for g0 in range(0, len(inst), 3)]
            gi = 0
            pre_done = 0
            step = 0
            while step < NT:
                ready = []
                while step + len(ready) < NT and 2 * (step + len(ready)) + 2 <= pre_done:
                    ready.append(step + len(ready))
                grp = []
                if gi < len(groups):
                    grp = groups[gi]
                    gi += 1
                ns_ = len(grp) + (2 if ready else 0)
                for q_ in K.streams(ns_):
                    if q_ < len(grp):
                        g_pre(grp[q_][0], grp[q_][1], q_)
                    else:
                        for s_ in ready:
                            g_chain(s_, q_ - len(grp))
                pre_done += len(grp)
                step += len(ready)
            stop_at("gdn_scan")
            for i in range(2, NT):
                K.op("dve", lambda e, i=i: e.tensor_tensor(on_[:], obuf[0][:, i, :], obuf[1][:, i, :], op=ALU.add),
                     reads=["obuf0", "obuf1"], writes=["on_"])
                K.op("act", lambda e: e.activation(oj[:], on_[:], AF.Square), reads=["on_"], writes=["oj"])
                K.op("dve", lambda e: e.reduce_sum(oss[:, 0:1], oj[:], axis=AX.X), reads=["oj"], writes=["oss"])
                K.op("act", lambda e: e.activation(oss[:, 1:2], oss[:, 0:1], AF.Sqrt, bias=eps_t[:, 0:1], scale=1.0 / 128), reads=["oss", "eps"], writes=["oss1"])
                K.op("dve", lambda e: e.reciprocal(oss[:, 2:3], oss[:, 1:2]), reads=["oss1"], writes=["oss2"])
                K.op("dve", lambda e: e.tensor_scalar(onn[:], on_[:], oss[:, 2:3], None, op0=ALU.mult), reads=["on_", "oss2"], writes=["onn"])
                pt = ps[i % 2]; pk = "ps%d" % (i % 2)
                K.op("pe", lambda e, pt=pt: e.transpose(pt[:, 0:128], onn[:], ident[:]), reads=["onn", "ident"], writes=[pk])
                gcol = pk2[:, PK2["GNORM"]:PK2["GNORM"] + 1]
                K.op("dve", lambda e, i=i, pt=pt, gcol=gcol: e.scalar_tensor_tensor(
                    mst[:, (i - 2) * 128:(i - 1) * 128], pt[:, 0:128], gcol, zs[:, i * 128:(i + 1) * 128], op0=ALU.mult, op1=ALU.mult),
                    reads=[pk, "pk2", "zs"], writes=["mst"])
            K.dma(mT_d[h, :, :], mst[:, :], reads=["mst"], writes=[("mT", h)], key="st_mst")

    K.barrier()
    pm_d = nc.dram_tensor("pm_s", [12, 128, T], F32).ap()
    lora_d = nc.dram_tensor("lora_s", [3, 128, T], BF16).ap()
    CW_ = float(np.exp(-0.5))
    NRW = n_rows
    with ExitStack() as pr:
        cidx_i = sb("cidx_i", [128, 15], I32, stack=pr)
        cidx = sb("cidx", [128, 15], stack=pr)
        mum = {nm: sb("mum_" + nm, [128, 15], stack=pr) for nm in ("om", "L", "R", "U", "D", "P", "N")}
        tmpm = sb("tmpm", [128, 15], stack=pr)
        K.op("pool", lambda e: e.iota(cidx_i[:], pattern=[[128, 15]], base=0, channel_multiplier=1), writes=["cidx_i"])
        K.op("dve", lambda e: e.tensor_copy(cidx[:], cidx_i[:]), reads=["cidx_i"], writes=["cidx"])
        mu_ap = pk2[:, PK2["MU"]:PK2["MU"] + 15]
        K.op("dve", lambda e: e.tensor_scalar(mum["om"][:], mu_ap, -1.0, 1.0, op0=ALU.mult, op1=ALU.add), reads=["pk2"], writes=["mum"])

        def band(nm, lo, hi):
            K.op("dve", lambda e: e.tensor_scalar(tmpm[:], cidx[:], float(lo), None, op0=ALU.is_ge), reads=["cidx"], writes=["tmpm"])
            K.op("dve", lambda e: e.scalar_tensor_tensor(tmpm[:], cidx[:], float(hi), tmpm[:], op0=ALU.is_lt, op1=ALU.mult), reads=["cidx", "tmpm"], writes=["tmpm"])
            K.op("dve", lambda e: e.tensor_tensor(mum[nm][:], tmpm[:], mu_ap, op=ALU.mult), reads=["tmpm", "pk2"], writes=["mum"])

        band("L", 0, 480); band("R", 480, 960); band("U", 960, 1440); band("D", 1440, 1920)
        band("P", 0, 960); band("N", 960, 1920)
        pins = [sb("rpin%d" % i, [128, T], stack=pr) for i in range(2)]
        pmx = [sb("pmx%d" % i, [128, T], stack=pr) for i in range(2)]
        lob = sb("lob", [128, T], BF16, stack=pr)
        for j in range(15):
            pin_ = pins[j % 2]; pk_ = "rpin%d" % (j % 2)
            po = pmx[j % 2]; ok_ = "pmx%d" % (j % 2)
            K.dma(pin_[:, :], pT_d[17 + j, :, :], reads=[("pT", 17 + j)], writes=[pk_], key=pk_)
            K.op("dve", lambda e, j=j, pin_=pin_, po=po: e.tensor_scalar(po[:], pin_[:], mum["om"][:, j:j + 1], None, op0=ALU.mult),
                 reads=[pk_, "mum"], writes=[ok_])
            c0, c1 = j * 128, j * 128 + 128

            def has(lo, hi):
                return c0 < hi and c1 > lo

            def acc(dst, src, nm, j=j, pin_=pin_, po=po, pk_=pk_, ok_=ok_, eng="dve"):
                K.op("dve", lambda e: e.scalar_tensor_tensor(dst(po), src(pin_), mum[nm][:, j:j + 1], dst(po), op0=ALU.mult, op1=ALU.add),
                     reads=[pk_, "mum", ok_], writes=[ok_])

            lat = lambda t: t[:, TC:T].rearrange("p (r w) -> p r w", w=64)
            if has(0, 960):
                acc(lambda t: t[:, 1:TC], lambda t: t[:, 0:TC - 1], "P")
            if has(960, 1920):
                acc(lambda t: t[:, 0:TC - 1], lambda t: t[:, 1:TC], "N")
            if has(0, 480):
                acc(lambda t: lat(t)[:, :, 1:64], lambda t: lat(t)[:, :, 0:63], "L")
            if has(480, 960):
                acc(lambda t: lat(t)[:, :, 0:63], lambda t: lat(t)[:, :, 1:64], "R")
            if has(960, 1440) and NRW > 1:
                acc(lambda t: lat(t)[:, 1:NRW, :], lambda t: lat(t)[:, 0:NRW - 1, :], "U")
            if has(1440, 1920) and NRW > 1:
                acc(lambda t: lat(t)[:, 0:NRW - 1, :], lambda t: lat(t)[:, 1:NRW, :], "D")
            if j < 12:
                K.dma(pm_d[j, :, :], po[:, :], reads=[ok_], writes=[("pm", j)], key="st_" + ok_)
            else:
                fn_ = {12: AF.Tanh, 13: AF.Identity, 14: AF.Sigmoid}[j]
                K.op("act", lambda e, po=po, fn_=fn_: e.activation(lob[:], po[:], fn_), reads=[ok_], writes=["lob"])
                K.dma(lora_d[j - 12, :, :], lob[:, :], reads=["lob"], writes=[("lora", j - 12)], key="st_lob")
    K.barrier()
    stop_at("rw_mix")
    if "pm" in dbg:
        d_o = dbgt("pm", [12, 128, T])
        K.dma(d_o[:, :, :], pm_d[:, :, :], reads=[("pm", j) for j in range(12)], writes=["dbgpm"], key="dbg")

    comb_d = nc.dram_tensor("comb_s", [16384, 2 * D], BF16).ap()
    with ExitStack() as pw:
        nm_tiles.clear()
        RW_NDT = BF16 if RW_NM[0] == "bf16" else F32
        nmode['cur'] = RW_NM[0]
        for tag in ("n0", "n1", "n2", "n3"):
            nm_tiles[tag] = dict(PR=[sb(tag + "rPR%d" % i, [128, 256], RW_NDT, stack=pw) for i in range(2)],
                                 Pt=[sb(tag + "rPt%d" % i, [128, 128], RW_NDT, stack=pw) for i in range(2)],
                                 fin=sb(tag + "rfin", [128, 128], BF16, stack=pw))
        BL = min(256, T)
        blocks = [(b0, min(BL, T - b0)) for b0 in range(0, T, BL)]
        wst_ = sb("rw_wst", [128, 3, 512], stack=pw)
        wlb = sb("rw_wlb", [128, 3, 512], BF16, stack=pw)
        for q_, src in enumerate((w2_d, a2_d, g2w_d)):
            K.dma(wst_[:, q_, :], src[:, :], writes=["rw_wst"], key="rw_wst")
        K.op("dve", lambda e: e.tensor_copy(wlb[:], wst_[:]), reads=["rw_wst"], writes=["wlb"])
        bones = sb("bones", [128, 128], stack=pw)
        K.op("pool", lambda e: e.memset(bones[:], 0.0), writes=["bones"])
        K.op("pool", lambda e: e.memset(bones[0:64, 0:64], 1.0), writes=["bones"])
        K.op("pool", lambda e: e.memset(bones[64:128, 64:128], 1.0), writes=["bones"])
        rmask = sb("rmask", [128, BL], stack=pw)
        K.op("pool", lambda e: e.memset(rmask[:], 1.0), writes=["rmask"])
        K.op("pool", lambda e: e.memset(rmask[:].rearrange("p (n t) -> p n t", t=128)[:, :, 0:1], 0.0), writes=["rmask"])
        mask4 = [sb("mask4_%d" % d_, [128, 512], stack=pw) for d_ in range(2)]
        for d_ in range(2):
            ms_, mi_ = (m_lt, m_le) if d_ == 0 else (m_gt, m_ge)
            for q_ in range(4):
                src = ms_ if q_ % 2 == 0 else mi_
                K.op("dve", lambda e, d_=d_, q_=q_, src=src: e.tensor_copy(mask4[d_][:, q_ * 128:(q_ + 1) * 128], src[:]),
                     reads=["m_lt", "m_le", "m_gt", "m_ge"], writes=["mask4"])
        rT = [sb("rT%d" % d_, [128, T], BF16, stack=pw) for d_ in range(2)]
        kpT = [sb("kpT%d" % d_, [128, T], BF16, stack=pw) for d_ in range(2)]
        ktT = [sb("ktT%d" % d_, [128, T], BF16, stack=pw) for d_ in range(2)]
        nbT = [sb("nbT%d" % d_, [128, T], BF16, stack=pw) for d_ in range(2)]
        Lam = [sb("Lam%d" % d_, [128, NT], stack=pw) for d_ in range(2)]
        vtk = sb("rvtok", [128, NT, 128], BF16, stack=pw)
        bonus = sb("bonus", [128, T], BF16, stack=pw)
        gateT = sb("gateT", [128, T], BF16, stack=pw)
        ybuf = [sb("ybuf%d" % d_, [128, NT, 128], BF16, stack=pw) for d_ in range(2)]
        rmst = sb("rmst", [128, TL], BF16, stack=pw)
        bt = {nm: sb("b_" + nm, [128, BL], stack=pw) for nm in
              ("r", "k", "v", "kap", "sig", "cum", "w", "iw", "wp", "a", "t1", "t2", "rk")}
        lbt = sb("b_lora", [128, 3, BL], BF16, stack=pw)
        vb16 = sb("b_vb16", [128, BL], BF16, stack=pw)
        Z = [sb("Z%d" % d_, [128, 128], stack=pw) for d_ in range(2)]
        Zb = [sb("Zb%d" % d_, [128, 128], BF16, stack=pw) for d_ in range(2)]
        AK = [[sb("AK%d_%d" % (d_, z_), [128, 512], BF16, stack=pw) for z_ in range(2)] for d_ in range(2)]
        ANB = [[sb("ANB%d_%d" % (d_, z_), [128, 512], BF16, stack=pw) for z_ in range(2)] for d_ in range(2)]
        YY = [[sb("YY%d%d" % (d_, hh), [128, 128], RW_NDT, stack=pw) for hh in range(2)] for d_ in range(2)]
        YYt = [[sb("YYt%d%d" % (d_, hh), [128, 128], RW_NDT, stack=pw) for hh in range(2)] for d_ in range(2)]
        AinvS = [[[sb("Ainv%d%d_%d" % (d_, hh, z_), [128, 128], BF16, stack=pw) for z_ in range(2)] for hh in range(2)] for d_ in range(2)]
        ktok_ = [[sb("rktok%d_%d" % (d_, z_), [128, 128], BF16, stack=pw) for z_ in range(2)] for d_ in range(2)]
        nbtok_ = [[sb("rnbtok%d_%d" % (d_, z_), [128, 128], BF16, stack=pw) for z_ in range(2)] for d_ in range(2)]
        P1b = [sb("P1b%d" % d_, [128, 128], BF16, stack=pw) for d_ in range(2)]
        Ub = [sb("Ub%d" % d_, [128, 128], BF16, stack=pw) for d_ in range(2)]
        zt = [sb("zt%d" % d_, [128, 128], stack=pw) for d_ in range(2)]
        yo = sb("yo", [128, 128], stack=pw)
        yc = sb("yc", [128, 128], stack=pw)
        ysq = sb("ysq", [128, 128], stack=pw)
        yst = sb("yst", [128, 8], stack=pw)
        yt2 = sb("yt2", [128, 128], stack=pw)
        cst = [sb("cst%d" % i_, [128, 2, D], stack=pw) for i_ in range(2)]
        cbf = [sb("cbf%d" % i_, [128, 2 * D], BF16, stack=pw) for i_ in range(2)]
        conv_n = [0, 0]

        def conv_load():
            c_ = conv_n[0]
            if c_ >= 128:
                return
            conv_n[0] += 1
            st_ = cst[c_ % 2]; sk_ = "cst%d" % (c_ % 2)
            K.dma(st_[:, 0, :], down_d[c_ * 128:(c_ + 1) * 128, :], writes=[sk_], key=sk_)
            K.dma(st_[:, 1, :], up_d[c_ * 128:(c_ + 1) * 128, :], writes=[sk_], key=sk_)

        def conv_emit():
            c_ = conv_n[1]
            if c_ >= 128:
                return
            conv_n[1] += 1
            conv_load()
            st_ = cst[c_ % 2]; bf_ = cbf[c_ % 2]
            sk_ = "cst%d" % (c_ % 2); bk_ = "cbf%d" % (c_ % 2)
            K.op("act", lambda e, st_=st_, bf_=bf_: e.copy(bf_[:, 0:D], st_[:, 0, :]), reads=[sk_], writes=[bk_ + "a"])
            K.op("pool", lambda e, st_=st_, bf_=bf_: e.tensor_copy(bf_[:, D:2 * D], st_[:, 1, :]), reads=[sk_], writes=[bk_ + "b"])
            K.dma(comb_d[c_ * 128:(c_ + 1) * 128, :], bf_[:, :], reads=[bk_ + "a", bk_ + "b"], writes=["comb"], key="st_cbf")

        conv_load()

        for P in range(4):
            ch = slice(P * 128, (P + 1) * 128)
            for (b0, bn) in blocks:
                bs = slice(b0, b0 + bn)
                ntb = bn // 128
                for nm, jj in (("r", P), ("k", 4 + P), ("v", 8 + P)):
                    K.dma(bt[nm][:, 0:bn], pm_d[jj, :, bs], reads=[("pm", jj)], writes=["b_" + nm], key="b_" + nm)
                K.dma(lbt[:, :, 0:bn], lora_d[:, :, bs].rearrange("q p t -> p q t"), reads=[("lora", 0), ("lora", 1), ("lora", 2)], writes=["b_lora"], key="b_lora")
                K.op("pool", lambda e, bn=bn: e.tensor_copy(vb16[:, 0:bn], bt["v"][:, 0:bn]), reads=["b_v"], writes=["vb16"])
                for ii in range(ntb):
                    gi = b0 // 128 + ii
                    pt = psb[6 + (ii % 2)]; pk = "ps%d" % (6 + (ii % 2))
                    K.op("pe", lambda e, ii=ii, pt=pt: e.transpose(pt[:, 0:128], vb16[:, ii * 128:(ii + 1) * 128], identb[:]), reads=["vb16", "identb"], writes=[pk])
                    K.op("act", lambda e, gi=gi, pt=pt: e.copy(vtk[:, gi, :], pt[:, 0:128]), reads=[pk], writes=["rvtok"])
                kkc = pk2[:, PK2["KK"] + P:PK2["KK"] + P + 1]
                K.op("dve", lambda e, bn=bn, kkc=kkc: e.tensor_scalar(bt["kap"][:, 0:bn], bt["k"][:, 0:bn], kkc, None, op0=ALU.mult), reads=["b_k", "pk2"], writes=["b_kap"])
                K.op("pool", lambda e, bn=bn: e.tensor_tensor(bt["t1"][:, 0:bn], bt["kap"][:, 0:bn], bt["kap"][:, 0:bn], op=ALU.mult), reads=["b_kap"], writes=["b_t1"])
                for n in range((bn + 511) // 512):
                    t0 = n * 512; tn = min(512, bn - t0)
                    K.op("pe", lambda e, t0=t0, tn=tn: e.matmul(ps[0][:, 0:tn], lhsT=bones[:], rhs=bt["t1"][:, t0:t0 + tn], start=True, stop=True), reads=["bones", "b_t1"], writes=["ps0"])
                    K.op("act", lambda e, t0=t0, tn=tn: e.activation(bt["t2"][:, t0:t0 + tn], ps[0][:, 0:tn], AF.Sqrt, bias=eps_t[:, 1:2]), reads=["ps0", "eps"], writes=["b_t2"])
                K.op("dve", lambda e, bn=bn: e.reciprocal(bt["t2"][:, 0:bn], bt["t2"][:, 0:bn]), reads=["b_t2"], writes=["b_t2"])
                K.op("dve", lambda e, bn=bn: e.tensor_tensor(bt["kap"][:, 0:bn], bt["kap"][:, 0:bn], bt["t2"][:, 0:bn], op=ALU.mult), reads=["b_kap", "b_t2"], writes=["b_kap"])
                for n in range((bn + 511) // 512):
                    t0 = n * 512; tn = min(512, bn - t0)
                    K.op("pe", lambda e, t0=t0, tn=tn: e.matmul(ps[1][:, 0:tn], lhsT=wlb[:, 2, ch], rhs=lbt[:, 2, t0:t0 + tn], start=True, stop=True), reads=["wlb", "b_lora"], writes=["ps1"])
                    K.op("act", lambda e, t0=t0, tn=tn: e.copy(gateT[:, b0 + t0:b0 + t0 + tn], ps[1][:, 0:tn]), reads=["ps1"], writes=["gateT"])
                first_rk = True
                for d_ in range(2):
                    ds = slice(d_ * 64, (d_ + 1) * 64)
                    w0c = pk2[:, PK2["W0"] + d_ * 4 + P:PK2["W0"] + d_ * 4 + P + 1]
                    a0c = pk2[:, PK2["A0"] + d_ * 4 + P:PK2["A0"] + d_ * 4 + P + 1]
                    for n in range((bn + 511) // 512):
                        t0 = n * 512; tn = min(512, bn - t0)
                        K.op("pe", lambda e, t0=t0, tn=tn, ds=ds: e.matmul(ps[2][:, 0:tn], lhsT=wlb[ds, 0, ch], rhs=lbt[ds, 0, t0:t0 + tn], start=True, stop=True), reads=["wlb", "b_lora"], writes=["ps2"])
                        K.op("act", lambda e, t0=t0, tn=tn, w0c=w0c: e.activation(bt["sig"][:, t0:t0 + tn], ps[2][:, 0:tn], AF.Sigmoid, bias=w0c), reads=["ps2", "pk2"], writes=["b_sig"])
                        K.op("pe", lambda e, t0=t0, tn=tn, ds=ds: e.matmul(ps[3][:, 0:tn], lhsT=wlb[ds, 1, ch], rhs=lbt[ds, 1, t0:t0 + tn], start=True, stop=True), reads=["wlb", "b_lora"], writes=["ps3"])
                        K.op("act", lambda e, t0=t0, tn=tn, a0c=a0c: e.activation(bt["a"][:, t0:t0 + tn], ps[3][:, 0:tn], AF.Sigmoid, bias=a0c), reads=["ps3", "pk2"], writes=["b_a"])
                    K.op("dve", lambda e, bn=bn: e.tensor_tensor_scan(bt["cum"][:, 0:bn], rmask[:, 0:bn], bt["sig"][:, 0:bn], 0.0, op0=ALU.mult, op1=ALU.add),
                         reads=["rmask", "b_sig"], writes=["b_cum"])
                    c3 = bt["cum"][:, 0:bn].rearrange("p (n t) -> p n t", t=128)
                    tot_b = c3[:, :, 127:128].to_broadcast([128, ntb, 128])
                    K.op("act", lambda e, d_=d_, c3=c3, ntb=ntb: e.activation(Lam[d_][:, b0 // 128:b0 // 128 + ntb], c3[:, :, 127], AF.Exp, scale=-CW_),
                         reads=["b_cum"], writes=["Lam%d" % d_])
                    if d_ == 1:
                        K.op("dve", lambda e, bn=bn, c3=c3, tot_b=tot_b, ntb=ntb: e.tensor_tensor(
                            bt["t1"][:, 0:bn].rearrange("p (n t) -> p n t", t=128), tot_b, c3, op=ALU.subtract), reads=["b_cum"], writes=["b_t1"])
                        K.op("dve", lambda e, bn=bn: e.tensor_tensor(bt["cum"][:, 0:bn], bt["t1"][:, 0:bn], bt["sig"][:, 0:bn], op=ALU.add),
                             reads=["b_t1", "b_sig"], writes=["b_cum"])
                    K.op("act", lambda e, bn=bn: e.activation(bt["w"][:, 0:bn], bt["cum"][:, 0:bn], AF.Exp, scale=-CW_), reads=["b_cum"], writes=["b_w"])
                    K.op("act", lambda e, bn=bn: e.activation(bt["iw"][:, 0:bn], bt["cum"][:, 0:bn], AF.Exp, scale=CW_), reads=["b_cum"], writes=["b_iw"])
                    K.op("pool", lambda e, bn=bn: e.tensor_tensor(bt["t1"][:, 0:bn], bt["cum"][:, 0:bn], bt["sig"][:, 0:bn], op=ALU.subtract), reads=["b_cum", "b_sig"], writes=["b_t1"])
                    K.op("act", lambda e, bn=bn: e.activation(bt["wp"][:, 0:bn], bt["t1"][:, 0:bn], AF.Exp, scale=-CW_), reads=["b_t1"], writes=["b_wp"])
                    K.op("dve", lambda e, d_=d_, bn=bn: e.tensor_tensor(rT[d_][:, bs], bt["r"][:, 0:bn], bt["w"][:, 0:bn], op=ALU.mult), reads=["b_r", "b_w"], writes=["rT%d" % d_])
                    K.op("pool", lambda e, d_=d_, bn=bn: e.tensor_tensor(kpT[d_][:, bs], bt["kap"][:, 0:bn], bt["wp"][:, 0:bn], op=ALU.mult), reads=["b_kap", "b_wp"], writes=["kpT%d" % d_])
                    kac = pk2[:, PK2["KA"] + P:PK2["KA"] + P + 1]
                    K.op("dve", lambda e, bn=bn, kac=kac: e.tensor_scalar(bt["t1"][:, 0:bn], bt["a"][:, 0:bn], -1.0, kac, op0=ALU.add, op1=ALU.mult), reads=["b_a", "pk2"], writes=["b_t1"])
                    K.op("dve", lambda e, bn=bn: e.scalar_tensor_tensor(bt["t1"][:, 0:bn], bt["t1"][:, 0:bn], 1.0, bt["k"][:, 0:bn], op0=ALU.add, op1=ALU.mult), reads=["b_t1", "b_k"], writes=["b_t1"])
                    K.op("dve", lambda e, d_=d_, bn=bn: e.tensor_tensor(ktT[d_][:, bs], bt["t1"][:, 0:bn], bt["iw"][:, 0:bn], op=ALU.mult), reads=["b_t1", "b_iw"], writes=["ktT%d" % d_])
                    if first_rk:
                        K.op("pool", lambda e, bn=bn: e.tensor_tensor(bt["rk"][:, 0:bn], bt["t1"][:, 0:bn], bt["r"][:, 0:bn], op=ALU.mult), reads=["b_t1", "b_r"], writes=["b_rk"])
                        first_rk = False
                    else:
                        K.op("pool", lambda e, bn=bn: e.tensor_tensor(bt["t2"][:, 0:bn], bt["t1"][:, 0:bn], bt["r"][:, 0:bn], op=ALU.mult), reads=["b_t1", "b_r"], writes=["b_t2"])
                        K.op("pool", lambda e, bn=bn: e.tensor_tensor(bt["rk"][:, 0:bn], bt["rk"][:, 0:bn], bt["t2"][:, 0:bn], op=ALU.add), reads=["b_rk", "b_t2"], writes=["b_rk"])
                    K.op("dve", lambda e, bn=bn: e.scalar_tensor_tensor(bt["t2"][:, 0:bn], bt["kap"][:, 0:bn], -1.0, bt["a"][:, 0:bn], op0=ALU.mult, op1=ALU.mult), reads=["b_kap", "b_a"], writes=["b_t2"])
                    K.op("dve", lambda e, d_=d_, bn=bn: e.tensor_tensor(nbT[d_][:, bs], bt["t2"][:, 0:bn], bt["iw"][:, 0:bn], op=ALU.mult), reads=["b_t2", "b_iw"], writes=["nbT%d" % d_])
                rkc = pk2[:, PK2["RK"] + P:PK2["RK"] + P + 1]
                K.op("dve", lambda e, bn=bn, rkc=rkc: e.tensor_scalar(bt["rk"][:, 0:bn], bt["rk"][:, 0:bn], rkc, None, op0=ALU.mult), reads=["b_rk", "pk2"], writes=["b_rk"])
                for n in range((bn + 511) // 512):
                    t0 = n * 512; tn = min(512, bn - t0)
                    K.op("pe", lambda e, t0=t0, tn=tn: e.matmul(ps[4][:, 0:tn], lhsT=bones[:], rhs=bt["rk"][:, t0:t0 + tn], start=True, stop=True), reads=["bones", "b_rk"], writes=["ps4"])
                    K.op("dve", lambda e, t0=t0, tn=tn: e.tensor_tensor(bonus[:, b0 + t0:b0 + t0 + tn], ps[4][:, 0:tn], bt["v"][:, t0:t0 + tn], op=ALU.mult), reads=["ps4", "b_v"], writes=["bonus"])
            stop_at("rw_prep")
            for d_ in range(2):
                K.op("pool", lambda e, d_=d_: e.memset(Z[d_][:], 0.0), writes=["Z%d" % d_])
                K.op("pool", lambda e, d_=d_: e.memset(Zb[d_][:], 0.0), writes=["Zb%d" % d_])
            def r1(step, d_):
                i = fwd_order[step] if d_ == 0 else bwd_order[step]
                sl = slice(i * 128, (i + 1) * 128)
                want_o = i >= 2
                dk_ = "%d" % d_
                pz = step % 2
                pk_ = "%d_%d" % (d_, pz)
                b_ = d_ * 4
                for hh in range(2):
                    hs = slice(hh * 64, (hh + 1) * 64)
                    pt = ps[b_ + hh]; pk = "ps%d" % (b_ + hh)
                    for qq, src in enumerate((ktT, nbT)):
                        K.op("pe", lambda e, src=src, pt=pt, qq=qq, hs=hs, d_=d_, sl=sl: e.matmul(pt[:, qq * 256:qq * 256 + 128], lhsT=src[d_][hs, sl], rhs=kpT[d_][hs, sl], start=True, stop=True),
                             reads=["ktT" + dk_, "nbT" + dk_, "kpT" + dk_], writes=[pk])
                        K.op("pe", lambda e, src=src, pt=pt, qq=qq, hs=hs, d_=d_, sl=sl: e.matmul(pt[:, qq * 256 + 128:qq * 256 + 256], lhsT=src[d_][hs, sl], rhs=rT[d_][hs, sl], start=True, stop=True),
                             reads=["ktT" + dk_, "nbT" + dk_, "rT" + dk_], writes=[pk])
                for hh in range(2):
                    K.op("dve", lambda e, pz=pz, d_=d_, hh=hh: e.tensor_tensor(AK[d_][pz][:, hh * 256:(hh + 1) * 256], ps[d_ * 4 + hh][:, 0:256], mask4[d_][:, 0:256], op=ALU.mult),
                         reads=["ps%d" % (b_ + hh), "mask4"], writes=["AK" + pk_])
                    K.op("dve", lambda e, pz=pz, d_=d_, hh=hh: e.tensor_tensor(ANB[d_][pz][:, hh * 256:(hh + 1) * 256], ps[d_ * 4 + hh][:, 256:512], mask4[d_][:, 0:256], op=ALU.mult),
                         reads=["ps%d" % (b_ + hh), "mask4"], writes=["ANB" + pk_])
                pT2 = psb[b_ + 2]; kT2 = "ps%d" % (b_ + 2)
                K.op("pe", lambda e, pz=pz, d_=d_, sl=sl, pT2=pT2: e.transpose(pT2[:, 0:128], ktT[d_][:, sl], identb[:]), reads=["ktT" + dk_, "identb"], writes=[kT2])
                K.op("pe", lambda e, pz=pz, d_=d_, sl=sl, pT2=pT2: e.transpose(pT2[:, 128:256], nbT[d_][:, sl], identb[:]), reads=["nbT" + dk_, "identb"], writes=[kT2])
                K.op("act", lambda e, pz=pz, d_=d_, pT2=pT2: e.copy(ktok_[d_][pz][:], pT2[:, 0:128]), reads=[kT2], writes=["rktok" + pk_])
                K.op("act", lambda e, pz=pz, d_=d_, pT2=pT2: e.copy(nbtok_[d_][pz][:], pT2[:, 128:256]), reads=[kT2], writes=["rnbtok" + pk_])
            def r2(step, d_, hh):
                dk_ = "%d" % d_
                pz = step % 2
                pk_ = "%d_%d" % (d_, pz)
                tag = "n%d" % (d_ * 2 + hh)
                bank = d_ * 4 + hh
                kB2 = "ps%d" % bank
                K.op("pool", lambda e: e.tensor_copy(YY[d_][hh][:], ANB[d_][pz][:, hh * 256:hh * 256 + 128]), reads=["ANB" + pk_], writes=[tag + "Y"])
                pB = (psb if RW_NM[0] == "bf16" else ps)[bank]
                idn_ = identb if RW_NM[0] == "bf16" else ident
                K.op("pe", lambda e: e.transpose(pB[:, 0:128], YY[d_][hh][:], idn_[:]), reads=[tag + "Y", "identb", "ident"], writes=[kB2])
                K.op("act", lambda e: e.copy(YYt[d_][hh][:], pB[:, 0:128]), reads=[kB2], writes=[tag + "Yt"])
                Ai, kAi = neumann(YY[d_][hh], YYt[d_][hh], tag, bank, pb=(bank, 384))
                K.op("dve", lambda e: e.tensor_copy(AinvS[d_][hh][pz][:], Ai), reads=[kAi], writes=["Ainv%d%d_%d" % (d_, hh, pz)])

            def r3(step, d_):
                i = fwd_order[step] if d_ == 0 else bwd_order[step]
                sl = slice(i * 128, (i + 1) * 128)
                want_o = i >= 2
                dk_ = "%d" % d_
                pz = step % 2
                pk_ = "%d_%d" % (d_, pz)
                b_ = d_ * 4
                pC = ps[b_ + 3]; kC = "ps%d" % (b_ + 3)
                for hh in range(2):
                    K.op("pe", lambda e, pz=pz, d_=d_, hh=hh, i=i, pC=pC: e.matmul(pC[:, hh * 64:(hh + 1) * 64], lhsT=AK[d_][pz][:, hh * 256:hh * 256 + 128], rhs=vtk[:, i, hh * 64:(hh + 1) * 64], start=(hh == 0), stop=False),
                         reads=["AK" + pk_, "rvtok"], writes=[kC])
                K.op("pe", lambda e, pz=pz, d_=d_, sl=sl, pC=pC: e.matmul(pC[:, 0:128], lhsT=kpT[d_][:, sl], rhs=Zb[d_][:], start=False, stop=True), reads=["kpT" + dk_, "Zb" + dk_], writes=[kC])
                K.op("act", lambda e, pz=pz, d_=d_, pC=pC: e.copy(P1b[d_][:], pC[:, 0:128]), reads=[kC], writes=["P1b" + dk_])
                for hh in range(2):
                    K.op("pe", lambda e, pz=pz, d_=d_, hh=hh, pC=pC: e.matmul(pC[:, 128 + hh * 64:128 + (hh + 1) * 64], lhsT=AinvS[d_][hh][pz][:], rhs=P1b[d_][:, hh * 64:(hh + 1) * 64], start=True, stop=True),
                         reads=["Ainv%d%d_%d" % (d_, hh, pz), "P1b" + dk_], writes=[kC])
                K.op("dve", lambda e, pz=pz, d_=d_, pC=pC: e.tensor_copy(Ub[d_][:], pC[:, 128:256]), reads=[kC], writes=["Ub" + dk_])
                if want_o:
                    for hh in range(2):
                        K.op("pe", lambda e, pz=pz, d_=d_, hh=hh, i=i, pC=pC: e.matmul(pC[:, 256 + hh * 64:256 + (hh + 1) * 64], lhsT=AK[d_][pz][:, hh * 256 + 128:hh * 256 + 256], rhs=vtk[:, i, hh * 64:(hh + 1) * 64], start=(hh == 0), stop=False),
                             reads=["AK" + pk_, "rvtok"], writes=[kC])
                    K.op("pe", lambda e, pz=pz, d_=d_, sl=sl, pC=pC: e.matmul(pC[:, 256:384], lhsT=rT[d_][:, sl], rhs=Zb[d_][:], start=False, stop=False), reads=["rT" + dk_, "Zb" + dk_], writes=[kC])
                    for hh in range(2):
                        K.op("pe", lambda e, pz=pz, d_=d_, hh=hh, pC=pC: e.matmul(pC[:, 256 + hh * 64:256 + (hh + 1) * 64], lhsT=ANB[d_][pz][:, hh * 256 + 128:hh * 256 + 256], rhs=Ub[d_][:, hh * 64:(hh + 1) * 64], start=False, stop=(hh == 1)),
                             reads=["ANB" + pk_, "Ub" + dk_], writes=[kC])
                    K.op("act", lambda e, pz=pz, d_=d_, i=i, pC=pC: e.copy(ybuf[d_][:, i, :], pC[:, 256:384]), reads=[kC], writes=["ybuf" + dk_])
                pD = ps[b_ + 3]; kD = "ps%d" % (b_ + 3)
                K.op("pe", lambda e, pz=pz, d_=d_, i=i, pD=pD: e.matmul(pD[:, 384:512], lhsT=ktok_[d_][pz][:], rhs=vtk[:, i, :], start=True, stop=False), reads=["rktok" + pk_, "rvtok"], writes=[kD])
                K.op("pe", lambda e, pz=pz, d_=d_, pD=pD: e.matmul(pD[:, 384:512], lhsT=nbtok_[d_][pz][:], rhs=Ub[d_][:], start=False, stop=True), reads=["rnbtok" + pk_, "Ub" + dk_], writes=[kD])
                K.op("dve", lambda e, pz=pz, d_=d_, pD=pD: e.tensor_tensor(zt[d_][:], pD[:, 384:512], Z[d_][:], op=ALU.add), reads=[kD, "Z" + dk_], writes=["zt" + dk_])
                K.op("dve", lambda e, pz=pz, d_=d_, i=i: e.scalar_tensor_tensor(Z[d_][:], zt[d_][:], Lam[d_][:, i:i + 1], bones[:], op0=ALU.mult, op1=ALU.mult),
                     reads=["zt" + dk_, "Lam" + dk_, "bones"], writes=["Z" + dk_])
                K.op("act", lambda e, pz=pz, d_=d_: e.copy(Zb[d_][:], Z[d_][:]), reads=["Z" + dk_], writes=["Zb" + dk_])
            for d_ in K.streams(2):
                r1(0, d_)
            for q_ in K.streams(4):
                r2(0, q_ // 2, q_ % 2)
            for step in range(NT):
                conv_emit()
                if step + 1 < NT:
                    for d_ in K.streams(2):
                        r1(step + 1, d_)
                    for q_ in K.streams(6):
                        if q_ < 2:
                            r3(step, q_)
                        else:
                            r2(step + 1, (q_ - 2) // 2, (q_ - 2) % 2)
                else:
                    for d_ in K.streams(2):
                        r3(step, d_)
            stop_at("rw_scan")
            gwc = pk2[:, PK2["GNW"] + P:PK2["GNW"] + P + 1]
            gbc = pk2[:, PK2["GNB"] + P:PK2["GNB"] + P + 1]
            for i in range(2, NT):
                K.op("dve", lambda e, i=i: e.tensor_tensor(yo[:], ybuf[0][:, i, :], ybuf[1][:, i, :], op=ALU.add), reads=["ybuf0", "ybuf1"], writes=["yo"])
                y3 = yo[:].rearrange("p (h c) -> p h c", c=64)
                K.op("dve", lambda e, y3=y3: e.reduce_sum(yst[:, 0:2], y3, axis=AX.X), reads=["yo"], writes=["yst0"])
                K.op("dve", lambda e: e.tensor_scalar(yst[:, 2:4], yst[:, 0:2], 1.0 / 64, None, op0=ALU.mult), reads=["yst0"], writes=["yst1"])
                K.op("dve", lambda e, y3=y3: e.tensor_tensor(yc[:].rearrange("p (h c) -> p h c", c=64), y3, yst[:, 2:4].unsqueeze(2).to_broadcast([128, 2, 64]), op=ALU.subtract),
                     reads=["yo", "yst1"], writes=["yc"])
                K.op("act", lambda e: e.activation(ysq[:], yc[:], AF.Square), reads=["yc"], writes=["ysq"])
                K.op("dve", lambda e: e.reduce_sum(yst[:, 4:6], ysq[:].rearrange("p (h c) -> p h c", c=64), axis=AX.X), reads=["ysq"], writes=["yst2"])
                K.op("act", lambda e: e.activation(yst[:, 6:8], yst[:, 4:6], AF.Sqrt, bias=eps_t[:, 2:3], scale=1.0 / 64), reads=["yst2", "eps"], writes=["yst3"])
                K.op("dve", lambda e: e.reciprocal(yst[:, 6:8], yst[:, 6:8]), reads=["yst3"], writes=["yst3"])
                K.op("dve", lambda e: e.tensor_tensor(yt2[:].rearrange("p (h c) -> p h c", c=64), yc[:].rearrange("p (h c) -> p h c", c=64),
                                                      yst[:, 6:8].unsqueeze(2).to_broadcast([128, 2, 64]), op=ALU.mult), reads=["yc", "yst3"], writes=["yt2"])
                pt = ps[i % 2]; pk = "ps%d" % (i % 2)
                K.op("pe", lambda e, pt=pt: e.transpose(pt[:, 0:128], yt2[:], ident[:]), reads=["yt2", "ident"], writes=[pk])
                K.op("act", lambda e, pt=pt: e.activation(yc[:], pt[:, 0:128], AF.Identity, bias=gbc, scale=gwc), reads=[pk, "pk2", "yc"], writes=["yc"])
                K.op("dve", lambda e, i=i: e.tensor_tensor(yc[:], yc[:], bonus[:, i * 128:(i + 1) * 128], op=ALU.add), reads=["yc", "bonus"], writes=["yc"])
                K.op("dve", lambda e, i=i: e.tensor_tensor(rmst[:, (i - 2) * 128:(i - 1) * 128], yc[:], gateT[:, i * 128:(i + 1) * 128], op=ALU.mult), reads=["yc", "gateT"], writes=["rmst"])
            K.dma(mT_d[4 + P, :, :], rmst[:, :], reads=["rmst"], writes=[("mT", 4 + P)], key="st_rmst")
        while conv_n[1] < 128:
            conv_emit()
    K.barrier()

    stop_at("mix_done")
    with ExitStack() as pp_:
        mTs = [sb("mTs%d" % i_, [128, 8, 128], BF16, stack=pp_) for i_ in range(2)]
        woutb = sb("woutb", [128, 8, D], BF16, stack=pp_)
        wqb = sb("wqb", [128, 8, 2048], BF16, stack=pp_)
        skT = sb("skT", [128, 16, 128], BF16, stack=pp_)
        with ExitStack() as pset:
            wstg = sb("wstg", [128, 8, 512], stack=pset)
            skst = sb("skst", [128, 16, 128], stack=pset)
            for hf in range(2):
                K.dma(wstg[:, :, :], wout_d[:, hf * 512:(hf + 1) * 512].rearrange("(k p) c -> p k c", p=128), writes=["wstg"], key="wstg")
                K.op("pool", lambda e, hf=hf: e.tensor_copy(woutb[:, :, hf * 512:(hf + 1) * 512], wstg[:]), reads=["wstg"], writes=["woutb"])
            for hf in range(4):
                K.dma(wstg[:, :, :], wq_d[:, hf * 512:(hf + 1) * 512].rearrange("(k p) c -> p k c", p=128), writes=["wstg"], key="wstg")
                K.op("pool", lambda e, hf=hf: e.tensor_copy(wqb[:, :, hf * 512:(hf + 1) * 512], wstg[:]), reads=["wstg"], writes=["wqb"])
            K.dma(skst[:, :, :], sk_d[:, :, :].rearrange("g k d -> k g d"), writes=["skst"], key="skst")
            for g in range(16):
                pt = ps[g % 2]; pk = "ps%d" % (g % 2)
                K.op("pe", lambda e, g=g, pt=pt: e.transpose(pt[:, 0:128], skst[:, g, :], ident[:]), reads=["skst", "ident"], writes=[pk])
                K.op("act", lambda e, g=g, pt=pt: e.copy(skT[:, g, :], pt[:, 0:128]), reads=[pk], writes=["skT"])
            K.barrier()
        gtB = sb("gtB2", [128, 4, D], stack=pp_)
        K.dma(gtB[:].rearrange("p q d -> p (q d)"), gt_d[:, :], reads=["gt_d"], writes=["gtB"], key="gtB2")
        A2row = sb("A2row", [128, D], stack=pp_)
        fgB = sb("fgB", [128, D], stack=pp_)
        K.dma(A2row[:, :], g2row_d.partition_broadcast(128), writes=["A2row"], key="A2row")
        K.dma(fgB[:, :], fng_d.partition_broadcast(128), writes=["fgB"], key="fgB")
        K.op("dve", lambda e: e.scalar_tensor_tensor(A2row[:], gtB[:, 2, :], 1.0, A2row[:], op0=ALU.add, op1=ALU.mult), reads=["gtB", "A2row"], writes=["A2row"])
        iota16i = sb("iota16i", [128, 16], I32, stack=pp_)
        iota16 = sb("iota16", [128, 16], stack=pp_)
        K.op("pool", lambda e: e.iota(iota16i[:], pattern=[[1, 16]], base=0, channel_multiplier=0), writes=["iota16i"])
        K.op("dve", lambda e: e.tensor_copy(iota16[:], iota16i[:]), reads=["iota16i"], writes=["iota16"])

        xt_ = sb("p_xt", [128, D], stack=pp_)
        x1 = sb("p_x1", [128, D], stack=pp_)
        h2 = sb("p_h2", [128, D], stack=pp_)
        yacc = sb("p_y", [128, D], stack=pp_)
        pj = sb("p_junk", [128, D], stack=pp_)
        pss = sb("p_ss", [128, 1], stack=pp_)
        prs = sb("p_rs", [128, 2], stack=pp_)
        pxs = sb("p_xs", [128, D], stack=pp_)
        h2T = sb("p_h2T", [128, 8, 128], BF16, stack=pp_)
        qTs = sb("p_qT", [128, 16, 128], BF16, stack=pp_)
        scs = sb("p_sc", [128, 16, 128], stack=pp_)
        tmp1 = sb("p_tmp1", [128, 16, 128], stack=pp_)
        tv = sb("p_tv", [128, 16, 16], stack=pp_)
        tiu = sb("p_tiu", [128, 16, 16], U32, stack=pp_)
        tif = sb("p_tif", [128, 16, 16], stack=pp_)
        cand = sb("p_cand", [128, 8, 256], stack=pp_)
        tmp2 = sb("p_tmp2", [128, 8, 256], stack=pp_)
        eq = tmp2
        bsv = sb("p_bs", [128, 8, 16], stack=pp_)
        posu = sb("p_posu", [128, 8, 16], U32, stack=pp_)
        pau = sb("p_pau", [128, 8, 16], U32, stack=pp_)
        pbu = sb("p_pbu", [128, 8, 16], U32, stack=pp_)
        paf = sb("p_paf", [128, 8, 16], stack=pp_)
        pbf = sb("p_pbf", [128, 8, 16], stack=pp_)
        i0f = sb("p_i0f", [128, 8, 16], stack=pp_)
        i1f = sb("p_i1f", [128, 8, 16], stack=pp_)
        eidx = sb("p_eidx", [128, 128], U32, stack=pp_)
        gat = sb("p_gate", [128, 8, 16], stack=pp_)
        gsum = sb("p_gsum", [128, 8], stack=pp_)
        apre = sb("p_apre", [128, 128], stack=pp_)
        coef = sb("p_coef", [128, 128], stack=pp_)
        NRB = 8
        rowc = [sb("p_rowc%d" % i, [128, 2 * D], BF16, stack=pp_) for i in range(NRB)]
        pjb = sb("p_junkb", [128, D], BF16, stack=pp_)
        h2b = sb("p_h2b", [128, D], BF16, stack=pp_)
        dgs = [sb("p_dg%d" % i, [128, 128], BF16, stack=pp_) for i in range(4)]
        gflat = sb("p_gflat", [128, 128], stack=pp_)
        NEG = -1.0e30

        for i in range(NTL):
            tsl = slice(i * 128, (i + 1) * 128)
            K.dma(xt_[:, :], x_d[tsl, :], writes=["p_xt"], key="p_xt")
            mTt = mTs[i % 2]; mk_ = "mTs%d" % (i % 2)
            K.dma(mTt[:, :, :], mT_d[:, :, tsl].rearrange("k p t -> p k t"), reads=[("mT", j) for j in range(8)], writes=[mk_], key=mk_)
            for hf in range(2):
                pt = ps[hf]; pk = "ps%d" % hf
                for k in range(8):
                    K.op("pe", lambda e, k=k, hf=hf, pt=pt, mTt=mTt: e.matmul(pt[:, :], lhsT=mTt[:, k, :], rhs=woutb[:, k, hf * 512:(hf + 1) * 512], start=(k == 0), stop=(k == 7)),
                         reads=[mk_, "woutb"], writes=[pk])
                K.op("dve", lambda e, hf=hf, pt=pt: e.tensor_tensor(x1[:, hf * 512:(hf + 1) * 512], pt[:, :], gtB[:, 0, hf * 512:(hf + 1) * 512], op=ALU.mult),
                     reads=[pk, "gtB"], writes=["p_x1"])
            K.op("pool", lambda e: e.tensor_tensor(x1[:], x1[:], xt_[:], op=ALU.add), reads=["p_x1", "p_xt"], writes=["p_x1"])
            K.op("act", lambda e: e.activation(pj[:], x1[:], AF.Square), reads=["p_x1"], writes=["p_junk"])
            K.op("dve", lambda e: e.reduce_sum(pss[:, 0:1], pj[:], axis=AX.X), reads=["p_junk"], writes=["p_ss"])
            K.op("act", lambda e: e.activation(prs[:, 0:1], pss[:, 0:1], AF.Sqrt, bias=eps_t[:, 0:1], scale=1.0 / D), reads=["p_ss", "eps"], writes=["p_rs"])
            K.op("dve", lambda e: e.reciprocal(prs[:, 1:2], prs[:, 0:1]), reads=["p_rs"], writes=["p_rs2"])
            K.op("dve", lambda e: e.tensor_scalar(pxs[:], x1[:], prs[:, 1:2], None, op0=ALU.mult), reads=["p_x1", "p_rs2"], writes=["p_xs"])
            for hf in range(2):
                pt = ps[2 + hf]; pk = "ps%d" % (2 + hf)
                for kk in range(4):
                    k = hf * 4 + kk
                    K.op("pe", lambda e, k=k, kk=kk, pt=pt: e.transpose(pt[:, kk * 128:(kk + 1) * 128], pxs[:, k * 128:(k + 1) * 128], ident[:]), reads=["p_xs", "ident"], writes=[pk])
                for kk in range(4):
                    k = hf * 4 + kk
                    K.op("act", lambda e, k=k, kk=kk, pt=pt: e.activation(h2T[:, k, :], pt[:, kk * 128:(kk + 1) * 128], AF.Identity, bias=B2[:, k:k + 1], scale=A2[:, k:k + 1]),
                         reads=[pk, "mods"], writes=["p_h2T"])
            K.op("dve", lambda e: e.tensor_tensor(h2[:], pxs[:], A2row[:], op=ALU.mult), reads=["p_xs", "A2row"], writes=["p_h2"])
            K.op("pool", lambda e: e.tensor_tensor(h2[:], h2[:], gtB[:, 1, :], op=ALU.add), reads=["p_h2", "gtB"], writes=["p_h2"])
            for g in range(16):
                pt = ps[4 + (g % 2)]; pk = "ps%d" % (4 + (g % 2))
                for k in range(8):
                    K.op("pe", lambda e, g=g, k=k, pt=pt: e.matmul(pt[:, 0:128], lhsT=wqb[:, k, g * 128:(g + 1) * 128], rhs=h2T[:, k, :], start=(k == 0), stop=(k == 7)),
                         reads=["wqb", "p_h2T"], writes=[pk])
                K.op("act", lambda e, g=g, pt=pt: e.copy(qTs[:, g, :], pt[:, 0:128]), reads=[pk], writes=[("p_qT", g)])
            for g in range(16):
                pt = ps[6 + (g // 4) % 2]; pk = "ps%d" % (6 + (g // 4) % 2)
                K.op("pe", lambda e, g=g, pt=pt: e.matmul(pt[:, (g % 4) * 128:(g % 4 + 1) * 128], lhsT=qTs[:, g, :], rhs=skT[:, g, :], start=True, stop=True),
                     reads=[("p_qT", g), "skT"], writes=[pk])
                if g % 4 == 3:
                    K.op("dve", lambda e, g=g, pt=pt: e.tensor_copy(scs[:, g - 3:g + 1, :].rearrange("p g k -> p (g k)"), pt[:, :]), reads=[pk], writes=[("p_sc", g // 4)])
            for g in range(16):
                K.op("dve", lambda e, g=g: e.max(tv[:, g, 0:8], scs[:, g, :]), reads=[("p_sc", g // 4)], writes=[("tv", g)])
            for g in range(16):
                K.op("dve", lambda e, g=g: e.max_index(tiu[:, g, 0:8], tv[:, g, 0:8], scs[:, g, :]), reads=[("p_sc", g // 4), ("tv", g)], writes=[("tiu", g)])
            for g in range(16):
                K.op("dve", lambda e, g=g: e.match_replace(tmp1[:, g, :], tv[:, g, 0:8], scs[:, g, :], NEG), reads=[("p_sc", g // 4), ("tv", g)], writes=[("tmp1", g)])
            for g in range(16):
                K.op("dve", lambda e, g=g: e.max(tv[:, g, 8:16], tmp1[:, g, :]), reads=[("tmp1", g)], writes=[("tv2", g)])
            for g in range(16):
                K.op("dve", lambda e, g=g: e.max_index(tiu[:, g, 8:16], tv[:, g, 8:16], tmp1[:, g, :]), reads=[("tmp1", g), ("tv2", g)], writes=[("tiu2", g)])
            allg = [("tv", g) for g in range(16)] + [("tv2", g) for g in range(16)]
            alli = [("tiu", g) for g in range(16)] + [("tiu2", g) for g in range(16)]
            K.op("dve", lambda e: e.tensor_copy(tif[:], tiu[:]), reads=alli, writes=["p_tif"])
            tvv = tv[:].rearrange("p (h q) a -> p h q a", q=2)
            tfv = tif[:].rearrange("p (h q) a -> p h q a", q=2)
            c4 = cand[:].rearrange("p h (a b) -> p h a b", b=16)
            K.op("dve", lambda e: e.tensor_tensor(c4, tvv[:, :, 0, :].unsqueeze(3).to_broadcast([128, 8, 16, 16]),
                                                  tvv[:, :, 1, :].unsqueeze(2).to_broadcast([128, 8, 16, 16]), op=ALU.add), reads=allg, writes=["p_cand"])
            for hh in range(8):
                K.op("dve", lambda e, hh=hh: e.max(bsv[:, hh, 0:8], cand[:, hh, :]), reads=["p_cand"], writes=[("bs", hh)])
            for hh in range(8):
                K.op("dve", lambda e, hh=hh: e.max_index(posu[:, hh, 0:8], bsv[:, hh, 0:8], cand[:, hh, :]), reads=["p_cand", ("bs", hh)], writes=[("pos", hh)])
            for hh in range(8):
                K.op("dve", lambda e, hh=hh: e.match_replace(tmp2[:, hh, :], bsv[:, hh, 0:8], cand[:, hh, :], NEG), reads=["p_cand", ("bs", hh), "p_eq"], writes=[("tmp2", hh)])
            for hh in range(8):
                K.op("dve", lambda e, hh=hh: e.max(bsv[:, hh, 8:16], tmp2[:, hh, :]), reads=[("tmp2", hh)], writes=[("bs2", hh)])
            for hh in range(8):
                K.op("dve", lambda e, hh=hh: e.max_index(posu[:, hh, 8:16], bsv[:, hh, 8:16], tmp2[:, hh, :]), reads=[("tmp2", hh), ("bs2", hh)], writes=[("pos2", hh)])
            allb = [("bs", hh) for hh in range(8)] + [("bs2", hh) for hh in range(8)]
            allp = [("pos", hh) for hh in range(8)] + [("pos2", hh) for hh in range(8)]
            K.op("dve", lambda e: e.tensor_single_scalar(pau[:], posu[:], 4, op=ALU.logical_shift_right), reads=allp, writes=["p_pau"])
            K.op("dve", lambda e: e.tensor_single_scalar(pbu[:], posu[:], 15, op=ALU.bitwise_and), reads=allp, writes=["p_pbu"])
            K.op("dve", lambda e: e.tensor_copy(paf[:], pau[:]), reads=["p_pau"], writes=["p_paf"])
            K.op("dve", lambda e: e.tensor_copy(pbf[:], pbu[:]), reads=["p_pbu"], writes=["p_pbf"])
            e4 = eq[:].rearrange("p h (k a) -> p h k a", a=16)
            io4 = iota16[:].unsqueeze(1).unsqueeze(1).to_broadcast([128, 8, 16, 16])
            for (pf, q_, dst, nm) in ((paf, 0, i0f, "i0f"), (pbf, 1, i1f, "i1f")):
                K.op("dve", lambda e, pf=pf: e.tensor_tensor(e4, pf[:].unsqueeze(3).to_broadcast([128, 8, 16, 16]), io4, op=ALU.is_equal),
                     reads=["p_paf", "p_pbf", "iota16"], writes=["p_eq"] + [("tmp2", hh_) for hh_ in range(8)])
                K.op("dve", lambda e, q_=q_: e.tensor_tensor(e4, e4, tfv[:, :, q_, :].unsqueeze(2).to_broadcast([128, 8, 16, 16]), op=ALU.mult),
                     reads=["p_eq", "p_tif"], writes=["p_eq"])
                K.op("dve", lambda e, dst=dst: e.reduce_sum(dst[:], e4, axis=AX.X), reads=["p_eq"], writes=["p_" + nm])
            K.op("dve", lambda e: e.scalar_tensor_tensor(i0f[:], i0f[:], 128.0, i1f[:], op0=ALU.mult, op1=ALU.add), reads=["p_i0f", "p_i1f"], writes=["p_i0f"])
            K.op("dve", lambda e: e.tensor_copy(eidx[:], i0f[:].rearrange("p h k -> p (h k)")), reads=["p_i0f"], writes=["p_eidx"])
            K.op("dve", lambda e: e.tensor_tensor(gat[:], bsv[:], bsv[:, :, 0:1].to_broadcast([128, 8, 16]), op=ALU.subtract), reads=allb, writes=["p_gate"])
            K.op("act", lambda e: e.activation(gat[:], gat[:], AF.Exp), reads=["p_gate"], writes=["p_gate"])
            K.op("dve", lambda e: e.reduce_sum(gsum[:], gat[:], axis=AX.X), reads=["p_gate"], writes=["p_gsum"])
            K.op("dve", lambda e: e.reciprocal(gsum[:], gsum[:]), reads=["p_gsum"], writes=["p_gsum"])
            K.op("dve", lambda e: e.tensor_tensor(gat[:], gat[:], gsum[:].unsqueeze(2).to_broadcast([128, 8, 16]), op=ALU.mult), reads=["p_gate", "p_gsum"], writes=["p_gate"])
            K.op("act", lambda e: e.copy(h2b[:], h2[:]), reads=["p_h2"], writes=["p_h2b"])
            K.op("dve", lambda e: e.tensor_copy(gflat[:], gat[:].rearrange("p h k -> p (h k)")), reads=["p_gate"], writes=["p_gflat"])
            GRP = 2
            for g0 in range(0, 128, GRP):
                for kslot in range(g0, g0 + GRP):
                    rb = rowc[kslot % NRB]; rk_ = "p_rowc%d" % (kslot % NRB)
                    K.gather(rb[:, :], comb_d[:, :], eidx[:, kslot:kslot + 1], reads=["p_eidx", "comb"], writes=[rk_], key=rk_)
                    K.op("dve", lambda e, rb=rb, kslot=kslot: e.scalar_tensor_tensor(pjb[:], rb[:, 0:D], 1.0, h2b[:], op0=ALU.mult, op1=ALU.mult, accum_out=apre[:, kslot:kslot + 1]),
                         reads=[rk_, "p_h2b"], writes=["p_junkb", ("apre", g0 // GRP)])
                K.op("dve", lambda e, g0=g0: e.tensor_copy(coef[:, g0:g0 + GRP], apre[:, g0:g0 + GRP]), reads=[("apre", g0 // GRP)], writes=[("cf0", g0 // GRP)])
                K.op("act", lambda e, g0=g0: e.activation(coef[:, g0:g0 + GRP], coef[:, g0:g0 + GRP], AF.Gelu), reads=[("cf0", g0 // GRP)], writes=[("cf1", g0 // GRP)])
                K.op("dve", lambda e, g0=g0: e.tensor_tensor(coef[:, g0:g0 + GRP], coef[:, g0:g0 + GRP], gflat[:, g0:g0 + GRP], op=ALU.mult),
                     reads=[("cf1", g0 // GRP), "p_gflat"], writes=[("cf2", g0 // GRP)])
                for kslot in range(g0, g0 + GRP):
                    rb = rowc[kslot % NRB]; rk_ = "p_rowc%d" % (kslot % NRB)
                    dg = dgs[kslot % 4]; dk__ = "p_dg%d" % (kslot % 4)
                    K.op("act", lambda e, dg=dg, kslot=kslot: e.activation(dg[:], identb[:], AF.Identity, scale=coef[:, kslot:kslot + 1]),
                         reads=["identb", ("cf2", g0 // GRP)], writes=[dk__])
                    for hf in range(2):
                        K.op("pe", lambda e, dg=dg, rb=rb, hf=hf, kslot=kslot: e.matmul(ps[hf][:, :], lhsT=dg[:], rhs=rb[:, D + hf * 512:D + (hf + 1) * 512],
                                                                                       start=(kslot == 0), stop=(kslot == 127)),
                             reads=[dk__, rk_], writes=["ps%d" % hf])
            for hf in range(2):
                K.op("act", lambda e, hf=hf: e.copy(yacc[:, hf * 512:(hf + 1) * 512], ps[hf][:, :]), reads=["ps%d" % hf], writes=["p_y"])
            K.op("dve", lambda e: e.tensor_tensor(yacc[:], yacc[:], gtB[:, 3, :], op=ALU.mult), reads=["p_y", "gtB"], writes=["p_y"])
            K.op("pool", lambda e: e.tensor_tensor(yacc[:], yacc[:], x1[:], op=ALU.add), reads=["p_y", "p_x1"], writes=["p_y"])
            K.op("act", lambda e: e.activation(pj[:], yacc[:], AF.Square), reads=["p_y"], writes=["p_junk"])
            K.op("dve", lambda e: e.reduce_sum(pss[:, 0:1], pj[:], axis=AX.X), reads=["p_junk"], writes=["p_ss"])
            K.op("act", lambda e: e.activation(prs[:, 0:1], pss[:, 0:1], AF.Sqrt, bias=eps_t[:, 0:1], scale=1.0 / D), reads=["p_ss", "eps"], writes=["p_rs"])
            K.op("dve", lambda e: e.reciprocal(prs[:, 1:2], prs[:, 0:1]), reads=["p_rs"], writes=["p_rs2"])
            K.op("dve", lambda e: e.scalar_tensor_tensor(pxs[:], yacc[:], prs[:, 1:2], fgB[:], op0=ALU.mult, op1=ALU.mult), reads=["p_y", "p_rs2", "fgB"], writes=["p_xs"])
            K.dma(out_d[tsl, :], pxs[:, :], reads=["p_xs"], writes=["outdone"], key="st_out")
            if i == 0:
                stop_at("peer_t0")
    K.barrier()
    if "mT" in dbg:
        d_o = dbgt("mT", [8, 128, TL], BF16)
        K.dma(d_o[:, :, :], mT_d[:, :, :], reads=[("mT", j) for j in range(8)], writes=["dbgmT"], key="dbg")

    K.finish([k for k in K.st.keys() if (isinstance(k, str) and k.startswith("dbg")) or k == "outdone"])
    return dbg_out


def _inputs_for_core(inp, b, n_rows):
    TL = 64 * n_rows
    f = lambda a: np.ascontiguousarray(np.asarray(a, dtype=np.float32))
    m = {
        "x": f(inp["x"][b, :TL]),
        "c": f(inp["c"][b:b + 1]),
        "ctx": f(inp["ctx"][b]),
        "c_ctx": f(inp["c_ctx"][None, :]),
        "ada_w": f(inp["ada_w"][0]),
        "ada_b": f(inp["ada_b"][0].reshape(48, 128)),
        "ada_b_row": f(inp["ada_b"][0].reshape(1, 6144)),
        "norm1_g": f(inp["norm1_g"][0].reshape(8, 128)),
        "w_in": f(inp["w_in"][0]),
        "gdn_conv_w": f(inp["gdn_conv_w"][0].reshape(60, 128)),
        "gdn_a_log": f(inp["gdn_a_log"][0].reshape(1, 8)),
        "gdn_dt_bias": f(inp["gdn_dt_bias"][0].reshape(1, 8)),
        "gdn_norm_w": f(inp["gdn_norm_w"][0].reshape(1, 128)),
        "rwkv_mu": f(inp["rwkv_mu"][0].reshape(15, 128)),
        "rwkv_w0": f(inp["rwkv_w0"][0].reshape(8, 128)),
        "rwkv_w2": f(inp["rwkv_w2"][0].reshape(128, 512)),
        "rwkv_a0": f(inp["rwkv_a0"][0].reshape(8, 128)),
        "rwkv_a2": f(inp["rwkv_a2"][0].reshape(128, 512)),
        "rwkv_g2": f(inp["rwkv_g2"][0]),
        "rwkv_k_k": f(inp["rwkv_k_k"][0].reshape(4, 128)),
        "rwkv_k_a": f(inp["rwkv_k_a"][0].reshape(4, 128)),
        "rwkv_r_k": f(inp["rwkv_r_k"][0].reshape(4, 128)),
        "rwkv_gn_w": f(inp["rwkv_gn_w"][0].reshape(4, 128)),
        "rwkv_gn_b": f(inp["rwkv_gn_b"][0].reshape(4, 128)),
        "w_out": f(inp["w_out"][0]),
        "norm2_g": f(inp["norm2_g"][0].reshape(8, 128)),
        "peer_w_query": f(inp["peer_w_query"][0]),
        "peer_sub_keys": f(inp["peer_sub_keys"][0].reshape(16, 128, 128)),
        "peer_down": f(inp["peer_down"][0]),
        "peer_up": f(inp["peer_up"][0]),
        "final_norm_g": f(inp["final_norm_g"][None, :]),
        "norm2_g_row": f(inp["norm2_g"][0].reshape(1, 1024)),
    }
    return m


def run(inp, n_rows=64, cores=None, dbg=(), stop=None):
    nb = inp["x"].shape[0]
    cores = list(range(nb)) if cores is None else cores
    nc = bass.Bass("TRN2", target_bir_lowering=False)
    build(nc, n_rows=n_rows, dbg=dbg, stop=stop)
    in_maps = [_inputs_for_core(inp, b, n_rows) for b in cores]
    res = run_bass_kernel_spmd(nc, in_maps, core_ids=list(range(len(cores))))
    return res.results


def kernel(**inputs):
    res = run(inputs, n_rows=64)
    return np.stack([np.asarray(r["out"], dtype=np.float32) for r in res], axis=0)
```
